# Optimizing a Trainium2 kernel written in Bass

```python
import jax, jax.numpy as jnp
from jax import lax
import numpy as np

D_MODEL = 1024
BATCH = 8
SEQ = 4096
DEPTH = 2

N_BRANCH = 3
BRANCH_W = D_MODEL // 2
NORM_EPS = 1e-6
MAX_POS_OFFSET = 1024
MLA_HEADS = 8
MLA_NOPE = 64
MLA_ROPE = 32
MLA_V = BRANCH_W // MLA_HEADS
MLA_Q_LORA = 256
MLA_KV_LORA = 128
ROPE_BASE = 10000.0
Q_BLOCK = 128
RWKV_HEADS = 8
RWKV_HEAD = BRANCH_W // RWKV_HEADS
RWKV_DECAY_RANK = 64
RWKV_ICLR_RANK = 64
RWKV_IN = 3 * BRANCH_W + RWKV_DECAY_RANK + RWKV_ICLR_RANK
RWKV_GN_EPS = 64e-5
GLA_HEADS = 4
GLA_DK = 64
GLA_DV = BRANCH_W // GLA_HEADS
GLA_GATE_RANK = 16
GLA_TAU = 16.0
GLA_CHUNK = 64
IN_SIZES = (MLA_Q_LORA, MLA_KV_LORA, MLA_ROPE, RWKV_IN,
            GLA_HEADS * GLA_DK, GLA_HEADS * GLA_DK, GLA_HEADS * GLA_DV, GLA_GATE_RANK,
            N_BRANCH * BRANCH_W, N_BRANCH * D_MODEL)
N_IN = sum(IN_SIZES)

kernel_name = "hybrid_mla_rwkv7_gla_gated_merge"


def rms_norm(x, g, eps=NORM_EPS):
    xf = x.astype(jnp.float32)
    y = xf * lax.rsqrt(jnp.mean(xf * xf, axis=-1, keepdims=True) + eps)
    return (y * g.astype(jnp.float32)).astype(x.dtype)


def rope_tables(positions):
    inv = 1.0 / (ROPE_BASE ** (jnp.arange(0, MLA_ROPE, 2, dtype=jnp.float32) / MLA_ROPE))
    ang = positions.astype(jnp.float32)[..., None] * inv
    return jnp.cos(ang), jnp.sin(ang)


def apply_rope(x, cos, sin):
    xf = x.astype(jnp.float32)
    x1, x2 = xf[..., : MLA_ROPE // 2], xf[..., MLA_ROPE // 2:]
    return jnp.concatenate([x1 * cos - x2 * sin, x2 * cos + x1 * sin], axis=-1).astype(x.dtype)


def mla_branch(c_q, c_kv, k_rope, cos, sin, q_norm, kv_norm, w_uq, w_ukv):
    B, S, _ = c_q.shape
    q = (rms_norm(c_q, q_norm) @ w_uq).reshape(B, S, MLA_HEADS, MLA_NOPE + MLA_ROPE)
    q_nope = q[..., :MLA_NOPE]
    q_rope = apply_rope(q[..., MLA_NOPE:], cos[:, :, None, :], sin[:, :, None, :])
    kv = (rms_norm(c_kv, kv_norm) @ w_ukv).reshape(B, S, MLA_HEADS, MLA_NOPE + MLA_V)
    k_nope, v = kv[..., :MLA_NOPE], kv[..., MLA_NOPE:]
    k_r = apply_rope(k_rope, cos, sin)
    scale = (MLA_NOPE + MLA_ROPE) ** -0.5
    n_blk = S // Q_BLOCK
    qn_b = q_nope.reshape(B, n_blk, Q_BLOCK, MLA_HEADS, MLA_NOPE).transpose(1, 0, 2, 3, 4)
    qr_b = q_rope.reshape(B, n_blk, Q_BLOCK, MLA_HEADS, MLA_ROPE).transpose(1, 0, 2, 3, 4)
    k_idx = jnp.arange(S)

    def block(args):
        qn, qr, i = args
        s = (jnp.einsum('bqhd,bkhd->bhqk', qn, k_nope)
             + jnp.einsum('bqhr,bkr->bhqk', qr, k_r)).astype(jnp.float32) * scale
        q_idx = i * Q_BLOCK + jnp.arange(Q_BLOCK)
        s = jnp.where(k_idx[None, :] <= q_idx[:, None], s, -jnp.inf)
        p = jax.nn.softmax(s, axis=-1).astype(v.dtype)
        return jnp.einsum('bhqk,bkhd->bqhd', p, v)

    o = lax.map(block, (qn_b, qr_b, jnp.arange(n_blk)))
    return o.transpose(1, 0, 2, 3, 4).reshape(B, S, MLA_HEADS * MLA_V)


def rwkv7_branch(u, mu, w0, w_up, a0, a_up, k_k, k_a, r_k, ln_w, ln_b):
    B, S, _ = u.shape
    W = BRANCH_W
    u_prev = jnp.pad(u[:, :-1], ((0, 0), (1, 0), (0, 0)))
    u = u + (u_prev - u) * mu
    r, k, v, lw, la = jnp.split(u, [W, 2 * W, 3 * W, 3 * W + RWKV_DECAY_RANK], axis=-1)
    w = -jax.nn.softplus(-(w0 + jnp.tanh(lw) @ w_up)) - 0.5
    decay = jnp.exp(-jnp.exp(w.astype(jnp.float32)))
    a = jax.nn.sigmoid(a0 + la @ a_up)
    heads = lambda t: t.astype(jnp.float32).reshape(B, S, RWKV_HEADS, RWKV_HEAD)
    kk = heads(k * k_k)
    kk = kk / jnp.maximum(jnp.sqrt(jnp.sum(kk * kk, axis=-1, keepdims=True)), 1e-12)
    k = k * (1.0 + (a - 1.0) * k_a)
    r_h, k_h, v_h, a_h, w_h = heads(r), heads(k), heads(v), heads(a), heads(decay)

    def step(state, inp):
        r_t, w_t, k_t, v_t, kk_t, a_t = inp
        sa = jnp.einsum('bhvk,bhk->bhv', state, -kk_t)
        state = (state * w_t[:, :, None, :] + sa[..., None] * (kk_t * a_t)[:, :, None, :]
                 + v_t[..., None] * k_t[:, :, None, :])
        return state, jnp.einsum('bhvk,bhk->bhv', state, r_t)

    xs = tuple(t.transpose(1, 0, 2, 3) for t in (r_h, w_h, k_h, v_h, kk, a_h))
    state0 = jnp.zeros((B, RWKV_HEADS, RWKV_HEAD, RWKV_HEAD), jnp.float32)
    _, y = lax.scan(step, state0, xs)
    y = y.transpose(1, 0, 2, 3)
    mean = jnp.mean(y, axis=-1, keepdims=True)
    var = jnp.mean(jnp.square(y - mean), axis=-1, keepdims=True)
    y = ((y - mean) * lax.rsqrt(var + RWKV_GN_EPS)).reshape(B, S, W)
    y = y * ln_w.astype(jnp.float32) + ln_b.astype(jnp.float32)
    bonus = jnp.sum(r_h * k_h * r_k.astype(jnp.float32), axis=-1, keepdims=True) * v_h
    return (y + bonus.reshape(B, S, W)).astype(u.dtype)


def gla_branch(q, k, v, lat, a_up, a_b, g_norm):
    B, S, _ = q.shape
    H, C = GLA_HEADS, GLA_CHUNK
    n_c = S // C
    log_a = jax.nn.log_sigmoid((lat @ a_up + a_b).astype(jnp.float32)) / GLA_TAU

    def chunks(t, d):
        return t.astype(jnp.float32).reshape(B, n_c, C, H, d).transpose(1, 0, 2, 3, 4)

    qc = chunks(q * GLA_DK ** -0.5, GLA_DK)
    kc = chunks(k, GLA_DK)
    vc = chunks(v, GLA_DV)
    bc = jnp.cumsum(chunks(log_a, GLA_DK), axis=2)
    causal = jnp.tril(jnp.ones((C, C), dtype=bool))[None, :, :, None, None]

    def step(state, inp):
        q_t, k_t, v_t, b_t = inp
        diff = b_t[:, :, None] - b_t[:, None, :]
        dec = jnp.exp(jnp.where(causal, diff, -jnp.inf))
        att = jnp.einsum('bihd,bjhd,bijhd->bhij', q_t, k_t, dec)
        o = (jnp.einsum('bhij,bjhv->bihv', att, v_t)
             + jnp.einsum('bihk,bhkv->bihv', q_t * jnp.exp(b_t), state))
        b_last = b_t[:, -1]
        state = (state * jnp.exp(b_last)[..., None]
                 + jnp.einsum('bjhk,bjhv->bhkv', k_t * jnp.exp(b_last[:, None] - b_t), v_t))
        return state, o

    state0 = jnp.zeros((B, H, GLA_DK, GLA_DV), jnp.float32)
    _, o = lax.scan(step, state0, (qc, kc, vc, bc))
    o = o.transpose(1, 0, 2, 3, 4).reshape(B, S, H, GLA_DV)
    o = rms_norm(o, g_norm.reshape(H, GLA_DV))
    return o.reshape(B, S, H * GLA_DV).astype(q.dtype)


def setup_inputs(seed: int = 0) -> dict:
    key = jax.random.key(seed)
    ks = jax.random.split(key, 24)
    L, W = DEPTH, BRANCH_W

    def dense(k, shape, fan_in, scale=1.0):
        return scale * fan_in ** -0.5 * jax.random.normal(k, shape, jnp.float32)

    def gain(k, shape):
        return 1.0 + 0.05 * jax.random.normal(k, shape, jnp.float32)

    x = jax.random.normal(ks[0], (BATCH, SEQ, D_MODEL), jnp.float32)
    offset = jax.random.randint(ks[1], (BATCH, 1), 0, MAX_POS_OFFSET, dtype=jnp.int32)
    positions = offset + jnp.arange(SEQ, dtype=jnp.int32)[None, :]
    return {
        "x": x,
        "positions": positions,
        "norm_pre": gain(ks[2], (L, D_MODEL)),
        "w_in": dense(ks[3], (L, D_MODEL, N_IN), D_MODEL),
        "mla_q_norm": gain(ks[4], (L, MLA_Q_LORA)),
        "mla_kv_norm": gain(ks[5], (L, MLA_KV_LORA)),
        "mla_w_uq": dense(ks[6], (L, MLA_Q_LORA, MLA_HEADS * (MLA_NOPE + MLA_ROPE)), MLA_Q_LORA),
        "mla_w_ukv": dense(ks[7], (L, MLA_KV_LORA, MLA_HEADS * (MLA_NOPE + MLA_V)), MLA_KV_LORA),
        "rwkv_mu": jax.random.uniform(ks[8], (L, RWKV_IN), jnp.float32),
        "rwkv_w0": jax.random.uniform(ks[9], (L, W), jnp.float32, -6.0, -1.0),
        "rwkv_w_up": dense(ks[10], (L, RWKV_DECAY_RANK, W), RWKV_DECAY_RANK, 0.5),
        "rwkv_a0": 0.1 * jax.random.normal(ks[11], (L, W), jnp.float32),
        "rwkv_a_up": dense(ks[12], (L, RWKV_ICLR_RANK, W), RWKV_ICLR_RANK, 0.5),
        "rwkv_k_k": 0.85 + 0.05 * jax.random.normal(ks[13], (L, W), jnp.float32),
        "rwkv_k_a": 1.0 + 0.05 * jax.random.normal(ks[14], (L, W), jnp.float32),
        "rwkv_r_k": 0.1 * jax.random.normal(ks[15], (L, RWKV_HEADS, RWKV_HEAD), jnp.float32),
        "rwkv_ln_w": gain(ks[16], (L, W)),
        "rwkv_ln_b": 0.01 * jax.random.normal(ks[17], (L, W), jnp.float32),
        "gla_a_up": dense(ks[18], (L, GLA_GATE_RANK, GLA_HEADS * GLA_DK), GLA_GATE_RANK),
        "gla_a_b": 0.1 * jax.random.normal(ks[19], (L, GLA_HEADS * GLA_DK), jnp.float32),
        "gla_norm": gain(ks[20], (L, W)),
        "w_branch_out": dense(ks[21], (L, N_BRANCH, W, D_MODEL), W),
        "w_out": dense(ks[22], (L, D_MODEL, D_MODEL), D_MODEL),
        "norm_post": gain(ks[23], (L, D_MODEL)),
    }


def reference(x, positions, norm_pre, w_in, mla_q_norm, mla_kv_norm, mla_w_uq, mla_w_ukv,
              rwkv_mu, rwkv_w0, rwkv_w_up, rwkv_a0, rwkv_a_up, rwkv_k_k, rwkv_k_a, rwkv_r_k,
              rwkv_ln_w, rwkv_ln_b, gla_a_up, gla_a_b, gla_norm, w_branch_out, w_out, norm_post):
    B, S, _ = x.shape
    splits = [int(i) for i in np.cumsum(IN_SIZES)[:-1]]
    cos, sin = rope_tables(positions)
    for l in range(DEPTH):
        h = rms_norm(x, norm_pre[l])
        proj = h @ w_in[l]
        (c_q, c_kv, k_rope, u_rwkv, g_q, g_k, g_v, g_lat,
         br_gate, merge_gate) = jnp.split(proj, splits, axis=-1)
        y_mla = mla_branch(c_q, c_kv, k_rope, cos, sin, mla_q_norm[l], mla_kv_norm[l],
                           mla_w_uq[l], mla_w_ukv[l])
        y_rwkv = rwkv7_branch(u_rwkv, rwkv_mu[l], rwkv_w0[l], rwkv_w_up[l], rwkv_a0[l],
                              rwkv_a_up[l], rwkv_k_k[l], rwkv_k_a[l], rwkv_r_k[l],
                              rwkv_ln_w[l], rwkv_ln_b[l])
        y_gla = gla_branch(g_q, g_k, g_v, g_lat, gla_a_up[l], gla_a_b[l], gla_norm[l])
        ys = (jnp.stack([y_mla, y_rwkv, y_gla], axis=2)
              * jax.nn.silu(br_gate).reshape(B, S, N_BRANCH, BRANCH_W))
        branch = jnp.einsum('bsnw,nwd->bsnd', ys, w_branch_out[l])
        merged = jnp.sum(jax.nn.sigmoid(merge_gate).reshape(B, S, N_BRANCH, D_MODEL) * branch, axis=2)
        x = x + rms_norm(merged @ w_out[l], norm_post[l])
    return x
```

```python
import math
import numpy as np
import concourse.bass as bass
import concourse.mybir as mybir
from concourse.bass_utils import run_bass_kernel_spmd

F32 = mybir.dt.float32
BF16 = mybir.dt.bfloat16
I32 = mybir.dt.int32
ALU = mybir.AluOpType
AF = mybir.ActivationFunctionType
AX = mybir.AxisListType

SEM_CAP = 30000
D = 1024
NIN = 7728
L = 2
EPS = 1e-6


class Buf:
    __slots__ = ("name", "w", "r")

    def __init__(self, name):
        self.name = name
        self.w = None
        self.r = {}


class T:
    def __init__(self, t, name):
        self.t = t
        self.b = Buf(name)

    def __getitem__(self, k):
        return self.t[k]


def _fs(ap):
    n = 1
    for d in ap.shape[1:]:
        n *= int(d)
    return n


def _nbytes(ap):
    n = 1
    for d in ap.shape:
        n *= int(d)
    return n * mybir.dt.size(ap.dtype)


class Prog:
    CE = ("pe", "act", "dve", "pool")
    QE = ("sp",)
    WINDOW = 24
    SYNC = 120.0

    def __init__(self, nc, n_dma_sems=32):
        self.nc = nc
        self.sems = {}
        self._ctx = []
        self._semctx = []
        self.cur = {}
        self.seen = {e: {} for e in self.CE + self.QE}
        self.nsem = 0
        self.old_sems = []
        for e in self.CE:
            self._new_eng_sem(e)
        self.dma_sems = []
        for i in range(n_dma_sems):
            k = self._alloc_sem("dma%d" % i)
            self.dma_sems.append([k, 0])
        self.dma_rr = 0
        self.ninst = 0
        self.ops = []
        self.uid = 0
        self.model_time = 0.0

    def _alloc_sem(self, name):
        cm = self.nc.semaphore(name)
        h = cm.__enter__()
        self._semctx.append(cm)
        self.sems[name] = h
        self.nsem += 1
        return name

    def _new_eng_sem(self, e):
        if e in self.cur:
            self.old_sems.append(tuple(self.cur[e]))
        k = self._alloc_sem("%s_s%d" % (e, self.nsem))
        self.cur[e] = [k, 0]

    def sbuf(self, name, shape, dt):
        self.uid += 1
        name = "%s_u%d" % (name, self.uid)
        cm = self.nc.sbuf_tensor(name, list(shape), dt)
        t = cm.__enter__()
        self._ctx.append(cm)
        return T(t, name)

    def psum(self, name, shape, dt):
        self.uid += 1
        name = "%s_u%d" % (name, self.uid)
        cm = self.nc.psum_tensor(name, list(shape), dt)
        t = cm.__enter__()
        self._ctx.append(cm)
        return T(t, name)

    def dram(self, name, shape, dt, kind="Internal"):
        t = self.nc.dram_tensor(name, list(shape), dt, kind=kind).ap()
        return T(t, name)

    def op(self, eng, fn, R=(), W=(), signal=True, dur=500.0):
        self.ops.append((eng, fn, [x.b for x in R], [x.b for x in W], float(dur), float(dur), False))
        self.ninst += 1

    def dma(self, out_ap, in_ap, R=(), W=(), q="sp", **kw):
        lat = 2000.0 + _nbytes(out_ap) / 150.0
        self.ops.append((q, lambda e: e.dma_start(out=out_ap, in_=in_ap, **kw),
                         [x.b for x in R], [x.b for x in W], 80.0, lat, True))
        self.ninst += 1

    def mm(self, out, lhsT, rhs, start, stop, R, W, **kw):
        n = _fs(rhs)
        d = (50.0 + 1.0 * max(n, 64)) if lhsT.dtype == F32 else (40.0 + 0.42 * max(n, 64))
        self.op("pe", lambda e: e.matmul(out, lhsT, rhs, start=start, stop=stop, **kw), R, W, dur=d)

    def tr(self, out, in_, ident, R, W, signal=True):
        self.op("pe", lambda e: e.transpose(out, in_, ident), R, W, dur=110.0)

    def act(self, out, in_, func, R, W, **kw):
        self.op("act", lambda e: e.activation(out, in_, func, **kw), R, W, dur=260.0 + 0.83 * _fs(out))

    def _vd(self, eng, out, two_src):
        n = _fs(out)
        if eng == "pool":
            return 150.0 + 1.7 * n
        b16 = mybir.dt.size(out.dtype) == 2
        if two_src:
            return 160.0 + (1.04 * n)
        return 160.0 + (0.55 * n)

    def tt(self, eng, out, a, b, op, R, W):
        self.op(eng, lambda e: e.tensor_tensor(out, a, b, op), R, W, dur=self._vd(eng, out, True))

    def ts(self, eng, out, a, s1, s2, op0, op1, R, W):
        d = self._vd(eng, out, False)
        if s2 is None:
            self.op(eng, lambda e: e.tensor_scalar(out, a, s1, None, op0), R, W, dur=d)
        else:
            self.op(eng, lambda e: e.tensor_scalar(out, a, s1, s2, op0, op1), R, W, dur=d)

    def stt(self, eng, out, a, s, b, op0, op1, R, W):
        self.op(eng, lambda e: e.scalar_tensor_tensor(out, a, s, b, op0, op1), R, W, dur=self._vd(eng, out, True))

    def cp(self, eng, out, in_, R, W):
        if eng == "act":
            self.op(eng, lambda e: e.copy(out, in_), R, W, dur=260.0 + 0.83 * _fs(out))
        else:
            self.op(eng, lambda e: e.tensor_copy(out, in_), R, W, dur=self._vd(eng, out, False))

    def memset(self, eng, ap, val, W):
        self.op(eng, lambda e: e.memset(ap, val), (), W, dur=self._vd(eng, ap, False))

    def _eng(self, eng):
        nc = self.nc
        return {"pe": nc.tensor, "act": nc.scalar, "dve": nc.vector, "pool": nc.gpsimd, "sp": nc.sync}[eng]

    def flush(self):
        import bisect
        ops = self.ops
        self.ops = []
        n = len(ops)
        if n == 0:
            return
        engs = self.CE + self.QE
        lastw = {}
        readers = {}
        deps = [None] * n
        for i in range(n):
            eng, fn, R, W, occ, lat, isd = ops[i]
            d = set()
            for b in R:
                w = lastw.get(id(b))
                if w is not None:
                    d.add(w)
            for b in W:
                w = lastw.get(id(b))
                if w is not None:
                    d.add(w)
                rl = readers.get(id(b))
                if rl:
                    d.update(rl)
            d.discard(i)
            deps[i] = d
            for b in R:
                readers.setdefault(id(b), []).append(i)
            for b in W:
                lastw[id(b)] = i
                readers[id(b)] = []
        succ = [[] for _ in range(n)]
        nd = [0] * n
        for i in range(n):
            nd[i] = len(deps[i])
            for j in deps[i]:
                succ[j].append(i)
        ready = [0.0] * n
        free = {e: 0.0 for e in engs}
        cand = {e: [] for e in engs}
        for i in range(n):
            if nd[i] == 0:
                cand[ops[i][0]].append(i)
        order = {e: [] for e in engs}
        done = 0
        W_ = self.WINDOW
        SY = self.SYNC
        tmax = 0.0
        while done < n:
            best = None
            for e in engs:
                c = cand[e]
                if not c:
                    continue
                fe = free[e]
                bst = None
                for i in c[:W_]:
                    st = ready[i] if ready[i] > fe else fe
                    if bst is None or st < bst[0]:
                        bst = (st, i)
                if best is None or bst < best[:2]:
                    best = (bst[0], bst[1], e)
            st, i, e = best
            cand[e].remove(i)
            order[e].append(i)
            occ, lat = ops[i][4], ops[i][5]
            free[e] = st + occ
            fin = st + lat
            if fin > tmax:
                tmax = fin
            for s_ in succ[i]:
                rt = fin + (SY if ops[s_][0] != e else (0.0 if e == "pe" else 60.0))
                if rt > ready[s_]:
                    ready[s_] = rt
                nd[s_] -= 1
                if nd[s_] == 0:
                    bisect.insort(cand[ops[s_][0]], s_)
            done += 1
        self.model_time += tmax
        need = [False] * n
        for i in range(n):
            e = ops[i][0]
            if ops[i][6] or not succ[i]:
                need[i] = True
                continue
            for s_ in succ[i]:
                if ops[s_][0] != e or e != "pe":
                    need[i] = True
                    break
        tick = [None] * n
        reuse_wait = {}
        for e in engs:
            for i in order[e]:
                if ops[i][6]:
                    slot = self.dma_sems[self.dma_rr]
                    self.dma_rr = (self.dma_rr + 1) % len(self.dma_sems)
                    if slot[1] > 0:
                        reuse_wait[i] = (slot[0], slot[1])
                    slot[1] += 16
                    tick[i] = (slot[0], slot[1])
                elif need[i]:
                    cur = self.cur[e]
                    if cur[1] >= SEM_CAP:
                        self._new_eng_sem(e)
                        cur = self.cur[e]
                    cur[1] += 1
                    tick[i] = (cur[0], cur[1])
        for e in engs:
            eo = self._eng(e)
            seen = self.seen[e]
            sems = self.sems
            for i in order[e]:
                w = {}
                for j in deps[i]:
                    if e == "pe" and ops[j][0] == "pe":
                        continue
                    k, v = tick[j]
                    if w.get(k, 0) < v:
                        w[k] = v
                if i in reuse_wait:
                    k, v = reuse_wait[i]
                    if w.get(k, 0) < v:
                        w[k] = v
                for k, v in w.items():
                    if seen.get(k, 0) < v:
                        seen[k] = v
                        eo.wait_ge(sems[k], v)
                ins = ops[i][1](eo)
                if tick[i] is not None:
                    ins.then_inc(sems[tick[i][0]], 16 if ops[i][6] else 1)

    def barrier(self):
        self.flush()
        allv = [(k, v) for (k, v) in [tuple(x) for x in self.cur.values()] if v > 0]
        allv += [(k, v) for (k, v) in self.old_sems]
        allv += [(k, v) for (k, v) in [tuple(x) for x in self.dma_sems] if v > 0]
        for eng in self.CE + self.QE:
            eo = self._eng(eng)
            for k, v in allv:
                if self.seen[eng].get(k, 0) < v:
                    self.seen[eng][k] = v
                    eo.wait_ge(self.sems[k], v)

    def mark(self):
        return len(self._ctx)

    def release(self, mark):
        self.barrier()
        while len(self._ctx) > mark:
            self._ctx.pop().__exit__(None, None, None)

    def finish(self, final):
        self.barrier()
        while self._ctx:
            self._ctx.pop().__exit__(None, None, None)
        while self._semctx:
            self._semctx.pop().__exit__(None, None, None)


C_CQ, C_CKV, C_KR, C_U = 0, 256, 384, 416
C_GQ, C_GK, C_GV, C_GL, C_BG, C_MG = 2080, 2336, 2592, 3104, 3120, 4656
SCALE = (64 + 32) ** -0.5


class Ctx:
    pass


def setup_consts(p, S, pos_d):
    c = Ctx()
    idf = p.sbuf("idf", [128, 128], F32)
    p.memset("pool", idf[:], 1.0, [idf])
    p.op("pool", lambda e: e.affine_select(idf[:], idf[:], [[1, 128]], ALU.is_equal, 0.0,
                                           base=0, channel_multiplier=-1), [idf], [idf])
    idb = p.sbuf("idb", [128, 128], BF16)
    p.cp("dve", idb[:], idf[:], [idf], [idb])
    c.idf, c.idb = idf, idb
    onesb = p.sbuf("onesb", [128, 128], BF16)
    p.memset("pool", onesb[:], 1.0, [onesb])
    c.onesb = onesb
    epsb = p.sbuf("epsb", [128, 1], F32)
    p.memset("pool", epsb[:], EPS, [epsb])
    c.epsb = epsb
    lnsc = p.sbuf("lnsc", [128, 1], F32)
    p.memset("pool", lnsc[:], math.log(SCALE), [lnsc])
    c.lnsc = lnsc
    inv = (1.0 / (10000.0 ** (np.arange(0, 32, 2, dtype=np.float32) / np.float32(32)))).astype(np.float32)
    row = p.sbuf("roperow", [1, 64], F32)
    for j in range(16):
        p.memset("pool", row[0:1, j:j + 1], float(inv[j]), [row])
        p.memset("pool", row[0:1, 16 + j:17 + j], float(inv[j]), [row])
    p.memset("pool", row[0:1, 32:48], -1.0, [row])
    p.memset("pool", row[0:1, 48:64], 1.0, [row])
    rd = p.dram("rope_d", [64], F32)
    p.dma(rd[:].rearrange("(o n) -> o n", o=1), row[:], [row], [rd])
    ctab = p.sbuf("ctab", [96, 2], F32)
    p.dma(ctab[64:96, :], rd[:].rearrange("(c p) -> p c", p=32), [rd], [ctab], allow_slow_non_contiguous=True)
    cos2 = p.sbuf("cos2", [96, S], F32)
    sin2 = p.sbuf("sin2", [96, S], F32)
    mk = p.mark()
    posi = p.sbuf("posi", [96, S], I32)
    p.dma(posi[64:96, :], pos_d.partition_broadcast(32), [], [posi])
    ang = p.sbuf("ang", [96, S], F32)
    p.cp("dve", ang[64:96, :], posi[64:96, :], [posi], [ang])
    p.ts("dve", ang[64:96, :], ang[64:96, :], ctab[64:96, 0:1], None, ALU.mult, None, [ang, ctab], [ang])
    kf = p.sbuf("kf", [96, S], F32)
    sl = slice(64, 96)

    def sin_of(dst, shift):
        a = dst
        p.ts("dve", a[sl, :], ang[sl, :], shift, None, ALU.add, None, [ang], [dst])
        p.ts("dve", kf[sl, :], a[sl, :], 1.0 / (2 * math.pi), None, ALU.mult, None, [dst], [kf])
        p.cp("dve", posi[sl, :], kf[sl, :], [kf], [posi])
        p.cp("dve", kf[sl, :], posi[sl, :], [posi], [kf])
        p.stt("dve", a[sl, :], kf[sl, :], -2 * math.pi, a[sl, :], ALU.mult, ALU.add, [kf, dst], [dst])
        p.ts("dve", kf[sl, :], a[sl, :], math.pi, -2 * math.pi, ALU.is_gt, ALU.mult, [dst], [kf])
        p.tt("dve", a[sl, :], a[sl, :], kf[sl, :], ALU.add, [dst, kf], [dst])
        p.ts("dve", kf[sl, :], a[sl, :], -math.pi, 2 * math.pi, ALU.is_lt, ALU.mult, [dst], [kf])
        p.tt("dve", a[sl, :], a[sl, :], kf[sl, :], ALU.add, [dst, kf], [dst])
        p.act(a[sl, :], a[sl, :], AF.Sin, [dst], [dst])
    sin_of(cos2, math.pi / 2)
    sin_of(sin2, 0.0)
    p.ts("dve", sin2[sl, :], sin2[sl, :], ctab[sl, 1:2], None, ALU.mult, None, [sin2, ctab], [sin2])
    p.release(mk)
    c.cos2, c.sin2 = cos2, sin2
    return c


def colvec(p, name, src_ap, n, dt=F32):
    t = p.sbuf(name, [128, n], dt)
    p.dma(t[:], src_ap.rearrange("(c p) -> p c", p=128), [], [t], allow_slow_non_contiguous=True)
    return t


def phase1(p, c, S, l, xin, W, sc):
    NT_ = S // 128
    NB = S // 512
    hT = p.sbuf("hT%d" % l, [128, 8, S], BF16)
    gbc = p.sbuf("gbc%d" % l, [128, D], F32)
    p.dma(gbc[:], W["norm_pre"][l:l + 1, :].partition_broadcast(128), [], [gbc])
    mk1 = p.mark()
    xt = [p.sbuf("xt%d_%d" % (l, i), [128, D], F32) for i in range(2)]
    hb = [p.sbuf("hb%d_%d" % (l, i), [128, D], BF16) for i in range(2)]
    junk = p.sbuf("junk%d" % l, [128, D], BF16)
    ss = [p.sbuf("ss%d_%d" % (l, i), [128, 1], F32) for i in range(2)]
    ptr = [p.psum("ptr%d_%d" % (l, i), [128, 8, 128], BF16) for i in range(2)]
    for i in range(NT_):
        s = i % 2
        p.dma(xt[s][:], xin[i * 128:(i + 1) * 128, :], [xin], [xt[s]])
        p.act(junk[:], xt[s][:], AF.Square, [xt[s]], [junk, ss[s]], accum_out=ss[s][:])
        p.act(ss[s][:], ss[s][:], AF.Ln, [ss[s], c.epsb], [ss[s]], bias=c.epsb[:], scale=1.0 / D)
        p.act(ss[s][:], ss[s][:], AF.Exp, [ss[s]], [ss[s]], scale=-0.5)
        p.stt("dve", hb[s][:], xt[s][:], ss[s][:, 0:1], gbc[:], ALU.mult, ALU.mult,
              [xt[s], ss[s], gbc], [hb[s]])
        for k in range(8):
            p.tr(ptr[s][:, k, :], hb[s][:, k * 128:(k + 1) * 128], c.idb[:], [hb[s], c.idb], [ptr[s]],
                 signal=(k == 7))
        p.cp("act", hT[:, :, i * 128:(i + 1) * 128], ptr[s][:], [ptr[s]], [hT])

    p.release(mk1)
    mk2 = p.mark()
    wa32 = p.sbuf("wa32_%d" % l, [128, 8, 384], F32)
    p.dma(wa32[:], W["w_in"][l, :, 0:384].rearrange("(c p) n -> p c n", p=128), [], [wa32])
    wa = p.sbuf("wa_%d" % l, [128, 8, 384], BF16)
    p.cp("pool", wa[:], wa32[:], [wa32], [wa])
    wk32 = p.sbuf("wk32_%d" % l, [128, 8, 192], F32)
    p.dma(wk32[:], W["wkr2"][l].rearrange("(c p) n -> p c n", p=128), [], [wk32])
    wkr = p.sbuf("wkr_%d" % l, [128, 8, 192], BF16)
    p.cp("pool", wkr[:], wk32[:], [wk32], [wkr])
    wq32 = p.sbuf("wq32_%d" % l, [128, 2, 1536], F32)
    p.dma(wq32[:, :, 0:768], W["mla_w_uq"][l].rearrange("(c p) n -> p c n", p=128), [], [wq32])
    p.dma(wq32[:, :, 768:1536], W["wuq_sw"][l].rearrange("(c p) n -> p c n", p=128), [], [wq32])
    wq = p.sbuf("wq_%d" % l, [128, 2, 1536], BF16)
    p.cp("pool", wq[:], wq32[:], [wq32], [wq])
    wkv32 = p.sbuf("wkv32_%d" % l, [128, 1024], F32)
    p.dma(wkv32[:, 0:512], W["wuk"][l], [], [wkv32])
    p.dma(wkv32[:, 512:1024], W["wuv"][l], [], [wkv32])
    wkv = p.sbuf("wkv_%d" % l, [128, 1024], BF16)
    p.cp("pool", wkv[:], wkv32[:], [wkv32], [wkv])
    gq = colvec(p, "gq%d" % l, W["mla_q_norm"][l], 2)
    gkv = colvec(p, "gkv%d" % l, W["mla_kv_norm"][l], 1)

    pq = [p.psum("pq%d_%d" % (l, i), [128, 512], F32) for i in range(2)]
    pkv = p.psum("pkv%d" % l, [128, 512], F32)
    pkr = p.psum("pkr%d" % l, [96, 2, 512], F32)
    pn = p.psum("pn%d" % l, [128, 512], F32)
    pw = [p.psum("pw%d_%d" % (l, i), [128, 512], F32) for i in range(2)]
    sq = p.sbuf("sq%d" % l, [128, 2, 512], BF16)
    rq = p.sbuf("rq%d" % l, [128, 512], F32)
    cqn = p.sbuf("cqn%d" % l, [128, 2, 512], BF16)
    ckvn = p.sbuf("ckvn%d" % l, [128, 512], BF16)
    qst = p.sbuf("qst%d" % l, [96, 8, 512], BF16)
    kst = p.sbuf("kst%d" % l, [128, 4, 512], BF16)
    krs = p.sbuf("krs%d" % l, [96, 512], BF16)
    t1 = p.sbuf("t1_%d" % l, [96, 512], F32)
    t2 = p.sbuf("t2_%d" % l, [96, 512], F32)
    vst = p.sbuf("vst%d" % l, [128, 4, 8, 65], BF16)
    p.memset("pool", vst[:], 1.0, [vst])
    rs = slice(64, 96)
    for tb in range(NB):
        ts_ = slice(tb * 512, (tb + 1) * 512)
        for cc in range(2):
            for k in range(8):
                p.mm(pq[cc][:], wa[:, k, cc * 128:(cc + 1) * 128], hT[:, k, ts_], k == 0, k == 7, [wa, hT], [pq[cc]])
        for k in range(8):
            p.mm(pkv[:], wa[:, k, 256:384], hT[:, k, ts_], k == 0, k == 7, [wa, hT], [pkv])
        for v in range(2):
            for k in range(8):
                p.mm(pkr[:, v, :], wkr[:, k, v * 96:(v + 1) * 96], hT[:, k, ts_], k == 0, k == 7, [wkr, hT], [pkr])
        for cc in range(2):
            p.act(sq[:, cc, :], pq[cc][:], AF.Square, [pq[cc]], [sq])
        for cc in range(2):
            p.mm(pn[:], c.onesb[:], sq[:, cc, :], cc == 0, cc == 1, [c.onesb, sq], [pn])
        p.act(rq[:], pn[:], AF.Ln, [pn, c.epsb], [rq], bias=c.epsb[:], scale=1.0 / 256)
        p.act(rq[:], rq[:], AF.Exp, [rq, c.lnsc], [rq], bias=c.lnsc[:], scale=-0.5)
        for cc in range(2):
            p.stt("dve", cqn[:, cc, :], pq[cc][:], gq[:, cc:cc + 1], rq[:], ALU.mult, ALU.mult,
                  [pq[cc], gq, rq], [cqn])
        p.act(sq[:, 0, :], pkv[:], AF.Square, [pkv], [sq])
        p.mm(pn[:], c.onesb[:], sq[:, 0, :], True, True, [c.onesb, sq], [pn])
        p.act(rq[:], pn[:], AF.Ln, [pn, c.epsb], [rq], bias=c.epsb[:], scale=1.0 / 128)
        p.act(rq[:], rq[:], AF.Exp, [rq], [rq], scale=-0.5)
        p.stt("dve", ckvn[:], pkv[:], gkv[:, 0:1], rq[:], ALU.mult, ALU.mult, [pkv, gkv, rq], [ckvn])
        p.tt("dve", t1[rs, :], pkr[rs, 0, :], c.cos2[rs, ts_], ALU.mult, [pkr, c.cos2], [t1])
        p.tt("dve", t2[rs, :], pkr[rs, 1, :], c.sin2[rs, ts_], ALU.mult, [pkr, c.sin2], [t2])
        p.tt("pool", krs[rs, :], t1[rs, :], t2[rs, :], ALU.add, [t1, t2], [krs])
        for h in range(8):
            p.dma(sc["KT"][h, 64:96, ts_], krs[rs, :], [krs], [sc["KT"]])
        for h in range(8):
            a, b = pw[0], pw[1]
            for k in range(2):
                p.mm(a[0:96, :], wq[:, k, h * 96:(h + 1) * 96], cqn[:, k, :], k == 0, k == 1, [wq, cqn], [a])
            for k in range(2):
                p.mm(b[0:96, :], wq[:, k, 768 + h * 96:768 + (h + 1) * 96], cqn[:, k, :], k == 0, k == 1, [wq, cqn], [b])
            p.cp("act", qst[0:64, h, :], a[0:64, :], [a], [qst])
            p.tt("dve", t1[rs, :], a[rs, :], c.cos2[rs, ts_], ALU.mult, [a, c.cos2], [t1])
            p.tt("dve", t2[rs, :], b[rs, :], c.sin2[rs, ts_], ALU.mult, [b, c.sin2], [t2])
            p.tt("pool", qst[rs, h, :], t1[rs, :], t2[rs, :], ALU.add, [t1, t2], [qst])
        p.dma(sc["QT"][:, :, ts_].rearrange("h p t -> p h t"), qst[:], [qst], [sc["QT"]])
        for j in range(4):
            a = pw[j % 2]
            p.mm(a[:], wkv[:, j * 128:(j + 1) * 128], ckvn[:], True, True, [wkv, ckvn], [a])
            p.cp("act", kst[:, j, :], a[:], [a], [kst])
        for two in range(2):
            p.dma(sc["KT"][:, 0:64, ts_].rearrange("(j two) p t -> two p j t", two=2)[two],
                  kst[two * 64:(two + 1) * 64, :, :], [kst], [sc["KT"]])
        for tt_ in range(4):
            a = pw[tt_ % 2]
            p.mm(a[:], ckvn[:, tt_ * 128:(tt_ + 1) * 128], wkv[:, 512:1024], True, True, [ckvn, wkv], [a])
            p.cp("act", vst[:, tt_, :, 0:64], a[:].rearrange("p (h v) -> p h v", v=64), [a], [vst])
        p.dma(sc["VA"][ts_, :].rearrange("(tt p) n -> p tt n", p=128),
              vst[:].rearrange("p t h v -> p t (h v)"), [vst], [sc["VA"]])

    p.release(mk2)
    mk3 = p.mark()
    pw = [p.psum("pwb%d_%d" % (l, i), [128, 512], F32) for i in range(2)]
    chunks = []
    for i in range(13):
        chunks.append((C_U + i * 128, 128, "copy32", "U", i * 128))
    for i in range(2):
        chunks.append((C_GQ + i * 128, 128, "copy32", "GQ", i * 128))
    for i in range(2):
        chunks.append((C_GK + i * 128, 128, "copy32", "GK", i * 128))
    chunks.append((C_GL, 16, "copy32", "GL", 0))
    for i in range(12):
        chunks.append((C_BG + i * 128, 128, "silu", "BG", i * 128))
    for i in range(24):
        chunks.append((C_MG + i * 128, 128, "sigm", "MG", i * 128))
    chunks.sort(key=lambda x: {"copy32": 0, "silu": 1, "sigm": 2}[x[2]])
    wc32 = [p.sbuf("wc32_%d_%d" % (l, i), [128, 8, 128], F32) for i in range(2)]
    wcb = [p.sbuf("wcb_%d_%d" % (l, i), [128, 8, 128], BF16) for i in range(2)]
    st32 = [p.sbuf("st32_%d_%d" % (l, i), [128, 512], F32) for i in range(2)]
    st16 = [p.sbuf("st16_%d_%d" % (l, i), [128, 512], BF16) for i in range(2)]
    n = 0
    for ci, (col0, m, kind, dest, row0) in enumerate(chunks):
        s = ci % 2
        p.dma(wc32[s][:, :, 0:m], W["w_in"][l, :, col0:col0 + m].rearrange("(c p) n -> p c n", p=128), [], [wc32[s]])
        p.cp("pool", wcb[s][:, :, 0:m], wc32[s][:, :, 0:m], [wc32[s]], [wcb[s]])
        for tb in range(NB):
            ts_ = slice(tb * 512, (tb + 1) * 512)
            a = pw[n % 2]
            for k in range(8):
                p.mm(a[0:m, :], wcb[s][:, k, 0:m], hT[:, k, ts_], k == 0, k == 7, [wcb[s], hT], [a])
            if kind == "copy32":
                o = st32[n % 2]
                if n % 4 < 2:
                    p.cp("act", o[0:m, :], a[0:m, :], [a], [o])
                else:
                    p.cp("dve", o[0:m, :], a[0:m, :], [a], [o])
            else:
                o = st16[n % 2]
                p.act(o[0:m, :], a[0:m, :], AF.Silu if kind == "silu" else AF.Sigmoid, [a], [o])
            p.dma(sc[dest][row0:row0 + m, ts_], o[0:m, :], [o], [sc[dest]])
            n += 1
    p.release(mk3)
    mk4 = p.mark()
    pw = [p.psum("pwc%d_%d" % (l, i), [128, 512], F32) for i in range(2)]
    st16 = [p.sbuf("st16c_%d_%d" % (l, i), [128, 512], BF16) for i in range(2)]
    wg32 = p.sbuf("wg32_%d" % l, [128, 8, 512], F32)
    p.dma(wg32[:], W["w_in"][l, :, C_GV:C_GV + 512].rearrange("(c p) n -> p c n", p=128), [], [wg32])
    wg = p.sbuf("wg_%d" % l, [128, 8, 512], BF16)
    p.cp("pool", wg[:], wg32[:], [wg32], [wg])
    for i in range(NT_):
        a = pw[i % 2]
        o = st16[i % 2]
        for k in range(8):
            p.mm(a[:], hT[:, k, i * 128:(i + 1) * 128], wg[:, k, :], k == 0, k == 7, [hT, wg], [a])
        p.cp("act", o[:], a[:], [a], [o])
        p.dma(sc["GV"][i * 128:(i + 1) * 128, :], o[:], [o], [sc["GV"]])
    p.release(mk4)


def phase_mla(p, c, S, l, sc):
    NB = S // 512
    NT_ = S // 128
    kt = p.sbuf("kt", [96, 8, S], BF16)
    for h in range(8):
        p.dma(kt[:, h, :], sc["KT"][h], [sc["KT"]], [kt])
    va = p.sbuf("va", [128, NT_, 520], BF16)
    p.dma(va[:], sc["VA"].t.rearrange("(n p) f -> p n f", p=128), [sc["VA"]], [va])
    qts = [p.sbuf("qt%d" % i, [96, 8, 512], BF16) for i in range(2)]
    bgs = [p.sbuf("bg%d" % i, [128, 4, 512], BF16) for i in range(2)]
    pts = [p.sbuf("pt%d" % i, [128, 512], BF16) for i in range(3)]
    ytm = p.sbuf("ytm", [128, 4, 512], F32)
    ys = [p.sbuf("ysm%d" % i, [128, 4, 512], BF16) for i in range(2)]
    rec = p.sbuf("rec", [128, 4], F32)
    pst = [p.psum("pst%d" % i, [128, 512], F32) for i in range(2)]
    acc = [p.psum("acc%d" % i, [128, 512], F32) for i in range(4)]
    pT = p.psum("pT", [128, 4, 128], F32)
    n = 0
    for qb in range(NB):
        qt = qts[qb % 2]
        bg = bgs[qb % 2]
        cols = slice(qb * 512, (qb + 1) * 512)
        p.dma(qt[:], sc["QT"][:, :, cols].rearrange("h p t -> p h t"), [sc["QT"]], [qt])
        p.dma(bg[:], sc["BG"][0:512, cols].rearrange("(c p) t -> p c t", p=128), [sc["BG"]], [bg])
        for h in range(8):
            nkt = 4 * (qb + 1)
            for kti in range(nkt):
                j = kti - 4 * qb
                q0 = 128 * j if j > 0 else 0
                N = 512 - q0
                st = pst[n % 2]
                pt = pts[n % 3]
                p.mm(st[:, 0:N], kt[:, h, kti * 128:(kti + 1) * 128], qt[:, h, q0:512], True, True, [kt, qt], [st])
                p.act(pt[:, 0:N], st[:, 0:N], AF.Exp, [st], [pt])
                if j >= 0:
                    p.op("pool", lambda e, pt=pt: e.affine_select(pt[:, 0:128], pt[:, 0:128], [[1, 128]], ALU.is_ge, 0.0,
                                                                  base=0, channel_multiplier=-1), [pt], [pt])
                for qs in range(q0 // 128, 4):
                    p.mm(acc[qs][:, 0:65], pt[:, qs * 128 - q0:(qs + 1) * 128 - q0], va[:, kti, h * 65:(h + 1) * 65],
                         kti == 0, kti == 4 * qb + qs, [pt, va], [acc[qs]])
                n += 1
            for qs in range(4):
                p.op("dve", lambda e, qs=qs: e.reciprocal(rec[:, qs:qs + 1], acc[qs][:, 64:65]), [acc[qs]], [rec])
                p.ts("dve", ytm[:, qs, h * 64:(h + 1) * 64], acc[qs][:, 0:64], rec[:, qs:qs + 1], None, ALU.mult, None,
                     [acc[qs], rec], [ytm])
        yo = ys[qb % 2]
        for fc in range(4):
            for qs in range(4):
                p.tr(pT[:, qs, :], ytm[:, qs, fc * 128:(fc + 1) * 128], c.idf[:], [ytm, c.idf], [pT], signal=(qs == 3))
            p.tt("dve", yo[:, fc, :], pT[:].rearrange("p a b -> p (a b)"), bg[:, fc, :], ALU.mult, [pT, bg], [yo])
        p.dma(sc["YS"][0:512, cols].rearrange("(c p) t -> p c t", p=128), yo[:], [yo], [sc["YS"]])


def phase_gla(p, c, S, l, W, sc):
    NB = S // 512
    H = 4
    aup = p.sbuf("g_aup", [16, 256], F32)
    p.dma(aup[:], W["gla_a_up"][l], [], [aup])
    nab = p.sbuf("g_nab", [64, H], F32)
    p.dma(nab[:], W["gla_a_b"][l].rearrange("(h d) -> d h", d=64), [], [nab], allow_slow_non_contiguous=True)
    p.ts("dve", nab[:], nab[:], -1.0, None, ALU.mult, None, [nab], [nab])
    gn = colvec(p, "g_gn", W["gla_norm"][l], 4)
    one1 = p.sbuf("g_one1", [128, 1], F32)
    p.memset("pool", one1[:], 1.0, [one1])
    maskr = p.sbuf("g_maskr", [64, 512], F32)
    p.memset("pool", maskr[:], 1.0, [maskr])
    p.memset("pool", maskr[:].rearrange("p (c t) -> p c t", t=64)[:, :, 0:1], 0.0, [maskr])
    msk = p.sbuf("g_msk", [64, H, 64], F32)
    p.memset("pool", msk[:], 1.0, [msk])
    for h in range(H):
        p.op("pool", lambda e, h=h: e.affine_select(msk[:, h, :], msk[:, h, :], [[1, 64]], ALU.is_ge, 0.0,
                                                    base=0, channel_multiplier=-1), [msk], [msk])
    st = p.sbuf("g_st", [64, H, 128], F32)
    stb = p.sbuf("g_stb", [64, H, 128], BF16)
    st2 = p.sbuf("g_st2", [64, H, 128], F32)
    p.memset("pool", st[:], 0.0, [st])
    p.memset("pool", stb[:], 0.0, [stb])
    gq = [p.sbuf("g_q%d" % i, [64, H, 512], F32) for i in range(2)]
    gk = [p.sbuf("g_k%d" % i, [64, H, 512], F32) for i in range(2)]
    gl = [p.sbuf("g_l%d" % i, [16, 512], F32) for i in range(2)]
    gv = [p.sbuf("g_v%d" % i, [64, 8, 512], BF16) for i in range(2)]
    bg = [p.sbuf("g_bg%d" % i, [128, H, 512], BF16) for i in range(2)]
    e1 = p.sbuf("g_e1", [64, H, 512], F32)
    cs = p.sbuf("g_cs", [64, H, 512], F32)
    eb = p.sbuf("g_eb", [64, H, 512], F32)
    enb = p.sbuf("g_enb", [64, H, 512], F32)
    qe = p.sbuf("g_qe", [64, H, 512], BF16)
    ke = p.sbuf("g_ke", [64, H, 512], BF16)
    att = [p.sbuf("g_att%d" % i, [64, H, 64], BF16) for i in range(2)]
    ketm = [p.sbuf("g_ketm%d" % i, [64, H, 64], BF16) for i in range(2)]
    sq = p.sbuf("g_sq", [64, H, 128], F32)
    ssg = p.sbuf("g_ss", [64, H], F32)
    yn = [p.sbuf("g_yn%d" % i, [64, H, 128], F32) for i in range(2)]
    yT = p.sbuf("g_yT", [128, H, 512], F32)
    ysg = [p.sbuf("g_ys%d" % i, [128, H, 512], BF16) for i in range(2)]
    pz = [p.psum("g_pz%d" % i, [64, 512], F32) for i in range(2)]
    pat = p.psum("g_pat", [64, H, 64], F32)
    po = p.psum("g_po", [64, H, 128], F32)
    pkt = p.psum("g_pkt", [64, H, 64], BF16)
    pst = p.psum("g_pst", [64, H, 128], F32)
    pyT = p.psum("g_pyT", [128, H, 64], F32)
    for tb in range(NB):
        s_ = tb % 2
        cols = slice(tb * 512, (tb + 1) * 512)
        p.dma(gq[s_][:], sc["GQ"][:, cols].rearrange("(h d) t -> d h t", d=64), [sc["GQ"]], [gq[s_]])
        p.dma(gk[s_][:], sc["GK"][:, cols].rearrange("(h d) t -> d h t", d=64), [sc["GK"]], [gk[s_]])
        p.dma(gl[s_][:], sc["GL"][:, cols], [sc["GL"]], [gl[s_]])
        p.dma(gv[s_][:], sc["GV"][cols, :].rearrange("(c p) f -> p c f", p=64), [sc["GV"]], [gv[s_]])
        p.dma(bg[s_][:], sc["BG"][1024:1536, cols].rearrange("(c p) t -> p c t", p=128), [sc["BG"]], [bg[s_]])
        for h in range(H):
            z = pz[h % 2]
            p.mm(z[:], aup[:, h * 64:(h + 1) * 64], gl[s_][:], True, True, [aup, gl[s_]], [z])
            p.act(e1[:, h, :], z[:], AF.Exp, [z, nab], [e1], bias=nab[:, h:h + 1], scale=-1.0)
        p.act(e1[:], e1[:], AF.Ln, [e1, one1], [e1], bias=one1[0:64, :], scale=1.0)
        for h in range(H):
            p.op("dve", lambda e, h=h: e.tensor_tensor_scan(cs[:, h, :], maskr[:], e1[:, h, :], 0.0, ALU.mult, ALU.add),
                 [maskr, e1], [cs])
        p.act(eb[:], cs[:], AF.Exp, [cs], [eb], scale=-1.0 / 16)
        p.act(enb[:], cs[:], AF.Exp, [cs], [enb], scale=1.0 / 16)
        p.stt("dve", qe[:], gq[s_][:], 0.125, eb[:], ALU.mult, ALU.mult, [gq[s_], eb], [qe])
        p.tt("pool", ke[:], gk[s_][:], enb[:], ALU.mult, [gk[s_], enb], [ke])
        for cch in range(8):
            cc = slice(cch * 64, (cch + 1) * 64)
            a_ = att[cch % 2]
            kt_ = ketm[cch % 2]
            y_ = yn[cch % 2]
            for h in range(H):
                p.mm(pat[:, h, :], ke[:, h, cc], qe[:, h, cc], True, True, [ke, qe], [pat])
            p.tt("dve", a_[:], pat[:], msk[:], ALU.mult, [pat, msk], [a_])
            for h in range(H):
                p.tr(pkt[:, h, :], ke[:, h, cc], c.idb[0:64, 0:64], [ke, c.idb], [pkt], signal=(h == H - 1))
            p.cp("act", kt_[:], pkt[:], [pkt], [kt_])
            for h in range(H):
                p.mm(po[:, h, :], a_[:, h, :], gv[s_][:, cch, h * 128:(h + 1) * 128], True, False, [a_, gv[s_]], [po])
                p.mm(po[:, h, :], qe[:, h, cc], stb[:, h, :], False, True, [qe, stb], [po])
            for h in range(H):
                p.mm(pst[:, h, :], kt_[:, h, :], gv[s_][:, cch, h * 128:(h + 1) * 128], True, True, [kt_, gv[s_]], [pst])
            p.tt("dve", st2[:], st[:], pst[:], ALU.add, [st, pst], [st2])
            ebl = eb[:, :, cch * 64 + 63:cch * 64 + 64].broadcast_to([64, H, 128])
            p.tt("dve", st[:], st2[:], ebl, ALU.mult, [st2, eb], [st])
            p.cp("pool", stb[:], st[:], [st], [stb])
            p.act(sq[:], po[:], AF.Square, [po], [sq])
            p.op("dve", lambda e: e.tensor_reduce(ssg[:], sq[:], AX.X, ALU.add), [sq], [ssg])
            p.act(ssg[:], ssg[:], AF.Ln, [ssg, c.epsb], [ssg], bias=c.epsb[0:64, :], scale=1.0 / 128)
            p.act(ssg[:], ssg[:], AF.Exp, [ssg], [ssg], scale=-0.5)
            p.tt("dve", y_[:], po[:], ssg[:].unsqueeze(2).broadcast_to([64, H, 128]), ALU.mult, [po, ssg], [y_])
            for h in range(H):
                p.tr(pyT[:, h, :], y_[:, h, :], c.idf[0:64, 0:64], [y_, c.idf], [pyT], signal=(h == H - 1))
            p.cp("act", yT[:, :, cc], pyT[:], [pyT], [yT])
        o_ = ysg[s_]
        for h in range(H):
            p.stt("dve", o_[:, h, :], yT[:, h, :], gn[:, h:h + 1], bg[s_][:, h, :], ALU.mult, ALU.mult, [yT, gn, bg[s_]], [o_])
        p.dma(sc["YS"][1024:1536, cols].rearrange("(c p) t -> p c t", p=128), o_[:], [o_], [sc["YS"]])


def phase_rwkv(p, c, S, l, W, sc, TB=128):
    NB = S // TB
    NCH = TB // 64
    H = 8
    C0 = math.exp(-0.5)
    vec = lambda name, ap: _hv(p, name, ap)
    mu_r = vec("r_mur", W["rwkv_mu"][l, 0:512])
    mu_k = vec("r_muk", W["rwkv_mu"][l, 512:1024])
    mu_v = vec("r_muv", W["rwkv_mu"][l, 1024:1536])
    mu_w = p.sbuf("r_muw", [64, 2], F32)
    p.dma(mu_w[:], W["rwkv_mu"][l, 1536:1664].rearrange("(c p) -> p c", p=64), [], [mu_w], allow_slow_non_contiguous=True)
    w0 = vec("r_w0", W["rwkv_w0"][l])
    a0 = vec("r_a0", W["rwkv_a0"][l])
    k_k = vec("r_kk", W["rwkv_k_k"][l])
    k_a = vec("r_ka", W["rwkv_k_a"][l])
    r_k = vec("r_rk", W["rwkv_r_k"][l])
    lnw = vec("r_lnw", W["rwkv_ln_w"][l])
    lnb = vec("r_lnb", W["rwkv_ln_b"][l])
    wup = p.sbuf("r_wup", [64, 512], F32)
    p.dma(wup[:], W["rwkv_w_up"][l], [], [wup])
    aup = p.sbuf("r_aup", [64, 512], F32)
    p.dma(aup[:], W["rwkv_a_up"][l], [], [aup])
    ones = p.sbuf("r_ones", [64, 64], F32)
    p.memset("pool", ones[:], 1.0, [ones])
    gne = p.sbuf("r_gne", [64, 1], F32)
    p.memset("pool", gne[:], 64e-5, [gne])
    maskr = p.sbuf("r_maskr", [64, TB], F32)
    p.memset("pool", maskr[:], 1.0, [maskr])
    p.memset("pool", maskr[:].rearrange("p (c t) -> p c t", t=64)[:, :, 0:1], 0.0, [maskr])

    def mk_mask(name, pat, cm, op):
        m = p.sbuf(name, [64, H, 64], F32)
        p.memset("pool", m[:], 1.0, [m])
        for h in range(H):
            p.op("pool", lambda e, h=h: e.affine_select(m[:, h, :], m[:, h, :], pat, op, 0.0, base=0, channel_multiplier=cm), [m], [m])
        return m
    mL = mk_mask("r_mL", [[-1, 64]], 1, ALU.is_gt)
    mSU = mk_mask("r_mSU", [[1, 64]], -1, ALU.is_gt)
    mU = mk_mask("r_mU", [[1, 64]], -1, ALU.is_ge)
    idh = mk_mask("r_idh", [[1, 64]], -1, ALU.is_equal)

    def A3(name, dt=F32, n=TB):
        return p.sbuf(name, [64, H, n], dt)
    ur, uk, uv = A3("r_ur", n=TB + 1), A3("r_uk", n=TB + 1), A3("r_uv", n=TB + 1)
    uw = p.sbuf("r_uw", [64, 2, TB + 1], F32)
    r_, k_, v_ = A3("r_r"), A3("r_k"), A3("r_v")
    lwa = p.sbuf("r_lwa", [64, 2, TB], F32)
    tmp = A3("r_tmp")
    sg, a_, cs = A3("r_sg"), A3("r_a"), A3("r_cs")
    g_, gi, gp = A3("r_g"), A3("r_gi"), A3("r_gp")
    kkn, kp = A3("r_kkn"), A3("r_kp")
    Ab, Bb, Kb, Rb = A3("r_Ab"), A3("r_Bb"), A3("r_Kb"), A3("r_Rb")
    bon = A3("r_bon")
    yfm = A3("r_yfm")
    bgt = A3("r_bgt", BF16)
    yso = A3("r_yso", BF16)
    T0 = p.sbuf("r_T0", [64, H, 64], F32)
    p.memset("pool", T0[:], 0.0, [T0])
    T1 = p.sbuf("r_T1", [64, H, 64], F32)

    def S3(name):
        return p.sbuf(name, [64, H, 64], F32)
    P_ = [S3("r_P0"), S3("r_P1")]
    PT_ = [S3("r_PT0"), S3("r_PT1")]
    NT = S3("r_NT")
    LakT, ArbT, ArkT = S3("r_LakT"), S3("r_ArbT"), S3("r_ArkT")
    Vtm, Btm, Ktm = S3("r_Vtm"), S3("r_Btm"), S3("r_Ktm")
    X_, U_ = S3("r_X"), S3("r_U")
    yc, ysq, ynm = S3("r_yc"), S3("r_ysq"), S3("r_ynm")
    mean = p.sbuf("r_mean", [64, H], F32)
    var = p.sbuf("r_var", [64, H], F32)
    pz = [p.psum("r_pz%d" % i, [64, 2, TB], F32) for i in range(2)]
    pg = [p.psum("r_pg%d" % i, [64, H, 64], F32) for i in range(6)]
    pgi = [0]

    def PG():
        t = pg[pgi[0] % 6]
        pgi[0] += 1
        return t

    def bc(t2):
        return t2[:].unsqueeze(2).broadcast_to([64, H, TB])

    for tb in range(NB):
        t0 = tb * TB
        cols = slice(t0, t0 + TB)
        for (dst, r0) in ((ur, 0), (uk, 512), (uv, 1024)):
            if tb == 0:
                p.memset("pool", dst[:, :, 0:1], 0.0, [dst])
                p.dma(dst[:, :, 1:TB + 1], sc["U"][r0:r0 + 512, cols].rearrange("(h d) t -> d h t", d=64), [sc["U"]], [dst])
            else:
                p.dma(dst[:], sc["U"][r0:r0 + 512, t0 - 1:t0 + TB].rearrange("(h d) t -> d h t", d=64), [sc["U"]], [dst])
        if tb == 0:
            p.memset("pool", uw[:, :, 0:1], 0.0, [uw])
            p.dma(uw[:, :, 1:TB + 1], sc["U"][1536:1664, cols].rearrange("(c p) t -> p c t", p=64), [sc["U"]], [uw])
        else:
            p.dma(uw[:], sc["U"][1536:1664, t0 - 1:t0 + TB].rearrange("(c p) t -> p c t", p=64), [sc["U"]], [uw])
        p.dma(bgt[:], sc["BG"][512:1024, cols].rearrange("(h d) t -> d h t", d=64), [sc["BG"]], [bgt])
        for (src, dst, mu) in ((ur, r_, mu_r), (uk, k_, mu_k), (uv, v_, mu_v)):
            p.tt("dve", tmp[:], src[:, :, 0:TB], src[:, :, 1:TB + 1], ALU.subtract, [src], [tmp])
            p.tt("pool", tmp[:], tmp[:], bc(mu), ALU.mult, [tmp, mu], [tmp])
            p.tt("dve", dst[:], tmp[:], src[:, :, 1:TB + 1], ALU.add, [tmp, src], [dst])
        p.tt("dve", lwa[:], uw[:, :, 0:TB], uw[:, :, 1:TB + 1], ALU.subtract, [uw], [lwa])
        p.tt("pool", lwa[:], lwa[:], mu_w[:].unsqueeze(2).broadcast_to([64, 2, TB]), ALU.mult, [lwa, mu_w], [lwa])
        p.tt("dve", lwa[:], lwa[:], uw[:, :, 1:TB + 1], ALU.add, [lwa, uw], [lwa])
        p.act(lwa[:, 0, :], lwa[:, 0, :], AF.Tanh, [lwa], [lwa])
        for h in range(H):
            z = pz[h % 2]
            p.mm(z[:, 0, :], wup[:, h * 64:(h + 1) * 64], lwa[:, 0, :], True, True, [wup, lwa], [z])
            p.mm(z[:, 1, :], aup[:, h * 64:(h + 1) * 64], lwa[:, 1, :], True, True, [aup, lwa], [z])
            p.act(sg[:, h, :], z[:, 0, :], AF.Sigmoid, [z, w0], [sg], bias=w0[:, h:h + 1], scale=1.0)
            p.act(a_[:, h, :], z[:, 1, :], AF.Sigmoid, [z, a0], [a_], bias=a0[:, h:h + 1], scale=1.0)
        for h in range(H):
            p.op("dve", lambda e, h=h: e.tensor_tensor_scan(cs[:, h, :], maskr[:], sg[:, h, :], 0.0, ALU.mult, ALU.add),
                 [maskr, sg], [cs])
        p.act(g_[:], cs[:], AF.Exp, [cs], [g_], scale=-C0)
        p.act(gi[:], cs[:], AF.Exp, [cs], [gi], scale=C0)
        p.tt("pool", tmp[:], cs[:], sg[:], ALU.subtract, [cs, sg], [tmp])
        p.act(gp[:], tmp[:], AF.Exp, [tmp], [gp], scale=-C0)
        p.tt("dve", kkn[:], k_[:], bc(k_k), ALU.mult, [k_, k_k], [kkn])
        p.act(tmp[:], kkn[:], AF.Square, [kkn], [tmp])
        for hp in range(4):
            z = pz[hp % 2]
            for j in range(2):
                p.mm(z[:, j, :], ones[:], tmp[:, hp * 2 + j, :], True, True, [ones, tmp], [z])
            p.ts("dve", sg[:, hp * 2:hp * 2 + 2, :], z[:], 1e-24, None, ALU.max, None, [z], [sg])
        p.act(sg[:], sg[:], AF.Ln, [sg], [sg])
        p.act(sg[:], sg[:], AF.Exp, [sg], [sg], scale=-0.5)
        p.tt("dve", kkn[:], kkn[:], sg[:], ALU.mult, [kkn, sg], [kkn])
        p.stt("dve", tmp[:], a_[:], -1.0, bc(k_a), ALU.add, ALU.mult, [a_, k_a], [tmp])
        p.stt("dve", kp[:], tmp[:], 1.0, k_[:], ALU.add, ALU.mult, [tmp, k_], [kp])
        p.stt("dve", Ab[:], kkn[:], -1.0, gp[:], ALU.mult, ALU.mult, [kkn, gp], [Ab])
        p.tt("pool", tmp[:], kkn[:], a_[:], ALU.mult, [kkn, a_], [tmp])
        p.tt("dve", Bb[:], tmp[:], gi[:], ALU.mult, [tmp, gi], [Bb])
        p.tt("pool", Kb[:], kp[:], gi[:], ALU.mult, [kp, gi], [Kb])
        p.tt("dve", Rb[:], r_[:], g_[:], ALU.mult, [r_, g_], [Rb])
        p.tt("pool", tmp[:], r_[:], kp[:], ALU.mult, [r_, kp], [tmp])
        p.tt("dve", tmp[:], tmp[:], bc(r_k), ALU.mult, [tmp, r_k], [tmp])
        for hp in range(4):
            z = pz[hp % 2]
            for j in range(2):
                p.mm(z[:, j, :], ones[:], tmp[:, hp * 2 + j, :], True, True, [ones, tmp], [z])
            p.tt("dve", bon[:, hp * 2:hp * 2 + 2, :], z[:], v_[:, hp * 2:hp * 2 + 2, :], ALU.mult, [z, v_], [bon])
        for cch in range(NCH):
            cc = slice(cch * 64, (cch + 1) * 64)

            def scores(lh, rh, mask, dst, eng):
                ps = PG()
                for h in range(H):
                    p.mm(ps[:, h, :], lh[:, h, cc], rh[:, h, cc], True, True, [lh, rh], [ps])
                p.tt(eng, dst[:], ps[:], mask[:], ALU.mult, [ps, mask], [dst])
            P, PT = P_[0], PT_[0]
            scores(Ab, Bb, mL, P, "dve")
            scores(Bb, Ab, mSU, PT, "dve")
            p.tt("pool", NT[:], PT[:], idh[:], ALU.add, [PT, idh], [NT])
            scores(Kb, Ab, mSU, LakT, "dve")
            scores(Bb, Rb, mU, ArbT, "dve")
            scores(Kb, Rb, mU, ArkT, "dve")

            def transp(src, dst):
                ps = PG()
                for h in range(H):
                    p.tr(ps[:, h, :], src[:, h, cc], c.idf[0:64, 0:64], [src, c.idf], [ps], signal=(h == H - 1))
                p.cp("act", dst[:], ps[:], [ps], [dst])
            transp(v_, Vtm)
            transp(Bb, Btm)
            transp(Kb, Ktm)
            cur = 0
            for step in range(5):
                Pn, PTn = P_[1 - cur], PT_[1 - cur]
                ps = PG()
                for h in range(H):
                    p.mm(ps[:, h, :], PT_[cur][:, h, :], P_[cur][:, h, :], True, True, [PT_[cur], P_[cur]], [ps])
                p.cp("act", Pn[:], ps[:], [ps], [Pn])
                if step < 4:
                    ps2 = PG()
                    for h in range(H):
                        p.mm(ps2[:, h, :], P_[cur][:, h, :], PT_[cur][:, h, :], True, True, [PT_[cur], P_[cur]], [ps2])
                    p.cp("dve", PTn[:], ps2[:], [ps2], [PTn])
                ps3 = PG()
                for h in range(H):
                    p.mm(ps3[:, h, :], Pn[:, h, :], NT[:, h, :], True, True, [Pn, NT], [ps3])
                p.tt("dve", NT[:], NT[:], ps3[:], ALU.add, [NT, ps3], [NT])
                cur = 1 - cur
            ps = PG()
            for h in range(H):
                p.mm(ps[:, h, :], Ab[:, h, cc], T0[:, h, :], True, False, [Ab, T0], [ps])
                p.mm(ps[:, h, :], LakT[:, h, :], Vtm[:, h, :], False, True, [LakT, Vtm], [ps])
            p.cp("act", X_[:], ps[:], [ps], [X_])
            ps = PG()
            for h in range(H):
                p.mm(ps[:, h, :], NT[:, h, :], X_[:, h, :], True, True, [NT, X_], [ps])
            p.cp("act", U_[:], ps[:], [ps], [U_])
            py = PG()
            for h in range(H):
                p.mm(py[:, h, :], Rb[:, h, cc], T0[:, h, :], True, False, [Rb, T0], [py])
                p.mm(py[:, h, :], ArbT[:, h, :], U_[:, h, :], False, False, [ArbT, U_], [py])
                p.mm(py[:, h, :], ArkT[:, h, :], Vtm[:, h, :], False, True, [ArkT, Vtm], [py])
            ps = PG()
            for h in range(H):
                p.mm(ps[:, h, :], Btm[:, h, :], U_[:, h, :], True, False, [Btm, U_], [ps])
                p.mm(ps[:, h, :], Ktm[:, h, :], Vtm[:, h, :], False, True, [Ktm, Vtm], [ps])
            p.tt("dve", T1[:], T0[:], ps[:], ALU.add, [T0, ps], [T1])
            gC = g_[:, :, cch * 64 + 63:cch * 64 + 64].broadcast_to([64, H, 64])
            p.tt("dve", T0[:], T1[:], gC, ALU.mult, [T1, g_], [T0])
            p.op("dve", lambda e, py=py: e.tensor_reduce(mean[:], py[:], AX.X, ALU.add), [py], [mean])
            p.ts("dve", mean[:], mean[:], 1.0 / 64, None, ALU.mult, None, [mean], [mean])
            p.tt("dve", yc[:], py[:], mean[:].unsqueeze(2).broadcast_to([64, H, 64]), ALU.subtract, [py, mean], [yc])
            p.act(ysq[:], yc[:], AF.Square, [yc], [ysq])
            p.op("dve", lambda e: e.tensor_reduce(var[:], ysq[:], AX.X, ALU.add), [ysq], [var])
            p.act(var[:], var[:], AF.Ln, [var, gne], [var], bias=gne[:], scale=1.0 / 64)
            p.act(var[:], var[:], AF.Exp, [var], [var], scale=-0.5)
            p.tt("pool", ynm[:], yc[:], var[:].unsqueeze(2).broadcast_to([64, H, 64]), ALU.mult, [yc, var], [ynm])
            ps = PG()
            for h in range(H):
                p.tr(ps[:, h, :], ynm[:, h, :], c.idf[0:64, 0:64], [ynm, c.idf], [ps], signal=(h == H - 1))
            p.cp("act", yfm[:, :, cc], ps[:], [ps], [yfm])
        p.tt("dve", yfm[:], yfm[:], bc(lnw), ALU.mult, [yfm, lnw], [yfm])
        p.tt("pool", yfm[:], yfm[:], bc(lnb), ALU.add, [yfm, lnb], [yfm])
        p.tt("dve", yfm[:], yfm[:], bon[:], ALU.add, [yfm, bon], [yfm])
        p.tt("dve", yso[:], yfm[:], bgt[:], ALU.mult, [yfm, bgt], [yso])
        p.dma(sc["YS"][512:1024, cols].rearrange("(h d) t -> d h t", d=64), yso[:], [yso], [sc["YS"]])


def _hv(p, name, ap):
    t = p.sbuf(name, [64, 8], F32)
    p.dma(t[:], ap.rearrange("(h d) -> d h", d=64), [], [t], allow_slow_non_contiguous=True)
    return t


def phase_out(p, c, S, l, xin, xout, W, sc):
    NB = S // 512
    wbo = p.sbuf("wbo", [128, 12, D], BF16)
    wout = p.sbuf("wout", [128, 8, D], BF16)
    gpb = p.sbuf("gpb", [128, D], F32)
    p.dma(gpb[:], W["norm_post"][l:l + 1, :].partition_broadcast(128), [], [gpb])
    mk = p.mark()
    stg = [p.sbuf("wstg%d" % i, [128, 4, D], F32) for i in range(2)]
    for n in range(3):
        p.dma(stg[n % 2][:], W["w_branch_out"][l, n].rearrange("(k p) d -> p k d", p=128), [], [stg[n % 2]])
        p.cp("pool", wbo[:, n * 4:(n + 1) * 4, :], stg[n % 2][:], [stg[n % 2]], [wbo])
    for hf in range(2):
        p.dma(stg[(hf + 1) % 2][:], W["w_out"][l, hf * 512:(hf + 1) * 512, :].rearrange("(k p) d -> p k d", p=128), [], [stg[(hf + 1) % 2]])
        p.cp("pool", wout[:, hf * 4:(hf + 1) * 4, :], stg[(hf + 1) % 2][:], [stg[(hf + 1) % 2]], [wout])
    p.release(mk)
    ysb = [p.sbuf("ysb%d" % i, [128, 12, 512], BF16) for i in range(2)]
    mgb = [p.sbuf("mgb%d" % i, [128, 24, 512], BF16) for i in range(2)]
    mT = p.sbuf("mT", [128, 8, 512], BF16)
    m32 = p.sbuf("m32", [128, 512], F32)
    t32 = [p.sbuf("t32_%d" % i, [128, 512], F32) for i in range(2)]
    xt = [p.sbuf("xo%d" % i, [128, D], F32) for i in range(2)]
    o32 = [p.sbuf("o32_%d" % i, [128, D], F32) for i in range(2)]
    junk = p.sbuf("junko", [128, 512], BF16)
    ssq = p.sbuf("ssq", [128, 2], F32)
    rstd = p.sbuf("rstdo", [128, 1], F32)
    pb = [p.psum("pb%d" % i, [128, 512], F32) for i in range(3)]
    po = [p.psum("po%d" % i, [128, 512], F32) for i in range(2)]
    it = 0
    for tb in range(NB):
        cols = slice(tb * 512, (tb + 1) * 512)
        ysx, mgx = ysb[tb % 2], mgb[tb % 2]
        p.dma(ysx[:], sc["YS"][:, cols].rearrange("(c p) t -> p c t", p=128), [sc["YS"]], [ysx])
        p.dma(mgx[:], sc["MG"][:, cols].rearrange("(c p) t -> p c t", p=128), [sc["MG"]], [mgx])
        for cch in range(8):
            for n in range(3):
                for k in range(4):
                    p.mm(pb[n][:], wbo[:, n * 4 + k, cch * 128:(cch + 1) * 128], ysx[:, n * 4 + k, :], k == 0, k == 3,
                         [wbo, ysx], [pb[n]])
            p.tt("dve", m32[:], pb[0][:], mgx[:, cch, :], ALU.mult, [pb[0], mgx], [m32])
            p.tt("dve", t32[0][:], pb[1][:], mgx[:, 8 + cch, :], ALU.mult, [pb[1], mgx], [t32[0]])
            p.tt("dve", t32[1][:], pb[2][:], mgx[:, 16 + cch, :], ALU.mult, [pb[2], mgx], [t32[1]])
            p.tt("pool", m32[:], m32[:], t32[0][:], ALU.add, [m32, t32[0]], [m32])
            p.tt("pool", mT[:, cch, :], m32[:], t32[1][:], ALU.add, [m32, t32[1]], [mT])
        for tsub in range(4):
            s_ = it % 2
            it += 1
            r0 = tb * 512 + tsub * 128
            p.dma(xt[s_][:], xin[r0:r0 + 128, :], [xin], [xt[s_]])
            for hf in range(2):
                for k in range(8):
                    p.mm(po[hf][:], mT[:, k, tsub * 128:(tsub + 1) * 128], wout[:, k, hf * 512:(hf + 1) * 512], k == 0, k == 7,
                         [mT, wout], [po[hf]])
                p.act(junk[:], po[hf][:], AF.Square, [po[hf]], [junk, ssq], accum_out=ssq[:, hf:hf + 1])
            p.tt("dve", rstd[:], ssq[:, 0:1], ssq[:, 1:2], ALU.add, [ssq], [rstd])
            p.act(rstd[:], rstd[:], AF.Ln, [rstd, c.epsb], [rstd], bias=c.epsb[:], scale=1.0 / D)
            p.act(rstd[:], rstd[:], AF.Exp, [rstd], [rstd], scale=-0.5)
            for hf in range(2):
                p.stt("dve", o32[s_][:, hf * 512:(hf + 1) * 512], po[hf][:], rstd[:, 0:1], gpb[:, hf * 512:(hf + 1) * 512],
                      ALU.mult, ALU.mult, [po[hf], rstd, gpb], [o32[s_]])
            p.tt("pool", o32[s_][:], o32[s_][:], xt[s_][:], ALU.add, [o32[s_], xt[s_]], [o32[s_]])
            p.dma(xout[r0:r0 + 128, :], o32[s_][:], [o32[s_]], [xout])


def make_scratch(p, S, dbg):
    kind = "ExternalOutput" if dbg else "Internal"
    sc = {}
    sc["QT"] = p.dram("QT", [8, 96, S], BF16, kind)
    sc["KT"] = p.dram("KT", [8, 96, S], BF16, kind)
    sc["VA"] = p.dram("VA", [S, 520], BF16, kind)
    sc["U"] = p.dram("U", [1664, S], F32, kind)
    sc["GQ"] = p.dram("GQ", [256, S], F32, kind)
    sc["GK"] = p.dram("GK", [256, S], F32, kind)
    sc["GV"] = p.dram("GV", [S, 512], BF16, kind)
    sc["GL"] = p.dram("GL", [16, S], F32, kind)
    sc["BG"] = p.dram("BG", [1536, S], BF16, kind)
    sc["MG"] = p.dram("MG", [3072, S], BF16, kind)
    sc["YS"] = p.dram("YS", [1536, S], BF16, kind)
    sc["X1"] = p.dram("X1", [S, D], F32, kind)
    return sc


WSPEC = {
    "norm_pre": ([L, D], F32), "w_in": ([L, D, NIN], F32), "wkr2": ([L, D, 192], F32),
    "mla_q_norm": ([L, 256], F32), "mla_kv_norm": ([L, 128], F32),
    "mla_w_uq": ([L, 256, 768], F32), "wuq_sw": ([L, 256, 768], F32),
    "wuk": ([L, 128, 512], F32), "wuv": ([L, 128, 512], F32),
    "rwkv_mu": ([L, 1664], F32), "rwkv_w0": ([L, 512], F32), "rwkv_w_up": ([L, 64, 512], F32),
    "rwkv_a0": ([L, 512], F32), "rwkv_a_up": ([L, 64, 512], F32), "rwkv_k_k": ([L, 512], F32),
    "rwkv_k_a": ([L, 512], F32), "rwkv_r_k": ([L, 512], F32), "rwkv_ln_w": ([L, 512], F32),
    "rwkv_ln_b": ([L, 512], F32), "gla_a_up": ([L, 16, 256], F32), "gla_a_b": ([L, 256], F32),
    "gla_norm": ([L, 512], F32), "w_branch_out": ([L, 3, 512, D], F32), "w_out": ([L, D, D], F32),
    "norm_post": ([L, D], F32),
}


def host_weights(inp):
    w = {}
    for k in WSPEC:
        if k in inp:
            w[k] = np.ascontiguousarray(np.asarray(inp[k], dtype=np.float32))
    win = w["w_in"]
    w["wkr2"] = np.ascontiguousarray(np.concatenate(
        [win[:, :, 320:416], win[:, :, 320:384], win[:, :, 400:416], win[:, :, 384:400]], axis=2))
    uq = w["mla_w_uq"].reshape(L, 256, 8, 96)
    w["wuq_sw"] = np.ascontiguousarray(np.concatenate(
        [uq[..., 0:64], uq[..., 80:96], uq[..., 64:80]], axis=-1).reshape(L, 256, 768))
    ukv = w["mla_w_ukv"] if "mla_w_ukv" in w else np.asarray(inp["mla_w_ukv"], dtype=np.float32)
    ukv = ukv.reshape(L, 128, 8, 128)
    w["wuk"] = np.ascontiguousarray(ukv[..., 0:64].reshape(L, 128, 512))
    w["wuv"] = np.ascontiguousarray(ukv[..., 64:128].reshape(L, 128, 512))
    w["rwkv_r_k"] = w["rwkv_r_k"].reshape(L, 512) if "rwkv_r_k" in w else np.asarray(inp["rwkv_r_k"], np.float32).reshape(L, 512)
    return w


def build(S, phases=("p1",), dbg=True, nlayers=1):
    nc = bass.Bass("TRN2", target_bir_lowering=False)
    p = Prog(nc)
    x_d = p.dram("x", [S, D], F32, "ExternalInput")
    pos_d = p.dram("pos", [1, S], I32, "ExternalInput")
    W = {k: p.dram(k, sh, dt, "ExternalInput").t for k, (sh, dt) in WSPEC.items()}
    out_d = p.dram("out", [S, D], F32, "ExternalOutput")
    sc = make_scratch(p, S, dbg)
    c = setup_consts(p, S, pos_d.t)
    final = [out_d]
    for l in range(nlayers):
        xin = x_d if l == 0 else sc["X1"]
        if "p1" in phases:
            mk = p.mark()
            phase1(p, c, S, l, xin, W, sc)
            p.release(mk)
        if "mla" in phases:
            mk = p.mark()
            phase_mla(p, c, S, l, sc)
            p.release(mk)
        if "rwkv" in phases:
            mk = p.mark()
            phase_rwkv(p, c, S, l, W, sc)
            p.release(mk)
        if "gla" in phases:
            mk = p.mark()
            phase_gla(p, c, S, l, W, sc)
            p.release(mk)
        if "out" in phases:
            mk = p.mark()
            xo = out_d if l == nlayers - 1 else sc["X1"]
            phase_out(p, c, S, l, xin, xo, W, sc)
            p.release(mk)
    if dbg:
        final = list(sc.values()) + [out_d]
    p.finish(final)
    return nc, p


ALL_PHASES = ("p1", "mla", "rwkv", "gla", "out")
_CACHE = {}


def kernel(**inputs):
    x = np.asarray(inputs["x"], dtype=np.float32)
    pos = np.asarray(inputs["positions"], dtype=np.int32)
    B, S, _ = x.shape
    Wh = host_weights(inputs)
    nc, _p = build(S, phases=ALL_PHASES, dbg=False, nlayers=L)
    in_maps = []
    for b in range(B):
        m = {"x": np.ascontiguousarray(x[b]), "pos": np.ascontiguousarray(pos[b:b + 1])}
        for k in WSPEC:
            m[k] = Wh[k]
        in_maps.append(m)
    res = run_bass_kernel_spmd(nc, in_maps, core_ids=list(range(B)))
    return np.stack([np.asarray(r["out"]) for r in res.results], axis=0).astype(np.float32)
```

```python
import math
import numpy as np
import concourse.bass as bass
import concourse.mybir as mybir
from concourse.bass_utils import run_bass_kernel_spmd

F32 = mybir.dt.float32
BF16 = mybir.dt.bfloat16
I32 = mybir.dt.int32
ALU = mybir.AluOpType
AF = mybir.ActivationFunctionType
AX = mybir.AxisListType

SEM_CAP = 30000
D = 1024
NIN = 7728
L = 2
EPS = 1e-6


class Buf:
    __slots__ = ("name", "w", "r")

    def __init__(self, name):
        self.name = name
        self.w = None
        self.r = {}


class T:
    def __init__(self, t, name):
        self.t = t
        self.b = Buf(name)

    def __getitem__(self, k):
        return self.t[k]


def _fs(ap):
    n = 1
    for d in ap.shape[1:]:
        n *= int(d)
    return n


def _nbytes(ap):
    n = 1
    for d in ap.shape:
        n *= int(d)
    return n * mybir.dt.size(ap.dtype)


class Prog:
    CE = ("pe", "act", "dve", "pool")
    QE = ("sp",)
    WINDOW = 48
    SYNC = 300.0

    def __init__(self, nc, n_dma_sems=32):
        self.nc = nc
        self.sems = {}
        self._ctx = []
        self._semctx = []
        self.cur = {}
        self.seen = {e: {} for e in self.CE + self.QE}
        self.nsem = 0
        self.old_sems = []
        for e in self.CE:
            self._new_eng_sem(e)
        self.dma_sems = []
        for i in range(n_dma_sems):
            k = self._alloc_sem("dma%d" % i)
            self.dma_sems.append([k, 0])
        self.dma_rr = 0
        self.ninst = 0
        self.ops = []
        self.uid = 0
        self.model_time = 0.0

    def _alloc_sem(self, name):
        cm = self.nc.semaphore(name)
        h = cm.__enter__()
        self._semctx.append(cm)
        self.sems[name] = h
        self.nsem += 1
        return name

    def _new_eng_sem(self, e):
        if e in self.cur:
            self.old_sems.append(tuple(self.cur[e]))
        k = self._alloc_sem("%s_s%d" % (e, self.nsem))
        self.cur[e] = [k, 0]

    def sbuf(self, name, shape, dt):
        self.uid += 1
        name = "%s_u%d" % (name, self.uid)
        cm = self.nc.sbuf_tensor(name, list(shape), dt)
        t = cm.__enter__()
        self._ctx.append(cm)
        return T(t, name)

    def psum(self, name, shape, dt):
        self.uid += 1
        name = "%s_u%d" % (name, self.uid)
        cm = self.nc.psum_tensor(name, list(shape), dt)
        t = cm.__enter__()
        self._ctx.append(cm)
        return T(t, name)

    def dram(self, name, shape, dt, kind="Internal"):
        t = self.nc.dram_tensor(name, list(shape), dt, kind=kind).ap()
        return T(t, name)

    def op(self, eng, fn, R=(), W=(), signal=True, dur=500.0):
        self.ops.append((eng, fn, [x.b for x in R], [x.b for x in W], float(dur), float(dur), False))
        self.ninst += 1

    def dma(self, out_ap, in_ap, R=(), W=(), q="sp", **kw):
        lat = 2000.0 + _nbytes(out_ap) / 150.0
        self.ops.append((q, lambda e: e.dma_start(out=out_ap, in_=in_ap, **kw),
                         [x.b for x in R], [x.b for x in W], 80.0, lat, True))
        self.ninst += 1

    def mm(self, out, lhsT, rhs, start, stop, R, W, **kw):
        n = _fs(rhs)
        d = (50.0 + 1.0 * max(n, 64)) if lhsT.dtype == F32 else (40.0 + 0.42 * max(n, 64))
        self.op("pe", lambda e: e.matmul(out, lhsT, rhs, start=start, stop=stop, **kw), R, W, dur=d)

    def tr(self, out, in_, ident, R, W, signal=True):
        self.op("pe", lambda e: e.transpose(out, in_, ident), R, W, dur=110.0)

    def act(self, out, in_, func, R, W, **kw):
        self.op("act", lambda e: e.activation(out, in_, func, **kw), R, W, dur=260.0 + 0.83 * _fs(out))

    def _vd(self, eng, out, two_src):
        n = _fs(out)
        if eng == "pool":
            return 150.0 + 2.2 * n
        b16 = mybir.dt.size(out.dtype) == 2
        if two_src:
            return 160.0 + (1.04 * n)
        return 160.0 + (0.55 * n)

    def tt(self, eng, out, a, b, op, R, W):
        self.op(eng, lambda e: e.tensor_tensor(out, a, b, op), R, W, dur=self._vd(eng, out, True))

    def ts(self, eng, out, a, s1, s2, op0, op1, R, W):
        d = self._vd(eng, out, False)
        if s2 is None:
            self.op(eng, lambda e: e.tensor_scalar(out, a, s1, None, op0), R, W, dur=d)
        else:
            self.op(eng, lambda e: e.tensor_scalar(out, a, s1, s2, op0, op1), R, W, dur=d)

    def stt(self, eng, out, a, s, b, op0, op1, R, W):
        self.op(eng, lambda e: e.scalar_tensor_tensor(out, a, s, b, op0, op1), R, W, dur=self._vd(eng, out, True))

    def cp(self, eng, out, in_, R, W):
        if eng == "act":
            self.op(eng, lambda e: e.copy(out, in_), R, W, dur=260.0 + 0.83 * _fs(out))
        else:
            self.op(eng, lambda e: e.tensor_copy(out, in_), R, W, dur=self._vd(eng, out, False))

    def memset(self, eng, ap, val, W):
        self.op(eng, lambda e: e.memset(ap, val), (), W, dur=self._vd(eng, ap, False))

    def _eng(self, eng):
        nc = self.nc
        return {"pe": nc.tensor, "act": nc.scalar, "dve": nc.vector, "pool": nc.gpsimd, "sp": nc.sync}[eng]

    def flush(self):
        import bisect
        ops = self.ops
        self.ops = []
        n = len(ops)
        if n == 0:
            return
        engs = self.CE + self.QE
        lastw = {}
        readers = {}
        deps = [None] * n
        for i in range(n):
            eng, fn, R, W, occ, lat, isd = ops[i]
            d = set()
            for b in R:
                w = lastw.get(id(b))
                if w is not None:
                    d.add(w)
            for b in W:
                w = lastw.get(id(b))
                if w is not None:
                    d.add(w)
                rl = readers.get(id(b))
                if rl:
                    d.update(rl)
            d.discard(i)
            deps[i] = d
            for b in R:
                readers.setdefault(id(b), []).append(i)
            for b in W:
                lastw[id(b)] = i
                readers[id(b)] = []
        succ = [[] for _ in range(n)]
        nd = [0] * n
        for i in range(n):
            nd[i] = len(deps[i])
            for j in deps[i]:
                succ[j].append(i)
        bl = [0.0] * n
        for i in range(n - 1, -1, -1):
            m = 0.0
            for s_ in succ[i]:
                if bl[s_] > m:
                    m = bl[s_]
            bl[i] = m + ops[i][5] + 100.0
        ready = [0.0] * n
        free = {e: 0.0 for e in engs}
        cand = {e: [] for e in engs}
        for i in range(n):
            if nd[i] == 0:
                cand[ops[i][0]].append(i)
        order = {e: [] for e in engs}
        done = 0
        W_ = self.WINDOW
        SY = self.SYNC
        tmax = 0.0
        while done < n:
            best = None
            for e in engs:
                c = cand[e]
                if not c:
                    continue
                fe = free[e]
                bst = None
                for i in c[:W_]:
                    st = ready[i] if ready[i] > fe else fe
                    key = (st, -bl[i])
                    if bst is None or key < bst[2]:
                        bst = (st, i, key)
                if best is None or bst[2] < best[3]:
                    best = (bst[0], bst[1], e, bst[2])
            st, i, e = best[0], best[1], best[2]
            cand[e].remove(i)
            order[e].append(i)
            occ, lat = ops[i][4], ops[i][5]
            free[e] = st + occ
            fin = st + lat
            if fin > tmax:
                tmax = fin
            for s_ in succ[i]:
                rt = fin + (SY if ops[s_][0] != e else (0.0 if e == "pe" else 200.0))
                if rt > ready[s_]:
                    ready[s_] = rt
                nd[s_] -= 1
                if nd[s_] == 0:
                    bisect.insort(cand[ops[s_][0]], s_)
            done += 1
        self.model_time += tmax
        busy = {e: 0.0 for e in engs}
        for i in range(n):
            busy[ops[i][0]] += ops[i][4]
        if not hasattr(self, "regions"):
            self.regions = []
        self.regions.append((tmax, busy, n))
        need = [False] * n
        for i in range(n):
            e = ops[i][0]
            if ops[i][6] or not succ[i]:
                need[i] = True
                continue
            for s_ in succ[i]:
                if ops[s_][0] != e or e != "pe":
                    need[i] = True
                    break
        tick = [None] * n
        reuse_wait = {}
        for e in engs:
            for i in order[e]:
                if ops[i][6]:
                    slot = self.dma_sems[self.dma_rr]
                    self.dma_rr = (self.dma_rr + 1) % len(self.dma_sems)
                    if slot[1] > 0:
                        reuse_wait[i] = (slot[0], slot[1])
                    slot[1] += 16
                    tick[i] = (slot[0], slot[1])
                elif need[i]:
                    cur = self.cur[e]
                    if cur[1] >= SEM_CAP:
                        self._new_eng_sem(e)
                        cur = self.cur[e]
                    cur[1] += 1
                    tick[i] = (cur[0], cur[1])
        for e in engs:
            eo = self._eng(e)
            seen = self.seen[e]
            sems = self.sems
            for i in order[e]:
                w = {}
                for j in deps[i]:
                    if e == "pe" and ops[j][0] == "pe":
                        continue
                    k, v = tick[j]
                    if w.get(k, 0) < v:
                        w[k] = v
                if i in reuse_wait:
                    k, v = reuse_wait[i]
                    if w.get(k, 0) < v:
                        w[k] = v
                for k, v in w.items():
                    if seen.get(k, 0) < v:
                        seen[k] = v
                        eo.wait_ge(sems[k], v)
                ins = ops[i][1](eo)
                if tick[i] is not None:
                    ins.then_inc(sems[tick[i][0]], 16 if ops[i][6] else 1)

    def barrier(self):
        self.flush()
        allv = [(k, v) for (k, v) in [tuple(x) for x in self.cur.values()] if v > 0]
        allv += [(k, v) for (k, v) in self.old_sems]
        allv += [(k, v) for (k, v) in [tuple(x) for x in self.dma_sems] if v > 0]
        for eng in self.CE + self.QE:
            eo = self._eng(eng)
            for k, v in allv:
                if self.seen[eng].get(k, 0) < v:
                    self.seen[eng][k] = v
                    eo.wait_ge(self.sems[k], v)

    def mark(self):
        return len(self._ctx)

    def release(self, mark):
        self.barrier()
        while len(self._ctx) > mark:
            self._ctx.pop().__exit__(None, None, None)

    def finish(self, final):
        self.barrier()
        while self._ctx:
            self._ctx.pop().__exit__(None, None, None)
        while self._semctx:
            self._semctx.pop().__exit__(None, None, None)


C_CQ, C_CKV, C_KR, C_U = 0, 256, 384, 416
C_GQ, C_GK, C_GV, C_GL, C_BG, C_MG = 2080, 2336, 2592, 3104, 3120, 4656
SCALE = (64 + 32) ** -0.5


class Ctx:
    pass


def setup_consts(p, S, pos_d):
    c = Ctx()
    idf = p.sbuf("idf", [128, 128], F32)
    p.memset("pool", idf[:], 1.0, [idf])
    p.op("pool", lambda e: e.affine_select(idf[:], idf[:], [[1, 128]], ALU.is_equal, 0.0,
                                           base=0, channel_multiplier=-1), [idf], [idf])
    idb = p.sbuf("idb", [128, 128], BF16)
    p.cp("dve", idb[:], idf[:], [idf], [idb])
    c.idf, c.idb = idf, idb
    onesb = p.sbuf("onesb", [128, 128], BF16)
    p.memset("pool", onesb[:], 1.0, [onesb])
    c.onesb = onesb
    epsb = p.sbuf("epsb", [128, 1], F32)
    p.memset("pool", epsb[:], EPS, [epsb])
    c.epsb = epsb
    lnsc = p.sbuf("lnsc", [128, 1], F32)
    p.memset("pool", lnsc[:], math.log(SCALE), [lnsc])
    c.lnsc = lnsc
    inv = (1.0 / (10000.0 ** (np.arange(0, 32, 2, dtype=np.float32) / np.float32(32)))).astype(np.float32)
    row = p.sbuf("roperow", [1, 64], F32)
    for j in range(16):
        p.memset("pool", row[0:1, j:j + 1], float(inv[j]), [row])
        p.memset("pool", row[0:1, 16 + j:17 + j], float(inv[j]), [row])
    p.memset("pool", row[0:1, 32:48], -1.0, [row])
    p.memset("pool", row[0:1, 48:64], 1.0, [row])
    rd = p.dram("rope_d", [64], F32)
    p.dma(rd[:].rearrange("(o n) -> o n", o=1), row[:], [row], [rd])
    ctab = p.sbuf("ctab", [96, 2], F32)
    p.dma(ctab[64:96, :], rd[:].rearrange("(c p) -> p c", p=32), [rd], [ctab], allow_slow_non_contiguous=True)
    cos2 = p.sbuf("cos2", [96, S], F32)
    sin2 = p.sbuf("sin2", [96, S], F32)
    mk = p.mark()
    posi = p.sbuf("posi", [96, S], I32)
    p.dma(posi[64:96, :], pos_d.partition_broadcast(32), [], [posi])
    ang = p.sbuf("ang", [96, S], F32)
    p.cp("dve", ang[64:96, :], posi[64:96, :], [posi], [ang])
    p.ts("dve", ang[64:96, :], ang[64:96, :], ctab[64:96, 0:1], None, ALU.mult, None, [ang, ctab], [ang])
    kf = p.sbuf("kf", [96, S], F32)
    sl = slice(64, 96)

    def sin_of(dst, shift):
        a = dst
        p.ts("dve", a[sl, :], ang[sl, :], shift, None, ALU.add, None, [ang], [dst])
        p.ts("dve", kf[sl, :], a[sl, :], 1.0 / (2 * math.pi), None, ALU.mult, None, [dst], [kf])
        p.cp("dve", posi[sl, :], kf[sl, :], [kf], [posi])
        p.cp("dve", kf[sl, :], posi[sl, :], [posi], [kf])
        p.stt("dve", a[sl, :], kf[sl, :], -2 * math.pi, a[sl, :], ALU.mult, ALU.add, [kf, dst], [dst])
        p.ts("dve", kf[sl, :], a[sl, :], math.pi, -2 * math.pi, ALU.is_gt, ALU.mult, [dst], [kf])
        p.tt("dve", a[sl, :], a[sl, :], kf[sl, :], ALU.add, [dst, kf], [dst])
        p.ts("dve", kf[sl, :], a[sl, :], -math.pi, 2 * math.pi, ALU.is_lt, ALU.mult, [dst], [kf])
        p.tt("dve", a[sl, :], a[sl, :], kf[sl, :], ALU.add, [dst, kf], [dst])
        p.act(a[sl, :], a[sl, :], AF.Sin, [dst], [dst])
    sin_of(cos2, math.pi / 2)
    sin_of(sin2, 0.0)
    p.ts("dve", sin2[sl, :], sin2[sl, :], ctab[sl, 1:2], None, ALU.mult, None, [sin2, ctab], [sin2])
    p.release(mk)
    c.cos2, c.sin2 = cos2, sin2
    return c


def colvec(p, name, src_ap, n, dt=F32):
    t = p.sbuf(name, [128, n], dt)
    p.dma(t[:], src_ap.rearrange("(c p) -> p c", p=128), [], [t], allow_slow_non_contiguous=True)
    return t


def phase1(p, c, S, l, xin, W, sc):
    NT_ = S // 128
    NB = S // 512
    hT = p.sbuf("hT%d" % l, [128, 8, S], BF16)
    gbc = p.sbuf("gbc%d" % l, [128, D], F32)
    p.dma(gbc[:], W["norm_pre"][l:l + 1, :].partition_broadcast(128), [], [gbc])
    mk1 = p.mark()
    xt = [p.sbuf("xt%d_%d" % (l, i), [128, D], F32) for i in range(2)]
    hb = [p.sbuf("hb%d_%d" % (l, i), [128, D], BF16) for i in range(2)]
    junk = p.sbuf("junk%d" % l, [128, D], BF16)
    ss = [p.sbuf("ss%d_%d" % (l, i), [128, 1], F32) for i in range(2)]
    ptr = [p.psum("ptr%d_%d" % (l, i), [128, 8, 128], BF16) for i in range(2)]
    for i in range(NT_):
        s = i % 2
        p.dma(xt[s][:], xin[i * 128:(i + 1) * 128, :], [xin], [xt[s]])
        p.act(junk[:], xt[s][:], AF.Square, [xt[s]], [junk, ss[s]], accum_out=ss[s][:])
        p.act(ss[s][:], ss[s][:], AF.Ln, [ss[s], c.epsb], [ss[s]], bias=c.epsb[:], scale=1.0 / D)
        p.act(ss[s][:], ss[s][:], AF.Exp, [ss[s]], [ss[s]], scale=-0.5)
        p.stt("dve", hb[s][:], xt[s][:], ss[s][:, 0:1], gbc[:], ALU.mult, ALU.mult,
              [xt[s], ss[s], gbc], [hb[s]])
        for k in range(8):
            p.tr(ptr[s][:, k, :], hb[s][:, k * 128:(k + 1) * 128], c.idb[:], [hb[s], c.idb], [ptr[s]],
                 signal=(k == 7))
        p.cp("act", hT[:, :, i * 128:(i + 1) * 128], ptr[s][:], [ptr[s]], [hT])

    p.release(mk1)
    mk2 = p.mark()
    wa32 = p.sbuf("wa32_%d" % l, [128, 8, 384], F32)
    p.dma(wa32[:], W["w_in"][l, :, 0:384].rearrange("(c p) n -> p c n", p=128), [], [wa32])
    wa = p.sbuf("wa_%d" % l, [128, 8, 384], BF16)
    p.cp("pool", wa[:], wa32[:], [wa32], [wa])
    wk32 = p.sbuf("wk32_%d" % l, [128, 8, 192], F32)
    p.dma(wk32[:], W["wkr2"][l].rearrange("(c p) n -> p c n", p=128), [], [wk32])
    wkr = p.sbuf("wkr_%d" % l, [128, 8, 192], BF16)
    p.cp("pool", wkr[:], wk32[:], [wk32], [wkr])
    wq32 = p.sbuf("wq32_%d" % l, [128, 2, 1536], F32)
    p.dma(wq32[:, :, 0:768], W["mla_w_uq"][l].rearrange("(c p) n -> p c n", p=128), [], [wq32])
    p.dma(wq32[:, :, 768:1536], W["wuq_sw"][l].rearrange("(c p) n -> p c n", p=128), [], [wq32])
    wq = p.sbuf("wq_%d" % l, [128, 2, 1536], BF16)
    p.cp("pool", wq[:], wq32[:], [wq32], [wq])
    wkv32 = p.sbuf("wkv32_%d" % l, [128, 1024], F32)
    p.dma(wkv32[:, 0:512], W["wuk"][l], [], [wkv32])
    p.dma(wkv32[:, 512:1024], W["wuv"][l], [], [wkv32])
    wkv = p.sbuf("wkv_%d" % l, [128, 1024], BF16)
    p.cp("pool", wkv[:], wkv32[:], [wkv32], [wkv])
    gq = colvec(p, "gq%d" % l, W["mla_q_norm"][l], 2)
    gkv = colvec(p, "gkv%d" % l, W["mla_kv_norm"][l], 1)

    pq = [p.psum("pq%d_%d" % (l, i), [128, 512], F32) for i in range(2)]
    pkv = p.psum("pkv%d" % l, [128, 512], F32)
    pkr = p.psum("pkr%d" % l, [96, 2, 512], F32)
    pn = p.psum("pn%d" % l, [128, 512], F32)
    pw = [p.psum("pw%d_%d" % (l, i), [128, 512], F32) for i in range(2)]
    sq = p.sbuf("sq%d" % l, [128, 2, 512], BF16)
    rq = p.sbuf("rq%d" % l, [128, 512], F32)
    cqn = p.sbuf("cqn%d" % l, [128, 2, 512], BF16)
    ckvn = p.sbuf("ckvn%d" % l, [128, 512], BF16)
    qst = p.sbuf("qst%d" % l, [96, 8, 512], BF16)
    kst = p.sbuf("kst%d" % l, [128, 4, 512], BF16)
    krs = p.sbuf("krs%d" % l, [96, 512], BF16)
    t1 = p.sbuf("t1_%d" % l, [96, 512], F32)
    t2 = p.sbuf("t2_%d" % l, [96, 512], F32)
    vst = p.sbuf("vst%d" % l, [128, 4, 8, 65], BF16)
    p.memset("pool", vst[:], 1.0, [vst])
    rs = slice(64, 96)
    for tb in range(NB):
        ts_ = slice(tb * 512, (tb + 1) * 512)
        for cc in range(2):
            for k in range(8):
                p.mm(pq[cc][:], wa[:, k, cc * 128:(cc + 1) * 128], hT[:, k, ts_], k == 0, k == 7, [wa, hT], [pq[cc]])
        for k in range(8):
            p.mm(pkv[:], wa[:, k, 256:384], hT[:, k, ts_], k == 0, k == 7, [wa, hT], [pkv])
        for v in range(2):
            for k in range(8):
                p.mm(pkr[:, v, :], wkr[:, k, v * 96:(v + 1) * 96], hT[:, k, ts_], k == 0, k == 7, [wkr, hT], [pkr])
        for cc in range(2):
            p.act(sq[:, cc, :], pq[cc][:], AF.Square, [pq[cc]], [sq])
        for cc in range(2):
            p.mm(pn[:], c.onesb[:], sq[:, cc, :], cc == 0, cc == 1, [c.onesb, sq], [pn])
        p.act(rq[:], pn[:], AF.Ln, [pn, c.epsb], [rq], bias=c.epsb[:], scale=1.0 / 256)
        p.act(rq[:], rq[:], AF.Exp, [rq, c.lnsc], [rq], bias=c.lnsc[:], scale=-0.5)
        for cc in range(2):
            p.stt("dve", cqn[:, cc, :], pq[cc][:], gq[:, cc:cc + 1], rq[:], ALU.mult, ALU.mult,
                  [pq[cc], gq, rq], [cqn])
        p.act(sq[:, 0, :], pkv[:], AF.Square, [pkv], [sq])
        p.mm(pn[:], c.onesb[:], sq[:, 0, :], True, True, [c.onesb, sq], [pn])
        p.act(rq[:], pn[:], AF.Ln, [pn, c.epsb], [rq], bias=c.epsb[:], scale=1.0 / 128)
        p.act(rq[:], rq[:], AF.Exp, [rq], [rq], scale=-0.5)
        p.stt("dve", ckvn[:], pkv[:], gkv[:, 0:1], rq[:], ALU.mult, ALU.mult, [pkv, gkv, rq], [ckvn])
        p.tt("dve", t1[rs, :], pkr[rs, 0, :], c.cos2[rs, ts_], ALU.mult, [pkr, c.cos2], [t1])
        p.tt("dve", t2[rs, :], pkr[rs, 1, :], c.sin2[rs, ts_], ALU.mult, [pkr, c.sin2], [t2])
        p.tt("pool", krs[rs, :], t1[rs, :], t2[rs, :], ALU.add, [t1, t2], [krs])
        for h in range(8):
            p.dma(sc["KT"][h, 64:96, ts_], krs[rs, :], [krs], [sc["KT"]])
        for h in range(8):
            a, b = pw[0], pw[1]
            for k in range(2):
                p.mm(a[0:96, :], wq[:, k, h * 96:(h + 1) * 96], cqn[:, k, :], k == 0, k == 1, [wq, cqn], [a])
            for k in range(2):
                p.mm(b[0:96, :], wq[:, k, 768 + h * 96:768 + (h + 1) * 96], cqn[:, k, :], k == 0, k == 1, [wq, cqn], [b])
            p.cp("act", qst[0:64, h, :], a[0:64, :], [a], [qst])
            p.tt("dve", t1[rs, :], a[rs, :], c.cos2[rs, ts_], ALU.mult, [a, c.cos2], [t1])
            p.tt("dve", t2[rs, :], b[rs, :], c.sin2[rs, ts_], ALU.mult, [b, c.sin2], [t2])
            p.tt("pool", qst[rs, h, :], t1[rs, :], t2[rs, :], ALU.add, [t1, t2], [qst])
        p.dma(sc["QT"][:, :, ts_].rearrange("h p t -> p h t"), qst[:], [qst], [sc["QT"]])
        for j in range(4):
            a = pw[j % 2]
            p.mm(a[:], wkv[:, j * 128:(j + 1) * 128], ckvn[:], True, True, [wkv, ckvn], [a])
            p.cp("act", kst[:, j, :], a[:], [a], [kst])
        for two in range(2):
            p.dma(sc["KT"][:, 0:64, ts_].rearrange("(j two) p t -> two p j t", two=2)[two],
                  kst[two * 64:(two + 1) * 64, :, :], [kst], [sc["KT"]])
        for tt_ in range(4):
            a = pw[tt_ % 2]
            p.mm(a[:], ckvn[:, tt_ * 128:(tt_ + 1) * 128], wkv[:, 512:1024], True, True, [ckvn, wkv], [a])
            p.cp("act", vst[:, tt_, :, 0:64], a[:].rearrange("p (h v) -> p h v", v=64), [a], [vst])
        p.dma(sc["VA"][ts_, :].rearrange("(tt p) n -> p tt n", p=128),
              vst[:].rearrange("p t h v -> p t (h v)"), [vst], [sc["VA"]])

    p.release(mk2)
    mk3 = p.mark()
    pw = [p.psum("pwb%d_%d" % (l, i), [128, 512], F32) for i in range(2)]
    chunks = []
    for i in range(13):
        chunks.append((C_U + i * 128, 128, "mix", "U", i * 128))
    for i in range(2):
        chunks.append((C_GQ + i * 128, 128, "copy32", "GQ", i * 128))
    for i in range(2):
        chunks.append((C_GK + i * 128, 128, "copy32", "GK", i * 128))
    chunks.append((C_GL, 16, "copy32", "GL", 0))
    for i in range(12):
        chunks.append((C_BG + i * 128, 128, "silu", "BG", i * 128))
    for i in range(24):
        chunks.append((C_MG + i * 128, 128, "sigm", "MG", i * 128))
    chunks.sort(key=lambda x: {"mix": 0, "copy32": 0, "silu": 1, "sigm": 2}[x[2]])
    pw = pw + [p.psum("pwd%d_%d" % (l, i), [128, 512], F32) for i in range(2)]
    mu13 = colvec(p, "mu13_%d" % l, W["rwkv_mu"][l], 13)
    wc32 = [p.sbuf("wc32_%d_%d" % (l, i), [128, 8, 128], F32) for i in range(3)]
    wcb = [p.sbuf("wcb_%d_%d" % (l, i), [128, 8, 128], BF16) for i in range(3)]
    st32 = [p.sbuf("st32_%d_%d" % (l, i), [128, 512], F32) for i in range(4)]
    st16 = [p.sbuf("st16_%d_%d" % (l, i), [128, 512], BF16) for i in range(4)]
    stgm = [p.sbuf("stgm_%d_%d" % (l, i), [128, 513], F32) for i in range(3)]
    dmx = [p.sbuf("dmx_%d_%d" % (l, i), [128, 512], F32) for i in range(2)]
    wg32 = p.sbuf("wg32_%d" % l, [128, 8, 512], F32)
    p.dma(wg32[:], W["w_in"][l, :, C_GV:C_GV + 512].rearrange("(c p) n -> p c n", p=128), [], [wg32])
    wg = p.sbuf("wg_%d" % l, [128, 8, 512], BF16)
    p.cp("pool", wg[:], wg32[:], [wg32], [wg])
    n = 0
    nm = 0
    for ci, (col0, m, kind, dest, row0) in enumerate(chunks):
        s = ci % 3
        p.dma(wc32[s][:, :, 0:m], W["w_in"][l, :, col0:col0 + m].rearrange("(c p) n -> p c n", p=128), [], [wc32[s]])
        p.cp("pool", wcb[s][:, :, 0:m], wc32[s][:, :, 0:m], [wc32[s]], [wcb[s]])
        prev = None
        for tb in range(NB):
            ts_ = slice(tb * 512, (tb + 1) * 512)
            a = pw[n % 4]
            for k in range(8):
                p.mm(a[0:m, :], wcb[s][:, k, 0:m], hT[:, k, ts_], k == 0, k == 7, [wcb[s], hT], [a])
            if kind == "mix":
                sg_ = stgm[nm % 3]
                dd = dmx[nm % 2]
                nm += 1
                o = st32[n % 4]
                if prev is None:
                    p.memset("pool", sg_[:, 0:1], 0.0, [sg_])
                else:
                    p.cp("pool", sg_[:, 0:1], prev[:, 512:513], [prev], [sg_])
                p.cp("act", sg_[:, 1:513], a[:], [a], [sg_])
                p.tt("dve", dd[:], sg_[:, 0:512], sg_[:, 1:513], ALU.subtract, [sg_], [dd])
                mi = row0 // 128
                p.stt("dve", o[:], dd[:], mu13[:, mi:mi + 1], sg_[:, 1:513], ALU.mult, ALU.add, [dd, mu13, sg_], [o])
                prev = sg_
            elif kind == "copy32":
                o = st32[n % 4]
                if n % 2 == 0:
                    p.cp("act", o[0:m, :], a[0:m, :], [a], [o])
                else:
                    p.cp("dve", o[0:m, :], a[0:m, :], [a], [o])
            else:
                o = st16[n % 4]
                p.act(o[0:m, :], a[0:m, :], AF.Silu if kind == "silu" else AF.Sigmoid, [a], [o])
            p.dma(sc[dest][row0:row0 + m, ts_], o[0:m, :], [o], [sc[dest]])
            n += 1
    for i in range(NT_):
        a = pw[n % 4]
        o = st16[n % 4]
        n += 1
        for k in range(8):
            p.mm(a[:], hT[:, k, i * 128:(i + 1) * 128], wg[:, k, :], k == 0, k == 7, [hT, wg], [a])
        p.cp("act", o[:], a[:], [a], [o])
        p.dma(sc["GV"][i * 128:(i + 1) * 128, :], o[:], [o], [sc["GV"]])
    p.release(mk3)


def phase_mla(p, c, S, l, sc):
    NB = S // 512
    NT_ = S // 128
    kt = p.sbuf("kt", [96, 8, S], BF16)
    for h in range(8):
        p.dma(kt[:, h, :], sc["KT"][h], [sc["KT"]], [kt])
    va = p.sbuf("va", [128, NT_, 520], BF16)
    p.dma(va[:], sc["VA"].t.rearrange("(n p) f -> p n f", p=128), [sc["VA"]], [va])
    qts = [p.sbuf("qt%d" % i, [96, 8, 512], BF16) for i in range(2)]
    bgs = [p.sbuf("bg%d" % i, [128, 4, 512], BF16) for i in range(2)]
    pts = [p.sbuf("pt%d" % i, [128, 512], BF16) for i in range(3)]
    ytm = p.sbuf("ytm", [128, 4, 512], F32)
    ys = [p.sbuf("ysm%d" % i, [128, 4, 512], BF16) for i in range(2)]
    rec = p.sbuf("rec", [128, 4], F32)
    pst = [p.psum("pst%d" % i, [128, 512], F32) for i in range(2)]
    acc = [p.psum("acc%d" % i, [128, 512], F32) for i in range(4)]
    pT = p.psum("pT", [128, 4, 128], F32)
    n = 0
    for qb in range(NB):
        qt = qts[qb % 2]
        bg = bgs[qb % 2]
        cols = slice(qb * 512, (qb + 1) * 512)
        p.dma(qt[:], sc["QT"][:, :, cols].rearrange("h p t -> p h t"), [sc["QT"]], [qt])
        p.dma(bg[:], sc["BG"][0:512, cols].rearrange("(c p) t -> p c t", p=128), [sc["BG"]], [bg])
        for h in range(8):
            nkt = 4 * (qb + 1)
            for kti in range(nkt):
                j = kti - 4 * qb
                q0 = 128 * j if j > 0 else 0
                N = 512 - q0
                st = pst[n % 2]
                pt = pts[n % 3]
                p.mm(st[:, 0:N], kt[:, h, kti * 128:(kti + 1) * 128], qt[:, h, q0:512], True, True, [kt, qt], [st])
                p.act(pt[:, 0:N], st[:, 0:N], AF.Exp, [st], [pt])
                if j >= 0:
                    p.op("pool", lambda e, pt=pt: e.affine_select(pt[:, 0:128], pt[:, 0:128], [[1, 128]], ALU.is_ge, 0.0,
                                                                  base=0, channel_multiplier=-1), [pt], [pt])
                for qs in range(q0 // 128, 4):
                    p.mm(acc[qs][:, 0:65], pt[:, qs * 128 - q0:(qs + 1) * 128 - q0], va[:, kti, h * 65:(h + 1) * 65],
                         kti == 0, kti == 4 * qb + qs, [pt, va], [acc[qs]])
                n += 1
            for qs in range(4):
                p.op("dve", lambda e, qs=qs: e.reciprocal(rec[:, qs:qs + 1], acc[qs][:, 64:65]), [acc[qs]], [rec])
                p.ts("dve", ytm[:, qs, h * 64:(h + 1) * 64], acc[qs][:, 0:64], rec[:, qs:qs + 1], None, ALU.mult, None,
                     [acc[qs], rec], [ytm])
        yo = ys[qb % 2]
        for fc in range(4):
            for qs in range(4):
                p.tr(pT[:, qs, :], ytm[:, qs, fc * 128:(fc + 1) * 128], c.idf[:], [ytm, c.idf], [pT], signal=(qs == 3))
            p.tt("dve", yo[:, fc, :], pT[:].rearrange("p a b -> p (a b)"), bg[:, fc, :], ALU.mult, [pT, bg], [yo])
        p.dma(sc["YS"][0:512, cols].rearrange("(c p) t -> p c t", p=128), yo[:], [yo], [sc["YS"]])


def phase_gla(p, c, S, l, W, sc):
    NB = S // 512
    H = 4
    aup = p.sbuf("g_aup", [16, 256], F32)
    p.dma(aup[:], W["gla_a_up"][l], [], [aup])
    nab = p.sbuf("g_nab", [64, H], F32)
    p.dma(nab[:], W["gla_a_b"][l].rearrange("(h d) -> d h", d=64), [], [nab], allow_slow_non_contiguous=True)
    p.ts("dve", nab[:], nab[:], -1.0, None, ALU.mult, None, [nab], [nab])
    gn = colvec(p, "g_gn", W["gla_norm"][l], 4)
    one1 = p.sbuf("g_one1", [128, 1], F32)
    p.memset("pool", one1[:], 1.0, [one1])
    maskr = p.sbuf("g_maskr", [64, 512], F32)
    p.memset("pool", maskr[:], 1.0, [maskr])
    p.memset("pool", maskr[:].rearrange("p (c t) -> p c t", t=64)[:, :, 0:1], 0.0, [maskr])
    msk = p.sbuf("g_msk", [64, H, 64], F32)
    p.memset("pool", msk[:], 1.0, [msk])
    for h in range(H):
        p.op("pool", lambda e, h=h: e.affine_select(msk[:, h, :], msk[:, h, :], [[1, 64]], ALU.is_ge, 0.0,
                                                    base=0, channel_multiplier=-1), [msk], [msk])
    st = p.sbuf("g_st", [64, H, 128], F32)
    stb = p.sbuf("g_stb", [64, H, 128], BF16)
    st2 = p.sbuf("g_st2", [64, H, 128], F32)
    p.memset("pool", st[:], 0.0, [st])
    p.memset("pool", stb[:], 0.0, [stb])
    gq = [p.sbuf("g_q%d" % i, [64, H, 512], F32) for i in range(2)]
    gk = [p.sbuf("g_k%d" % i, [64, H, 512], F32) for i in range(2)]
    gl = [p.sbuf("g_l%d" % i, [16, 512], F32) for i in range(2)]
    gv = [p.sbuf("g_v%d" % i, [64, 8, 512], BF16) for i in range(2)]
    bg = [p.sbuf("g_bg%d" % i, [128, H, 512], BF16) for i in range(2)]
    e1 = p.sbuf("g_e1", [64, H, 512], F32)
    cs = p.sbuf("g_cs", [64, H, 512], F32)
    eb = p.sbuf("g_eb", [64, H, 512], F32)
    enb = p.sbuf("g_enb", [64, H, 512], F32)
    qe = p.sbuf("g_qe", [64, H, 512], BF16)
    ke = p.sbuf("g_ke", [64, H, 512], BF16)
    att = [p.sbuf("g_att%d" % i, [64, H, 64], BF16) for i in range(2)]
    ketm = [p.sbuf("g_ketm%d" % i, [64, H, 64], BF16) for i in range(2)]
    sq = p.sbuf("g_sq", [64, H, 128], F32)
    ssg = p.sbuf("g_ss", [64, H], F32)
    yn = [p.sbuf("g_yn%d" % i, [64, H, 128], F32) for i in range(2)]
    yT = p.sbuf("g_yT", [128, H, 512], F32)
    ysg = [p.sbuf("g_ys%d" % i, [128, H, 512], BF16) for i in range(2)]
    pz = [p.psum("g_pz%d" % i, [64, 512], F32) for i in range(2)]
    pat = p.psum("g_pat", [64, H, 64], F32)
    po = p.psum("g_po", [64, H, 128], F32)
    pkt = p.psum("g_pkt", [64, H, 64], BF16)
    pst = p.psum("g_pst", [64, H, 128], F32)
    pyT = p.psum("g_pyT", [128, H, 64], F32)
    for tb in range(NB):
        s_ = tb % 2
        cols = slice(tb * 512, (tb + 1) * 512)
        p.dma(gq[s_][:], sc["GQ"][:, cols].rearrange("(h d) t -> d h t", d=64), [sc["GQ"]], [gq[s_]])
        p.dma(gk[s_][:], sc["GK"][:, cols].rearrange("(h d) t -> d h t", d=64), [sc["GK"]], [gk[s_]])
        p.dma(gl[s_][:], sc["GL"][:, cols], [sc["GL"]], [gl[s_]])
        p.dma(gv[s_][:], sc["GV"][cols, :].rearrange("(c p) f -> p c f", p=64), [sc["GV"]], [gv[s_]])
        p.dma(bg[s_][:], sc["BG"][1024:1536, cols].rearrange("(c p) t -> p c t", p=128), [sc["BG"]], [bg[s_]])
        for h in range(H):
            z = pz[h % 2]
            p.mm(z[:], aup[:, h * 64:(h + 1) * 64], gl[s_][:], True, True, [aup, gl[s_]], [z])
            p.act(e1[:, h, :], z[:], AF.Exp, [z, nab], [e1], bias=nab[:, h:h + 1], scale=-1.0)
        p.act(e1[:], e1[:], AF.Ln, [e1, one1], [e1], bias=one1[0:64, :], scale=1.0)
        for h in range(H):
            p.op("dve", lambda e, h=h: e.tensor_tensor_scan(cs[:, h, :], maskr[:], e1[:, h, :], 0.0, ALU.mult, ALU.add),
                 [maskr, e1], [cs])
        p.act(eb[:], cs[:], AF.Exp, [cs], [eb], scale=-1.0 / 16)
        p.act(enb[:], cs[:], AF.Exp, [cs], [enb], scale=1.0 / 16)
        p.stt("dve", qe[:], gq[s_][:], 0.125, eb[:], ALU.mult, ALU.mult, [gq[s_], eb], [qe])
        p.tt("pool", ke[:], gk[s_][:], enb[:], ALU.mult, [gk[s_], enb], [ke])
        for cch in range(8):
            cc = slice(cch * 64, (cch + 1) * 64)
            a_ = att[cch % 2]
            kt_ = ketm[cch % 2]
            y_ = yn[cch % 2]
            for h in range(H):
                p.mm(pat[:, h, :], ke[:, h, cc], qe[:, h, cc], True, True, [ke, qe], [pat])
            p.tt("dve", a_[:], pat[:], msk[:], ALU.mult, [pat, msk], [a_])
            for h in range(H):
                p.tr(pkt[:, h, :], ke[:, h, cc], c.idb[0:64, 0:64], [ke, c.idb], [pkt], signal=(h == H - 1))
            p.cp("act", kt_[:], pkt[:], [pkt], [kt_])
            for h in range(H):
                p.mm(po[:, h, :], a_[:, h, :], gv[s_][:, cch, h * 128:(h + 1) * 128], True, False, [a_, gv[s_]], [po])
                p.mm(po[:, h, :], qe[:, h, cc], stb[:, h, :], False, True, [qe, stb], [po])
            for h in range(H):
                p.mm(pst[:, h, :], kt_[:, h, :], gv[s_][:, cch, h * 128:(h + 1) * 128], True, True, [kt_, gv[s_]], [pst])
            p.tt("dve", st2[:], st[:], pst[:], ALU.add, [st, pst], [st2])
            ebl = eb[:, :, cch * 64 + 63:cch * 64 + 64].broadcast_to([64, H, 128])
            p.tt("dve", st[:], st2[:], ebl, ALU.mult, [st2, eb], [st])
            p.cp("pool", stb[:], st[:], [st], [stb])
            p.act(sq[:], po[:], AF.Square, [po], [sq])
            p.op("dve", lambda e: e.tensor_reduce(ssg[:], sq[:], AX.X, ALU.add), [sq], [ssg])
            p.act(ssg[:], ssg[:], AF.Ln, [ssg, c.epsb], [ssg], bias=c.epsb[0:64, :], scale=1.0 / 128)
            p.act(ssg[:], ssg[:], AF.Exp, [ssg], [ssg], scale=-0.5)
            p.tt("dve", y_[:], po[:], ssg[:].unsqueeze(2).broadcast_to([64, H, 128]), ALU.mult, [po, ssg], [y_])
            for h in range(H):
                p.tr(pyT[:, h, :], y_[:, h, :], c.idf[0:64, 0:64], [y_, c.idf], [pyT], signal=(h == H - 1))
            p.cp("act", yT[:, :, cc], pyT[:], [pyT], [yT])
        o_ = ysg[s_]
        for h in range(H):
            p.stt("dve", o_[:, h, :], yT[:, h, :], gn[:, h:h + 1], bg[s_][:, h, :], ALU.mult, ALU.mult, [yT, gn, bg[s_]], [o_])
        p.dma(sc["YS"][1024:1536, cols].rearrange("(c p) t -> p c t", p=128), o_[:], [o_], [sc["YS"]])


def phase_rwkv(p, c, S, l, W, sc, TB=128):
    NB = S // TB
    NCH = TB // 64
    H = 8
    C0 = math.exp(-0.5)
    vec = lambda name, ap: _hv(p, name, ap)
    mu_r = vec("r_mur", W["rwkv_mu"][l, 0:512])
    mu_k = vec("r_muk", W["rwkv_mu"][l, 512:1024])
    mu_v = vec("r_muv", W["rwkv_mu"][l, 1024:1536])
    mu_w = p.sbuf("r_muw", [64, 2], F32)
    p.dma(mu_w[:], W["rwkv_mu"][l, 1536:1664].rearrange("(c p) -> p c", p=64), [], [mu_w], allow_slow_non_contiguous=True)
    w0 = vec("r_w0", W["rwkv_w0"][l])
    a0 = vec("r_a0", W["rwkv_a0"][l])
    k_k = vec("r_kk", W["rwkv_k_k"][l])
    k_a = vec("r_ka", W["rwkv_k_a"][l])
    r_k = vec("r_rk", W["rwkv_r_k"][l])
    lnw = vec("r_lnw", W["rwkv_ln_w"][l])
    lnb = vec("r_lnb", W["rwkv_ln_b"][l])
    wup = p.sbuf("r_wup", [64, 512], F32)
    p.dma(wup[:], W["rwkv_w_up"][l], [], [wup])
    aup = p.sbuf("r_aup", [64, 512], F32)
    p.dma(aup[:], W["rwkv_a_up"][l], [], [aup])
    ones = p.sbuf("r_ones", [64, 64], F32)
    p.memset("pool", ones[:], 1.0, [ones])
    gne = p.sbuf("r_gne", [64, 1], F32)
    p.memset("pool", gne[:], 64e-5, [gne])
    maskr = p.sbuf("r_maskr", [64, TB], F32)
    p.memset("pool", maskr[:], 1.0, [maskr])
    p.memset("pool", maskr[:].rearrange("p (c t) -> p c t", t=64)[:, :, 0:1], 0.0, [maskr])

    def mk_mask(name, pat, cm, op, dt=F32):
        m = p.sbuf(name, [64, H, 64], F32)
        p.memset("pool", m[:], 1.0, [m])
        for h in range(H):
            p.op("pool", lambda e, h=h: e.affine_select(m[:, h, :], m[:, h, :], pat, op, 0.0, base=0, channel_multiplier=cm), [m], [m])
        if dt == F32:
            return m
        mb = p.sbuf(name + "b", [64, H, 64], dt)
        p.cp("pool", mb[:], m[:], [m], [mb])
        return mb
    mL = mk_mask("r_mL", [[-1, 64]], 1, ALU.is_gt)
    mSU = mk_mask("r_mSU", [[1, 64]], -1, ALU.is_gt)
    mU = mk_mask("r_mU", [[1, 64]], -1, ALU.is_ge)
    idh = mk_mask("r_idh", [[1, 64]], -1, ALU.is_equal, BF16)
    id64 = c.idb[0:64, 0:64]

    def A3(name, dt=F32, n=TB):
        return p.sbuf(name, [64, H, n], dt)
    r_, k_, vf = A3("r_r"), A3("r_k"), A3("r_v")
    lwa = p.sbuf("r_lwa", [64, 2, TB], F32)
    tmp, tmp2 = A3("r_tmp"), A3("r_tmp2")
    sg, a_, cs = A3("r_sg"), A3("r_a"), A3("r_cs")
    gi, gp = A3("r_gi"), A3("r_gp")
    kkn, kp = A3("r_kkn"), A3("r_kp")
    D2 = lambda name, dt: [A3(name + "0", dt), A3(name + "1", dt)]
    vb2, Ab2, Bb2, Kb2, Rb2 = D2("r_vb", BF16), D2("r_Ab", BF16), D2("r_Bb", BF16), D2("r_Kb", BF16), D2("r_Rb", BF16)
    g2, bon2, yfm2 = D2("r_g", F32), D2("r_bon", F32), D2("r_yfm", F32)
    bgt2, yso2 = D2("r_bgt", BF16), D2("r_yso", BF16)
    T0 = p.sbuf("r_T0", [64, H, 64], F32)
    p.memset("pool", T0[:], 0.0, [T0])
    T0b = p.sbuf("r_T0b", [64, H, 64], BF16)
    p.memset("pool", T0b[:], 0.0, [T0b])
    T1 = p.sbuf("r_T1", [64, H, 64], F32)

    def S3(name, dt=BF16):
        return p.sbuf(name, [64, H, 64], dt)
    S2 = lambda name: [S3(name + "0"), S3(name + "1")]
    P_ = S2("r_P")
    PT_ = S2("r_PT")
    NTa = [S2("r_NTa"), S2("r_NTb")]
    LakT2, ArbT2, ArkT2 = S2("r_LakT"), S2("r_ArbT"), S2("r_ArkT")
    Vtm2, Btm2, Ktm2 = S2("r_Vtm"), S2("r_Btm"), S2("r_Ktm")
    X_, U_ = S3("r_X"), S3("r_U")
    yc, ysq, ynm = S3("r_yc", F32), S3("r_ysq", F32), S3("r_ynm", F32)
    mean = p.sbuf("r_mean", [64, H], F32)
    var = p.sbuf("r_var", [64, H], F32)
    pz = p.psum("r_pz", [64, 2, TB], F32)
    pg = [p.psum("r_pg%d" % i, [64, H, 64], F32) for i in range(5)]
    pb = [p.psum("r_pb%d" % i, [64, H, 64], BF16) for i in range(2)]
    pgi = [0, 0]

    def PG():
        t = pg[pgi[0] % 5]
        pgi[0] += 1
        return t

    def PB():
        t = pb[pgi[1] % 2]
        pgi[1] += 1
        return t

    def bc(t2):
        return t2[:].unsqueeze(2).broadcast_to([64, H, TB])

    def prep(tb):
        t0 = tb * TB
        cols = slice(t0, t0 + TB)
        bp = tb % 2
        vb, Ab, Bb, Kb, Rb, g_, bon, bgt = vb2[bp], Ab2[bp], Bb2[bp], Kb2[bp], Rb2[bp], g2[bp], bon2[bp], bgt2[bp]
        for (dst, r0) in ((r_, 0), (k_, 512), (vf, 1024)):
            p.dma(dst[:], sc["U"][r0:r0 + 512, cols].rearrange("(h d) t -> d h t", d=64), [sc["U"]], [dst])
        p.dma(lwa[:], sc["U"][1536:1664, cols].rearrange("(c p) t -> p c t", p=64), [sc["U"]], [lwa])
        p.dma(bgt[:], sc["BG"][512:1024, cols].rearrange("(h d) t -> d h t", d=64), [sc["BG"]], [bgt])
        p.cp("act", vb[:], vf[:], [vf], [vb])
        p.act(lwa[:, 0, :], lwa[:, 0, :], AF.Tanh, [lwa], [lwa])
        for h in range(H):
            p.mm(pz[:, 0, :], wup[:, h * 64:(h + 1) * 64], lwa[:, 0, :], True, True, [wup, lwa], [pz])
            p.mm(pz[:, 1, :], aup[:, h * 64:(h + 1) * 64], lwa[:, 1, :], True, True, [aup, lwa], [pz])
            p.act(sg[:, h, :], pz[:, 0, :], AF.Sigmoid, [pz, w0], [sg], bias=w0[:, h:h + 1], scale=1.0)
            p.act(a_[:, h, :], pz[:, 1, :], AF.Sigmoid, [pz, a0], [a_], bias=a0[:, h:h + 1], scale=1.0)
        for h in range(H):
            p.op("dve", lambda e, h=h: e.tensor_tensor_scan(cs[:, h, :], maskr[:], sg[:, h, :], 0.0, ALU.mult, ALU.add),
                 [maskr, sg], [cs], dur=100 + 2.1 * TB)
        p.act(g_[:], cs[:], AF.Exp, [cs], [g_], scale=-C0)
        p.act(gi[:], cs[:], AF.Exp, [cs], [gi], scale=C0)
        p.tt("pool", tmp[:], cs[:], sg[:], ALU.subtract, [cs, sg], [tmp])
        p.act(gp[:], tmp[:], AF.Exp, [tmp], [gp], scale=-C0)
        p.tt("dve", kkn[:], k_[:], bc(k_k), ALU.mult, [k_, k_k], [kkn])
        p.act(tmp2[:], kkn[:], AF.Square, [kkn], [tmp2])
        for hp in range(4):
            for j in range(2):
                p.mm(pz[:, j, :], ones[:], tmp2[:, hp * 2 + j, :], True, True, [ones, tmp2], [pz])
            p.ts("dve", sg[:, hp * 2:hp * 2 + 2, :], pz[:], 1e-24, None, ALU.max, None, [pz], [sg])
        p.act(sg[:], sg[:], AF.Ln, [sg], [sg])
        p.act(sg[:], sg[:], AF.Exp, [sg], [sg], scale=-0.5)
        p.tt("pool", kkn[:], kkn[:], sg[:], ALU.mult, [kkn, sg], [kkn])
        p.stt("dve", tmp[:], a_[:], -1.0, bc(k_a), ALU.add, ALU.mult, [a_, k_a], [tmp])
        p.stt("dve", kp[:], tmp[:], 1.0, k_[:], ALU.add, ALU.mult, [tmp, k_], [kp])
        p.stt("dve", Ab[:], kkn[:], -1.0, gp[:], ALU.mult, ALU.mult, [kkn, gp], [Ab])
        p.tt("pool", tmp2[:], kkn[:], a_[:], ALU.mult, [kkn, a_], [tmp2])
        p.tt("dve", Bb[:], tmp2[:], gi[:], ALU.mult, [tmp2, gi], [Bb])
        p.tt("pool", Kb[:], kp[:], gi[:], ALU.mult, [kp, gi], [Kb])
        p.tt("dve", Rb[:], r_[:], g_[:], ALU.mult, [r_, g_], [Rb])
        p.tt("pool", tmp[:], r_[:], kp[:], ALU.mult, [r_, kp], [tmp])
        p.tt("pool", tmp[:], tmp[:], bc(r_k), ALU.mult, [tmp, r_k], [tmp])
        for hp in range(4):
            for j in range(2):
                p.mm(pz[:, j, :], ones[:], tmp[:, hp * 2 + j, :], True, True, [ones, tmp], [pz])
            p.tt("dve", bon[:, hp * 2:hp * 2 + 2, :], pz[:], vf[:, hp * 2:hp * 2 + 2, :], ALU.mult, [pz, vf], [bon])

    def chunk(tb, cch):
        bp = tb % 2
        g = tb * NCH + cch
        cp_ = g % 2
        cc = slice(cch * 64, (cch + 1) * 64)
        vb, Ab, Bb, Kb, Rb, g_, yfm = vb2[bp], Ab2[bp], Bb2[bp], Kb2[bp], Rb2[bp], g2[bp], yfm2[bp]
        LakT, ArbT, ArkT, Vtm, Btm, Ktm = LakT2[cp_], ArbT2[cp_], ArkT2[cp_], Vtm2[cp_], Btm2[cp_], Ktm2[cp_]
        NTp = NTa[cp_]

        def scores(lh, rh, mask, dst, eng):
            ps = PG()
            for h in range(H):
                p.mm(ps[:, h, :], lh[:, h, cc], rh[:, h, cc], True, True, [lh, rh], [ps])
            p.tt(eng, dst[:], ps[:], mask[:], ALU.mult, [ps, mask], [dst])
        scores(Ab, Bb, mL, P_[0], "dve")
        scores(Bb, Ab, mSU, PT_[0], "dve")
        p.tt("pool", NTp[0][:], PT_[0][:], idh[:], ALU.add, [PT_[0], idh], [NTp[0]])
        scores(Kb, Ab, mSU, LakT, "dve")
        scores(Bb, Rb, mU, ArbT, "dve")
        scores(Kb, Rb, mU, ArkT, "dve")

        def transp(src, dst):
            ps = PB()
            for h in range(H):
                p.tr(ps[:, h, :], src[:, h, cc], id64, [src, c.idb], [ps])
            p.cp("act", dst[:], ps[:], [ps], [dst])
        transp(vb, Vtm)
        transp(Bb, Btm)
        transp(Kb, Ktm)
        cur = 0
        nti = 0
        for step in range(5):
            Pn, PTn = P_[1 - cur], PT_[1 - cur]
            ps = PG()
            for h in range(H):
                p.mm(ps[:, h, :], PT_[cur][:, h, :], P_[cur][:, h, :], True, True, [PT_[cur], P_[cur]], [ps])
            p.cp("act", Pn[:], ps[:], [ps], [Pn])
            if step < 4:
                ps2 = PG()
                for h in range(H):
                    p.mm(ps2[:, h, :], P_[cur][:, h, :], PT_[cur][:, h, :], True, True, [PT_[cur], P_[cur]], [ps2])
                p.cp("dve", PTn[:], ps2[:], [ps2], [PTn])
            ps3 = PG()
            NTo, NTn = NTp[nti], NTp[1 - nti]
            for h in range(H):
                p.mm(ps3[:, h, :], id64, NTo[:, h, :], True, False, [c.idb, NTo], [ps3])
                p.mm(ps3[:, h, :], Pn[:, h, :], NTo[:, h, :], False, True, [Pn, NTo], [ps3])
            p.cp("act", NTn[:], ps3[:], [ps3], [NTn])
            nti = 1 - nti
            cur = 1 - cur
        NT = NTp[nti]
        ps = PG()
        for h in range(H):
            p.mm(ps[:, h, :], Ab[:, h, cc], T0b[:, h, :], True, False, [Ab, T0b], [ps])
            p.mm(ps[:, h, :], LakT[:, h, :], Vtm[:, h, :], False, True, [LakT, Vtm], [ps])
        p.cp("act", X_[:], ps[:], [ps], [X_])
        ps = PG()
        for h in range(H):
            p.mm(ps[:, h, :], NT[:, h, :], X_[:, h, :], True, True, [NT, X_], [ps])
        p.cp("act", U_[:], ps[:], [ps], [U_])
        py = PG()
        for h in range(H):
            p.mm(py[:, h, :], Rb[:, h, cc], T0b[:, h, :], True, False, [Rb, T0b], [py])
            p.mm(py[:, h, :], ArbT[:, h, :], U_[:, h, :], False, False, [ArbT, U_], [py])
            p.mm(py[:, h, :], ArkT[:, h, :], Vtm[:, h, :], False, True, [ArkT, Vtm], [py])
        ps = PG()
        for h in range(H):
            p.mm(ps[:, h, :], Btm[:, h, :], U_[:, h, :], True, False, [Btm, U_], [ps])
            p.mm(ps[:, h, :], Ktm[:, h, :], Vtm[:, h, :], False, True, [Ktm, Vtm], [ps])
        p.tt("dve", T1[:], T0[:], ps[:], ALU.add, [T0, ps], [T1])
        gC = g_[:, :, cch * 64 + 63:cch * 64 + 64].broadcast_to([64, H, 64])
        p.tt("dve", T0[:], T1[:], gC, ALU.mult, [T1, g_], [T0])
        p.cp("pool", T0b[:], T0[:], [T0], [T0b])
        p.op("dve", lambda e, py=py: e.tensor_reduce(mean[:], py[:], AX.X, ALU.add), [py], [mean], dur=700)
        p.ts("dve", mean[:], mean[:], 1.0 / 64, None, ALU.mult, None, [mean], [mean])
        p.tt("dve", yc[:], py[:], mean[:].unsqueeze(2).broadcast_to([64, H, 64]), ALU.subtract, [py, mean], [yc])
        p.act(ysq[:], yc[:], AF.Square, [yc], [ysq])
        p.op("dve", lambda e: e.tensor_reduce(var[:], ysq[:], AX.X, ALU.add), [ysq], [var], dur=700)
        p.act(var[:], var[:], AF.Ln, [var, gne], [var], bias=gne[:], scale=1.0 / 64)
        p.act(var[:], var[:], AF.Exp, [var], [var], scale=-0.5)
        p.tt("pool", ynm[:], yc[:], var[:].unsqueeze(2).broadcast_to([64, H, 64]), ALU.mult, [yc, var], [ynm])
        ps = PG()
        for h in range(H):
            p.tr(ps[:, h, :], ynm[:, h, :], c.idf[0:64, 0:64], [ynm, c.idf], [ps])
        p.cp("act", yfm[:, :, cc], ps[:], [ps], [yfm])

    def epilogue(tb):
        bp = tb % 2
        cols = slice(tb * TB, (tb + 1) * TB)
        yfm, bon, bgt, yso = yfm2[bp], bon2[bp], bgt2[bp], yso2[bp]
        p.tt("dve", yfm[:], yfm[:], bc(lnw), ALU.mult, [yfm, lnw], [yfm])
        p.tt("pool", yfm[:], yfm[:], bc(lnb), ALU.add, [yfm, lnb], [yfm])
        p.tt("pool", yfm[:], yfm[:], bon[:], ALU.add, [yfm, bon], [yfm])
        p.tt("dve", yso[:], yfm[:], bgt[:], ALU.mult, [yfm, bgt], [yso])
        p.dma(sc["YS"][512:1024, cols].rearrange("(h d) t -> d h t", d=64), yso[:], [yso], [sc["YS"]])

    for tb in range(NB):
        prep(tb)
        for cch in range(NCH):
            chunk(tb, cch)
        epilogue(tb)


def _hv(p, name, ap):
    t = p.sbuf(name, [64, 8], F32)
    p.dma(t[:], ap.rearrange("(h d) -> d h", d=64), [], [t], allow_slow_non_contiguous=True)
    return t


def phase_out(p, c, S, l, xin, xout, W, sc):
    NB = S // 512
    wbo = p.sbuf("wbo", [128, 12, D], BF16)
    wout = p.sbuf("wout", [128, 8, D], BF16)
    gpb = p.sbuf("gpb", [128, D], F32)
    p.dma(gpb[:], W["norm_post"][l:l + 1, :].partition_broadcast(128), [], [gpb])
    mk = p.mark()
    stg = [p.sbuf("wstg%d" % i, [128, 4, D], F32) for i in range(2)]
    for n in range(3):
        p.dma(stg[n % 2][:], W["w_branch_out"][l, n].rearrange("(k p) d -> p k d", p=128), [], [stg[n % 2]])
        p.cp("pool", wbo[:, n * 4:(n + 1) * 4, :], stg[n % 2][:], [stg[n % 2]], [wbo])
    for hf in range(2):
        p.dma(stg[(hf + 1) % 2][:], W["w_out"][l, hf * 512:(hf + 1) * 512, :].rearrange("(k p) d -> p k d", p=128), [], [stg[(hf + 1) % 2]])
        p.cp("pool", wout[:, hf * 4:(hf + 1) * 4, :], stg[(hf + 1) % 2][:], [stg[(hf + 1) % 2]], [wout])
    p.release(mk)
    ysb = [p.sbuf("ysb%d" % i, [128, 12, 512], BF16) for i in range(2)]
    mgb = [p.sbuf("mgb%d" % i, [128, 24, 512], BF16) for i in range(2)]
    mT = p.sbuf("mT", [128, 8, 512], BF16)
    m32 = p.sbuf("m32", [128, 512], F32)
    t32 = [p.sbuf("t32_%d" % i, [128, 512], F32) for i in range(2)]
    xt = [p.sbuf("xo%d" % i, [128, D], F32) for i in range(2)]
    o32 = [p.sbuf("o32_%d" % i, [128, D], F32) for i in range(2)]
    junk = p.sbuf("junko", [128, 512], BF16)
    ssq = p.sbuf("ssq", [128, 2], F32)
    rstd = p.sbuf("rstdo", [128, 1], F32)
    pb = [p.psum("pb%d" % i, [128, 512], F32) for i in range(3)]
    po = [p.psum("po%d" % i, [128, 512], F32) for i in range(2)]
    it = 0
    for tb in range(NB):
        cols = slice(tb * 512, (tb + 1) * 512)
        ysx, mgx = ysb[tb % 2], mgb[tb % 2]
        p.dma(ysx[:], sc["YS"][:, cols].rearrange("(c p) t -> p c t", p=128), [sc["YS"]], [ysx])
        p.dma(mgx[:], sc["MG"][:, cols].rearrange("(c p) t -> p c t", p=128), [sc["MG"]], [mgx])
        for cch in range(8):
            for n in range(3):
                for k in range(4):
                    p.mm(pb[n][:], wbo[:, n * 4 + k, cch * 128:(cch + 1) * 128], ysx[:, n * 4 + k, :], k == 0, k == 3,
                         [wbo, ysx], [pb[n]])
            p.tt("dve", m32[:], pb[0][:], mgx[:, cch, :], ALU.mult, [pb[0], mgx], [m32])
            p.tt("dve", t32[0][:], pb[1][:], mgx[:, 8 + cch, :], ALU.mult, [pb[1], mgx], [t32[0]])
            p.tt("dve", t32[1][:], pb[2][:], mgx[:, 16 + cch, :], ALU.mult, [pb[2], mgx], [t32[1]])
            p.tt("pool", m32[:], m32[:], t32[0][:], ALU.add, [m32, t32[0]], [m32])
            p.tt("pool", mT[:, cch, :], m32[:], t32[1][:], ALU.add, [m32, t32[1]], [mT])
        for tsub in range(4):
            s_ = it % 2
            it += 1
            r0 = tb * 512 + tsub * 128
            p.dma(xt[s_][:], xin[r0:r0 + 128, :], [xin], [xt[s_]])
            for hf in range(2):
                for k in range(8):
                    p.mm(po[hf][:], mT[:, k, tsub * 128:(tsub + 1) * 128], wout[:, k, hf * 512:(hf + 1) * 512], k == 0, k == 7,
                         [mT, wout], [po[hf]])
                p.act(junk[:], po[hf][:], AF.Square, [po[hf]], [junk, ssq], accum_out=ssq[:, hf:hf + 1])
            p.tt("dve", rstd[:], ssq[:, 0:1], ssq[:, 1:2], ALU.add, [ssq], [rstd])
            p.act(rstd[:], rstd[:], AF.Ln, [rstd, c.epsb], [rstd], bias=c.epsb[:], scale=1.0 / D)
            p.act(rstd[:], rstd[:], AF.Exp, [rstd], [rstd], scale=-0.5)
            for hf in range(2):
                p.stt("dve", o32[s_][:, hf * 512:(hf + 1) * 512], po[hf][:], rstd[:, 0:1], gpb[:, hf * 512:(hf + 1) * 512],
                      ALU.mult, ALU.mult, [po[hf], rstd, gpb], [o32[s_]])
            p.tt("pool", o32[s_][:], o32[s_][:], xt[s_][:], ALU.add, [o32[s_], xt[s_]], [o32[s_]])
            p.dma(xout[r0:r0 + 128, :], o32[s_][:], [o32[s_]], [xout])


def make_scratch(p, S, dbg):
    kind = "ExternalOutput" if dbg else "Internal"
    sc = {}
    sc["QT"] = p.dram("QT", [8, 96, S], BF16, kind)
    sc["KT"] = p.dram("KT", [8, 96, S], BF16, kind)
    sc["VA"] = p.dram("VA", [S, 520], BF16, kind)
    sc["U"] = p.dram("U", [1664, S], F32, kind)
    sc["GQ"] = p.dram("GQ", [256, S], F32, kind)
    sc["GK"] = p.dram("GK", [256, S], F32, kind)
    sc["GV"] = p.dram("GV", [S, 512], BF16, kind)
    sc["GL"] = p.dram("GL", [16, S], F32, kind)
    sc["BG"] = p.dram("BG", [1536, S], BF16, kind)
    sc["MG"] = p.dram("MG", [3072, S], BF16, kind)
    sc["YS"] = p.dram("YS", [1536, S], BF16, kind)
    sc["X1"] = p.dram("X1", [S, D], F32, kind)
    return sc


WSPEC = {
    "norm_pre": ([L, D], F32), "w_in": ([L, D, NIN], F32), "wkr2": ([L, D, 192], F32),
    "mla_q_norm": ([L, 256], F32), "mla_kv_norm": ([L, 128], F32),
    "mla_w_uq": ([L, 256, 768], F32), "wuq_sw": ([L, 256, 768], F32),
    "wuk": ([L, 128, 512], F32), "wuv": ([L, 128, 512], F32),
    "rwkv_mu": ([L, 1664], F32), "rwkv_w0": ([L, 512], F32), "rwkv_w_up": ([L, 64, 512], F32),
    "rwkv_a0": ([L, 512], F32), "rwkv_a_up": ([L, 64, 512], F32), "rwkv_k_k": ([L, 512], F32),
    "rwkv_k_a": ([L, 512], F32), "rwkv_r_k": ([L, 512], F32), "rwkv_ln_w": ([L, 512], F32),
    "rwkv_ln_b": ([L, 512], F32), "gla_a_up": ([L, 16, 256], F32), "gla_a_b": ([L, 256], F32),
    "gla_norm": ([L, 512], F32), "w_branch_out": ([L, 3, 512, D], F32), "w_out": ([L, D, D], F32),
    "norm_post": ([L, D], F32),
}


def host_weights(inp):
    w = {}
    for k in WSPEC:
        if k in inp:
            w[k] = np.ascontiguousarray(np.asarray(inp[k], dtype=np.float32))
    win = w["w_in"]
    w["wkr2"] = np.ascontiguousarray(np.concatenate(
        [win[:, :, 320:416], win[:, :, 320:384], win[:, :, 400:416], win[:, :, 384:400]], axis=2))
    uq = w["mla_w_uq"].reshape(L, 256, 8, 96)
    w["wuq_sw"] = np.ascontiguousarray(np.concatenate(
        [uq[..., 0:64], uq[..., 80:96], uq[..., 64:80]], axis=-1).reshape(L, 256, 768))
    ukv = w["mla_w_ukv"] if "mla_w_ukv" in w else np.asarray(inp["mla_w_ukv"], dtype=np.float32)
    ukv = ukv.reshape(L, 128, 8, 128)
    w["wuk"] = np.ascontiguousarray(ukv[..., 0:64].reshape(L, 128, 512))
    w["wuv"] = np.ascontiguousarray(ukv[..., 64:128].reshape(L, 128, 512))
    w["rwkv_r_k"] = w["rwkv_r_k"].reshape(L, 512) if "rwkv_r_k" in w else np.asarray(inp["rwkv_r_k"], np.float32).reshape(L, 512)
    return w


def build(S, phases=("p1",), dbg=True, nlayers=1):
    nc = bass.Bass("TRN2", target_bir_lowering=False)
    p = Prog(nc)
    x_d = p.dram("x", [S, D], F32, "ExternalInput")
    pos_d = p.dram("pos", [1, S], I32, "ExternalInput")
    W = {k: p.dram(k, sh, dt, "ExternalInput").t for k, (sh, dt) in WSPEC.items()}
    out_d = p.dram("out", [S, D], F32, "ExternalOutput")
    sc = make_scratch(p, S, dbg)
    c = setup_consts(p, S, pos_d.t)
    final = [out_d]
    for l in range(nlayers):
        xin = x_d if l == 0 else sc["X1"]
        if "p1" in phases:
            mk = p.mark()
            phase1(p, c, S, l, xin, W, sc)
            p.release(mk)
        if "mla" in phases:
            mk = p.mark()
            phase_mla(p, c, S, l, sc)
            p.release(mk)
        if "rwkv" in phases:
            mk = p.mark()
            phase_rwkv(p, c, S, l, W, sc)
            p.release(mk)
        if "gla" in phases:
            mk = p.mark()
            phase_gla(p, c, S, l, W, sc)
            p.release(mk)
        if "out" in phases:
            mk = p.mark()
            xo = out_d if l == nlayers - 1 else sc["X1"]
            phase_out(p, c, S, l, xin, xo, W, sc)
            p.release(mk)
    if dbg:
        final = list(sc.values()) + [out_d]
    p.finish(final)
    return nc, p


ALL_PHASES = ("p1", "mla", "rwkv", "gla", "out")
_CACHE = {}


def kernel(**inputs):
    x = np.asarray(inputs["x"], dtype=np.float32)
    pos = np.asarray(inputs["positions"], dtype=np.int32)
    B, S, _ = x.shape
    Wh = host_weights(inputs)
    nc, _p = build(S, phases=ALL_PHASES, dbg=False, nlayers=L)
    in_maps = []
    for b in range(B):
        m = {"x": np.ascontiguousarray(x[b]), "pos": np.ascontiguousarray(pos[b:b + 1])}
        for k in WSPEC:
            m[k] = Wh[k]
        in_maps.append(m)
    res = run_bass_kernel_spmd(nc, in_maps, core_ids=list(range(B)))
    return np.stack([np.asarray(r["out"]) for r in res.results], axis=0).astype(np.float32)
```

```python
import math
import numpy as np
import concourse.bass as bass
import concourse.mybir as mybir
from concourse.bass_utils import run_bass_kernel_spmd

F32 = mybir.dt.float32
BF16 = mybir.dt.bfloat16
I32 = mybir.dt.int32
ALU = mybir.AluOpType
AF = mybir.ActivationFunctionType
AX = mybir.AxisListType

SEM_CAP = 30000
D = 1024
NIN = 7728
L = 2
EPS = 1e-6


class Buf:
    __slots__ = ("name", "w", "r")

    def __init__(self, name):
        self.name = name
        self.w = None
        self.r = {}


class T:
    def __init__(self, t, name):
        self.t = t
        self.b = Buf(name)

    def __getitem__(self, k):
        return self.t[k]


def _fs(ap):
    n = 1
    for d in ap.shape[1:]:
        n *= int(d)
    return n


def _nbytes(ap):
    n = 1
    for d in ap.shape:
        n *= int(d)
    return n * mybir.dt.size(ap.dtype)


class Prog:
    CE = ("pe", "act", "dve", "pool")
    QE = ("sp",)
    WINDOW = 48
    SYNC = 300.0

    def __init__(self, nc, n_dma_sems=32):
        self.nc = nc
        self.sems = {}
        self._ctx = []
        self._semctx = []
        self.cur = {}
        self.seen = {e: {} for e in self.CE + self.QE}
        self.nsem = 0
        self.old_sems = []
        for e in self.CE:
            self._new_eng_sem(e)
        self.dma_sems = []
        for i in range(n_dma_sems):
            k = self._alloc_sem("dma%d" % i)
            self.dma_sems.append([k, 0])
        self.dma_rr = 0
        self.ninst = 0
        self.ops = []
        self.uid = 0
        self.model_time = 0.0

    def _alloc_sem(self, name):
        cm = self.nc.semaphore(name)
        h = cm.__enter__()
        self._semctx.append(cm)
        self.sems[name] = h
        self.nsem += 1
        return name

    def _new_eng_sem(self, e):
        if e in self.cur:
            self.old_sems.append(tuple(self.cur[e]))
        k = self._alloc_sem("%s_s%d" % (e, self.nsem))
        self.cur[e] = [k, 0]

    def sbuf(self, name, shape, dt):
        self.uid += 1
        name = "%s_u%d" % (name, self.uid)
        cm = self.nc.sbuf_tensor(name, list(shape), dt)
        t = cm.__enter__()
        self._ctx.append(cm)
        return T(t, name)

    def psum(self, name, shape, dt):
        self.uid += 1
        name = "%s_u%d" % (name, self.uid)
        cm = self.nc.psum_tensor(name, list(shape), dt)
        t = cm.__enter__()
        self._ctx.append(cm)
        return T(t, name)

    def dram(self, name, shape, dt, kind="Internal"):
        t = self.nc.dram_tensor(name, list(shape), dt, kind=kind).ap()
        return T(t, name)

    def op(self, eng, fn, R=(), W=(), signal=True, dur=500.0):
        self.ops.append((eng, fn, [x.b for x in R], [x.b for x in W], float(dur), float(dur), False))
        self.ninst += 1

    def dma(self, out_ap, in_ap, R=(), W=(), q="sp", **kw):
        lat = 2000.0 + _nbytes(out_ap) / 150.0
        self.ops.append((q, lambda e: e.dma_start(out=out_ap, in_=in_ap, **kw),
                         [x.b for x in R], [x.b for x in W], 80.0, lat, True))
        self.ninst += 1

    def mm(self, out, lhsT, rhs, start, stop, R, W, **kw):
        n = _fs(rhs)
        d = (50.0 + 1.0 * max(n, 64)) if lhsT.dtype == F32 else (40.0 + 0.42 * max(n, 64))
        self.op("pe", lambda e: e.matmul(out, lhsT, rhs, start=start, stop=stop, **kw), R, W, dur=d)

    def tr(self, out, in_, ident, R, W, signal=True):
        self.op("pe", lambda e: e.transpose(out, in_, ident), R, W, dur=110.0)

    def act(self, out, in_, func, R, W, **kw):
        self.op("act", lambda e: e.activation(out, in_, func, **kw), R, W, dur=260.0 + 0.83 * _fs(out))

    def _vd(self, eng, out, two_src):
        n = _fs(out)
        if eng == "pool":
            return 150.0 + 2.2 * n
        b16 = mybir.dt.size(out.dtype) == 2
        if two_src:
            return 160.0 + (1.04 * n)
        return 160.0 + (0.55 * n)

    def tt(self, eng, out, a, b, op, R, W):
        self.op(eng, lambda e: e.tensor_tensor(out, a, b, op), R, W, dur=self._vd(eng, out, True))

    def ts(self, eng, out, a, s1, s2, op0, op1, R, W):
        d = self._vd(eng, out, False)
        if s2 is None:
            self.op(eng, lambda e: e.tensor_scalar(out, a, s1, None, op0), R, W, dur=d)
        else:
            self.op(eng, lambda e: e.tensor_scalar(out, a, s1, s2, op0, op1), R, W, dur=d)

    def stt(self, eng, out, a, s, b, op0, op1, R, W):
        self.op(eng, lambda e: e.scalar_tensor_tensor(out, a, s, b, op0, op1), R, W, dur=self._vd(eng, out, True))

    def cp(self, eng, out, in_, R, W):
        if eng == "act":
            self.op(eng, lambda e: e.copy(out, in_), R, W, dur=260.0 + 0.83 * _fs(out))
        else:
            self.op(eng, lambda e: e.tensor_copy(out, in_), R, W, dur=self._vd(eng, out, False))

    def memset(self, eng, ap, val, W):
        self.op(eng, lambda e: e.memset(ap, val), (), W, dur=self._vd(eng, ap, False))

    def _eng(self, eng):
        nc = self.nc
        return {"pe": nc.tensor, "act": nc.scalar, "dve": nc.vector, "pool": nc.gpsimd, "sp": nc.sync}[eng]

    def flush(self):
        import bisect
        ops = self.ops
        self.ops = []
        n = len(ops)
        if n == 0:
            return
        engs = self.CE + self.QE
        lastw = {}
        readers = {}
        deps = [None] * n
        for i in range(n):
            eng, fn, R, W, occ, lat, isd = ops[i]
            d = set()
            for b in R:
                w = lastw.get(id(b))
                if w is not None:
                    d.add(w)
            for b in W:
                w = lastw.get(id(b))
                if w is not None:
                    d.add(w)
                rl = readers.get(id(b))
                if rl:
                    d.update(rl)
            d.discard(i)
            deps[i] = d
            for b in R:
                readers.setdefault(id(b), []).append(i)
            for b in W:
                lastw[id(b)] = i
                readers[id(b)] = []
        succ = [[] for _ in range(n)]
        nd = [0] * n
        for i in range(n):
            nd[i] = len(deps[i])
            for j in deps[i]:
                succ[j].append(i)
        bl = [0.0] * n
        for i in range(n - 1, -1, -1):
            m = 0.0
            for s_ in succ[i]:
                if bl[s_] > m:
                    m = bl[s_]
            bl[i] = m + ops[i][5] + 100.0
        ready = [0.0] * n
        free = {e: 0.0 for e in engs}
        cand = {e: [] for e in engs}
        for i in range(n):
            if nd[i] == 0:
                cand[ops[i][0]].append(i)
        order = {e: [] for e in engs}
        done = 0
        W_ = self.WINDOW
        SY = self.SYNC
        tmax = 0.0
        while done < n:
            best = None
            for e in engs:
                c = cand[e]
                if not c:
                    continue
                fe = free[e]
                bst = None
                for i in c[:W_]:
                    st = ready[i] if ready[i] > fe else fe
                    key = (st, -bl[i])
                    if bst is None or key < bst[2]:
                        bst = (st, i, key)
                if best is None or bst[2] < best[3]:
                    best = (bst[0], bst[1], e, bst[2])
            st, i, e = best[0], best[1], best[2]
            cand[e].remove(i)
            order[e].append(i)
            occ, lat = ops[i][4], ops[i][5]
            free[e] = st + occ
            fin = st + lat
            if fin > tmax:
                tmax = fin
            for s_ in succ[i]:
                rt = fin + (SY if ops[s_][0] != e else (0.0 if e == "pe" else 200.0))
                if rt > ready[s_]:
                    ready[s_] = rt
                nd[s_] -= 1
                if nd[s_] == 0:
                    bisect.insort(cand[ops[s_][0]], s_)
            done += 1
        self.model_time += tmax
        busy = {e: 0.0 for e in engs}
        for i in range(n):
            busy[ops[i][0]] += ops[i][4]
        if not hasattr(self, "regions"):
            self.regions = []
        self.regions.append((tmax, busy, n))
        need = [False] * n
        for i in range(n):
            e = ops[i][0]
            if ops[i][6] or not succ[i]:
                need[i] = True
                continue
            for s_ in succ[i]:
                if ops[s_][0] != e or e != "pe":
                    need[i] = True
                    break
        tick = [None] * n
        reuse_wait = {}
        for e in engs:
            for i in order[e]:
                if ops[i][6]:
                    slot = self.dma_sems[self.dma_rr]
                    self.dma_rr = (self.dma_rr + 1) % len(self.dma_sems)
                    if slot[1] > 0:
                        reuse_wait[i] = (slot[0], slot[1])
                    slot[1] += 16
                    tick[i] = (slot[0], slot[1])
                elif need[i]:
                    cur = self.cur[e]
                    if cur[1] >= SEM_CAP:
                        self._new_eng_sem(e)
                        cur = self.cur[e]
                    cur[1] += 1
                    tick[i] = (cur[0], cur[1])
        for e in engs:
            eo = self._eng(e)
            seen = self.seen[e]
            sems = self.sems
            for i in order[e]:
                w = {}
                for j in deps[i]:
                    if e == "pe" and ops[j][0] == "pe":
                        continue
                    k, v = tick[j]
                    if w.get(k, 0) < v:
                        w[k] = v
                if i in reuse_wait:
                    k, v = reuse_wait[i]
                    if w.get(k, 0) < v:
                        w[k] = v
                for k, v in w.items():
                    if seen.get(k, 0) < v:
                        seen[k] = v
                        eo.wait_ge(sems[k], v)
                ins = ops[i][1](eo)
                if tick[i] is not None:
                    ins.then_inc(sems[tick[i][0]], 16 if ops[i][6] else 1)

    def barrier(self):
        self.flush()
        allv = [(k, v) for (k, v) in [tuple(x) for x in self.cur.values()] if v > 0]
        allv += [(k, v) for (k, v) in self.old_sems]
        allv += [(k, v) for (k, v) in [tuple(x) for x in self.dma_sems] if v > 0]
        for eng in self.CE + self.QE:
            eo = self._eng(eng)
            for k, v in allv:
                if self.seen[eng].get(k, 0) < v:
                    self.seen[eng][k] = v
                    eo.wait_ge(self.sems[k], v)

    def mark(self):
        return len(self._ctx)

    def release(self, mark):
        self.barrier()
        while len(self._ctx) > mark:
            self._ctx.pop().__exit__(None, None, None)

    def finish(self, final):
        self.barrier()
        while self._ctx:
            self._ctx.pop().__exit__(None, None, None)
        while self._semctx:
            self._semctx.pop().__exit__(None, None, None)


C_CQ, C_CKV, C_KR, C_U = 0, 256, 384, 416
C_GQ, C_GK, C_GV, C_GL, C_BG, C_MG = 2080, 2336, 2592, 3104, 3120, 4656
SCALE = (64 + 32) ** -0.5


class Ctx:
    pass


def setup_consts(p, S, pos_d):
    c = Ctx()
    idf = p.sbuf("idf", [128, 128], F32)
    p.memset("pool", idf[:], 1.0, [idf])
    p.op("pool", lambda e: e.affine_select(idf[:], idf[:], [[1, 128]], ALU.is_equal, 0.0,
                                           base=0, channel_multiplier=-1), [idf], [idf])
    idb = p.sbuf("idb", [128, 128], BF16)
    p.cp("dve", idb[:], idf[:], [idf], [idb])
    c.idf, c.idb = idf, idb
    onesb = p.sbuf("onesb", [128, 128], BF16)
    p.memset("pool", onesb[:], 1.0, [onesb])
    c.onesb = onesb
    epsb = p.sbuf("epsb", [128, 1], F32)
    p.memset("pool", epsb[:], EPS, [epsb])
    c.epsb = epsb
    lnsc = p.sbuf("lnsc", [128, 1], F32)
    p.memset("pool", lnsc[:], math.log(SCALE), [lnsc])
    c.lnsc = lnsc
    inv = (1.0 / (10000.0 ** (np.arange(0, 32, 2, dtype=np.float32) / np.float32(32)))).astype(np.float32)
    row = p.sbuf("roperow", [1, 64], F32)
    for j in range(16):
        p.memset("pool", row[0:1, j:j + 1], float(inv[j]), [row])
        p.memset("pool", row[0:1, 16 + j:17 + j], float(inv[j]), [row])
    p.memset("pool", row[0:1, 32:48], -1.0, [row])
    p.memset("pool", row[0:1, 48:64], 1.0, [row])
    rd = p.dram("rope_d", [64], F32)
    p.dma(rd[:].rearrange("(o n) -> o n", o=1), row[:], [row], [rd])
    ctab = p.sbuf("ctab", [96, 2], F32)
    p.dma(ctab[64:96, :], rd[:].rearrange("(c p) -> p c", p=32), [rd], [ctab], allow_slow_non_contiguous=True)
    mk = p.mark()
    cos2 = p.sbuf("cos2", [96, S], F32)
    sin2 = p.sbuf("sin2", [96, S], F32)
    posi = p.sbuf("posi", [96, S], I32)
    p.dma(posi[64:96, :], pos_d.partition_broadcast(32), [], [posi])
    ang = p.sbuf("ang", [96, S], F32)
    p.cp("dve", ang[64:96, :], posi[64:96, :], [posi], [ang])
    p.ts("dve", ang[64:96, :], ang[64:96, :], ctab[64:96, 0:1], None, ALU.mult, None, [ang, ctab], [ang])
    kf = p.sbuf("kf", [96, S], F32)
    sl = slice(64, 96)

    def sin_of(dst, shift):
        a = dst
        p.ts("dve", a[sl, :], ang[sl, :], shift, None, ALU.add, None, [ang], [dst])
        p.ts("dve", kf[sl, :], a[sl, :], 1.0 / (2 * math.pi), None, ALU.mult, None, [dst], [kf])
        p.cp("dve", posi[sl, :], kf[sl, :], [kf], [posi])
        p.cp("dve", kf[sl, :], posi[sl, :], [posi], [kf])
        p.stt("dve", a[sl, :], kf[sl, :], -2 * math.pi, a[sl, :], ALU.mult, ALU.add, [kf, dst], [dst])
        p.ts("dve", kf[sl, :], a[sl, :], math.pi, -2 * math.pi, ALU.is_gt, ALU.mult, [dst], [kf])
        p.tt("dve", a[sl, :], a[sl, :], kf[sl, :], ALU.add, [dst, kf], [dst])
        p.ts("dve", kf[sl, :], a[sl, :], -math.pi, 2 * math.pi, ALU.is_lt, ALU.mult, [dst], [kf])
        p.tt("dve", a[sl, :], a[sl, :], kf[sl, :], ALU.add, [dst, kf], [dst])
        p.act(a[sl, :], a[sl, :], AF.Sin, [dst], [dst])
    sin_of(cos2, math.pi / 2)
    sin_of(sin2, 0.0)
    p.ts("dve", sin2[sl, :], sin2[sl, :], ctab[sl, 1:2], None, ALU.mult, None, [sin2, ctab], [sin2])
    c.rope = p.dram("rope_tab", [2, 32, S], F32)
    p.dma(c.rope[0], cos2[sl, :], [cos2], [c.rope])
    p.dma(c.rope[1], sin2[sl, :], [sin2], [c.rope])
    p.release(mk)
    return c


def colvec(p, name, src_ap, n, dt=F32):
    t = p.sbuf(name, [128, n], dt)
    p.dma(t[:], src_ap.rearrange("(c p) -> p c", p=128), [], [t], allow_slow_non_contiguous=True)
    return t


def phase1(p, c, S, l, xin, W, sc):
    NT_ = S // 128
    NB = S // 512
    hT = p.sbuf("hT%d" % l, [128, 8, S], BF16)
    gbc = p.sbuf("gbc%d" % l, [128, D], F32)
    p.dma(gbc[:], W["norm_pre"][l:l + 1, :].partition_broadcast(128), [], [gbc])
    mk1 = p.mark()
    xt = [p.sbuf("xt%d_%d" % (l, i), [128, D], F32) for i in range(2)]
    hb = [p.sbuf("hb%d_%d" % (l, i), [128, D], BF16) for i in range(2)]
    junk = p.sbuf("junk%d" % l, [128, D], BF16)
    ss = [p.sbuf("ss%d_%d" % (l, i), [128, 1], F32) for i in range(2)]
    ptr = [p.psum("ptr%d_%d" % (l, i), [128, 8, 128], BF16) for i in range(2)]
    for i in range(NT_):
        s = i % 2
        p.dma(xt[s][:], xin[i * 128:(i + 1) * 128, :], [xin], [xt[s]])
        p.act(junk[:], xt[s][:], AF.Square, [xt[s]], [junk, ss[s]], accum_out=ss[s][:])
        p.act(ss[s][:], ss[s][:], AF.Ln, [ss[s], c.epsb], [ss[s]], bias=c.epsb[:], scale=1.0 / D)
        p.act(ss[s][:], ss[s][:], AF.Exp, [ss[s]], [ss[s]], scale=-0.5)
        p.stt("dve", hb[s][:], xt[s][:], ss[s][:, 0:1], gbc[:], ALU.mult, ALU.mult,
              [xt[s], ss[s], gbc], [hb[s]])
        for k in range(8):
            p.tr(ptr[s][:, k, :], hb[s][:, k * 128:(k + 1) * 128], c.idb[:], [hb[s], c.idb], [ptr[s]],
                 signal=(k == 7))
        p.cp("act", hT[:, :, i * 128:(i + 1) * 128], ptr[s][:], [ptr[s]], [hT])

    p.release(mk1)
    mk2 = p.mark()
    wa32 = p.sbuf("wa32_%d" % l, [128, 8, 384], F32)
    p.dma(wa32[:], W["w_in"][l, :, 0:384].rearrange("(c p) n -> p c n", p=128), [], [wa32])
    wa = p.sbuf("wa_%d" % l, [128, 8, 384], BF16)
    p.cp("pool", wa[:], wa32[:], [wa32], [wa])
    wk32 = p.sbuf("wk32_%d" % l, [128, 8, 192], F32)
    p.dma(wk32[:], W["wkr2"][l].rearrange("(c p) n -> p c n", p=128), [], [wk32])
    wkr = p.sbuf("wkr_%d" % l, [128, 8, 192], BF16)
    p.cp("pool", wkr[:], wk32[:], [wk32], [wkr])
    wq32 = p.sbuf("wq32_%d" % l, [128, 2, 1536], F32)
    p.dma(wq32[:, :, 0:768], W["mla_w_uq"][l].rearrange("(c p) n -> p c n", p=128), [], [wq32])
    p.dma(wq32[:, :, 768:1536], W["wuq_sw"][l].rearrange("(c p) n -> p c n", p=128), [], [wq32])
    wq = p.sbuf("wq_%d" % l, [128, 2, 1536], BF16)
    p.cp("pool", wq[:], wq32[:], [wq32], [wq])
    wkv32 = p.sbuf("wkv32_%d" % l, [128, 1024], F32)
    p.dma(wkv32[:, 0:512], W["wuk"][l], [], [wkv32])
    p.dma(wkv32[:, 512:1024], W["wuv"][l], [], [wkv32])
    wkv = p.sbuf("wkv_%d" % l, [128, 1024], BF16)
    p.cp("pool", wkv[:], wkv32[:], [wkv32], [wkv])
    cos2 = p.sbuf("cos2_%d" % l, [96, S], F32)
    sin2 = p.sbuf("sin2_%d" % l, [96, S], F32)
    p.dma(cos2[64:96, :], c.rope[0], [c.rope], [cos2])
    p.dma(sin2[64:96, :], c.rope[1], [c.rope], [sin2])
    gq = colvec(p, "gq%d" % l, W["mla_q_norm"][l], 2)
    gkv = colvec(p, "gkv%d" % l, W["mla_kv_norm"][l], 1)

    pq = [p.psum("pq%d_%d" % (l, i), [128, 512], F32) for i in range(2)]
    pkv = p.psum("pkv%d" % l, [128, 512], F32)
    pkr = p.psum("pkr%d" % l, [96, 2, 512], F32)
    pn = p.psum("pn%d" % l, [128, 512], F32)
    pw = [p.psum("pw%d_%d" % (l, i), [128, 512], F32) for i in range(2)]
    sq = p.sbuf("sq%d" % l, [128, 2, 512], BF16)
    rq = p.sbuf("rq%d" % l, [128, 512], F32)
    cqn = p.sbuf("cqn%d" % l, [128, 2, 512], BF16)
    ckvn = p.sbuf("ckvn%d" % l, [128, 512], BF16)
    qst = p.sbuf("qst%d" % l, [96, 8, 512], BF16)
    kst = p.sbuf("kst%d" % l, [128, 4, 512], BF16)
    krs = p.sbuf("krs%d" % l, [96, 512], BF16)
    t1 = p.sbuf("t1_%d" % l, [96, 512], F32)
    t2 = p.sbuf("t2_%d" % l, [96, 512], F32)
    vst = p.sbuf("vst%d" % l, [128, 4, 8, 65], BF16)
    p.memset("pool", vst[:], 1.0, [vst])
    rs = slice(64, 96)
    for tb in range(NB):
        ts_ = slice(tb * 512, (tb + 1) * 512)
        for cc in range(2):
            for k in range(8):
                p.mm(pq[cc][:], wa[:, k, cc * 128:(cc + 1) * 128], hT[:, k, ts_], k == 0, k == 7, [wa, hT], [pq[cc]])
        for k in range(8):
            p.mm(pkv[:], wa[:, k, 256:384], hT[:, k, ts_], k == 0, k == 7, [wa, hT], [pkv])
        for v in range(2):
            for k in range(8):
                p.mm(pkr[:, v, :], wkr[:, k, v * 96:(v + 1) * 96], hT[:, k, ts_], k == 0, k == 7, [wkr, hT], [pkr])
        for cc in range(2):
            p.act(sq[:, cc, :], pq[cc][:], AF.Square, [pq[cc]], [sq])
        for cc in range(2):
            p.mm(pn[:], c.onesb[:], sq[:, cc, :], cc == 0, cc == 1, [c.onesb, sq], [pn])
        p.act(rq[:], pn[:], AF.Ln, [pn, c.epsb], [rq], bias=c.epsb[:], scale=1.0 / 256)
        p.act(rq[:], rq[:], AF.Exp, [rq, c.lnsc], [rq], bias=c.lnsc[:], scale=-0.5)
        for cc in range(2):
            p.stt("dve", cqn[:, cc, :], pq[cc][:], gq[:, cc:cc + 1], rq[:], ALU.mult, ALU.mult,
                  [pq[cc], gq, rq], [cqn])
        p.act(sq[:, 0, :], pkv[:], AF.Square, [pkv], [sq])
        p.mm(pn[:], c.onesb[:], sq[:, 0, :], True, True, [c.onesb, sq], [pn])
        p.act(rq[:], pn[:], AF.Ln, [pn, c.epsb], [rq], bias=c.epsb[:], scale=1.0 / 128)
        p.act(rq[:], rq[:], AF.Exp, [rq], [rq], scale=-0.5)
        p.stt("dve", ckvn[:], pkv[:], gkv[:, 0:1], rq[:], ALU.mult, ALU.mult, [pkv, gkv, rq], [ckvn])
        p.tt("dve", t1[rs, :], pkr[rs, 0, :], cos2[rs, ts_], ALU.mult, [pkr, cos2], [t1])
        p.tt("dve", t2[rs, :], pkr[rs, 1, :], sin2[rs, ts_], ALU.mult, [pkr, sin2], [t2])
        p.tt("pool", krs[rs, :], t1[rs, :], t2[rs, :], ALU.add, [t1, t2], [krs])
        for h in range(8):
            p.dma(sc["KT"][h, 64:96, ts_], krs[rs, :], [krs], [sc["KT"]])
        for h in range(8):
            a, b = pw[0], pw[1]
            for k in range(2):
                p.mm(a[0:96, :], wq[:, k, h * 96:(h + 1) * 96], cqn[:, k, :], k == 0, k == 1, [wq, cqn], [a])
            for k in range(2):
                p.mm(b[0:96, :], wq[:, k, 768 + h * 96:768 + (h + 1) * 96], cqn[:, k, :], k == 0, k == 1, [wq, cqn], [b])
            p.cp("act", qst[0:64, h, :], a[0:64, :], [a], [qst])
            p.tt("dve", t1[rs, :], a[rs, :], cos2[rs, ts_], ALU.mult, [a, cos2], [t1])
            p.tt("dve", t2[rs, :], b[rs, :], sin2[rs, ts_], ALU.mult, [b, sin2], [t2])
            p.tt("pool", qst[rs, h, :], t1[rs, :], t2[rs, :], ALU.add, [t1, t2], [qst])
        p.dma(sc["QT"][:, :, ts_].rearrange("h p t -> p h t"), qst[:], [qst], [sc["QT"]])
        for j in range(4):
            a = pw[j % 2]
            p.mm(a[:], wkv[:, j * 128:(j + 1) * 128], ckvn[:], True, True, [wkv, ckvn], [a])
            p.cp("act", kst[:, j, :], a[:], [a], [kst])
        for two in range(2):
            p.dma(sc["KT"][:, 0:64, ts_].rearrange("(j two) p t -> two p j t", two=2)[two],
                  kst[two * 64:(two + 1) * 64, :, :], [kst], [sc["KT"]])
        for tt_ in range(4):
            a = pw[tt_ % 2]
            p.mm(a[:], ckvn[:, tt_ * 128:(tt_ + 1) * 128], wkv[:, 512:1024], True, True, [ckvn, wkv], [a])
            p.cp("act", vst[:, tt_, :, 0:64], a[:].rearrange("p (h v) -> p h v", v=64), [a], [vst])
        p.dma(sc["VA"][ts_, :].rearrange("(tt p) n -> p tt n", p=128),
              vst[:].rearrange("p t h v -> p t (h v)"), [vst], [sc["VA"]])

    p.release(mk2)
    mk3 = p.mark()
    pw = [p.psum("pwb%d_%d" % (l, i), [128, 512], F32) for i in range(2)]
    chunks = []
    for i in range(13):
        chunks.append((C_U + i * 128, 128, "mix", "U", i * 128))
    for i in range(2):
        chunks.append((C_GQ + i * 128, 128, "copy32", "GQ", i * 128))
    for i in range(2):
        chunks.append((C_GK + i * 128, 128, "copy32", "GK", i * 128))
    chunks.append((C_GL, 16, "copy32", "GL", 0))
    for i in range(12):
        chunks.append((C_BG + i * 128, 128, "silu", "BG", i * 128))
    for i in range(24):
        chunks.append((C_MG + i * 128, 128, "sigm", "MG", i * 128))
    chunks.sort(key=lambda x: {"mix": 0, "copy32": 0, "silu": 1, "sigm": 2}[x[2]])
    pw = pw + [p.psum("pwd%d_%d" % (l, i), [128, 512], F32) for i in range(2)]
    mu13 = colvec(p, "mu13_%d" % l, W["rwkv_mu"][l], 13)
    wc32 = [p.sbuf("wc32_%d_%d" % (l, i), [128, 8, 128], F32) for i in range(3)]
    wcb = [p.sbuf("wcb_%d_%d" % (l, i), [128, 8, 128], BF16) for i in range(3)]
    st32 = [p.sbuf("st32_%d_%d" % (l, i), [128, 512], F32) for i in range(4)]
    st16 = [p.sbuf("st16_%d_%d" % (l, i), [128, 512], BF16) for i in range(4)]
    stgm = [p.sbuf("stgm_%d_%d" % (l, i), [128, 513], F32) for i in range(3)]
    dmx = [p.sbuf("dmx_%d_%d" % (l, i), [128, 512], F32) for i in range(2)]
    wg32 = p.sbuf("wg32_%d" % l, [128, 8, 512], F32)
    p.dma(wg32[:], W["w_in"][l, :, C_GV:C_GV + 512].rearrange("(c p) n -> p c n", p=128), [], [wg32])
    wg = p.sbuf("wg_%d" % l, [128, 8, 512], BF16)
    p.cp("pool", wg[:], wg32[:], [wg32], [wg])
    n = 0
    nm = 0
    for ci, (col0, m, kind, dest, row0) in enumerate(chunks):
        s = ci % 3
        p.dma(wc32[s][:, :, 0:m], W["w_in"][l, :, col0:col0 + m].rearrange("(c p) n -> p c n", p=128), [], [wc32[s]])
        p.cp("pool", wcb[s][:, :, 0:m], wc32[s][:, :, 0:m], [wc32[s]], [wcb[s]])
        prev = None
        for tb in range(NB):
            ts_ = slice(tb * 512, (tb + 1) * 512)
            a = pw[n % 4]
            for k in range(8):
                p.mm(a[0:m, :], wcb[s][:, k, 0:m], hT[:, k, ts_], k == 0, k == 7, [wcb[s], hT], [a])
            if kind == "mix":
                sg_ = stgm[nm % 3]
                dd = dmx[nm % 2]
                nm += 1
                o = st32[n % 4]
                if prev is None:
                    p.memset("pool", sg_[:, 0:1], 0.0, [sg_])
                else:
                    p.cp("pool", sg_[:, 0:1], prev[:, 512:513], [prev], [sg_])
                p.cp("act", sg_[:, 1:513], a[:], [a], [sg_])
                p.tt("dve", dd[:], sg_[:, 0:512], sg_[:, 1:513], ALU.subtract, [sg_], [dd])
                mi = row0 // 128
                p.stt("dve", o[:], dd[:], mu13[:, mi:mi + 1], sg_[:, 1:513], ALU.mult, ALU.add, [dd, mu13, sg_], [o])
                prev = sg_
            elif kind == "copy32":
                o = st32[n % 4]
                if n % 2 == 0:
                    p.cp("act", o[0:m, :], a[0:m, :], [a], [o])
                else:
                    p.cp("dve", o[0:m, :], a[0:m, :], [a], [o])
            else:
                o = st16[n % 4]
                p.act(o[0:m, :], a[0:m, :], AF.Silu if kind == "silu" else AF.Sigmoid, [a], [o])
            p.dma(sc[dest][row0:row0 + m, ts_], o[0:m, :], [o], [sc[dest]])
            n += 1
    for i in range(NT_):
        a = pw[n % 4]
        o = st16[n % 4]
        n += 1
        for k in range(8):
            p.mm(a[:], hT[:, k, i * 128:(i + 1) * 128], wg[:, k, :], k == 0, k == 7, [hT, wg], [a])
        p.cp("act", o[:], a[:], [a], [o])
        p.dma(sc["GV"][i * 128:(i + 1) * 128, :], o[:], [o], [sc["GV"]])
    p.release(mk3)


def phase_mla(p, c, S, l, sc):
    NB = S // 512
    NT_ = S // 128
    kt = p.sbuf("kt", [96, 8, S], BF16)
    for h in range(8):
        p.dma(kt[:, h, :], sc["KT"][h], [sc["KT"]], [kt])
    va = p.sbuf("va", [128, NT_, 520], BF16)
    p.dma(va[:], sc["VA"].t.rearrange("(n p) f -> p n f", p=128), [sc["VA"]], [va])
    qts = [p.sbuf("qt%d" % i, [96, 8, 512], BF16) for i in range(2)]
    bgs = [p.sbuf("bg%d" % i, [128, 4, 512], BF16) for i in range(2)]
    pts = [p.sbuf("pt%d" % i, [128, 512], BF16) for i in range(3)]
    ytm = p.sbuf("ytm", [128, 4, 512], F32)
    ys = [p.sbuf("ysm%d" % i, [128, 4, 512], BF16) for i in range(2)]
    rec = p.sbuf("rec", [128, 4], F32)
    pst = [p.psum("pst%d" % i, [128, 512], F32) for i in range(2)]
    acc = [p.psum("acc%d" % i, [128, 512], F32) for i in range(4)]
    pT = p.psum("pT", [128, 4, 128], F32)
    n = 0
    for qb in range(NB):
        qt = qts[qb % 2]
        bg = bgs[qb % 2]
        cols = slice(qb * 512, (qb + 1) * 512)
        p.dma(qt[:], sc["QT"][:, :, cols].rearrange("h p t -> p h t"), [sc["QT"]], [qt])
        p.dma(bg[:], sc["BG"][0:512, cols].rearrange("(c p) t -> p c t", p=128), [sc["BG"]], [bg])
        for h in range(8):
            nkt = 4 * (qb + 1)
            for kti in range(nkt):
                j = kti - 4 * qb
                q0 = 128 * j if j > 0 else 0
                N = 512 - q0
                st = pst[n % 2]
                pt = pts[n % 3]
                p.mm(st[:, 0:N], kt[:, h, kti * 128:(kti + 1) * 128], qt[:, h, q0:512], True, True, [kt, qt], [st])
                p.act(pt[:, 0:N], st[:, 0:N], AF.Exp, [st], [pt])
                if j >= 0:
                    p.op("pool", lambda e, pt=pt: e.affine_select(pt[:, 0:128], pt[:, 0:128], [[1, 128]], ALU.is_ge, 0.0,
                                                                  base=0, channel_multiplier=-1), [pt], [pt])
                for qs in range(q0 // 128, 4):
                    p.mm(acc[qs][:, 0:65], pt[:, qs * 128 - q0:(qs + 1) * 128 - q0], va[:, kti, h * 65:(h + 1) * 65],
                         kti == 0, kti == 4 * qb + qs, [pt, va], [acc[qs]])
                n += 1
            for qs in range(4):
                p.op("dve", lambda e, qs=qs: e.reciprocal(rec[:, qs:qs + 1], acc[qs][:, 64:65]), [acc[qs]], [rec])
                p.ts("dve", ytm[:, qs, h * 64:(h + 1) * 64], acc[qs][:, 0:64], rec[:, qs:qs + 1], None, ALU.mult, None,
                     [acc[qs], rec], [ytm])
        yo = ys[qb % 2]
        for fc in range(4):
            for qs in range(4):
                p.tr(pT[:, qs, :], ytm[:, qs, fc * 128:(fc + 1) * 128], c.idf[:], [ytm, c.idf], [pT], signal=(qs == 3))
            p.tt("dve", yo[:, fc, :], pT[:].rearrange("p a b -> p (a b)"), bg[:, fc, :], ALU.mult, [pT, bg], [yo])
        p.dma(sc["YS"][0:512, cols].rearrange("(c p) t -> p c t", p=128), yo[:], [yo], [sc["YS"]])


def phase_gla(p, c, S, l, W, sc, ysblk=None):
    NB = S // 512
    H = 4
    aup = p.sbuf("g_aup", [16, 256], F32)
    p.dma(aup[:], W["gla_a_up"][l], [], [aup])
    nab = p.sbuf("g_nab", [64, H], F32)
    p.dma(nab[:], W["gla_a_b"][l].rearrange("(h d) -> d h", d=64), [], [nab], allow_slow_non_contiguous=True)
    p.ts("dve", nab[:], nab[:], -1.0, None, ALU.mult, None, [nab], [nab])
    gn = colvec(p, "g_gn", W["gla_norm"][l], 4)
    one1 = p.sbuf("g_one1", [128, 1], F32)
    p.memset("pool", one1[:], 1.0, [one1])
    maskr = p.sbuf("g_maskr", [64, 512], F32)
    p.memset("pool", maskr[:], 1.0, [maskr])
    p.memset("pool", maskr[:].rearrange("p (c t) -> p c t", t=64)[:, :, 0:1], 0.0, [maskr])
    msk = p.sbuf("g_msk", [64, H, 64], F32)
    p.memset("pool", msk[:], 1.0, [msk])
    for h in range(H):
        p.op("pool", lambda e, h=h: e.affine_select(msk[:, h, :], msk[:, h, :], [[1, 64]], ALU.is_ge, 0.0,
                                                    base=0, channel_multiplier=-1), [msk], [msk])
    st = p.sbuf("g_st", [64, H, 128], F32)
    stb = p.sbuf("g_stb", [64, H, 128], BF16)
    st2 = p.sbuf("g_st2", [64, H, 128], F32)
    p.memset("pool", st[:], 0.0, [st])
    p.memset("pool", stb[:], 0.0, [stb])
    gq1 = p.sbuf("g_q", [64, H, 512], F32)
    gk1 = p.sbuf("g_k", [64, H, 512], F32)
    gq, gk = [gq1, gq1], [gk1, gk1]
    gl1 = p.sbuf("g_l", [16, 512], F32)
    gl = [gl1, gl1]
    gv = [p.sbuf("g_v%d" % i, [64, 8, 512], BF16) for i in range(2)]
    bg1 = p.sbuf("g_bg", [128, H, 512], BF16)
    bg = [bg1, bg1]
    e1 = p.sbuf("g_e1", [64, H, 512], F32)
    cs = p.sbuf("g_cs", [64, H, 512], F32)
    eb = p.sbuf("g_eb", [64, H, 512], F32)
    enb = e1
    qe = p.sbuf("g_qe", [64, H, 512], BF16)
    ke = p.sbuf("g_ke", [64, H, 512], BF16)
    att = [p.sbuf("g_att%d" % i, [64, H, 64], BF16) for i in range(2)]
    ketm = [p.sbuf("g_ketm%d" % i, [64, H, 64], BF16) for i in range(2)]
    sq = p.sbuf("g_sq", [64, H, 128], F32)
    ssg = p.sbuf("g_ss", [64, H], F32)
    yn = [p.sbuf("g_yn%d" % i, [64, H, 128], F32) for i in range(2)]
    yT = p.sbuf("g_yT", [128, H, 512], F32)
    ysg1 = p.sbuf("g_ys", [128, H, 512], BF16)
    ysg = [ysg1, ysg1]
    gbank = [p.psum("g_bank%d" % i, [128, 512], F32) for i in range(3)]
    gbi = [0]

    def GB():
        t = gbank[gbi[0] % 3]
        gbi[0] += 1
        return t
    for tb in range(NB):
        s_ = tb % 2
        cols = slice(tb * 512, (tb + 1) * 512)
        p.dma(gq[s_][:], sc["GQ"][:, cols].rearrange("(h d) t -> d h t", d=64), [sc["GQ"]], [gq[s_]])
        p.dma(gk[s_][:], sc["GK"][:, cols].rearrange("(h d) t -> d h t", d=64), [sc["GK"]], [gk[s_]])
        p.dma(gl[s_][:], sc["GL"][:, cols], [sc["GL"]], [gl[s_]])
        p.dma(gv[s_][:], sc["GV"][cols, :].rearrange("(c p) f -> p c f", p=64), [sc["GV"]], [gv[s_]])
        p.dma(bg[s_][:], sc["BG"][1024:1536, cols].rearrange("(c p) t -> p c t", p=128), [sc["BG"]], [bg[s_]])
        for h in range(H):
            z = GB()
            p.mm(z[0:64, :], aup[:, h * 64:(h + 1) * 64], gl[s_][:], True, True, [aup, gl[s_]], [z])
            p.act(e1[:, h, :], z[0:64, :], AF.Exp, [z, nab], [e1], bias=nab[:, h:h + 1], scale=-1.0)
        p.act(e1[:], e1[:], AF.Ln, [e1, one1], [e1], bias=one1[0:64, :], scale=1.0)
        for h in range(H):
            p.op("dve", lambda e, h=h: e.tensor_tensor_scan(cs[:, h, :], maskr[:], e1[:, h, :], 0.0, ALU.mult, ALU.add),
                 [maskr, e1], [cs])
        p.act(eb[:], cs[:], AF.Exp, [cs], [eb], scale=-1.0 / 16)
        p.act(enb[:], cs[:], AF.Exp, [cs], [enb], scale=1.0 / 16)
        p.stt("dve", qe[:], gq[s_][:], 0.125, eb[:], ALU.mult, ALU.mult, [gq[s_], eb], [qe])
        p.tt("pool", ke[:], gk[s_][:], enb[:], ALU.mult, [gk[s_], enb], [ke])
        for cch in range(8):
            cc = slice(cch * 64, (cch + 1) * 64)
            a_ = att[cch % 2]
            kt_ = ketm[cch % 2]
            y_ = yn[cch % 2]
            b1 = GB()
            pat = b1[0:64, 0:256].rearrange("p (h i) -> p h i", h=H)
            for h in range(H):
                p.mm(pat[:, h, :], ke[:, h, cc], qe[:, h, cc], True, True, [ke, qe], [b1])
            p.tt("dve", a_[:], pat, msk[:], ALU.mult, [b1, msk], [a_])
            b2 = GB()
            pkt = b2[0:64, 0:128].bitcast(BF16).rearrange("p (h i) -> p h i", h=H)
            for h in range(H):
                p.tr(pkt[:, h, :], ke[:, h, cc], c.idb[0:64, 0:64], [ke, c.idb], [b2])
            p.cp("act", kt_[:], pkt, [b2], [kt_])
            b3 = GB()
            po = b3[0:64, :].rearrange("p (h v) -> p h v", h=H)
            for h in range(H):
                p.mm(po[:, h, :], a_[:, h, :], gv[s_][:, cch, h * 128:(h + 1) * 128], True, False, [a_, gv[s_]], [b3])
                p.mm(po[:, h, :], qe[:, h, cc], stb[:, h, :], False, True, [qe, stb], [b3])
            b4 = GB()
            pst = b4[0:64, :].rearrange("p (h v) -> p h v", h=H)
            for h in range(H):
                p.mm(pst[:, h, :], kt_[:, h, :], gv[s_][:, cch, h * 128:(h + 1) * 128], True, True, [kt_, gv[s_]], [b4])
            p.tt("dve", st2[:], st[:], pst, ALU.add, [st, b4], [st2])
            ebl = eb[:, :, cch * 64 + 63:cch * 64 + 64].broadcast_to([64, H, 128])
            p.tt("dve", st[:], st2[:], ebl, ALU.mult, [st2, eb], [st])
            p.cp("pool", stb[:], st[:], [st], [stb])
            p.act(sq[:], po, AF.Square, [b3], [sq])
            p.op("dve", lambda e: e.tensor_reduce(ssg[:], sq[:], AX.X, ALU.add), [sq], [ssg], dur=700)
            p.act(ssg[:], ssg[:], AF.Ln, [ssg, c.epsb], [ssg], bias=c.epsb[0:64, :], scale=1.0 / 128)
            p.act(ssg[:], ssg[:], AF.Exp, [ssg], [ssg], scale=-0.5)
            p.tt("dve", y_[:], po, ssg[:].unsqueeze(2).broadcast_to([64, H, 128]), ALU.mult, [b3, ssg], [y_])
            b5 = GB()
            pyT = b5[:, 0:256].rearrange("p (h t) -> p h t", h=H)
            for h in range(H):
                p.tr(pyT[:, h, :], y_[:, h, :], c.idf[0:64, 0:64], [y_, c.idf], [b5])
            p.cp("act", yT[:, :, cc], pyT, [b5], [yT])
        o_ = ysg[s_]
        for h in range(H):
            p.stt("dve", o_[:, h, :], yT[:, h, :], gn[:, h:h + 1], bg[s_][:, h, :], ALU.mult, ALU.mult, [yT, gn, bg[s_]], [o_])
        p.dma(sc["YS"][1024:1536, cols].rearrange("(c p) t -> p c t", p=128), o_[:], [o_],
              [sc["YS"] if ysblk is None else ysblk[tb]])


def phase_rwkv(p, c, S, l, W, sc, TB=128):
    NB = S // TB
    NCH = TB // 64
    H = 8
    C0 = math.exp(-0.5)
    vec = lambda name, ap: _hv(p, name, ap)
    mu_r = vec("r_mur", W["rwkv_mu"][l, 0:512])
    mu_k = vec("r_muk", W["rwkv_mu"][l, 512:1024])
    mu_v = vec("r_muv", W["rwkv_mu"][l, 1024:1536])
    mu_w = p.sbuf("r_muw", [64, 2], F32)
    p.dma(mu_w[:], W["rwkv_mu"][l, 1536:1664].rearrange("(c p) -> p c", p=64), [], [mu_w], allow_slow_non_contiguous=True)
    w0 = vec("r_w0", W["rwkv_w0"][l])
    a0 = vec("r_a0", W["rwkv_a0"][l])
    k_k = vec("r_kk", W["rwkv_k_k"][l])
    k_a = vec("r_ka", W["rwkv_k_a"][l])
    r_k = vec("r_rk", W["rwkv_r_k"][l])
    lnw = vec("r_lnw", W["rwkv_ln_w"][l])
    lnb = vec("r_lnb", W["rwkv_ln_b"][l])
    wup = p.sbuf("r_wup", [64, 512], F32)
    p.dma(wup[:], W["rwkv_w_up"][l], [], [wup])
    aup = p.sbuf("r_aup", [64, 512], F32)
    p.dma(aup[:], W["rwkv_a_up"][l], [], [aup])
    ones = p.sbuf("r_ones", [64, 64], F32)
    p.memset("pool", ones[:], 1.0, [ones])
    gne = p.sbuf("r_gne", [64, 1], F32)
    p.memset("pool", gne[:], 64e-5, [gne])
    maskr = p.sbuf("r_maskr", [64, TB], F32)
    p.memset("pool", maskr[:], 1.0, [maskr])
    p.memset("pool", maskr[:].rearrange("p (c t) -> p c t", t=64)[:, :, 0:1], 0.0, [maskr])

    def mk_mask(name, pat, cm, op, dt=F32):
        m = p.sbuf(name, [64, H, 64], F32)
        p.memset("pool", m[:], 1.0, [m])
        for h in range(H):
            p.op("pool", lambda e, h=h: e.affine_select(m[:, h, :], m[:, h, :], pat, op, 0.0, base=0, channel_multiplier=cm), [m], [m])
        if dt == F32:
            return m
        mb = p.sbuf(name + "b", [64, H, 64], dt)
        p.cp("pool", mb[:], m[:], [m], [mb])
        return mb
    mL = mk_mask("r_mL", [[-1, 64]], 1, ALU.is_gt)
    mSU = mk_mask("r_mSU", [[1, 64]], -1, ALU.is_gt)
    mU = mk_mask("r_mU", [[1, 64]], -1, ALU.is_ge)
    idh = mk_mask("r_idh", [[1, 64]], -1, ALU.is_equal, BF16)
    id64 = c.idb[0:64, 0:64]

    def A3(name, dt=F32, n=TB):
        return p.sbuf(name, [64, H, n], dt)
    r_, k_, vf = A3("r_r"), A3("r_k"), A3("r_v")
    lwa = p.sbuf("r_lwa", [64, 2, TB], F32)
    tmp, tmp2 = A3("r_tmp"), A3("r_tmp2")
    sg, a_, cs = A3("r_sg"), A3("r_a"), A3("r_cs")
    gi, gp = A3("r_gi"), A3("r_gp")
    kkn, kp = A3("r_kkn"), A3("r_kp")
    D2 = lambda name, dt: [A3(name + "0", dt), A3(name + "1", dt)]
    vb2, Ab2, Bb2, Kb2, Rb2 = D2("r_vb", BF16), D2("r_Ab", BF16), D2("r_Bb", BF16), D2("r_Kb", BF16), D2("r_Rb", BF16)
    g2, bon2, yfm2 = D2("r_g", F32), D2("r_bon", F32), D2("r_yfm", F32)
    bgt2, yso2 = D2("r_bgt", BF16), D2("r_yso", BF16)
    T0 = p.sbuf("r_T0", [64, H, 64], F32)
    p.memset("pool", T0[:], 0.0, [T0])
    T0b = p.sbuf("r_T0b", [64, H, 64], BF16)
    p.memset("pool", T0b[:], 0.0, [T0b])
    T1 = p.sbuf("r_T1", [64, H, 64], F32)

    def S3(name, dt=BF16):
        return p.sbuf(name, [64, H, 64], dt)
    S2 = lambda name: [S3(name + "0"), S3(name + "1")]
    P_ = S2("r_P")
    PT_ = S2("r_PT")
    NTa = [S2("r_NTa"), S2("r_NTb")]
    LakT2, ArbT2, ArkT2 = S2("r_LakT"), S2("r_ArbT"), S2("r_ArkT")
    Vtm2, Btm2, Ktm2 = S2("r_Vtm"), S2("r_Btm"), S2("r_Ktm")
    X_, U_ = S3("r_X"), S3("r_U")
    yc, ysq, ynm = S3("r_yc", F32), S3("r_ysq", F32), S3("r_ynm", F32)
    mean = p.sbuf("r_mean", [64, H], F32)
    var = p.sbuf("r_var", [64, H], F32)
    pz = p.psum("r_pz", [64, 2, TB], F32)
    pg = [p.psum("r_pg%d" % i, [64, H, 64], F32) for i in range(5)]
    pb = [p.psum("r_pb%d" % i, [64, H, 64], BF16) for i in range(2)]
    pgi = [0, 0]

    def PG():
        t = pg[pgi[0] % 5]
        pgi[0] += 1
        return t

    def PB():
        t = pb[pgi[1] % 2]
        pgi[1] += 1
        return t

    def bc(t2):
        return t2[:].unsqueeze(2).broadcast_to([64, H, TB])

    def prep(tb):
        t0 = tb * TB
        cols = slice(t0, t0 + TB)
        bp = tb % 2
        vb, Ab, Bb, Kb, Rb, g_, bon, bgt = vb2[bp], Ab2[bp], Bb2[bp], Kb2[bp], Rb2[bp], g2[bp], bon2[bp], bgt2[bp]
        for (dst, r0) in ((r_, 0), (k_, 512), (vf, 1024)):
            p.dma(dst[:], sc["U"][r0:r0 + 512, cols].rearrange("(h d) t -> d h t", d=64), [sc["U"]], [dst])
        p.dma(lwa[:], sc["U"][1536:1664, cols].rearrange("(c p) t -> p c t", p=64), [sc["U"]], [lwa])
        p.dma(bgt[:], sc["BG"][512:1024, cols].rearrange("(h d) t -> d h t", d=64), [sc["BG"]], [bgt])
        p.cp("act", vb[:], vf[:], [vf], [vb])
        p.act(lwa[:, 0, :], lwa[:, 0, :], AF.Tanh, [lwa], [lwa])
        for h in range(H):
            p.mm(pz[:, 0, :], wup[:, h * 64:(h + 1) * 64], lwa[:, 0, :], True, True, [wup, lwa], [pz])
            p.mm(pz[:, 1, :], aup[:, h * 64:(h + 1) * 64], lwa[:, 1, :], True, True, [aup, lwa], [pz])
            p.act(sg[:, h, :], pz[:, 0, :], AF.Sigmoid, [pz, w0], [sg], bias=w0[:, h:h + 1], scale=1.0)
            p.act(a_[:, h, :], pz[:, 1, :], AF.Sigmoid, [pz, a0], [a_], bias=a0[:, h:h + 1], scale=1.0)
        for h in range(H):
            p.op("dve", lambda e, h=h: e.tensor_tensor_scan(cs[:, h, :], maskr[:], sg[:, h, :], 0.0, ALU.mult, ALU.add),
                 [maskr, sg], [cs], dur=100 + 2.1 * TB)
        p.act(g_[:], cs[:], AF.Exp, [cs], [g_], scale=-C0)
        p.act(gi[:], cs[:], AF.Exp, [cs], [gi], scale=C0)
        p.tt("pool", tmp[:], cs[:], sg[:], ALU.subtract, [cs, sg], [tmp])
        p.act(gp[:], tmp[:], AF.Exp, [tmp], [gp], scale=-C0)
        p.tt("dve", kkn[:], k_[:], bc(k_k), ALU.mult, [k_, k_k], [kkn])
        p.act(tmp2[:], kkn[:], AF.Square, [kkn], [tmp2])
        for hp in range(4):
            for j in range(2):
                p.mm(pz[:, j, :], ones[:], tmp2[:, hp * 2 + j, :], True, True, [ones, tmp2], [pz])
            p.ts("dve", sg[:, hp * 2:hp * 2 + 2, :], pz[:], 1e-24, None, ALU.max, None, [pz], [sg])
        p.act(sg[:], sg[:], AF.Ln, [sg], [sg])
        p.act(sg[:], sg[:], AF.Exp, [sg], [sg], scale=-0.5)
        p.tt("pool", kkn[:], kkn[:], sg[:], ALU.mult, [kkn, sg], [kkn])
        p.stt("dve", tmp[:], a_[:], -1.0, bc(k_a), ALU.add, ALU.mult, [a_, k_a], [tmp])
        p.stt("dve", kp[:], tmp[:], 1.0, k_[:], ALU.add, ALU.mult, [tmp, k_], [kp])
        p.stt("dve", Ab[:], kkn[:], -1.0, gp[:], ALU.mult, ALU.mult, [kkn, gp], [Ab])
        p.tt("pool", tmp2[:], kkn[:], a_[:], ALU.mult, [kkn, a_], [tmp2])
        p.tt("dve", Bb[:], tmp2[:], gi[:], ALU.mult, [tmp2, gi], [Bb])
        p.tt("pool", Kb[:], kp[:], gi[:], ALU.mult, [kp, gi], [Kb])
        p.tt("dve", Rb[:], r_[:], g_[:], ALU.mult, [r_, g_], [Rb])
        p.tt("pool", tmp[:], r_[:], kp[:], ALU.mult, [r_, kp], [tmp])
        p.tt("pool", tmp[:], tmp[:], bc(r_k), ALU.mult, [tmp, r_k], [tmp])
        for hp in range(4):
            for j in range(2):
                p.mm(pz[:, j, :], ones[:], tmp[:, hp * 2 + j, :], True, True, [ones, tmp], [pz])
            p.tt("dve", bon[:, hp * 2:hp * 2 + 2, :], pz[:], vf[:, hp * 2:hp * 2 + 2, :], ALU.mult, [pz, vf], [bon])

    def chunk(tb, cch):
        bp = tb % 2
        g = tb * NCH + cch
        cp_ = g % 2
        cc = slice(cch * 64, (cch + 1) * 64)
        vb, Ab, Bb, Kb, Rb, g_, yfm = vb2[bp], Ab2[bp], Bb2[bp], Kb2[bp], Rb2[bp], g2[bp], yfm2[bp]
        LakT, ArbT, ArkT, Vtm, Btm, Ktm = LakT2[cp_], ArbT2[cp_], ArkT2[cp_], Vtm2[cp_], Btm2[cp_], Ktm2[cp_]
        NTp = NTa[cp_]

        def scores(lh, rh, mask, dst, eng):
            ps = PG()
            for h in range(H):
                p.mm(ps[:, h, :], lh[:, h, cc], rh[:, h, cc], True, True, [lh, rh], [ps])
            p.tt(eng, dst[:], ps[:], mask[:], ALU.mult, [ps, mask], [dst])
        scores(Ab, Bb, mL, P_[0], "dve")
        scores(Bb, Ab, mSU, PT_[0], "dve")
        p.tt("pool", NTp[0][:], PT_[0][:], idh[:], ALU.add, [PT_[0], idh], [NTp[0]])
        scores(Kb, Ab, mSU, LakT, "dve")
        scores(Bb, Rb, mU, ArbT, "dve")
        scores(Kb, Rb, mU, ArkT, "dve")

        def transp(src, dst):
            ps = PB()
            for h in range(H):
                p.tr(ps[:, h, :], src[:, h, cc], id64, [src, c.idb], [ps])
            p.cp("act", dst[:], ps[:], [ps], [dst])
        transp(vb, Vtm)
        transp(Bb, Btm)
        transp(Kb, Ktm)
        cur = 0
        nti = 0
        for step in range(5):
            Pn, PTn = P_[1 - cur], PT_[1 - cur]
            ps = PG()
            for h in range(H):
                p.mm(ps[:, h, :], PT_[cur][:, h, :], P_[cur][:, h, :], True, True, [PT_[cur], P_[cur]], [ps])
            p.cp("act", Pn[:], ps[:], [ps], [Pn])
            if step < 4:
                ps2 = PG()
                for h in range(H):
                    p.mm(ps2[:, h, :], P_[cur][:, h, :], PT_[cur][:, h, :], True, True, [PT_[cur], P_[cur]], [ps2])
                p.cp("dve", PTn[:], ps2[:], [ps2], [PTn])
            ps3 = PG()
            NTo, NTn = NTp[nti], NTp[1 - nti]
            for h in range(H):
                p.mm(ps3[:, h, :], id64, NTo[:, h, :], True, False, [c.idb, NTo], [ps3])
                p.mm(ps3[:, h, :], Pn[:, h, :], NTo[:, h, :], False, True, [Pn, NTo], [ps3])
            p.cp("act", NTn[:], ps3[:], [ps3], [NTn])
            nti = 1 - nti
            cur = 1 - cur
        NT = NTp[nti]
        ps = PG()
        for h in range(H):
            p.mm(ps[:, h, :], Ab[:, h, cc], T0b[:, h, :], True, False, [Ab, T0b], [ps])
            p.mm(ps[:, h, :], LakT[:, h, :], Vtm[:, h, :], False, True, [LakT, Vtm], [ps])
        p.cp("act", X_[:], ps[:], [ps], [X_])
        ps = PG()
        for h in range(H):
            p.mm(ps[:, h, :], NT[:, h, :], X_[:, h, :], True, True, [NT, X_], [ps])
        p.cp("act", U_[:], ps[:], [ps], [U_])
        py = PG()
        for h in range(H):
            p.mm(py[:, h, :], Rb[:, h, cc], T0b[:, h, :], True, False, [Rb, T0b], [py])
            p.mm(py[:, h, :], ArbT[:, h, :], U_[:, h, :], False, False, [ArbT, U_], [py])
            p.mm(py[:, h, :], ArkT[:, h, :], Vtm[:, h, :], False, True, [ArkT, Vtm], [py])
        ps = PG()
        for h in range(H):
            p.mm(ps[:, h, :], Btm[:, h, :], U_[:, h, :], True, False, [Btm, U_], [ps])
            p.mm(ps[:, h, :], Ktm[:, h, :], Vtm[:, h, :], False, True, [Ktm, Vtm], [ps])
        p.tt("dve", T1[:], T0[:], ps[:], ALU.add, [T0, ps], [T1])
        gC = g_[:, :, cch * 64 + 63:cch * 64 + 64].broadcast_to([64, H, 64])
        p.tt("dve", T0[:], T1[:], gC, ALU.mult, [T1, g_], [T0])
        p.cp("pool", T0b[:], T0[:], [T0], [T0b])
        p.op("dve", lambda e, py=py: e.tensor_reduce(mean[:], py[:], AX.X, ALU.add), [py], [mean], dur=700)
        p.ts("dve", mean[:], mean[:], 1.0 / 64, None, ALU.mult, None, [mean], [mean])
        p.tt("dve", yc[:], py[:], mean[:].unsqueeze(2).broadcast_to([64, H, 64]), ALU.subtract, [py, mean], [yc])
        p.act(ysq[:], yc[:], AF.Square, [yc], [ysq])
        p.op("dve", lambda e: e.tensor_reduce(var[:], ysq[:], AX.X, ALU.add), [ysq], [var], dur=700)
        p.act(var[:], var[:], AF.Ln, [var, gne], [var], bias=gne[:], scale=1.0 / 64)
        p.act(var[:], var[:], AF.Exp, [var], [var], scale=-0.5)
        p.tt("pool", ynm[:], yc[:], var[:].unsqueeze(2).broadcast_to([64, H, 64]), ALU.mult, [yc, var], [ynm])
        ps = PG()
        for h in range(H):
            p.tr(ps[:, h, :], ynm[:, h, :], c.idf[0:64, 0:64], [ynm, c.idf], [ps])
        p.cp("act", yfm[:, :, cc], ps[:], [ps], [yfm])

    def epilogue(tb):
        bp = tb % 2
        cols = slice(tb * TB, (tb + 1) * TB)
        yfm, bon, bgt, yso = yfm2[bp], bon2[bp], bgt2[bp], yso2[bp]
        p.tt("dve", yfm[:], yfm[:], bc(lnw), ALU.mult, [yfm, lnw], [yfm])
        p.tt("pool", yfm[:], yfm[:], bc(lnb), ALU.add, [yfm, lnb], [yfm])
        p.tt("pool", yfm[:], yfm[:], bon[:], ALU.add, [yfm, bon], [yfm])
        p.tt("dve", yso[:], yfm[:], bgt[:], ALU.mult, [yfm, bgt], [yso])
        p.dma(sc["YS"][512:1024, cols].rearrange("(h d) t -> d h t", d=64), yso[:], [yso], [sc["YS"]])

    for tb in range(NB):
        prep(tb)
        for cch in range(NCH):
            chunk(tb, cch)
        epilogue(tb)


def _hv(p, name, ap):
    t = p.sbuf(name, [64, 8], F32)
    p.dma(t[:], ap.rearrange("(h d) -> d h", d=64), [], [t], allow_slow_non_contiguous=True)
    return t


def phase_out_prep(p, c, S, l, W):
    wbo = p.sbuf("wbo", [128, 12, D], BF16)
    wout = p.sbuf("wout", [128, 8, D], BF16)
    gpb = p.sbuf("gpb", [128, D], F32)
    p.dma(gpb[:], W["norm_post"][l:l + 1, :].partition_broadcast(128), [], [gpb])
    mk = p.mark()
    stg = [p.sbuf("wstg%d" % i, [128, 4, D], F32) for i in range(2)]
    for n in range(3):
        p.dma(stg[n % 2][:], W["w_branch_out"][l, n].rearrange("(k p) d -> p k d", p=128), [], [stg[n % 2]])
        p.cp("pool", wbo[:, n * 4:(n + 1) * 4, :], stg[n % 2][:], [stg[n % 2]], [wbo])
    for hf in range(2):
        p.dma(stg[(hf + 1) % 2][:], W["w_out"][l, hf * 512:(hf + 1) * 512, :].rearrange("(k p) d -> p k d", p=128), [], [stg[(hf + 1) % 2]])
        p.cp("pool", wout[:, hf * 4:(hf + 1) * 4, :], stg[(hf + 1) % 2][:], [stg[(hf + 1) % 2]], [wout])
    p.release(mk)
    return wbo, wout, gpb


def phase_out(p, c, S, l, xin, xout, W, sc, pre=None, ysblk=None):
    NB = S // 512
    if pre is None:
        pre = phase_out_prep(p, c, S, l, W)
    wbo, wout, gpb = pre
    ysb = [p.sbuf("ysb%d" % i, [128, 12, 512], BF16) for i in range(2)]
    mgb = [p.sbuf("mgb%d" % i, [128, 3, 512], BF16) for i in range(3)]
    mT = p.sbuf("mT", [128, 8, 512], BF16)
    m32 = p.sbuf("m32", [128, 512], F32)
    t32 = [p.sbuf("t32_%d" % i, [128, 512], F32) for i in range(2)]
    xt = [p.sbuf("xo%d" % i, [128, D], F32) for i in range(2)]
    o32a = p.sbuf("o32_0", [128, D], F32)
    o32 = [o32a, o32a]
    junk = p.sbuf("junko", [128, 512], BF16)
    ssq = p.sbuf("ssq", [128, 2], F32)
    rstd = p.sbuf("rstdo", [128, 1], F32)
    pb = [p.psum("pb%d" % i, [128, 512], F32) for i in range(3)]
    po = [p.psum("po%d" % i, [128, 512], F32) for i in range(2)]
    it = 0
    im = 0
    for tb in range(NB):
        cols = slice(tb * 512, (tb + 1) * 512)
        ysx = ysb[tb % 2]
        p.dma(ysx[:], sc["YS"][:, cols].rearrange("(c p) t -> p c t", p=128),
              [sc["YS"] if ysblk is None else ysblk[tb]], [ysx])
        mgv = sc["MG"][:, cols].rearrange("(n c p) t -> p n c t", n=3, p=128)
        for cch in range(8):
            mgx = mgb[im % 3]
            im += 1
            p.dma(mgx[:], mgv[:, :, cch, :], [sc["MG"]], [mgx])
            for n in range(3):
                for k in range(4):
                    p.mm(pb[n][:], wbo[:, n * 4 + k, cch * 128:(cch + 1) * 128], ysx[:, n * 4 + k, :], k == 0, k == 3,
                         [wbo, ysx], [pb[n]])
            p.tt("dve", m32[:], pb[0][:], mgx[:, 0, :], ALU.mult, [pb[0], mgx], [m32])
            p.tt("dve", t32[0][:], pb[1][:], mgx[:, 1, :], ALU.mult, [pb[1], mgx], [t32[0]])
            p.tt("dve", t32[1][:], pb[2][:], mgx[:, 2, :], ALU.mult, [pb[2], mgx], [t32[1]])
            p.tt("pool", m32[:], m32[:], t32[0][:], ALU.add, [m32, t32[0]], [m32])
            p.tt("pool", mT[:, cch, :], m32[:], t32[1][:], ALU.add, [m32, t32[1]], [mT])
        for tsub in range(4):
            s_ = it % 2
            it += 1
            r0 = tb * 512 + tsub * 128
            p.dma(xt[s_][:], xin[r0:r0 + 128, :], [xin], [xt[s_]])
            for hf in range(2):
                for k in range(8):
                    p.mm(po[hf][:], mT[:, k, tsub * 128:(tsub + 1) * 128], wout[:, k, hf * 512:(hf + 1) * 512], k == 0, k == 7,
                         [mT, wout], [po[hf]])
                p.act(junk[:], po[hf][:], AF.Square, [po[hf]], [junk, ssq], accum_out=ssq[:, hf:hf + 1])
            p.tt("dve", rstd[:], ssq[:, 0:1], ssq[:, 1:2], ALU.add, [ssq], [rstd])
            p.act(rstd[:], rstd[:], AF.Ln, [rstd, c.epsb], [rstd], bias=c.epsb[:], scale=1.0 / D)
            p.act(rstd[:], rstd[:], AF.Exp, [rstd], [rstd], scale=-0.5)
            for hf in range(2):
                p.stt("dve", o32[s_][:, hf * 512:(hf + 1) * 512], po[hf][:], rstd[:, 0:1], gpb[:, hf * 512:(hf + 1) * 512],
                      ALU.mult, ALU.mult, [po[hf], rstd, gpb], [o32[s_]])
            p.tt("pool", o32[s_][:], o32[s_][:], xt[s_][:], ALU.add, [o32[s_], xt[s_]], [o32[s_]])
            p.dma(xout[r0:r0 + 128, :], o32[s_][:], [o32[s_]], [xout])


def make_scratch(p, S, dbg):
    kind = "ExternalOutput" if dbg else "Internal"
    sc = {}
    sc["QT"] = p.dram("QT", [8, 96, S], BF16, kind)
    sc["KT"] = p.dram("KT", [8, 96, S], BF16, kind)
    sc["VA"] = p.dram("VA", [S, 520], BF16, kind)
    sc["U"] = p.dram("U", [1664, S], F32, kind)
    sc["GQ"] = p.dram("GQ", [256, S], F32, kind)
    sc["GK"] = p.dram("GK", [256, S], F32, kind)
    sc["GV"] = p.dram("GV", [S, 512], BF16, kind)
    sc["GL"] = p.dram("GL", [16, S], F32, kind)
    sc["BG"] = p.dram("BG", [1536, S], BF16, kind)
    sc["MG"] = p.dram("MG", [3072, S], BF16, kind)
    sc["YS"] = p.dram("YS", [1536, S], BF16, kind)
    sc["X1"] = p.dram("X1", [S, D], F32, kind)
    return sc


WSPEC = {
    "norm_pre": ([L, D], F32), "w_in": ([L, D, NIN], F32), "wkr2": ([L, D, 192], F32),
    "mla_q_norm": ([L, 256], F32), "mla_kv_norm": ([L, 128], F32),
    "mla_w_uq": ([L, 256, 768], F32), "wuq_sw": ([L, 256, 768], F32),
    "wuk": ([L, 128, 512], F32), "wuv": ([L, 128, 512], F32),
    "rwkv_mu": ([L, 1664], F32), "rwkv_w0": ([L, 512], F32), "rwkv_w_up": ([L, 64, 512], F32),
    "rwkv_a0": ([L, 512], F32), "rwkv_a_up": ([L, 64, 512], F32), "rwkv_k_k": ([L, 512], F32),
    "rwkv_k_a": ([L, 512], F32), "rwkv_r_k": ([L, 512], F32), "rwkv_ln_w": ([L, 512], F32),
    "rwkv_ln_b": ([L, 512], F32), "gla_a_up": ([L, 16, 256], F32), "gla_a_b": ([L, 256], F32),
    "gla_norm": ([L, 512], F32), "w_branch_out": ([L, 3, 512, D], F32), "w_out": ([L, D, D], F32),
    "norm_post": ([L, D], F32),
}


def host_weights(inp):
    w = {}
    for k in WSPEC:
        if k in inp:
            w[k] = np.ascontiguousarray(np.asarray(inp[k], dtype=np.float32))
    win = w["w_in"]
    w["wkr2"] = np.ascontiguousarray(np.concatenate(
        [win[:, :, 320:416], win[:, :, 320:384], win[:, :, 400:416], win[:, :, 384:400]], axis=2))
    uq = w["mla_w_uq"].reshape(L, 256, 8, 96)
    w["wuq_sw"] = np.ascontiguousarray(np.concatenate(
        [uq[..., 0:64], uq[..., 80:96], uq[..., 64:80]], axis=-1).reshape(L, 256, 768))
    ukv = w["mla_w_ukv"] if "mla_w_ukv" in w else np.asarray(inp["mla_w_ukv"], dtype=np.float32)
    ukv = ukv.reshape(L, 128, 8, 128)
    w["wuk"] = np.ascontiguousarray(ukv[..., 0:64].reshape(L, 128, 512))
    w["wuv"] = np.ascontiguousarray(ukv[..., 64:128].reshape(L, 128, 512))
    w["rwkv_r_k"] = w["rwkv_r_k"].reshape(L, 512) if "rwkv_r_k" in w else np.asarray(inp["rwkv_r_k"], np.float32).reshape(L, 512)
    return w


def build(S, phases=("p1",), dbg=True, nlayers=1):
    nc = bass.Bass("TRN2", target_bir_lowering=False)
    p = Prog(nc)
    x_d = p.dram("x", [S, D], F32, "ExternalInput")
    pos_d = p.dram("pos", [1, S], I32, "ExternalInput")
    W = {k: p.dram(k, sh, dt, "ExternalInput").t for k, (sh, dt) in WSPEC.items()}
    out_d = p.dram("out", [S, D], F32, "ExternalOutput")
    sc = make_scratch(p, S, dbg)
    c = setup_consts(p, S, pos_d.t)
    final = [out_d]
    for l in range(nlayers):
        xin = x_d if l == 0 else sc["X1"]
        if "p1" in phases:
            mk = p.mark()
            phase1(p, c, S, l, xin, W, sc)
            p.release(mk)
        if "mla" in phases:
            mk = p.mark()
            phase_mla(p, c, S, l, sc)
            p.release(mk)
        if "rwkv" in phases:
            mk = p.mark()
            phase_rwkv(p, c, S, l, W, sc)
            p.release(mk)
        xo = out_d if l == nlayers - 1 else sc["X1"]
        if "gla" in phases and "out" in phases:
            mk = p.mark()
            pre = phase_out_prep(p, c, S, l, W)
            ysblk = [T(None, "ysblk%d" % i) for i in range(S // 512)]
            phase_gla(p, c, S, l, W, sc, ysblk)
            phase_out(p, c, S, l, xin, xo, W, sc, pre, ysblk)
            p.release(mk)
        else:
            if "gla" in phases:
                mk = p.mark()
                phase_gla(p, c, S, l, W, sc)
                p.release(mk)
            if "out" in phases:
                mk = p.mark()
                phase_out(p, c, S, l, xin, xo, W, sc)
                p.release(mk)
    if dbg:
        final = list(sc.values()) + [out_d]
    p.finish(final)
    return nc, p


ALL_PHASES = ("p1", "mla", "rwkv", "gla", "out")
_CACHE = {}


def kernel(**inputs):
    x = np.asarray(inputs["x"], dtype=np.float32)
    pos = np.asarray(inputs["positions"], dtype=np.int32)
    B, S, _ = x.shape
    Wh = host_weights(inputs)
    nc, _p = build(S, phases=ALL_PHASES, dbg=False, nlayers=L)
    in_maps = []
    for b in range(B):
        m = {"x": np.ascontiguousarray(x[b]), "pos": np.ascontiguousarray(pos[b:b + 1])}
        for k in WSPEC:
            m[k] = Wh[k]
        in_maps.append(m)
    res = run_bass_kernel_spmd(nc, in_maps, core_ids=list(range(B)))
    return np.stack([np.asarray(r["out"]) for r in res.results], axis=0).astype(np.float32)
```

```python
import math
import numpy as np
import concourse.bass as bass
import concourse.mybir as mybir
from concourse.bass_utils import run_bass_kernel_spmd

F32 = mybir.dt.float32
BF16 = mybir.dt.bfloat16
I32 = mybir.dt.int32
ALU = mybir.AluOpType
AF = mybir.ActivationFunctionType
AX = mybir.AxisListType

SEM_CAP = 30000
D = 1024
NIN = 7728
L = 2
EPS = 1e-6


class Buf:
    __slots__ = ("name", "w", "r")

    def __init__(self, name):
        self.name = name
        self.w = None
        self.r = {}


class T:
    def __init__(self, t, name):
        self.t = t
        self.b = Buf(name)

    def __getitem__(self, k):
        return self.t[k]


def _fs(ap):
    n = 1
    for d in ap.shape[1:]:
        n *= int(d)
    return n


def _nbytes(ap):
    n = 1
    for d in ap.shape:
        n *= int(d)
    return n * mybir.dt.size(ap.dtype)


class Prog:
    CE = ("pe", "act", "dve", "pool")
    QE = ("sp",)
    WINDOW = 48
    SYNC = 300.0

    def __init__(self, nc, n_dma_sems=32):
        self.nc = nc
        self.sems = {}
        self._ctx = []
        self._semctx = []
        self.cur = {}
        self.seen = {e: {} for e in self.CE + self.QE}
        self.nsem = 0
        self.old_sems = []
        for e in self.CE:
            self._new_eng_sem(e)
        self.dma_sems = []
        for i in range(n_dma_sems):
            k = self._alloc_sem("dma%d" % i)
            self.dma_sems.append([k, 0])
        self.dma_rr = 0
        self.ninst = 0
        self.ops = []
        self.uid = 0
        self.model_time = 0.0

    def _alloc_sem(self, name):
        cm = self.nc.semaphore(name)
        h = cm.__enter__()
        self._semctx.append(cm)
        self.sems[name] = h
        self.nsem += 1
        return name

    def _new_eng_sem(self, e):
        if e in self.cur:
            self.old_sems.append(tuple(self.cur[e]))
        k = self._alloc_sem("%s_s%d" % (e, self.nsem))
        self.cur[e] = [k, 0]

    def sbuf(self, name, shape, dt):
        self.uid += 1
        name = "%s_u%d" % (name, self.uid)
        cm = self.nc.sbuf_tensor(name, list(shape), dt)
        t = cm.__enter__()
        self._ctx.append(cm)
        return T(t, name)

    def psum(self, name, shape, dt):
        self.uid += 1
        name = "%s_u%d" % (name, self.uid)
        cm = self.nc.psum_tensor(name, list(shape), dt)
        t = cm.__enter__()
        self._ctx.append(cm)
        return T(t, name)

    def dram(self, name, shape, dt, kind="Internal"):
        t = self.nc.dram_tensor(name, list(shape), dt, kind=kind).ap()
        return T(t, name)

    def op(self, eng, fn, R=(), W=(), signal=True, dur=500.0):
        self.ops.append((eng, fn, [x.b for x in R], [x.b for x in W], float(dur), float(dur), False))
        self.ninst += 1

    def dma(self, out_ap, in_ap, R=(), W=(), q="sp", **kw):
        lat = 2000.0 + _nbytes(out_ap) / 150.0
        self.ops.append((q, lambda e: e.dma_start(out=out_ap, in_=in_ap, **kw),
                         [x.b for x in R], [x.b for x in W], 80.0, lat, True))
        self.ninst += 1

    def mm(self, out, lhsT, rhs, start, stop, R, W, **kw):
        n = _fs(rhs)
        d = (50.0 + 1.0 * max(n, 64)) if lhsT.dtype == F32 else (40.0 + 0.42 * max(n, 64))
        self.op("pe", lambda e: e.matmul(out, lhsT, rhs, start=start, stop=stop, **kw), R, W, dur=d)

    def tr(self, out, in_, ident, R, W, signal=True):
        self.op("pe", lambda e: e.transpose(out, in_, ident), R, W, dur=110.0)

    def act(self, out, in_, func, R, W, **kw):
        self.op("act", lambda e: e.activation(out, in_, func, **kw), R, W, dur=260.0 + 0.83 * _fs(out))

    def _vd(self, eng, out, two_src):
        n = _fs(out)
        if eng == "pool":
            return 150.0 + 2.2 * n
        b16 = mybir.dt.size(out.dtype) == 2
        if two_src:
            return 160.0 + (1.04 * n)
        return 160.0 + (0.55 * n)

    def tt(self, eng, out, a, b, op, R, W):
        self.op(eng, lambda e: e.tensor_tensor(out, a, b, op), R, W, dur=self._vd(eng, out, True))

    def ts(self, eng, out, a, s1, s2, op0, op1, R, W):
        d = self._vd(eng, out, False)
        if s2 is None:
            self.op(eng, lambda e: e.tensor_scalar(out, a, s1, None, op0), R, W, dur=d)
        else:
            self.op(eng, lambda e: e.tensor_scalar(out, a, s1, s2, op0, op1), R, W, dur=d)

    def stt(self, eng, out, a, s, b, op0, op1, R, W):
        self.op(eng, lambda e: e.scalar_tensor_tensor(out, a, s, b, op0, op1), R, W, dur=self._vd(eng, out, True))

    def cp(self, eng, out, in_, R, W):
        if eng == "act":
            self.op(eng, lambda e: e.copy(out, in_), R, W, dur=260.0 + 0.83 * _fs(out))
        else:
            self.op(eng, lambda e: e.tensor_copy(out, in_), R, W, dur=self._vd(eng, out, False))

    def memset(self, eng, ap, val, W):
        self.op(eng, lambda e: e.memset(ap, val), (), W, dur=self._vd(eng, ap, False))

    def _eng(self, eng):
        nc = self.nc
        return {"pe": nc.tensor, "act": nc.scalar, "dve": nc.vector, "pool": nc.gpsimd, "sp": nc.sync}[eng]

    def flush(self):
        import bisect
        ops = self.ops
        self.ops = []
        n = len(ops)
        if n == 0:
            return
        engs = self.CE + self.QE
        lastw = {}
        readers = {}
        deps = [None] * n
        for i in range(n):
            eng, fn, R, W, occ, lat, isd = ops[i]
            d = set()
            for b in R:
                w = lastw.get(id(b))
                if w is not None:
                    d.add(w)
            for b in W:
                w = lastw.get(id(b))
                if w is not None:
                    d.add(w)
                rl = readers.get(id(b))
                if rl:
                    d.update(rl)
            d.discard(i)
            deps[i] = d
            for b in R:
                readers.setdefault(id(b), []).append(i)
            for b in W:
                lastw[id(b)] = i
                readers[id(b)] = []
        succ = [[] for _ in range(n)]
        nd = [0] * n
        for i in range(n):
            nd[i] = len(deps[i])
            for j in deps[i]:
                succ[j].append(i)
        bl = [0.0] * n
        for i in range(n - 1, -1, -1):
            m = 0.0
            for s_ in succ[i]:
                if bl[s_] > m:
                    m = bl[s_]
            bl[i] = m + ops[i][5] + 100.0
        ready = [0.0] * n
        free = {e: 0.0 for e in engs}
        cand = {e: [] for e in engs}
        for i in range(n):
            if nd[i] == 0:
                cand[ops[i][0]].append(i)
        order = {e: [] for e in engs}
        done = 0
        W_ = self.WINDOW
        SY = self.SYNC
        tmax = 0.0
        while done < n:
            best = None
            for e in engs:
                c = cand[e]
                if not c:
                    continue
                fe = free[e]
                bst = None
                for i in c[:W_]:
                    st = ready[i] if ready[i] > fe else fe
                    key = (st, -bl[i])
                    if bst is None or key < bst[2]:
                        bst = (st, i, key)
                if best is None or bst[2] < best[3]:
                    best = (bst[0], bst[1], e, bst[2])
            st, i, e = best[0], best[1], best[2]
            cand[e].remove(i)
            order[e].append(i)
            occ, lat = ops[i][4], ops[i][5]
            free[e] = st + occ
            fin = st + lat
            if fin > tmax:
                tmax = fin
            for s_ in succ[i]:
                rt = fin + (SY if ops[s_][0] != e else (0.0 if e == "pe" else 200.0))
                if rt > ready[s_]:
                    ready[s_] = rt
                nd[s_] -= 1
                if nd[s_] == 0:
                    bisect.insort(cand[ops[s_][0]], s_)
            done += 1
        self.model_time += tmax
        busy = {e: 0.0 for e in engs}
        for i in range(n):
            busy[ops[i][0]] += ops[i][4]
        if not hasattr(self, "regions"):
            self.regions = []
        self.regions.append((tmax, busy, n))
        need = [False] * n
        for i in range(n):
            e = ops[i][0]
            if ops[i][6] or not succ[i]:
                need[i] = True
                continue
            for s_ in succ[i]:
                if ops[s_][0] != e or e != "pe":
                    need[i] = True
                    break
        tick = [None] * n
        reuse_wait = {}
        for e in engs:
            for i in order[e]:
                if ops[i][6]:
                    slot = self.dma_sems[self.dma_rr]
                    self.dma_rr = (self.dma_rr + 1) % len(self.dma_sems)
                    if slot[1] > 0:
                        reuse_wait[i] = (slot[0], slot[1])
                    slot[1] += 16
                    tick[i] = (slot[0], slot[1])
                elif need[i]:
                    cur = self.cur[e]
                    if cur[1] >= SEM_CAP:
                        self._new_eng_sem(e)
                        cur = self.cur[e]
                    cur[1] += 1
                    tick[i] = (cur[0], cur[1])
        for e in engs:
            eo = self._eng(e)
            seen = self.seen[e]
            sems = self.sems
            for i in order[e]:
                w = {}
                for j in deps[i]:
                    if e == "pe" and ops[j][0] == "pe":
                        continue
                    k, v = tick[j]
                    if w.get(k, 0) < v:
                        w[k] = v
                if i in reuse_wait:
                    k, v = reuse_wait[i]
                    if w.get(k, 0) < v:
                        w[k] = v
                for k, v in w.items():
                    if seen.get(k, 0) < v:
                        seen[k] = v
                        eo.wait_ge(sems[k], v)
                ins = ops[i][1](eo)
                if tick[i] is not None:
                    ins.then_inc(sems[tick[i][0]], 16 if ops[i][6] else 1)

    def barrier(self):
        self.flush()
        allv = [(k, v) for (k, v) in [tuple(x) for x in self.cur.values()] if v > 0]
        allv += [(k, v) for (k, v) in self.old_sems]
        allv += [(k, v) for (k, v) in [tuple(x) for x in self.dma_sems] if v > 0]
        for eng in self.CE + self.QE:
            eo = self._eng(eng)
            for k, v in allv:
                if self.seen[eng].get(k, 0) < v:
                    self.seen[eng][k] = v
                    eo.wait_ge(self.sems[k], v)

    def mark(self):
        return len(self._ctx)

    def release(self, mark):
        self.barrier()
        while len(self._ctx) > mark:
            self._ctx.pop().__exit__(None, None, None)

    def finish(self, final):
        self.barrier()
        while self._ctx:
            self._ctx.pop().__exit__(None, None, None)
        while self._semctx:
            self._semctx.pop().__exit__(None, None, None)


C_CQ, C_CKV, C_KR, C_U = 0, 256, 384, 416
C_GQ, C_GK, C_GV, C_GL, C_BG, C_MG = 2080, 2336, 2592, 3104, 3120, 4656
SCALE = (64 + 32) ** -0.5


class Ctx:
    pass


def setup_consts(p, S, pos_d):
    c = Ctx()
    idf = p.sbuf("idf", [128, 128], F32)
    p.memset("pool", idf[:], 1.0, [idf])
    p.op("pool", lambda e: e.affine_select(idf[:], idf[:], [[1, 128]], ALU.is_equal, 0.0,
                                           base=0, channel_multiplier=-1), [idf], [idf])
    idb = p.sbuf("idb", [128, 128], BF16)
    p.cp("dve", idb[:], idf[:], [idf], [idb])
    c.idf, c.idb = idf, idb
    onesb = p.sbuf("onesb", [128, 128], BF16)
    p.memset("pool", onesb[:], 1.0, [onesb])
    c.onesb = onesb
    epsb = p.sbuf("epsb", [128, 1], F32)
    p.memset("pool", epsb[:], EPS, [epsb])
    c.epsb = epsb
    lnsc = p.sbuf("lnsc", [128, 1], F32)
    p.memset("pool", lnsc[:], math.log(SCALE), [lnsc])
    c.lnsc = lnsc
    inv = (1.0 / (10000.0 ** (np.arange(0, 32, 2, dtype=np.float32) / np.float32(32)))).astype(np.float32)
    row = p.sbuf("roperow", [1, 64], F32)
    for j in range(16):
        p.memset("pool", row[0:1, j:j + 1], float(inv[j]), [row])
        p.memset("pool", row[0:1, 16 + j:17 + j], float(inv[j]), [row])
    p.memset("pool", row[0:1, 32:48], -1.0, [row])
    p.memset("pool", row[0:1, 48:64], 1.0, [row])
    rd = p.dram("rope_d", [64], F32)
    p.dma(rd[:].rearrange("(o n) -> o n", o=1), row[:], [row], [rd])
    ctab = p.sbuf("ctab", [96, 2], F32)
    p.dma(ctab[64:96, :], rd[:].rearrange("(c p) -> p c", p=32), [rd], [ctab], allow_slow_non_contiguous=True)
    mk = p.mark()
    cos2 = p.sbuf("cos2", [96, S], F32)
    sin2 = p.sbuf("sin2", [96, S], F32)
    posi = p.sbuf("posi", [96, S], I32)
    p.dma(posi[64:96, :], pos_d.partition_broadcast(32), [], [posi])
    ang = p.sbuf("ang", [96, S], F32)
    p.cp("dve", ang[64:96, :], posi[64:96, :], [posi], [ang])
    p.ts("dve", ang[64:96, :], ang[64:96, :], ctab[64:96, 0:1], None, ALU.mult, None, [ang, ctab], [ang])
    kf = p.sbuf("kf", [96, S], F32)
    sl = slice(64, 96)

    def sin_of(dst, shift):
        a = dst
        p.ts("dve", a[sl, :], ang[sl, :], shift, None, ALU.add, None, [ang], [dst])
        p.ts("dve", kf[sl, :], a[sl, :], 1.0 / (2 * math.pi), None, ALU.mult, None, [dst], [kf])
        p.cp("dve", posi[sl, :], kf[sl, :], [kf], [posi])
        p.cp("dve", kf[sl, :], posi[sl, :], [posi], [kf])
        p.stt("dve", a[sl, :], kf[sl, :], -2 * math.pi, a[sl, :], ALU.mult, ALU.add, [kf, dst], [dst])
        p.ts("dve", kf[sl, :], a[sl, :], math.pi, -2 * math.pi, ALU.is_gt, ALU.mult, [dst], [kf])
        p.tt("dve", a[sl, :], a[sl, :], kf[sl, :], ALU.add, [dst, kf], [dst])
        p.ts("dve", kf[sl, :], a[sl, :], -math.pi, 2 * math.pi, ALU.is_lt, ALU.mult, [dst], [kf])
        p.tt("dve", a[sl, :], a[sl, :], kf[sl, :], ALU.add, [dst, kf], [dst])
        p.act(a[sl, :], a[sl, :], AF.Sin, [dst], [dst])
    sin_of(cos2, math.pi / 2)
    sin_of(sin2, 0.0)
    p.ts("dve", sin2[sl, :], sin2[sl, :], ctab[sl, 1:2], None, ALU.mult, None, [sin2, ctab], [sin2])
    c.rope = p.dram("rope_tab", [2, 32, S], F32)
    p.dma(c.rope[0], cos2[sl, :], [cos2], [c.rope])
    p.dma(c.rope[1], sin2[sl, :], [sin2], [c.rope])
    p.release(mk)
    return c


def colvec(p, name, src_ap, n, dt=F32):
    t = p.sbuf(name, [128, n], dt)
    p.dma(t[:], src_ap.rearrange("(c p) -> p c", p=128), [], [t], allow_slow_non_contiguous=True)
    return t


def phase1(p, c, S, l, xin, W, sc):
    NT_ = S // 128
    NB = S // 512
    hT = p.sbuf("hT%d" % l, [128, 8, S], BF16)
    gbc = p.sbuf("gbc%d" % l, [128, D], F32)
    p.dma(gbc[:], W["norm_pre"][l:l + 1, :].partition_broadcast(128), [], [gbc])
    mk1 = p.mark()
    xt = [p.sbuf("xt%d_%d" % (l, i), [128, D], F32) for i in range(3)]
    hb = [p.sbuf("hb%d_%d" % (l, i), [128, D], BF16) for i in range(3)]
    junk = p.sbuf("junk%d" % l, [128, D], BF16)
    ss = [p.sbuf("ss%d_%d" % (l, i), [128, 1], F32) for i in range(3)]
    ptr = [p.psum("ptr%d_%d" % (l, i), [128, 8, 128], BF16) for i in range(3)]
    for i in range(NT_):
        s = i % 3
        p.dma(xt[s][:], xin[i * 128:(i + 1) * 128, :], [xin], [xt[s]])
        p.act(junk[:], xt[s][:], AF.Square, [xt[s]], [junk, ss[s]], accum_out=ss[s][:])
        p.act(ss[s][:], ss[s][:], AF.Ln, [ss[s], c.epsb], [ss[s]], bias=c.epsb[:], scale=1.0 / D)
        p.act(ss[s][:], ss[s][:], AF.Exp, [ss[s]], [ss[s]], scale=-0.5)
        p.stt("dve", hb[s][:], xt[s][:], ss[s][:, 0:1], gbc[:], ALU.mult, ALU.mult,
              [xt[s], ss[s], gbc], [hb[s]])
        for k in range(8):
            p.tr(ptr[s][:, k, :], hb[s][:, k * 128:(k + 1) * 128], c.idb[:], [hb[s], c.idb], [ptr[s]],
                 signal=(k == 7))
        p.cp("act", hT[:, :, i * 128:(i + 1) * 128], ptr[s][:], [ptr[s]], [hT])

    p.release(mk1)
    mk2 = p.mark()
    wa32 = p.sbuf("wa32_%d" % l, [128, 8, 384], F32)
    p.dma(wa32[:], W["w_in"][l, :, 0:384].rearrange("(c p) n -> p c n", p=128), [], [wa32])
    wa = p.sbuf("wa_%d" % l, [128, 8, 384], BF16)
    p.cp("pool", wa[:], wa32[:], [wa32], [wa])
    wk32 = p.sbuf("wk32_%d" % l, [128, 8, 192], F32)
    p.dma(wk32[:], W["wkr2"][l].rearrange("(c p) n -> p c n", p=128), [], [wk32])
    wkr = p.sbuf("wkr_%d" % l, [128, 8, 192], BF16)
    p.cp("pool", wkr[:], wk32[:], [wk32], [wkr])
    wq32 = p.sbuf("wq32_%d" % l, [128, 2, 1536], F32)
    p.dma(wq32[:, :, 0:768], W["mla_w_uq"][l].rearrange("(c p) n -> p c n", p=128), [], [wq32])
    p.dma(wq32[:, :, 768:1536], W["wuq_sw"][l].rearrange("(c p) n -> p c n", p=128), [], [wq32])
    wq = p.sbuf("wq_%d" % l, [128, 2, 1536], BF16)
    p.cp("pool", wq[:], wq32[:], [wq32], [wq])
    wkv32 = p.sbuf("wkv32_%d" % l, [128, 1024], F32)
    p.dma(wkv32[:, 0:512], W["wuk"][l], [], [wkv32])
    p.dma(wkv32[:, 512:1024], W["wuv"][l], [], [wkv32])
    wkv = p.sbuf("wkv_%d" % l, [128, 1024], BF16)
    p.cp("pool", wkv[:], wkv32[:], [wkv32], [wkv])
    cos2 = p.sbuf("cos2_%d" % l, [96, S], F32)
    sin2 = p.sbuf("sin2_%d" % l, [96, S], F32)
    p.dma(cos2[64:96, :], c.rope[0], [c.rope], [cos2])
    p.dma(sin2[64:96, :], c.rope[1], [c.rope], [sin2])
    gq = colvec(p, "gq%d" % l, W["mla_q_norm"][l], 2)
    gkv = colvec(p, "gkv%d" % l, W["mla_kv_norm"][l], 1)

    pq = [p.psum("pq%d_%d" % (l, i), [128, 512], F32) for i in range(2)]
    pkv = p.psum("pkv%d" % l, [128, 512], F32)
    pkr = p.psum("pkr%d" % l, [96, 2, 512], F32)
    pn = p.psum("pn%d" % l, [128, 512], F32)
    pw = [p.psum("pw%d_%d" % (l, i), [128, 512], F32) for i in range(2)]
    sq = p.sbuf("sq%d" % l, [128, 2, 512], BF16)
    rq = p.sbuf("rq%d" % l, [128, 512], F32)
    cqn = p.sbuf("cqn%d" % l, [128, 2, 512], BF16)
    ckvn = p.sbuf("ckvn%d" % l, [128, 512], BF16)
    qst = p.sbuf("qst%d" % l, [96, 8, 512], BF16)
    kst = p.sbuf("kst%d" % l, [128, 4, 512], BF16)
    krs = p.sbuf("krs%d" % l, [96, 512], BF16)
    t1s = [p.sbuf("t1_%d_%d" % (l, i), [96, 512], F32) for i in range(2)]
    t2s = [p.sbuf("t2_%d_%d" % (l, i), [96, 512], F32) for i in range(2)]
    t1, t2 = t1s[0], t2s[0]
    vst = p.sbuf("vst%d" % l, [128, 4, 8, 65], BF16)
    p.memset("pool", vst[:], 1.0, [vst])
    rs = slice(64, 96)
    for tb in range(NB):
        ts_ = slice(tb * 512, (tb + 1) * 512)
        for cc in range(2):
            for k in range(8):
                p.mm(pq[cc][:], wa[:, k, cc * 128:(cc + 1) * 128], hT[:, k, ts_], k == 0, k == 7, [wa, hT], [pq[cc]])
        for k in range(8):
            p.mm(pkv[:], wa[:, k, 256:384], hT[:, k, ts_], k == 0, k == 7, [wa, hT], [pkv])
        for v in range(2):
            for k in range(8):
                p.mm(pkr[:, v, :], wkr[:, k, v * 96:(v + 1) * 96], hT[:, k, ts_], k == 0, k == 7, [wkr, hT], [pkr])
        for cc in range(2):
            p.act(sq[:, cc, :], pq[cc][:], AF.Square, [pq[cc]], [sq])
        for cc in range(2):
            p.mm(pn[:], c.onesb[:], sq[:, cc, :], cc == 0, cc == 1, [c.onesb, sq], [pn])
        p.act(rq[:], pn[:], AF.Ln, [pn, c.epsb], [rq], bias=c.epsb[:], scale=1.0 / 256)
        p.act(rq[:], rq[:], AF.Exp, [rq, c.lnsc], [rq], bias=c.lnsc[:], scale=-0.5)
        for cc in range(2):
            p.stt("dve", cqn[:, cc, :], pq[cc][:], gq[:, cc:cc + 1], rq[:], ALU.mult, ALU.mult,
                  [pq[cc], gq, rq], [cqn])
        p.act(sq[:, 0, :], pkv[:], AF.Square, [pkv], [sq])
        p.mm(pn[:], c.onesb[:], sq[:, 0, :], True, True, [c.onesb, sq], [pn])
        p.act(rq[:], pn[:], AF.Ln, [pn, c.epsb], [rq], bias=c.epsb[:], scale=1.0 / 128)
        p.act(rq[:], rq[:], AF.Exp, [rq], [rq], scale=-0.5)
        p.stt("dve", ckvn[:], pkv[:], gkv[:, 0:1], rq[:], ALU.mult, ALU.mult, [pkv, gkv, rq], [ckvn])
        p.tt("dve", t1[rs, :], pkr[rs, 0, :], cos2[rs, ts_], ALU.mult, [pkr, cos2], [t1])
        p.tt("dve", t2[rs, :], pkr[rs, 1, :], sin2[rs, ts_], ALU.mult, [pkr, sin2], [t2])
        p.tt("pool", krs[rs, :], t1[rs, :], t2[rs, :], ALU.add, [t1, t2], [krs])
        for h in range(8):
            p.dma(sc["KT"][h, 64:96, ts_], krs[rs, :], [krs], [])
        for h in range(8):
            a, b = pw[0], pw[1]
            t1, t2 = t1s[h % 2], t2s[h % 2]
            for k in range(2):
                p.mm(a[0:96, :], wq[:, k, h * 96:(h + 1) * 96], cqn[:, k, :], k == 0, k == 1, [wq, cqn], [a])
            for k in range(2):
                p.mm(b[0:96, :], wq[:, k, 768 + h * 96:768 + (h + 1) * 96], cqn[:, k, :], k == 0, k == 1, [wq, cqn], [b])
            p.cp("act", qst[0:64, h, :], a[0:64, :], [a], [qst])
            p.tt("dve", t1[rs, :], a[rs, :], cos2[rs, ts_], ALU.mult, [a, cos2], [t1])
            p.tt("dve", t2[rs, :], b[rs, :], sin2[rs, ts_], ALU.mult, [b, sin2], [t2])
            p.tt("pool", qst[rs, h, :], t1[rs, :], t2[rs, :], ALU.add, [t1, t2], [qst])
        p.dma(sc["QT"][:, :, ts_].rearrange("h p t -> p h t"), qst[:], [qst], [])
        for j in range(4):
            a = pw[j % 2]
            p.mm(a[:], wkv[:, j * 128:(j + 1) * 128], ckvn[:], True, True, [wkv, ckvn], [a])
            p.cp("act", kst[:, j, :], a[:], [a], [kst])
        for two in range(2):
            p.dma(sc["KT"][:, 0:64, ts_].rearrange("(j two) p t -> two p j t", two=2)[two],
                  kst[two * 64:(two + 1) * 64, :, :], [kst], [])
        for tt_ in range(4):
            a = pw[tt_ % 2]
            p.mm(a[:], ckvn[:, tt_ * 128:(tt_ + 1) * 128], wkv[:, 512:1024], True, True, [ckvn, wkv], [a])
            p.cp("act", vst[:, tt_, :, 0:64], a[:].rearrange("p (h v) -> p h v", v=64), [a], [vst])
        p.dma(sc["VA"][ts_, :].rearrange("(tt p) n -> p tt n", p=128),
              vst[:].rearrange("p t h v -> p t (h v)"), [vst], [])

    p.release(mk2)
    mk3 = p.mark()
    pw = [p.psum("pwb%d_%d" % (l, i), [128, 512], F32) for i in range(2)]
    chunks = []
    for i in range(13):
        chunks.append((C_U + i * 128, 128, "mix", "U", i * 128))
    for i in range(2):
        chunks.append((C_GQ + i * 128, 128, "copy32", "GQ", i * 128))
    for i in range(2):
        chunks.append((C_GK + i * 128, 128, "copy32", "GK", i * 128))
    chunks.append((C_GL, 16, "copy32", "GL", 0))
    for i in range(12):
        chunks.append((C_BG + i * 128, 128, "silu", "BG", i * 128))
    for i in range(24):
        chunks.append((C_MG + i * 128, 128, "sigm", "MG", i * 128))
    chunks.sort(key=lambda x: {"mix": 0, "copy32": 0, "silu": 1, "sigm": 2}[x[2]])
    pw = pw + [p.psum("pwd%d_%d" % (l, i), [128, 512], F32) for i in range(2)]
    mu13 = colvec(p, "mu13_%d" % l, W["rwkv_mu"][l], 13)
    wc32 = [p.sbuf("wc32_%d_%d" % (l, i), [128, 8, 128], F32) for i in range(3)]
    wcb = [p.sbuf("wcb_%d_%d" % (l, i), [128, 8, 128], BF16) for i in range(3)]
    st32 = [p.sbuf("st32_%d_%d" % (l, i), [128, 512], F32) for i in range(4)]
    st16 = [p.sbuf("st16_%d_%d" % (l, i), [128, 512], BF16) for i in range(4)]
    stgm = [p.sbuf("stgm_%d_%d" % (l, i), [128, 513], F32) for i in range(3)]
    dmx = [p.sbuf("dmx_%d_%d" % (l, i), [128, 512], F32) for i in range(2)]
    wg32 = p.sbuf("wg32_%d" % l, [128, 8, 512], F32)
    p.dma(wg32[:], W["w_in"][l, :, C_GV:C_GV + 512].rearrange("(c p) n -> p c n", p=128), [], [wg32])
    wg = p.sbuf("wg_%d" % l, [128, 8, 512], BF16)
    p.cp("pool", wg[:], wg32[:], [wg32], [wg])
    n = 0
    nm = 0
    for ci, (col0, m, kind, dest, row0) in enumerate(chunks):
        s = ci % 3
        p.dma(wc32[s][:, :, 0:m], W["w_in"][l, :, col0:col0 + m].rearrange("(c p) n -> p c n", p=128), [], [wc32[s]])
        p.cp("pool", wcb[s][:, :, 0:m], wc32[s][:, :, 0:m], [wc32[s]], [wcb[s]])
        prev = None
        for tb in range(NB):
            ts_ = slice(tb * 512, (tb + 1) * 512)
            a = pw[n % 4]
            for k in range(8):
                p.mm(a[0:m, :], wcb[s][:, k, 0:m], hT[:, k, ts_], k == 0, k == 7, [wcb[s], hT], [a])
            if kind == "mix":
                sg_ = stgm[nm % 3]
                dd = dmx[nm % 2]
                nm += 1
                o = st32[n % 4]
                if prev is None:
                    p.memset("pool", sg_[:, 0:1], 0.0, [sg_])
                else:
                    p.cp("pool", sg_[:, 0:1], prev[:, 512:513], [prev], [sg_])
                p.cp("act", sg_[:, 1:513], a[:], [a], [sg_])
                p.tt("dve", dd[:], sg_[:, 0:512], sg_[:, 1:513], ALU.subtract, [sg_], [dd])
                mi = row0 // 128
                p.stt("dve", o[:], dd[:], mu13[:, mi:mi + 1], sg_[:, 1:513], ALU.mult, ALU.add, [dd, mu13, sg_], [o])
                prev = sg_
            elif kind == "copy32":
                o = st32[n % 4]
                if n % 2 == 0:
                    p.cp("act", o[0:m, :], a[0:m, :], [a], [o])
                else:
                    p.cp("dve", o[0:m, :], a[0:m, :], [a], [o])
            else:
                o = st16[n % 4]
                p.act(o[0:m, :], a[0:m, :], AF.Silu if kind == "silu" else AF.Sigmoid, [a], [o])
            p.dma(sc[dest][row0:row0 + m, ts_], o[0:m, :], [o], [])
            n += 1
    for i in range(NT_):
        a = pw[n % 4]
        o = st16[n % 4]
        n += 1
        for k in range(8):
            p.mm(a[:], hT[:, k, i * 128:(i + 1) * 128], wg[:, k, :], k == 0, k == 7, [hT, wg], [a])
        p.cp("act", o[:], a[:], [a], [o])
        p.dma(sc["GV"][i * 128:(i + 1) * 128, :], o[:], [o], [])
    p.release(mk3)


def phase_mla(p, c, S, l, sc):
    NB = S // 512
    NT_ = S // 128
    kt = p.sbuf("kt", [96, 8, S], BF16)
    for h in range(8):
        p.dma(kt[:, h, :], sc["KT"][h], [sc["KT"]], [kt])
    va = p.sbuf("va", [128, NT_, 520], BF16)
    p.dma(va[:], sc["VA"].t.rearrange("(n p) f -> p n f", p=128), [sc["VA"]], [va])
    qts = [p.sbuf("qt%d" % i, [96, 8, 512], BF16) for i in range(2)]
    bgs = [p.sbuf("bg%d" % i, [128, 4, 512], BF16) for i in range(2)]
    pts = [p.sbuf("pt%d" % i, [128, 512], BF16) for i in range(4)]
    ytm = p.sbuf("ytm", [128, 4, 512], F32)
    ys = [p.sbuf("ysm%d" % i, [128, 4, 512], BF16) for i in range(2)]
    recs = [p.sbuf("rec%d" % i, [128, 1], F32) for i in range(8)]
    pst = [p.psum("pst%d" % i, [128, 512], F32) for i in range(3)]
    acc = [p.psum("acc%d" % i, [128, 512], F32) for i in range(4)]
    pT = p.psum("pT", [128, 4, 128], F32)
    n = 0
    for qb in range(NB):
        qt = qts[qb % 2]
        bg = bgs[qb % 2]
        cols = slice(qb * 512, (qb + 1) * 512)
        p.dma(qt[:], sc["QT"][:, :, cols].rearrange("h p t -> p h t"), [sc["QT"]], [qt])
        p.dma(bg[:], sc["BG"][0:512, cols].rearrange("(c p) t -> p c t", p=128), [sc["BG"]], [bg])
        for h in range(8):
            nkt = 4 * (qb + 1)
            for kti in range(nkt):
                j = kti - 4 * qb
                q0 = 128 * j if j > 0 else 0
                N = 512 - q0
                st = pst[n % 3]
                pt = pts[n % 4]
                p.mm(st[:, 0:N], kt[:, h, kti * 128:(kti + 1) * 128], qt[:, h, q0:512], True, True, [kt, qt], [st])
                p.act(pt[:, 0:N], st[:, 0:N], AF.Exp, [st], [pt])
                if j >= 0:
                    p.op("pool", lambda e, pt=pt: e.affine_select(pt[:, 0:128], pt[:, 0:128], [[1, 128]], ALU.is_ge, 0.0,
                                                                  base=0, channel_multiplier=-1), [pt], [pt])
                for qs in range(q0 // 128, 4):
                    p.mm(acc[qs][:, 0:65], pt[:, qs * 128 - q0:(qs + 1) * 128 - q0], va[:, kti, h * 65:(h + 1) * 65],
                         kti == 0, kti == 4 * qb + qs, [pt, va], [acc[qs]])
                n += 1
            for qs in range(4):
                rc = recs[(h % 2) * 4 + qs]
                p.op("dve", lambda e, qs=qs, rc=rc: e.reciprocal(rc[:], acc[qs][:, 64:65]), [acc[qs]], [rc], dur=200)
                p.ts("dve", ytm[:, qs, h * 64:(h + 1) * 64], acc[qs][:, 0:64], rc[:, 0:1], None, ALU.mult, None,
                     [acc[qs], rc], [ytm])
        yo = ys[qb % 2]
        for fc in range(4):
            for qs in range(4):
                p.tr(pT[:, qs, :], ytm[:, qs, fc * 128:(fc + 1) * 128], c.idf[:], [ytm, c.idf], [pT], signal=(qs == 3))
            p.tt("dve", yo[:, fc, :], pT[:].rearrange("p a b -> p (a b)"), bg[:, fc, :], ALU.mult, [pT, bg], [yo])
        p.dma(sc["YS"][0:512, cols].rearrange("(c p) t -> p c t", p=128), yo[:], [yo], [])


def phase_gla(p, c, S, l, W, sc, ysblk=None):
    NB = S // 512
    H = 4
    aup = p.sbuf("g_aup", [16, 256], F32)
    p.dma(aup[:], W["gla_a_up"][l], [], [aup])
    nab = p.sbuf("g_nab", [64, H], F32)
    p.dma(nab[:], W["gla_a_b"][l].rearrange("(h d) -> d h", d=64), [], [nab], allow_slow_non_contiguous=True)
    p.ts("dve", nab[:], nab[:], -1.0, None, ALU.mult, None, [nab], [nab])
    gn = colvec(p, "g_gn", W["gla_norm"][l], 4)
    one1 = p.sbuf("g_one1", [128, 1], F32)
    p.memset("pool", one1[:], 1.0, [one1])
    maskr = p.sbuf("g_maskr", [64, 512], F32)
    p.memset("pool", maskr[:], 1.0, [maskr])
    p.memset("pool", maskr[:].rearrange("p (c t) -> p c t", t=64)[:, :, 0:1], 0.0, [maskr])
    msk = p.sbuf("g_msk", [64, H, 64], F32)
    p.memset("pool", msk[:], 1.0, [msk])
    for h in range(H):
        p.op("pool", lambda e, h=h: e.affine_select(msk[:, h, :], msk[:, h, :], [[1, 64]], ALU.is_ge, 0.0,
                                                    base=0, channel_multiplier=-1), [msk], [msk])
    st = p.sbuf("g_st", [64, H, 128], F32)
    stb = p.sbuf("g_stb", [64, H, 128], BF16)
    st2 = p.sbuf("g_st2", [64, H, 128], F32)
    p.memset("pool", st[:], 0.0, [st])
    p.memset("pool", stb[:], 0.0, [stb])
    gq1 = p.sbuf("g_q", [64, H, 512], F32)
    gk1 = p.sbuf("g_k", [64, H, 512], F32)
    gq, gk = [gq1, gq1], [gk1, gk1]
    gl1 = p.sbuf("g_l", [16, 512], F32)
    gl = [gl1, gl1]
    gv = [p.sbuf("g_v%d" % i, [64, 8, 512], BF16) for i in range(2)]
    bg1 = p.sbuf("g_bg", [128, H, 512], BF16)
    bg = [bg1, bg1]
    e1 = p.sbuf("g_e1", [64, H, 512], F32)
    cs = p.sbuf("g_cs", [64, H, 512], F32)
    eb = p.sbuf("g_eb", [64, H, 512], F32)
    enb = e1
    qe = p.sbuf("g_qe", [64, H, 512], BF16)
    ke = p.sbuf("g_ke", [64, H, 512], BF16)
    att = [p.sbuf("g_att%d" % i, [64, H, 64], BF16) for i in range(2)]
    ketm = [p.sbuf("g_ketm%d" % i, [64, H, 64], BF16) for i in range(2)]
    sq = p.sbuf("g_sq", [64, H, 128], F32)
    ssg = p.sbuf("g_ss", [64, H], F32)
    yn = [p.sbuf("g_yn%d" % i, [64, H, 128], F32) for i in range(2)]
    yT = p.sbuf("g_yT", [128, H, 512], F32)
    ysg1 = p.sbuf("g_ys", [128, H, 512], BF16)
    ysg = [ysg1, ysg1]
    gbank = [p.psum("g_bank%d" % i, [128, 512], F32) for i in range(3)]
    gbi = [0]

    def GB():
        t = gbank[gbi[0] % 3]
        gbi[0] += 1
        return t
    for tb in range(NB):
        s_ = tb % 2
        cols = slice(tb * 512, (tb + 1) * 512)
        p.dma(gq[s_][:], sc["GQ"][:, cols].rearrange("(h d) t -> d h t", d=64), [sc["GQ"]], [gq[s_]])
        p.dma(gk[s_][:], sc["GK"][:, cols].rearrange("(h d) t -> d h t", d=64), [sc["GK"]], [gk[s_]])
        p.dma(gl[s_][:], sc["GL"][:, cols], [sc["GL"]], [gl[s_]])
        p.dma(gv[s_][:], sc["GV"][cols, :].rearrange("(c p) f -> p c f", p=64), [sc["GV"]], [gv[s_]])
        p.dma(bg[s_][:], sc["BG"][1024:1536, cols].rearrange("(c p) t -> p c t", p=128), [sc["BG"]], [bg[s_]])
        for h in range(H):
            z = GB()
            p.mm(z[0:64, :], aup[:, h * 64:(h + 1) * 64], gl[s_][:], True, True, [aup, gl[s_]], [z])
            p.act(e1[:, h, :], z[0:64, :], AF.Exp, [z, nab], [e1], bias=nab[:, h:h + 1], scale=-1.0)
        p.act(e1[:], e1[:], AF.Ln, [e1, one1], [e1], bias=one1[0:64, :], scale=1.0)
        for h in range(H):
            p.op("dve", lambda e, h=h: e.tensor_tensor_scan(cs[:, h, :], maskr[:], e1[:, h, :], 0.0, ALU.mult, ALU.add),
                 [maskr, e1], [cs])
        p.act(eb[:], cs[:], AF.Exp, [cs], [eb], scale=-1.0 / 16)
        p.act(enb[:], cs[:], AF.Exp, [cs], [enb], scale=1.0 / 16)
        p.stt("dve", qe[:], gq[s_][:], 0.125, eb[:], ALU.mult, ALU.mult, [gq[s_], eb], [qe])
        p.tt("pool", ke[:], gk[s_][:], enb[:], ALU.mult, [gk[s_], enb], [ke])
        for cch in range(8):
            cc = slice(cch * 64, (cch + 1) * 64)
            a_ = att[cch % 2]
            kt_ = ketm[cch % 2]
            y_ = yn[cch % 2]
            b1 = GB()
            pat = b1[0:64, 0:256].rearrange("p (h i) -> p h i", h=H)
            for h in range(H):
                p.mm(pat[:, h, :], ke[:, h, cc], qe[:, h, cc], True, True, [ke, qe], [b1])
            p.tt("dve", a_[:], pat, msk[:], ALU.mult, [b1, msk], [a_])
            b2 = GB()
            pkt = b2[0:64, 0:128].bitcast(BF16).rearrange("p (h i) -> p h i", h=H)
            for h in range(H):
                p.tr(pkt[:, h, :], ke[:, h, cc], c.idb[0:64, 0:64], [ke, c.idb], [b2])
            p.cp("act", kt_[:], pkt, [b2], [kt_])
            b3 = GB()
            po = b3[0:64, :].rearrange("p (h v) -> p h v", h=H)
            for h in range(H):
                p.mm(po[:, h, :], a_[:, h, :], gv[s_][:, cch, h * 128:(h + 1) * 128], True, False, [a_, gv[s_]], [b3])
                p.mm(po[:, h, :], qe[:, h, cc], stb[:, h, :], False, True, [qe, stb], [b3])
            b4 = GB()
            pst = b4[0:64, :].rearrange("p (h v) -> p h v", h=H)
            for h in range(H):
                p.mm(pst[:, h, :], kt_[:, h, :], gv[s_][:, cch, h * 128:(h + 1) * 128], True, True, [kt_, gv[s_]], [b4])
            p.tt("dve", st2[:], st[:], pst, ALU.add, [st, b4], [st2])
            ebl = eb[:, :, cch * 64 + 63:cch * 64 + 64].broadcast_to([64, H, 128])
            p.tt("dve", st[:], st2[:], ebl, ALU.mult, [st2, eb], [st])
            p.cp("pool", stb[:], st[:], [st], [stb])
            p.act(sq[:], po, AF.Square, [b3], [sq])
            p.op("dve", lambda e: e.tensor_reduce(ssg[:], sq[:], AX.X, ALU.add), [sq], [ssg], dur=700)
            p.act(ssg[:], ssg[:], AF.Ln, [ssg, c.epsb], [ssg], bias=c.epsb[0:64, :], scale=1.0 / 128)
            p.act(ssg[:], ssg[:], AF.Exp, [ssg], [ssg], scale=-0.5)
            p.tt("dve", y_[:], po, ssg[:].unsqueeze(2).broadcast_to([64, H, 128]), ALU.mult, [b3, ssg], [y_])
            b5 = GB()
            pyT = b5[:, 0:256].rearrange("p (h t) -> p h t", h=H)
            for h in range(H):
                p.tr(pyT[:, h, :], y_[:, h, :], c.idf[0:64, 0:64], [y_, c.idf], [b5])
            p.cp("act", yT[:, :, cc], pyT, [b5], [yT])
        o_ = ysg[s_]
        for h in range(H):
            p.stt("dve", o_[:, h, :], yT[:, h, :], gn[:, h:h + 1], bg[s_][:, h, :], ALU.mult, ALU.mult, [yT, gn, bg[s_]], [o_])
        p.dma(sc["YS"][1024:1536, cols].rearrange("(c p) t -> p c t", p=128), o_[:], [o_],
              [] if ysblk is None else [ysblk[tb]])


def phase_rwkv(p, c, S, l, W, sc, TB=128):
    NB = S // TB
    NCH = TB // 64
    H = 8
    C0 = math.exp(-0.5)
    vec = lambda name, ap: _hv(p, name, ap)
    mu_r = vec("r_mur", W["rwkv_mu"][l, 0:512])
    mu_k = vec("r_muk", W["rwkv_mu"][l, 512:1024])
    mu_v = vec("r_muv", W["rwkv_mu"][l, 1024:1536])
    mu_w = p.sbuf("r_muw", [64, 2], F32)
    p.dma(mu_w[:], W["rwkv_mu"][l, 1536:1664].rearrange("(c p) -> p c", p=64), [], [mu_w], allow_slow_non_contiguous=True)
    w0 = vec("r_w0", W["rwkv_w0"][l])
    a0 = vec("r_a0", W["rwkv_a0"][l])
    k_k = vec("r_kk", W["rwkv_k_k"][l])
    k_a = vec("r_ka", W["rwkv_k_a"][l])
    r_k = vec("r_rk", W["rwkv_r_k"][l])
    lnw = vec("r_lnw", W["rwkv_ln_w"][l])
    lnb = vec("r_lnb", W["rwkv_ln_b"][l])
    wup = p.sbuf("r_wup", [64, 512], F32)
    p.dma(wup[:], W["rwkv_w_up"][l], [], [wup])
    aup = p.sbuf("r_aup", [64, 512], F32)
    p.dma(aup[:], W["rwkv_a_up"][l], [], [aup])
    ones = p.sbuf("r_ones", [64, 64], F32)
    p.memset("pool", ones[:], 1.0, [ones])
    gne = p.sbuf("r_gne", [64, 1], F32)
    p.memset("pool", gne[:], 64e-5, [gne])
    maskr = p.sbuf("r_maskr", [64, TB], F32)
    p.memset("pool", maskr[:], 1.0, [maskr])
    p.memset("pool", maskr[:].rearrange("p (c t) -> p c t", t=64)[:, :, 0:1], 0.0, [maskr])

    def mk_mask(name, pat, cm, op, dt=F32):
        m = p.sbuf(name, [64, H, 64], F32)
        p.memset("pool", m[:], 1.0, [m])
        for h in range(H):
            p.op("pool", lambda e, h=h: e.affine_select(m[:, h, :], m[:, h, :], pat, op, 0.0, base=0, channel_multiplier=cm), [m], [m])
        if dt == F32:
            return m
        mb = p.sbuf(name + "b", [64, H, 64], dt)
        p.cp("pool", mb[:], m[:], [m], [mb])
        return mb
    mL = mk_mask("r_mL", [[-1, 64]], 1, ALU.is_gt)
    mSU = mk_mask("r_mSU", [[1, 64]], -1, ALU.is_gt)
    mU = mk_mask("r_mU", [[1, 64]], -1, ALU.is_ge)
    idh = mk_mask("r_idh", [[1, 64]], -1, ALU.is_equal, BF16)
    id64 = c.idb[0:64, 0:64]

    def A3(name, dt=F32, n=TB):
        return p.sbuf(name, [64, H, n], dt)
    r_, k_, vf = A3("r_r"), A3("r_k"), A3("r_v")
    lwa = p.sbuf("r_lwa", [64, 2, TB], F32)
    tmp, tmp2 = A3("r_tmp"), A3("r_tmp2")
    sg, a_, cs = A3("r_sg"), A3("r_a"), A3("r_cs")
    gi, gp = A3("r_gi"), A3("r_gp")
    kkn, kp = A3("r_kkn"), A3("r_kp")
    D2 = lambda name, dt: [A3(name + "0", dt), A3(name + "1", dt)]
    vb2, Ab2, Bb2, Kb2, Rb2 = D2("r_vb", BF16), D2("r_Ab", BF16), D2("r_Bb", BF16), D2("r_Kb", BF16), D2("r_Rb", BF16)
    g2, bon2, yfm2 = D2("r_g", F32), D2("r_bon", F32), D2("r_yfm", F32)
    bgt2, yso2 = D2("r_bgt", BF16), D2("r_yso", BF16)
    T0 = p.sbuf("r_T0", [64, H, 64], F32)
    p.memset("pool", T0[:], 0.0, [T0])
    T0b = p.sbuf("r_T0b", [64, H, 64], BF16)
    p.memset("pool", T0b[:], 0.0, [T0b])
    T1 = p.sbuf("r_T1", [64, H, 64], F32)

    def S3(name, dt=BF16):
        return p.sbuf(name, [64, H, 64], dt)
    S2 = lambda name: [S3(name + "0"), S3(name + "1")]
    P_ = S2("r_P")
    PT_ = S2("r_PT")
    NTa = [S2("r_NTa"), S2("r_NTb")]
    LakT2, ArbT2, ArkT2 = S2("r_LakT"), S2("r_ArbT"), S2("r_ArkT")
    Vtm2, Btm2, Ktm2 = S2("r_Vtm"), S2("r_Btm"), S2("r_Ktm")
    X_, U_ = S3("r_X"), S3("r_U")
    yc, ysq, ynm = S3("r_yc", F32), S3("r_ysq", F32), S3("r_ynm", F32)
    mean = p.sbuf("r_mean", [64, H], F32)
    var = p.sbuf("r_var", [64, H], F32)
    pz = p.psum("r_pz", [64, 2, TB], F32)
    pg = [p.psum("r_pg%d" % i, [64, H, 64], F32) for i in range(5)]
    pb = [p.psum("r_pb%d" % i, [64, H, 64], BF16) for i in range(2)]
    pgi = [0, 0]

    def PG():
        t = pg[pgi[0] % 5]
        pgi[0] += 1
        return t

    def PB():
        t = pb[pgi[1] % 2]
        pgi[1] += 1
        return t

    def bc(t2):
        return t2[:].unsqueeze(2).broadcast_to([64, H, TB])

    def prep(tb):
        t0 = tb * TB
        cols = slice(t0, t0 + TB)
        bp = tb % 2
        vb, Ab, Bb, Kb, Rb, g_, bon, bgt = vb2[bp], Ab2[bp], Bb2[bp], Kb2[bp], Rb2[bp], g2[bp], bon2[bp], bgt2[bp]
        for (dst, r0) in ((r_, 0), (k_, 512), (vf, 1024)):
            p.dma(dst[:], sc["U"][r0:r0 + 512, cols].rearrange("(h d) t -> d h t", d=64), [sc["U"]], [dst])
        p.dma(lwa[:], sc["U"][1536:1664, cols].rearrange("(c p) t -> p c t", p=64), [sc["U"]], [lwa])
        p.dma(bgt[:], sc["BG"][512:1024, cols].rearrange("(h d) t -> d h t", d=64), [sc["BG"]], [bgt])
        p.cp("act", vb[:], vf[:], [vf], [vb])
        p.act(lwa[:, 0, :], lwa[:, 0, :], AF.Tanh, [lwa], [lwa])
        for h in range(H):
            p.mm(pz[:, 0, :], wup[:, h * 64:(h + 1) * 64], lwa[:, 0, :], True, True, [wup, lwa], [pz])
            p.mm(pz[:, 1, :], aup[:, h * 64:(h + 1) * 64], lwa[:, 1, :], True, True, [aup, lwa], [pz])
            p.act(sg[:, h, :], pz[:, 0, :], AF.Sigmoid, [pz, w0], [sg], bias=w0[:, h:h + 1], scale=1.0)
            p.act(a_[:, h, :], pz[:, 1, :], AF.Sigmoid, [pz, a0], [a_], bias=a0[:, h:h + 1], scale=1.0)
        for h in range(H):
            p.op("dve", lambda e, h=h: e.tensor_tensor_scan(cs[:, h, :], maskr[:], sg[:, h, :], 0.0, ALU.mult, ALU.add),
                 [maskr, sg], [cs], dur=100 + 2.1 * TB)
        p.act(g_[:], cs[:], AF.Exp, [cs], [g_], scale=-C0)
        p.act(gi[:], cs[:], AF.Exp, [cs], [gi], scale=C0)
        p.tt("pool", tmp[:], cs[:], sg[:], ALU.subtract, [cs, sg], [tmp])
        p.act(gp[:], tmp[:], AF.Exp, [tmp], [gp], scale=-C0)
        p.tt("dve", kkn[:], k_[:], bc(k_k), ALU.mult, [k_, k_k], [kkn])
        p.act(tmp2[:], kkn[:], AF.Square, [kkn], [tmp2])
        for hp in range(4):
            for j in range(2):
                p.mm(pz[:, j, :], ones[:], tmp2[:, hp * 2 + j, :], True, True, [ones, tmp2], [pz])
            p.ts("dve", sg[:, hp * 2:hp * 2 + 2, :], pz[:], 1e-24, None, ALU.max, None, [pz], [sg])
        p.act(sg[:], sg[:], AF.Ln, [sg], [sg])
        p.act(sg[:], sg[:], AF.Exp, [sg], [sg], scale=-0.5)
        p.tt("pool", kkn[:], kkn[:], sg[:], ALU.mult, [kkn, sg], [kkn])
        p.stt("dve", tmp[:], a_[:], -1.0, bc(k_a), ALU.add, ALU.mult, [a_, k_a], [tmp])
        p.stt("dve", kp[:], tmp[:], 1.0, k_[:], ALU.add, ALU.mult, [tmp, k_], [kp])
        p.stt("dve", Ab[:], kkn[:], -1.0, gp[:], ALU.mult, ALU.mult, [kkn, gp], [Ab])
        p.tt("pool", tmp2[:], kkn[:], a_[:], ALU.mult, [kkn, a_], [tmp2])
        p.tt("dve", Bb[:], tmp2[:], gi[:], ALU.mult, [tmp2, gi], [Bb])
        p.tt("pool", Kb[:], kp[:], gi[:], ALU.mult, [kp, gi], [Kb])
        p.tt("dve", Rb[:], r_[:], g_[:], ALU.mult, [r_, g_], [Rb])
        p.tt("pool", tmp[:], r_[:], kp[:], ALU.mult, [r_, kp], [tmp])
        p.tt("pool", tmp[:], tmp[:], bc(r_k), ALU.mult, [tmp, r_k], [tmp])
        for hp in range(4):
            for j in range(2):
                p.mm(pz[:, j, :], ones[:], tmp[:, hp * 2 + j, :], True, True, [ones, tmp], [pz])
            p.tt("dve", bon[:, hp * 2:hp * 2 + 2, :], pz[:], vf[:, hp * 2:hp * 2 + 2, :], ALU.mult, [pz, vf], [bon])

    def chunk(tb, cch):
        bp = tb % 2
        g = tb * NCH + cch
        cp_ = g % 2
        cc = slice(cch * 64, (cch + 1) * 64)
        vb, Ab, Bb, Kb, Rb, g_, yfm = vb2[bp], Ab2[bp], Bb2[bp], Kb2[bp], Rb2[bp], g2[bp], yfm2[bp]
        LakT, ArbT, ArkT, Vtm, Btm, Ktm = LakT2[cp_], ArbT2[cp_], ArkT2[cp_], Vtm2[cp_], Btm2[cp_], Ktm2[cp_]
        NTp = NTa[cp_]

        def scores(lh, rh, mask, dst, eng):
            ps = PG()
            for h in range(H):
                p.mm(ps[:, h, :], lh[:, h, cc], rh[:, h, cc], True, True, [lh, rh], [ps])
            p.tt(eng, dst[:], ps[:], mask[:], ALU.mult, [ps, mask], [dst])
        scores(Ab, Bb, mL, P_[0], "dve")
        scores(Bb, Ab, mSU, PT_[0], "dve")
        p.tt("pool", NTp[0][:], PT_[0][:], idh[:], ALU.add, [PT_[0], idh], [NTp[0]])
        scores(Kb, Ab, mSU, LakT, "dve")
        scores(Bb, Rb, mU, ArbT, "dve")
        scores(Kb, Rb, mU, ArkT, "dve")

        def transp(src, dst):
            ps = PB()
            for h in range(H):
                p.tr(ps[:, h, :], src[:, h, cc], id64, [src, c.idb], [ps])
            p.cp("act", dst[:], ps[:], [ps], [dst])
        transp(vb, Vtm)
        transp(Bb, Btm)
        transp(Kb, Ktm)
        cur = 0
        nti = 0
        for step in range(5):
            Pn, PTn = P_[1 - cur], PT_[1 - cur]
            ps = PG()
            for h in range(H):
                p.mm(ps[:, h, :], PT_[cur][:, h, :], P_[cur][:, h, :], True, True, [PT_[cur], P_[cur]], [ps])
            p.cp("act", Pn[:], ps[:], [ps], [Pn])
            if step < 4:
                ps2 = PG()
                for h in range(H):
                    p.mm(ps2[:, h, :], P_[cur][:, h, :], PT_[cur][:, h, :], True, True, [PT_[cur], P_[cur]], [ps2])
                p.cp("dve", PTn[:], ps2[:], [ps2], [PTn])
            ps3 = PG()
            NTo, NTn = NTp[nti], NTp[1 - nti]
            for h in range(H):
                p.mm(ps3[:, h, :], id64, NTo[:, h, :], True, False, [c.idb, NTo], [ps3])
                p.mm(ps3[:, h, :], Pn[:, h, :], NTo[:, h, :], False, True, [Pn, NTo], [ps3])
            p.cp("act", NTn[:], ps3[:], [ps3], [NTn])
            nti = 1 - nti
            cur = 1 - cur
        NT = NTp[nti]
        ps = PG()
        for h in range(H):
            p.mm(ps[:, h, :], Ab[:, h, cc], T0b[:, h, :], True, False, [Ab, T0b], [ps])
            p.mm(ps[:, h, :], LakT[:, h, :], Vtm[:, h, :], False, True, [LakT, Vtm], [ps])
        p.cp("act", X_[:], ps[:], [ps], [X_])
        ps = PG()
        for h in range(H):
            p.mm(ps[:, h, :], NT[:, h, :], X_[:, h, :], True, True, [NT, X_], [ps])
        p.cp("act", U_[:], ps[:], [ps], [U_])
        py = PG()
        for h in range(H):
            p.mm(py[:, h, :], Rb[:, h, cc], T0b[:, h, :], True, False, [Rb, T0b], [py])
            p.mm(py[:, h, :], ArbT[:, h, :], U_[:, h, :], False, False, [ArbT, U_], [py])
            p.mm(py[:, h, :], ArkT[:, h, :], Vtm[:, h, :], False, True, [ArkT, Vtm], [py])
        ps = PG()
        for h in range(H):
            p.mm(ps[:, h, :], Btm[:, h, :], U_[:, h, :], True, False, [Btm, U_], [ps])
            p.mm(ps[:, h, :], Ktm[:, h, :], Vtm[:, h, :], False, True, [Ktm, Vtm], [ps])
        p.tt("dve", T1[:], T0[:], ps[:], ALU.add, [T0, ps], [T1])
        gC = g_[:, :, cch * 64 + 63:cch * 64 + 64].broadcast_to([64, H, 64])
        p.tt("dve", T0[:], T1[:], gC, ALU.mult, [T1, g_], [T0])
        p.cp("pool", T0b[:], T0[:], [T0], [T0b])
        p.op("dve", lambda e, py=py: e.tensor_reduce(mean[:], py[:], AX.X, ALU.add), [py], [mean], dur=700)
        p.ts("dve", mean[:], mean[:], 1.0 / 64, None, ALU.mult, None, [mean], [mean])
        p.tt("dve", yc[:], py[:], mean[:].unsqueeze(2).broadcast_to([64, H, 64]), ALU.subtract, [py, mean], [yc])
        p.act(ysq[:], yc[:], AF.Square, [yc], [ysq])
        p.op("dve", lambda e: e.tensor_reduce(var[:], ysq[:], AX.X, ALU.add), [ysq], [var], dur=700)
        p.act(var[:], var[:], AF.Ln, [var, gne], [var], bias=gne[:], scale=1.0 / 64)
        p.act(var[:], var[:], AF.Exp, [var], [var], scale=-0.5)
        p.tt("pool", ynm[:], yc[:], var[:].unsqueeze(2).broadcast_to([64, H, 64]), ALU.mult, [yc, var], [ynm])
        ps = PG()
        for h in range(H):
            p.tr(ps[:, h, :], ynm[:, h, :], c.idf[0:64, 0:64], [ynm, c.idf], [ps])
        p.cp("act", yfm[:, :, cc], ps[:], [ps], [yfm])

    def epilogue(tb):
        bp = tb % 2
        cols = slice(tb * TB, (tb + 1) * TB)
        yfm, bon, bgt, yso = yfm2[bp], bon2[bp], bgt2[bp], yso2[bp]
        p.tt("dve", yfm[:], yfm[:], bc(lnw), ALU.mult, [yfm, lnw], [yfm])
        p.tt("pool", yfm[:], yfm[:], bc(lnb), ALU.add, [yfm, lnb], [yfm])
        p.tt("pool", yfm[:], yfm[:], bon[:], ALU.add, [yfm, bon], [yfm])
        p.tt("dve", yso[:], yfm[:], bgt[:], ALU.mult, [yfm, bgt], [yso])
        p.dma(sc["YS"][512:1024, cols].rearrange("(h d) t -> d h t", d=64), yso[:], [yso], [])

    for tb in range(NB):
        prep(tb)
        for cch in range(NCH):
            chunk(tb, cch)
        epilogue(tb)


def _hv(p, name, ap):
    t = p.sbuf(name, [64, 8], F32)
    p.dma(t[:], ap.rearrange("(h d) -> d h", d=64), [], [t], allow_slow_non_contiguous=True)
    return t


def phase_out_prep(p, c, S, l, W):
    wbo = p.sbuf("wbo", [128, 12, D], BF16)
    wout = p.sbuf("wout", [128, 8, D], BF16)
    gpb = p.sbuf("gpb", [128, D], F32)
    p.dma(gpb[:], W["norm_post"][l:l + 1, :].partition_broadcast(128), [], [gpb])
    mk = p.mark()
    stg = [p.sbuf("wstg%d" % i, [128, 4, D], F32) for i in range(2)]
    for n in range(3):
        p.dma(stg[n % 2][:], W["w_branch_out"][l, n].rearrange("(k p) d -> p k d", p=128), [], [stg[n % 2]])
        p.cp("pool", wbo[:, n * 4:(n + 1) * 4, :], stg[n % 2][:], [stg[n % 2]], [wbo])
    for hf in range(2):
        p.dma(stg[(hf + 1) % 2][:], W["w_out"][l, hf * 512:(hf + 1) * 512, :].rearrange("(k p) d -> p k d", p=128), [], [stg[(hf + 1) % 2]])
        p.cp("pool", wout[:, hf * 4:(hf + 1) * 4, :], stg[(hf + 1) % 2][:], [stg[(hf + 1) % 2]], [wout])
    p.release(mk)
    return wbo, wout, gpb


def phase_out(p, c, S, l, xin, xout, W, sc, pre=None, ysblk=None):
    NB = S // 512
    if pre is None:
        pre = phase_out_prep(p, c, S, l, W)
    wbo, wout, gpb = pre
    ysb = [p.sbuf("ysb%d" % i, [128, 12, 512], BF16) for i in range(2)]
    mgb = [p.sbuf("mgb%d" % i, [128, 3, 512], BF16) for i in range(3)]
    mT = p.sbuf("mT", [128, 8, 512], BF16)
    m32 = p.sbuf("m32", [128, 512], F32)
    t32 = [p.sbuf("t32_%d" % i, [128, 512], F32) for i in range(2)]
    xt = [p.sbuf("xo%d" % i, [128, D], F32) for i in range(2)]
    o32a = p.sbuf("o32_0", [128, D], F32)
    o32 = [o32a, o32a]
    junk = p.sbuf("junko", [128, 512], BF16)
    ssq = p.sbuf("ssq", [128, 2], F32)
    rstd = p.sbuf("rstdo", [128, 1], F32)
    pb = [p.psum("pb%d" % i, [128, 512], F32) for i in range(3)]
    po = [p.psum("po%d" % i, [128, 512], F32) for i in range(2)]
    it = 0
    im = 0
    for tb in range(NB):
        cols = slice(tb * 512, (tb + 1) * 512)
        ysx = ysb[tb % 2]
        p.dma(ysx[:], sc["YS"][:, cols].rearrange("(c p) t -> p c t", p=128),
              [sc["YS"] if ysblk is None else ysblk[tb]], [ysx])
        mgv = sc["MG"][:, cols].rearrange("(n c p) t -> p n c t", n=3, p=128)
        for cch in range(8):
            mgx = mgb[im % 3]
            im += 1
            p.dma(mgx[:], mgv[:, :, cch, :], [sc["MG"]], [mgx])
            for n in range(3):
                for k in range(4):
                    p.mm(pb[n][:], wbo[:, n * 4 + k, cch * 128:(cch + 1) * 128], ysx[:, n * 4 + k, :], k == 0, k == 3,
                         [wbo, ysx], [pb[n]])
            p.tt("dve", m32[:], pb[0][:], mgx[:, 0, :], ALU.mult, [pb[0], mgx], [m32])
            p.tt("dve", t32[0][:], pb[1][:], mgx[:, 1, :], ALU.mult, [pb[1], mgx], [t32[0]])
            p.tt("dve", t32[1][:], pb[2][:], mgx[:, 2, :], ALU.mult, [pb[2], mgx], [t32[1]])
            p.tt("pool", m32[:], m32[:], t32[0][:], ALU.add, [m32, t32[0]], [m32])
            p.tt("pool", mT[:, cch, :], m32[:], t32[1][:], ALU.add, [m32, t32[1]], [mT])
        for tsub in range(4):
            s_ = it % 2
            it += 1
            r0 = tb * 512 + tsub * 128
            p.dma(xt[s_][:], xin[r0:r0 + 128, :], [xin], [xt[s_]])
            for hf in range(2):
                for k in range(8):
                    p.mm(po[hf][:], mT[:, k, tsub * 128:(tsub + 1) * 128], wout[:, k, hf * 512:(hf + 1) * 512], k == 0, k == 7,
                         [mT, wout], [po[hf]])
                p.act(junk[:], po[hf][:], AF.Square, [po[hf]], [junk, ssq], accum_out=ssq[:, hf:hf + 1])
            p.tt("dve", rstd[:], ssq[:, 0:1], ssq[:, 1:2], ALU.add, [ssq], [rstd])
            p.act(rstd[:], rstd[:], AF.Ln, [rstd, c.epsb], [rstd], bias=c.epsb[:], scale=1.0 / D)
            p.act(rstd[:], rstd[:], AF.Exp, [rstd], [rstd], scale=-0.5)
            for hf in range(2):
                p.stt("dve", o32[s_][:, hf * 512:(hf + 1) * 512], po[hf][:], rstd[:, 0:1], gpb[:, hf * 512:(hf + 1) * 512],
                      ALU.mult, ALU.mult, [po[hf], rstd, gpb], [o32[s_]])
            p.tt("pool", o32[s_][:], o32[s_][:], xt[s_][:], ALU.add, [o32[s_], xt[s_]], [o32[s_]])
            p.dma(xout[r0:r0 + 128, :], o32[s_][:], [o32[s_]], [])


def make_scratch(p, S, dbg):
    kind = "ExternalOutput" if dbg else "Internal"
    sc = {}
    sc["QT"] = p.dram("QT", [8, 96, S], BF16, kind)
    sc["KT"] = p.dram("KT", [8, 96, S], BF16, kind)
    sc["VA"] = p.dram("VA", [S, 520], BF16, kind)
    sc["U"] = p.dram("U", [1664, S], F32, kind)
    sc["GQ"] = p.dram("GQ", [256, S], F32, kind)
    sc["GK"] = p.dram("GK", [256, S], F32, kind)
    sc["GV"] = p.dram("GV", [S, 512], BF16, kind)
    sc["GL"] = p.dram("GL", [16, S], F32, kind)
    sc["BG"] = p.dram("BG", [1536, S], BF16, kind)
    sc["MG"] = p.dram("MG", [3072, S], BF16, kind)
    sc["YS"] = p.dram("YS", [1536, S], BF16, kind)
    sc["X1"] = p.dram("X1", [S, D], F32, kind)
    return sc


WSPEC = {
    "norm_pre": ([L, D], F32), "w_in": ([L, D, NIN], F32), "wkr2": ([L, D, 192], F32),
    "mla_q_norm": ([L, 256], F32), "mla_kv_norm": ([L, 128], F32),
    "mla_w_uq": ([L, 256, 768], F32), "wuq_sw": ([L, 256, 768], F32),
    "wuk": ([L, 128, 512], F32), "wuv": ([L, 128, 512], F32),
    "rwkv_mu": ([L, 1664], F32), "rwkv_w0": ([L, 512], F32), "rwkv_w_up": ([L, 64, 512], F32),
    "rwkv_a0": ([L, 512], F32), "rwkv_a_up": ([L, 64, 512], F32), "rwkv_k_k": ([L, 512], F32),
    "rwkv_k_a": ([L, 512], F32), "rwkv_r_k": ([L, 512], F32), "rwkv_ln_w": ([L, 512], F32),
    "rwkv_ln_b": ([L, 512], F32), "gla_a_up": ([L, 16, 256], F32), "gla_a_b": ([L, 256], F32),
    "gla_norm": ([L, 512], F32), "w_branch_out": ([L, 3, 512, D], F32), "w_out": ([L, D, D], F32),
    "norm_post": ([L, D], F32),
}


def host_weights(inp):
    w = {}
    for k in WSPEC:
        if k in inp:
            w[k] = np.ascontiguousarray(np.asarray(inp[k], dtype=np.float32))
    win = w["w_in"]
    w["wkr2"] = np.ascontiguousarray(np.concatenate(
        [win[:, :, 320:416], win[:, :, 320:384], win[:, :, 400:416], win[:, :, 384:400]], axis=2))
    uq = w["mla_w_uq"].reshape(L, 256, 8, 96)
    w["wuq_sw"] = np.ascontiguousarray(np.concatenate(
        [uq[..., 0:64], uq[..., 80:96], uq[..., 64:80]], axis=-1).reshape(L, 256, 768))
    ukv = w["mla_w_ukv"] if "mla_w_ukv" in w else np.asarray(inp["mla_w_ukv"], dtype=np.float32)
    ukv = ukv.reshape(L, 128, 8, 128)
    w["wuk"] = np.ascontiguousarray(ukv[..., 0:64].reshape(L, 128, 512))
    w["wuv"] = np.ascontiguousarray(ukv[..., 64:128].reshape(L, 128, 512))
    w["rwkv_r_k"] = w["rwkv_r_k"].reshape(L, 512) if "rwkv_r_k" in w else np.asarray(inp["rwkv_r_k"], np.float32).reshape(L, 512)
    return w


def build(S, phases=("p1",), dbg=True, nlayers=1):
    nc = bass.Bass("TRN2", target_bir_lowering=False)
    p = Prog(nc)
    x_d = p.dram("x", [S, D], F32, "ExternalInput")
    pos_d = p.dram("pos", [1, S], I32, "ExternalInput")
    W = {k: p.dram(k, sh, dt, "ExternalInput").t for k, (sh, dt) in WSPEC.items()}
    out_d = p.dram("out", [S, D], F32, "ExternalOutput")
    sc = make_scratch(p, S, dbg)
    c = setup_consts(p, S, pos_d.t)
    final = [out_d]
    for l in range(nlayers):
        xin = x_d if l == 0 else sc["X1"]
        if "p1" in phases:
            mk = p.mark()
            phase1(p, c, S, l, xin, W, sc)
            p.release(mk)
        if "mla" in phases:
            mk = p.mark()
            phase_mla(p, c, S, l, sc)
            p.release(mk)
        if "rwkv" in phases:
            mk = p.mark()
            phase_rwkv(p, c, S, l, W, sc)
            p.release(mk)
        xo = out_d if l == nlayers - 1 else sc["X1"]
        if "gla" in phases and "out" in phases:
            mk = p.mark()
            pre = phase_out_prep(p, c, S, l, W)
            ysblk = [T(None, "ysblk%d" % i) for i in range(S // 512)]
            phase_gla(p, c, S, l, W, sc, ysblk)
            phase_out(p, c, S, l, xin, xo, W, sc, pre, ysblk)
            p.release(mk)
        else:
            if "gla" in phases:
                mk = p.mark()
                phase_gla(p, c, S, l, W, sc)
                p.release(mk)
            if "out" in phases:
                mk = p.mark()
                phase_out(p, c, S, l, xin, xo, W, sc)
                p.release(mk)
    if dbg:
        final = list(sc.values()) + [out_d]
    p.finish(final)
    return nc, p


ALL_PHASES = ("p1", "mla", "rwkv", "gla", "out")
_CACHE = {}


def kernel(**inputs):
    x = np.asarray(inputs["x"], dtype=np.float32)
    pos = np.asarray(inputs["positions"], dtype=np.int32)
    B, S, _ = x.shape
    Wh = host_weights(inputs)
    nc, _p = build(S, phases=ALL_PHASES, dbg=False, nlayers=L)
    in_maps = []
    for b in range(B):
        m = {"x": np.ascontiguousarray(x[b]), "pos": np.ascontiguousarray(pos[b:b + 1])}
        for k in WSPEC:
            m[k] = Wh[k]
        in_maps.append(m)
    res = run_bass_kernel_spmd(nc, in_maps, core_ids=list(range(B)))
    return np.stack([np.asarray(r["out"]) for r in res.results], axis=0).astype(np.float32)
```

```python
import math
import numpy as np
import concourse.bass as bass
import concourse.mybir as mybir
from concourse.bass_utils import run_bass_kernel_spmd

F32 = mybir.dt.float32
BF16 = mybir.dt.bfloat16
I32 = mybir.dt.int32
ALU = mybir.AluOpType
AF = mybir.ActivationFunctionType
AX = mybir.AxisListType

SEM_CAP = 30000
D = 1024
NIN = 7728
L = 2
EPS = 1e-6


class Buf:
    __slots__ = ("name", "w", "r")

    def __init__(self, name):
        self.name = name
        self.w = None
        self.r = {}


class T:
    def __init__(self, t, name):
        self.t = t
        self.b = Buf(name)

    def __getitem__(self, k):
        return self.t[k]


def _fs(ap):
    n = 1
    for d in ap.shape[1:]:
        n *= int(d)
    return n


def _nbytes(ap):
    n = 1
    for d in ap.shape:
        n *= int(d)
    return n * mybir.dt.size(ap.dtype)


class Prog:
    CE = ("pe", "act", "dve", "pool")
    QE = ("sp",)
    WINDOW = 48
    SYNC = 300.0

    def __init__(self, nc, n_dma_sems=32):
        self.nc = nc
        self.sems = {}
        self._ctx = []
        self._semctx = []
        self.cur = {}
        self.seen = {e: {} for e in self.CE + self.QE}
        self.nsem = 0
        self.old_sems = []
        for e in self.CE:
            self._new_eng_sem(e)
        self.dma_sems = []
        for i in range(n_dma_sems):
            k = self._alloc_sem("dma%d" % i)
            self.dma_sems.append([k, 0])
        self.dma_rr = 0
        self.ninst = 0
        self.ops = []
        self.uid = 0
        self.model_time = 0.0

    def _alloc_sem(self, name):
        cm = self.nc.semaphore(name)
        h = cm.__enter__()
        self._semctx.append(cm)
        self.sems[name] = h
        self.nsem += 1
        return name

    def _new_eng_sem(self, e):
        if e in self.cur:
            self.old_sems.append(tuple(self.cur[e]))
        k = self._alloc_sem("%s_s%d" % (e, self.nsem))
        self.cur[e] = [k, 0]

    def sbuf(self, name, shape, dt):
        self.uid += 1
        name = "%s_u%d" % (name, self.uid)
        cm = self.nc.sbuf_tensor(name, list(shape), dt)
        t = cm.__enter__()
        self._ctx.append(cm)
        return T(t, name)

    def psum(self, name, shape, dt):
        self.uid += 1
        name = "%s_u%d" % (name, self.uid)
        cm = self.nc.psum_tensor(name, list(shape), dt)
        t = cm.__enter__()
        self._ctx.append(cm)
        return T(t, name)

    def dram(self, name, shape, dt, kind="Internal"):
        t = self.nc.dram_tensor(name, list(shape), dt, kind=kind).ap()
        return T(t, name)

    def op(self, eng, fn, R=(), W=(), signal=True, dur=500.0):
        self.ops.append((eng, fn, [x.b for x in R], [x.b for x in W], float(dur), float(dur), False))
        self.ninst += 1

    def dma(self, out_ap, in_ap, R=(), W=(), q="sp", **kw):
        lat = 2000.0 + _nbytes(out_ap) / 150.0
        self.ops.append((q, lambda e: e.dma_start(out=out_ap, in_=in_ap, **kw),
                         [x.b for x in R], [x.b for x in W], 80.0, lat, True))
        self.ninst += 1

    def mm(self, out, lhsT, rhs, start, stop, R, W, **kw):
        n = _fs(rhs)
        d = (50.0 + 1.0 * max(n, 64)) if lhsT.dtype == F32 else (40.0 + 0.42 * max(n, 64))
        self.op("pe", lambda e: e.matmul(out, lhsT, rhs, start=start, stop=stop, **kw), R, W, dur=d)

    def tr(self, out, in_, ident, R, W, signal=True):
        self.op("pe", lambda e: e.transpose(out, in_, ident), R, W, dur=110.0)

    def act(self, out, in_, func, R, W, **kw):
        self.op("act", lambda e: e.activation(out, in_, func, **kw), R, W, dur=260.0 + 0.83 * _fs(out))

    def _vd(self, eng, out, two_src):
        n = _fs(out)
        if eng == "pool":
            return 150.0 + 2.2 * n
        b16 = mybir.dt.size(out.dtype) == 2
        if two_src:
            return 160.0 + (1.04 * n)
        return 160.0 + (0.55 * n)

    def tt(self, eng, out, a, b, op, R, W):
        self.op(eng, lambda e: e.tensor_tensor(out, a, b, op), R, W, dur=self._vd(eng, out, True))

    def ts(self, eng, out, a, s1, s2, op0, op1, R, W):
        d = self._vd(eng, out, False)
        if s2 is None:
            self.op(eng, lambda e: e.tensor_scalar(out, a, s1, None, op0), R, W, dur=d)
        else:
            self.op(eng, lambda e: e.tensor_scalar(out, a, s1, s2, op0, op1), R, W, dur=d)

    def stt(self, eng, out, a, s, b, op0, op1, R, W):
        self.op(eng, lambda e: e.scalar_tensor_tensor(out, a, s, b, op0, op1), R, W, dur=self._vd(eng, out, True))

    def cp(self, eng, out, in_, R, W):
        if eng == "act":
            self.op(eng, lambda e: e.copy(out, in_), R, W, dur=260.0 + 0.83 * _fs(out))
        else:
            self.op(eng, lambda e: e.tensor_copy(out, in_), R, W, dur=self._vd(eng, out, False))

    def memset(self, eng, ap, val, W):
        self.op(eng, lambda e: e.memset(ap, val), (), W, dur=self._vd(eng, ap, False))

    def _eng(self, eng):
        nc = self.nc
        return {"pe": nc.tensor, "act": nc.scalar, "dve": nc.vector, "pool": nc.gpsimd, "sp": nc.sync}[eng]

    def flush(self):
        import bisect
        ops = self.ops
        self.ops = []
        n = len(ops)
        if n == 0:
            return
        engs = self.CE + self.QE
        lastw = {}
        readers = {}
        deps = [None] * n
        for i in range(n):
            eng, fn, R, W, occ, lat, isd = ops[i]
            d = set()
            for b in R:
                w = lastw.get(id(b))
                if w is not None:
                    d.add(w)
            for b in W:
                w = lastw.get(id(b))
                if w is not None:
                    d.add(w)
                rl = readers.get(id(b))
                if rl:
                    d.update(rl)
            d.discard(i)
            deps[i] = d
            for b in R:
                readers.setdefault(id(b), []).append(i)
            for b in W:
                lastw[id(b)] = i
                readers[id(b)] = []
        succ = [[] for _ in range(n)]
        nd = [0] * n
        for i in range(n):
            nd[i] = len(deps[i])
            for j in deps[i]:
                succ[j].append(i)
        bl = [0.0] * n
        for i in range(n - 1, -1, -1):
            m = 0.0
            for s_ in succ[i]:
                if bl[s_] > m:
                    m = bl[s_]
            bl[i] = m + ops[i][5] + 100.0
        ready = [0.0] * n
        free = {e: 0.0 for e in engs}
        cand = {e: [] for e in engs}
        for i in range(n):
            if nd[i] == 0:
                cand[ops[i][0]].append(i)
        order = {e: [] for e in engs}
        done = 0
        W_ = self.WINDOW
        SY = self.SYNC
        tmax = 0.0
        while done < n:
            best = None
            for e in engs:
                c = cand[e]
                if not c:
                    continue
                fe = free[e]
                bst = None
                for i in c[:W_]:
                    st = ready[i] if ready[i] > fe else fe
                    key = (st, -bl[i])
                    if bst is None or key < bst[2]:
                        bst = (st, i, key)
                if best is None or bst[2] < best[3]:
                    best = (bst[0], bst[1], e, bst[2])
            st, i, e = best[0], best[1], best[2]
            cand[e].remove(i)
            order[e].append(i)
            occ, lat = ops[i][4], ops[i][5]
            free[e] = st + occ
            fin = st + lat
            if fin > tmax:
                tmax = fin
            for s_ in succ[i]:
                rt = fin + (SY if ops[s_][0] != e else (0.0 if e == "pe" else 200.0))
                if rt > ready[s_]:
                    ready[s_] = rt
                nd[s_] -= 1
                if nd[s_] == 0:
                    bisect.insort(cand[ops[s_][0]], s_)
            done += 1
        self.model_time += tmax
        busy = {e: 0.0 for e in engs}
        for i in range(n):
            busy[ops[i][0]] += ops[i][4]
        if not hasattr(self, "regions"):
            self.regions = []
        self.regions.append((tmax, busy, n))
        need = [False] * n
        for i in range(n):
            e = ops[i][0]
            if ops[i][6] or not succ[i]:
                need[i] = True
                continue
            for s_ in succ[i]:
                if ops[s_][0] != e or e != "pe":
                    need[i] = True
                    break
        tick = [None] * n
        reuse_wait = {}
        for e in engs:
            for i in order[e]:
                if ops[i][6]:
                    slot = self.dma_sems[self.dma_rr]
                    self.dma_rr = (self.dma_rr + 1) % len(self.dma_sems)
                    if slot[1] > 0:
                        reuse_wait[i] = (slot[0], slot[1])
                    slot[1] += 16
                    tick[i] = (slot[0], slot[1])
                elif need[i]:
                    cur = self.cur[e]
                    if cur[1] >= SEM_CAP:
                        self._new_eng_sem(e)
                        cur = self.cur[e]
                    cur[1] += 1
                    tick[i] = (cur[0], cur[1])
        for e in engs:
            eo = self._eng(e)
            seen = self.seen[e]
            sems = self.sems
            for i in order[e]:
                w = {}
                for j in deps[i]:
                    if e == "pe" and ops[j][0] == "pe":
                        continue
                    k, v = tick[j]
                    if w.get(k, 0) < v:
                        w[k] = v
                if i in reuse_wait:
                    k, v = reuse_wait[i]
                    if w.get(k, 0) < v:
                        w[k] = v
                for k, v in w.items():
                    if seen.get(k, 0) < v:
                        seen[k] = v
                        eo.wait_ge(sems[k], v)
                ins = ops[i][1](eo)
                if tick[i] is not None:
                    ins.then_inc(sems[tick[i][0]], 16 if ops[i][6] else 1)

    def barrier(self):
        self.flush()
        allv = [(k, v) for (k, v) in [tuple(x) for x in self.cur.values()] if v > 0]
        allv += [(k, v) for (k, v) in self.old_sems]
        allv += [(k, v) for (k, v) in [tuple(x) for x in self.dma_sems] if v > 0]
        for eng in self.CE + self.QE:
            eo = self._eng(eng)
            for k, v in allv:
                if self.seen[eng].get(k, 0) < v:
                    self.seen[eng][k] = v
                    eo.wait_ge(self.sems[k], v)

    def mark(self):
        return len(self._ctx)

    def release(self, mark):
        self.barrier()
        while len(self._ctx) > mark:
            self._ctx.pop().__exit__(None, None, None)

    def finish(self, final):
        self.barrier()
        while self._ctx:
            self._ctx.pop().__exit__(None, None, None)
        while self._semctx:
            self._semctx.pop().__exit__(None, None, None)


C_CQ, C_CKV, C_KR, C_U = 0, 256, 384, 416
C_GQ, C_GK, C_GV, C_GL, C_BG, C_MG = 2080, 2336, 2592, 3104, 3120, 4656
SCALE = (64 + 32) ** -0.5


class Ctx:
    pass


def setup_consts(p, S, pos_d):
    c = Ctx()
    idf = p.sbuf("idf", [128, 128], F32)
    p.memset("pool", idf[:], 1.0, [idf])
    p.op("pool", lambda e: e.affine_select(idf[:], idf[:], [[1, 128]], ALU.is_equal, 0.0,
                                           base=0, channel_multiplier=-1), [idf], [idf])
    idb = p.sbuf("idb", [128, 128], BF16)
    p.cp("dve", idb[:], idf[:], [idf], [idb])
    c.idf, c.idb = idf, idb
    onesb = p.sbuf("onesb", [128, 128], BF16)
    p.memset("pool", onesb[:], 1.0, [onesb])
    c.onesb = onesb
    epsb = p.sbuf("epsb", [128, 1], F32)
    p.memset("pool", epsb[:], EPS, [epsb])
    c.epsb = epsb
    lnsc = p.sbuf("lnsc", [128, 1], F32)
    p.memset("pool", lnsc[:], math.log(SCALE), [lnsc])
    c.lnsc = lnsc
    inv = (1.0 / (10000.0 ** (np.arange(0, 32, 2, dtype=np.float32) / np.float32(32)))).astype(np.float32)
    row = p.sbuf("roperow", [1, 64], F32)
    for j in range(16):
        p.memset("pool", row[0:1, j:j + 1], float(inv[j]), [row])
        p.memset("pool", row[0:1, 16 + j:17 + j], float(inv[j]), [row])
    p.memset("pool", row[0:1, 32:48], -1.0, [row])
    p.memset("pool", row[0:1, 48:64], 1.0, [row])
    rd = p.dram("rope_d", [64], F32)
    p.dma(rd[:].rearrange("(o n) -> o n", o=1), row[:], [row], [rd])
    ctab = p.sbuf("ctab", [96, 2], F32)
    p.dma(ctab[64:96, :], rd[:].rearrange("(c p) -> p c", p=32), [rd], [ctab], allow_slow_non_contiguous=True)
    mk = p.mark()
    cos2 = p.sbuf("cos2", [96, S], F32)
    sin2 = p.sbuf("sin2", [96, S], F32)
    posi = p.sbuf("posi", [96, S], I32)
    p.dma(posi[64:96, :], pos_d.partition_broadcast(32), [], [posi])
    ang = p.sbuf("ang", [96, S], F32)
    p.cp("dve", ang[64:96, :], posi[64:96, :], [posi], [ang])
    p.ts("dve", ang[64:96, :], ang[64:96, :], ctab[64:96, 0:1], None, ALU.mult, None, [ang, ctab], [ang])
    kf = p.sbuf("kf", [96, S], F32)
    sl = slice(64, 96)

    def sin_of(dst, shift):
        a = dst
        p.ts("dve", a[sl, :], ang[sl, :], shift, None, ALU.add, None, [ang], [dst])
        p.ts("dve", kf[sl, :], a[sl, :], 1.0 / (2 * math.pi), None, ALU.mult, None, [dst], [kf])
        p.cp("dve", posi[sl, :], kf[sl, :], [kf], [posi])
        p.cp("dve", kf[sl, :], posi[sl, :], [posi], [kf])
        p.stt("dve", a[sl, :], kf[sl, :], -2 * math.pi, a[sl, :], ALU.mult, ALU.add, [kf, dst], [dst])
        p.ts("dve", kf[sl, :], a[sl, :], math.pi, -2 * math.pi, ALU.is_gt, ALU.mult, [dst], [kf])
        p.tt("dve", a[sl, :], a[sl, :], kf[sl, :], ALU.add, [dst, kf], [dst])
        p.ts("dve", kf[sl, :], a[sl, :], -math.pi, 2 * math.pi, ALU.is_lt, ALU.mult, [dst], [kf])
        p.tt("dve", a[sl, :], a[sl, :], kf[sl, :], ALU.add, [dst, kf], [dst])
        p.act(a[sl, :], a[sl, :], AF.Sin, [dst], [dst])
    sin_of(cos2, math.pi / 2)
    sin_of(sin2, 0.0)
    p.ts("dve", sin2[sl, :], sin2[sl, :], ctab[sl, 1:2], None, ALU.mult, None, [sin2, ctab], [sin2])
    c.rope = p.dram("rope_tab", [2, 32, S], F32)
    p.dma(c.rope[0], cos2[sl, :], [cos2], [c.rope])
    p.dma(c.rope[1], sin2[sl, :], [sin2], [c.rope])
    p.release(mk)
    return c


def colvec(p, name, src_ap, n, dt=F32):
    t = p.sbuf(name, [128, n], dt)
    p.dma(t[:], src_ap.rearrange("(c p) -> p c", p=128), [], [t], allow_slow_non_contiguous=True)
    return t


def phase1(p, c, S, l, xin, W, sc):
    NT_ = S // 128
    NB = S // 512
    hT = p.sbuf("hT%d" % l, [128, 8, S], BF16)
    gbc = p.sbuf("gbc%d" % l, [128, D], F32)
    p.dma(gbc[:], W["norm_pre"][l:l + 1, :].partition_broadcast(128), [], [gbc])
    mk1 = p.mark()
    xt = [p.sbuf("xt%d_%d" % (l, i), [128, D], F32) for i in range(3)]
    hb = [p.sbuf("hb%d_%d" % (l, i), [128, D], BF16) for i in range(3)]
    junk = p.sbuf("junk%d" % l, [128, D], BF16)
    ss = [p.sbuf("ss%d_%d" % (l, i), [128, 1], F32) for i in range(3)]
    ptr = [p.psum("ptr%d_%d" % (l, i), [128, 8, 128], BF16) for i in range(3)]
    for i in range(NT_):
        s = i % 3
        p.dma(xt[s][:], xin[i * 128:(i + 1) * 128, :], [xin], [xt[s]])
        p.act(junk[:], xt[s][:], AF.Square, [xt[s]], [junk, ss[s]], accum_out=ss[s][:])
        p.act(ss[s][:], ss[s][:], AF.Ln, [ss[s], c.epsb], [ss[s]], bias=c.epsb[:], scale=1.0 / D)
        p.act(ss[s][:], ss[s][:], AF.Exp, [ss[s]], [ss[s]], scale=-0.5)
        p.stt("dve", hb[s][:], xt[s][:], ss[s][:, 0:1], gbc[:], ALU.mult, ALU.mult,
              [xt[s], ss[s], gbc], [hb[s]])
        for k in range(8):
            p.tr(ptr[s][:, k, :], hb[s][:, k * 128:(k + 1) * 128], c.idb[:], [hb[s], c.idb], [ptr[s]],
                 signal=(k == 7))
        p.cp("act", hT[:, :, i * 128:(i + 1) * 128], ptr[s][:], [ptr[s]], [hT])

    p.release(mk1)
    mk2 = p.mark()
    wa32 = p.sbuf("wa32_%d" % l, [128, 8, 384], F32)
    p.dma(wa32[:], W["w_in"][l, :, 0:384].rearrange("(c p) n -> p c n", p=128), [], [wa32])
    wa = p.sbuf("wa_%d" % l, [128, 8, 384], BF16)
    p.cp("dve", wa[:], wa32[:], [wa32], [wa])
    wk32 = p.sbuf("wk32_%d" % l, [128, 8, 192], F32)
    p.dma(wk32[:], W["wkr2"][l].rearrange("(c p) n -> p c n", p=128), [], [wk32])
    wkr = p.sbuf("wkr_%d" % l, [128, 8, 192], BF16)
    p.cp("act", wkr[:], wk32[:], [wk32], [wkr])
    wq32 = p.sbuf("wq32_%d" % l, [128, 2, 1536], F32)
    p.dma(wq32[:, :, 0:768], W["mla_w_uq"][l].rearrange("(c p) n -> p c n", p=128), [], [wq32])
    p.dma(wq32[:, :, 768:1536], W["wuq_sw"][l].rearrange("(c p) n -> p c n", p=128), [], [wq32])
    wq = p.sbuf("wq_%d" % l, [128, 2, 1536], BF16)
    p.cp("dve", wq[:], wq32[:], [wq32], [wq])
    wkv32 = p.sbuf("wkv32_%d" % l, [128, 1024], F32)
    p.dma(wkv32[:, 0:512], W["wuk"][l], [], [wkv32])
    p.dma(wkv32[:, 512:1024], W["wuv"][l], [], [wkv32])
    wkv = p.sbuf("wkv_%d" % l, [128, 1024], BF16)
    p.cp("act", wkv[:], wkv32[:], [wkv32], [wkv])
    cos2 = p.sbuf("cos2_%d" % l, [96, S], F32)
    sin2 = p.sbuf("sin2_%d" % l, [96, S], F32)
    p.dma(cos2[64:96, :], c.rope[0], [c.rope], [cos2])
    p.dma(sin2[64:96, :], c.rope[1], [c.rope], [sin2])
    gq = colvec(p, "gq%d" % l, W["mla_q_norm"][l], 2)
    gkv = colvec(p, "gkv%d" % l, W["mla_kv_norm"][l], 1)

    pq = [p.psum("pq%d_%d" % (l, i), [128, 512], F32) for i in range(2)]
    pkv = p.psum("pkv%d" % l, [128, 512], F32)
    pkr = p.psum("pkr%d" % l, [96, 2, 512], F32)
    pn = p.psum("pn%d" % l, [128, 512], F32)
    pw = [p.psum("pw%d_%d" % (l, i), [128, 512], F32) for i in range(2)]
    sq = p.sbuf("sq%d" % l, [128, 2, 512], BF16)
    rq = p.sbuf("rq%d" % l, [128, 512], F32)
    cqn = p.sbuf("cqn%d" % l, [128, 2, 512], BF16)
    ckvn = p.sbuf("ckvn%d" % l, [128, 512], BF16)
    qst = p.sbuf("qst%d" % l, [96, 8, 512], BF16)
    kst = p.sbuf("kst%d" % l, [128, 4, 512], BF16)
    krs = p.sbuf("krs%d" % l, [96, 512], BF16)
    t1s = [p.sbuf("t1_%d_%d" % (l, i), [96, 512], F32) for i in range(2)]
    t2s = [p.sbuf("t2_%d_%d" % (l, i), [96, 512], F32) for i in range(2)]
    t1, t2 = t1s[0], t2s[0]
    vst = p.sbuf("vst%d" % l, [128, 4, 8, 65], BF16)
    p.memset("pool", vst[:], 1.0, [vst])
    rs = slice(64, 96)
    for tb in range(NB):
        ts_ = slice(tb * 512, (tb + 1) * 512)
        for cc in range(2):
            for k in range(8):
                p.mm(pq[cc][:], wa[:, k, cc * 128:(cc + 1) * 128], hT[:, k, ts_], k == 0, k == 7, [wa, hT], [pq[cc]])
        for k in range(8):
            p.mm(pkv[:], wa[:, k, 256:384], hT[:, k, ts_], k == 0, k == 7, [wa, hT], [pkv])
        for v in range(2):
            for k in range(8):
                p.mm(pkr[:, v, :], wkr[:, k, v * 96:(v + 1) * 96], hT[:, k, ts_], k == 0, k == 7, [wkr, hT], [pkr])
        for cc in range(2):
            p.act(sq[:, cc, :], pq[cc][:], AF.Square, [pq[cc]], [sq])
        for cc in range(2):
            p.mm(pn[:], c.onesb[:], sq[:, cc, :], cc == 0, cc == 1, [c.onesb, sq], [pn])
        p.act(rq[:], pn[:], AF.Ln, [pn, c.epsb], [rq], bias=c.epsb[:], scale=1.0 / 256)
        p.act(rq[:], rq[:], AF.Exp, [rq, c.lnsc], [rq], bias=c.lnsc[:], scale=-0.5)
        for cc in range(2):
            p.stt("dve", cqn[:, cc, :], pq[cc][:], gq[:, cc:cc + 1], rq[:], ALU.mult, ALU.mult,
                  [pq[cc], gq, rq], [cqn])
        p.act(sq[:, 0, :], pkv[:], AF.Square, [pkv], [sq])
        p.mm(pn[:], c.onesb[:], sq[:, 0, :], True, True, [c.onesb, sq], [pn])
        p.act(rq[:], pn[:], AF.Ln, [pn, c.epsb], [rq], bias=c.epsb[:], scale=1.0 / 128)
        p.act(rq[:], rq[:], AF.Exp, [rq], [rq], scale=-0.5)
        p.stt("dve", ckvn[:], pkv[:], gkv[:, 0:1], rq[:], ALU.mult, ALU.mult, [pkv, gkv, rq], [ckvn])
        p.tt("dve", t1[rs, :], pkr[rs, 0, :], cos2[rs, ts_], ALU.mult, [pkr, cos2], [t1])
        p.tt("dve", t2[rs, :], pkr[rs, 1, :], sin2[rs, ts_], ALU.mult, [pkr, sin2], [t2])
        p.tt("pool", krs[rs, :], t1[rs, :], t2[rs, :], ALU.add, [t1, t2], [krs])
        for h in range(8):
            p.dma(sc["KT"][h, 64:96, ts_], krs[rs, :], [krs], [])
        for h in range(8):
            a, b = pw[0], pw[1]
            t1, t2 = t1s[h % 2], t2s[h % 2]
            for k in range(2):
                p.mm(a[0:96, :], wq[:, k, h * 96:(h + 1) * 96], cqn[:, k, :], k == 0, k == 1, [wq, cqn], [a])
            for k in range(2):
                p.mm(b[0:96, :], wq[:, k, 768 + h * 96:768 + (h + 1) * 96], cqn[:, k, :], k == 0, k == 1, [wq, cqn], [b])
            p.cp("act", qst[0:64, h, :], a[0:64, :], [a], [qst])
            p.tt("dve", t1[rs, :], a[rs, :], cos2[rs, ts_], ALU.mult, [a, cos2], [t1])
            p.tt("dve", t2[rs, :], b[rs, :], sin2[rs, ts_], ALU.mult, [b, sin2], [t2])
            p.tt("pool", qst[rs, h, :], t1[rs, :], t2[rs, :], ALU.add, [t1, t2], [qst])
        p.dma(sc["QT"][:, :, ts_].rearrange("h p t -> p h t"), qst[:], [qst], [])
        for j in range(4):
            a = pw[j % 2]
            p.mm(a[:], wkv[:, j * 128:(j + 1) * 128], ckvn[:], True, True, [wkv, ckvn], [a])
            p.cp("act", kst[:, j, :], a[:], [a], [kst])
        for two in range(2):
            p.dma(sc["KT"][:, 0:64, ts_].rearrange("(j two) p t -> two p j t", two=2)[two],
                  kst[two * 64:(two + 1) * 64, :, :], [kst], [])
        for tt_ in range(4):
            a = pw[tt_ % 2]
            p.mm(a[:], ckvn[:, tt_ * 128:(tt_ + 1) * 128], wkv[:, 512:1024], True, True, [ckvn, wkv], [a])
            p.cp("act", vst[:, tt_, :, 0:64], a[:].rearrange("p (h v) -> p h v", v=64), [a], [vst])
        p.dma(sc["VA"][ts_, :].rearrange("(tt p) n -> p tt n", p=128),
              vst[:].rearrange("p t h v -> p t (h v)"), [vst], [])

    p.release(mk2)
    mk3 = p.mark()
    pw = [p.psum("pwb%d_%d" % (l, i), [128, 512], F32) for i in range(2)]
    chunks = []
    for i in range(13):
        chunks.append((C_U + i * 128, 128, "mix", "U", i * 128))
    for i in range(2):
        chunks.append((C_GQ + i * 128, 128, "copy32", "GQ", i * 128))
    for i in range(2):
        chunks.append((C_GK + i * 128, 128, "copy32", "GK", i * 128))
    chunks.append((C_GL, 16, "copy32", "GL", 0))
    for i in range(12):
        chunks.append((C_BG + i * 128, 128, "silu", "BG", i * 128))
    for i in range(24):
        chunks.append((C_MG + i * 128, 128, "sigm", "MG", i * 128))
    chunks.sort(key=lambda x: {"mix": 0, "copy32": 0, "silu": 1, "sigm": 2}[x[2]])
    pw = pw + [p.psum("pwd%d_%d" % (l, i), [128, 512], F32) for i in range(2)]
    mu13 = colvec(p, "mu13_%d" % l, W["rwkv_mu"][l], 13)
    wc32 = [p.sbuf("wc32_%d_%d" % (l, i), [128, 8, 128], F32) for i in range(3)]
    wcb = [p.sbuf("wcb_%d_%d" % (l, i), [128, 8, 128], BF16) for i in range(3)]
    st32 = [p.sbuf("st32_%d_%d" % (l, i), [128, 512], F32) for i in range(4)]
    st16 = [p.sbuf("st16_%d_%d" % (l, i), [128, 512], BF16) for i in range(4)]
    stgm = [p.sbuf("stgm_%d_%d" % (l, i), [128, 513], F32) for i in range(3)]
    dmx = [p.sbuf("dmx_%d_%d" % (l, i), [128, 512], F32) for i in range(2)]
    wg32 = p.sbuf("wg32_%d" % l, [128, 8, 512], F32)
    p.dma(wg32[:], W["w_in"][l, :, C_GV:C_GV + 512].rearrange("(c p) n -> p c n", p=128), [], [wg32])
    wg = p.sbuf("wg_%d" % l, [128, 8, 512], BF16)
    p.cp("pool", wg[:], wg32[:], [wg32], [wg])
    n = 0
    nm = 0
    for ci, (col0, m, kind, dest, row0) in enumerate(chunks):
        s = ci % 3
        p.dma(wc32[s][:, :, 0:m], W["w_in"][l, :, col0:col0 + m].rearrange("(c p) n -> p c n", p=128), [], [wc32[s]])
        p.cp("pool", wcb[s][:, :, 0:m], wc32[s][:, :, 0:m], [wc32[s]], [wcb[s]])
        prev = None
        for tb in range(NB):
            ts_ = slice(tb * 512, (tb + 1) * 512)
            a = pw[n % 4]
            for k in range(8):
                p.mm(a[0:m, :], wcb[s][:, k, 0:m], hT[:, k, ts_], k == 0, k == 7, [wcb[s], hT], [a])
            if kind == "mix":
                sg_ = stgm[nm % 3]
                dd = dmx[nm % 2]
                nm += 1
                o = st32[n % 4]
                if prev is None:
                    p.memset("pool", sg_[:, 0:1], 0.0, [sg_])
                else:
                    p.cp("pool", sg_[:, 0:1], prev[:, 512:513], [prev], [sg_])
                p.cp("act", sg_[:, 1:513], a[:], [a], [sg_])
                p.tt("dve", dd[:], sg_[:, 0:512], sg_[:, 1:513], ALU.subtract, [sg_], [dd])
                mi = row0 // 128
                p.stt("dve", o[:], dd[:], mu13[:, mi:mi + 1], sg_[:, 1:513], ALU.mult, ALU.add, [dd, mu13, sg_], [o])
                prev = sg_
            elif kind == "copy32":
                o = st32[n % 4]
                if n % 2 == 0:
                    p.cp("act", o[0:m, :], a[0:m, :], [a], [o])
                else:
                    p.cp("dve", o[0:m, :], a[0:m, :], [a], [o])
            else:
                o = st16[n % 4]
                p.act(o[0:m, :], a[0:m, :], AF.Silu if kind == "silu" else AF.Sigmoid, [a], [o])
            p.dma(sc[dest][row0:row0 + m, ts_], o[0:m, :], [o], [])
            n += 1
    for i in range(NT_):
        a = pw[n % 4]
        o = st16[n % 4]
        n += 1
        for k in range(8):
            p.mm(a[:], hT[:, k, i * 128:(i + 1) * 128], wg[:, k, :], k == 0, k == 7, [hT, wg], [a])
        p.cp("act", o[:], a[:], [a], [o])
        p.dma(sc["GV"][i * 128:(i + 1) * 128, :], o[:], [o], [])
    p.release(mk3)


def phase_mla(p, c, S, l, sc):
    NB = S // 512
    NT_ = S // 128
    kt = p.sbuf("kt", [96, 8, S], BF16)
    for h in range(8):
        p.dma(kt[:, h, :], sc["KT"][h], [sc["KT"]], [kt])
    va = p.sbuf("va", [128, NT_, 520], BF16)
    p.dma(va[:], sc["VA"].t.rearrange("(n p) f -> p n f", p=128), [sc["VA"]], [va])
    qts = [p.sbuf("qt%d" % i, [96, 8, 512], BF16) for i in range(2)]
    bgs = [p.sbuf("bg%d" % i, [128, 4, 512], BF16) for i in range(2)]
    pts = [p.sbuf("pt%d" % i, [128, 512], BF16) for i in range(4)]
    ytm = p.sbuf("ytm", [128, 4, 512], F32)
    ys = [p.sbuf("ysm%d" % i, [128, 4, 512], BF16) for i in range(2)]
    recs = [p.sbuf("rec%d" % i, [128, 1], F32) for i in range(8)]
    pst = [p.psum("pst%d" % i, [128, 512], F32) for i in range(3)]
    acc = [p.psum("acc%d" % i, [128, 512], F32) for i in range(4)]
    pT = p.psum("pT", [128, 4, 128], F32)
    n = 0
    for qb in range(NB):
        qt = qts[qb % 2]
        bg = bgs[qb % 2]
        cols = slice(qb * 512, (qb + 1) * 512)
        p.dma(qt[:], sc["QT"][:, :, cols].rearrange("h p t -> p h t"), [sc["QT"]], [qt])
        p.dma(bg[:], sc["BG"][0:512, cols].rearrange("(c p) t -> p c t", p=128), [sc["BG"]], [bg])
        for h in range(8):
            nkt = 4 * (qb + 1)
            for kti in range(nkt):
                j = kti - 4 * qb
                q0 = 128 * j if j > 0 else 0
                N = 512 - q0
                st = pst[n % 3]
                pt = pts[n % 4]
                p.mm(st[:, 0:N], kt[:, h, kti * 128:(kti + 1) * 128], qt[:, h, q0:512], True, True, [kt, qt], [st])
                p.act(pt[:, 0:N], st[:, 0:N], AF.Exp, [st], [pt])
                if j >= 0:
                    p.op("pool", lambda e, pt=pt: e.affine_select(pt[:, 0:128], pt[:, 0:128], [[1, 128]], ALU.is_ge, 0.0,
                                                                  base=0, channel_multiplier=-1), [pt], [pt])
                for qs in range(q0 // 128, 4):
                    p.mm(acc[qs][:, 0:65], pt[:, qs * 128 - q0:(qs + 1) * 128 - q0], va[:, kti, h * 65:(h + 1) * 65],
                         kti == 0, kti == 4 * qb + qs, [pt, va], [acc[qs]])
                n += 1
            for qs in range(4):
                rc = recs[(h % 2) * 4 + qs]
                p.op("dve", lambda e, qs=qs, rc=rc: e.reciprocal(rc[:], acc[qs][:, 64:65]), [acc[qs]], [rc], dur=200)
                p.ts("dve", ytm[:, qs, h * 64:(h + 1) * 64], acc[qs][:, 0:64], rc[:, 0:1], None, ALU.mult, None,
                     [acc[qs], rc], [ytm])
        yo = ys[qb % 2]
        for fc in range(4):
            for qs in range(4):
                p.tr(pT[:, qs, :], ytm[:, qs, fc * 128:(fc + 1) * 128], c.idf[:], [ytm, c.idf], [pT], signal=(qs == 3))
            p.tt("dve", yo[:, fc, :], pT[:].rearrange("p a b -> p (a b)"), bg[:, fc, :], ALU.mult, [pT, bg], [yo])
        p.dma(sc["YS"][0:512, cols].rearrange("(c p) t -> p c t", p=128), yo[:], [yo], [])


def phase_gla(p, c, S, l, W, sc, ysblk=None):
    NB = S // 512
    H = 4
    aup = p.sbuf("g_aup", [16, 256], F32)
    p.dma(aup[:], W["gla_a_up"][l], [], [aup])
    nab = p.sbuf("g_nab", [64, H], F32)
    p.dma(nab[:], W["gla_a_b"][l].rearrange("(h d) -> d h", d=64), [], [nab], allow_slow_non_contiguous=True)
    p.ts("dve", nab[:], nab[:], -1.0, None, ALU.mult, None, [nab], [nab])
    gn = colvec(p, "g_gn", W["gla_norm"][l], 4)
    one1 = p.sbuf("g_one1", [128, 1], F32)
    p.memset("pool", one1[:], 1.0, [one1])
    maskr = p.sbuf("g_maskr", [64, 512], F32)
    p.memset("pool", maskr[:], 1.0, [maskr])
    p.memset("pool", maskr[:].rearrange("p (c t) -> p c t", t=64)[:, :, 0:1], 0.0, [maskr])
    msk = p.sbuf("g_msk", [64, H, 64], F32)
    p.memset("pool", msk[:], 1.0, [msk])
    for h in range(H):
        p.op("pool", lambda e, h=h: e.affine_select(msk[:, h, :], msk[:, h, :], [[1, 64]], ALU.is_ge, 0.0,
                                                    base=0, channel_multiplier=-1), [msk], [msk])
    st = p.sbuf("g_st", [64, H, 128], F32)
    stb = p.sbuf("g_stb", [64, H, 128], BF16)
    st2 = p.sbuf("g_st2", [64, H, 128], F32)
    p.memset("pool", st[:], 0.0, [st])
    p.memset("pool", stb[:], 0.0, [stb])
    gq1 = p.sbuf("g_q", [64, H, 512], F32)
    gk1 = p.sbuf("g_k", [64, H, 512], F32)
    gq, gk = [gq1, gq1], [gk1, gk1]
    gl1 = p.sbuf("g_l", [16, 512], F32)
    gl = [gl1, gl1]
    gv = [p.sbuf("g_v%d" % i, [64, 8, 512], BF16) for i in range(2)]
    bg1 = p.sbuf("g_bg", [128, H, 512], BF16)
    bg = [bg1, bg1]
    e1 = p.sbuf("g_e1", [64, H, 512], F32)
    cs = p.sbuf("g_cs", [64, H, 512], F32)
    eb = p.sbuf("g_eb", [64, H, 512], F32)
    enb = e1
    qe = p.sbuf("g_qe", [64, H, 512], BF16)
    ke = p.sbuf("g_ke", [64, H, 512], BF16)
    att = [p.sbuf("g_att%d" % i, [64, H, 64], BF16) for i in range(2)]
    ketm = [p.sbuf("g_ketm%d" % i, [64, H, 64], BF16) for i in range(2)]
    sq = p.sbuf("g_sq", [64, H, 128], F32)
    ssg = p.sbuf("g_ss", [64, H], F32)
    yn = [p.sbuf("g_yn%d" % i, [64, H, 128], F32) for i in range(2)]
    yT = p.sbuf("g_yT", [128, H, 512], F32)
    ysg1 = p.sbuf("g_ys", [128, H, 512], BF16)
    ysg = [ysg1, ysg1]
    gbank = [p.psum("g_bank%d" % i, [128, 512], F32) for i in range(3)]
    gbi = [0]

    def GB():
        t = gbank[gbi[0] % 3]
        gbi[0] += 1
        return t
    for tb in range(NB):
        s_ = tb % 2
        cols = slice(tb * 512, (tb + 1) * 512)
        p.dma(gq[s_][:], sc["GQ"][:, cols].rearrange("(h d) t -> d h t", d=64), [sc["GQ"]], [gq[s_]])
        p.dma(gk[s_][:], sc["GK"][:, cols].rearrange("(h d) t -> d h t", d=64), [sc["GK"]], [gk[s_]])
        p.dma(gl[s_][:], sc["GL"][:, cols], [sc["GL"]], [gl[s_]])
        p.dma(gv[s_][:], sc["GV"][cols, :].rearrange("(c p) f -> p c f", p=64), [sc["GV"]], [gv[s_]])
        p.dma(bg[s_][:], sc["BG"][1024:1536, cols].rearrange("(c p) t -> p c t", p=128), [sc["BG"]], [bg[s_]])
        for h in range(H):
            z = GB()
            p.mm(z[0:64, :], aup[:, h * 64:(h + 1) * 64], gl[s_][:], True, True, [aup, gl[s_]], [z])
            p.act(e1[:, h, :], z[0:64, :], AF.Exp, [z, nab], [e1], bias=nab[:, h:h + 1], scale=-1.0)
        p.act(e1[:], e1[:], AF.Ln, [e1, one1], [e1], bias=one1[0:64, :], scale=1.0)
        for h in range(H):
            p.op("dve", lambda e, h=h: e.tensor_tensor_scan(cs[:, h, :], maskr[:], e1[:, h, :], 0.0, ALU.mult, ALU.add),
                 [maskr, e1], [cs])
        p.act(eb[:], cs[:], AF.Exp, [cs], [eb], scale=-1.0 / 16)
        p.act(enb[:], cs[:], AF.Exp, [cs], [enb], scale=1.0 / 16)
        p.stt("dve", qe[:], gq[s_][:], 0.125, eb[:], ALU.mult, ALU.mult, [gq[s_], eb], [qe])
        p.tt("pool", ke[:], gk[s_][:], enb[:], ALU.mult, [gk[s_], enb], [ke])
        for cch in range(8):
            cc = slice(cch * 64, (cch + 1) * 64)
            a_ = att[cch % 2]
            kt_ = ketm[cch % 2]
            y_ = yn[cch % 2]
            b1 = GB()
            pat = b1[0:64, 0:256].rearrange("p (h i) -> p h i", h=H)
            for h in range(H):
                p.mm(pat[:, h, :], ke[:, h, cc], qe[:, h, cc], True, True, [ke, qe], [b1])
            p.tt("dve", a_[:], pat, msk[:], ALU.mult, [b1, msk], [a_])
            b2 = GB()
            pkt = b2[0:64, 0:128].bitcast(BF16).rearrange("p (h i) -> p h i", h=H)
            for h in range(H):
                p.tr(pkt[:, h, :], ke[:, h, cc], c.idb[0:64, 0:64], [ke, c.idb], [b2])
            p.cp("act", kt_[:], pkt, [b2], [kt_])
            b3 = GB()
            po = b3[0:64, :].rearrange("p (h v) -> p h v", h=H)
            for h in range(H):
                p.mm(po[:, h, :], a_[:, h, :], gv[s_][:, cch, h * 128:(h + 1) * 128], True, False, [a_, gv[s_]], [b3])
                p.mm(po[:, h, :], qe[:, h, cc], stb[:, h, :], False, True, [qe, stb], [b3])
            b4 = GB()
            pst = b4[0:64, :].rearrange("p (h v) -> p h v", h=H)
            for h in range(H):
                p.mm(pst[:, h, :], kt_[:, h, :], gv[s_][:, cch, h * 128:(h + 1) * 128], True, True, [kt_, gv[s_]], [b4])
            p.tt("dve", st2[:], st[:], pst, ALU.add, [st, b4], [st2])
            ebl = eb[:, :, cch * 64 + 63:cch * 64 + 64].broadcast_to([64, H, 128])
            p.tt("dve", st[:], st2[:], ebl, ALU.mult, [st2, eb], [st])
            p.cp("pool", stb[:], st[:], [st], [stb])
            p.act(sq[:], po, AF.Square, [b3], [sq])
            p.op("dve", lambda e: e.tensor_reduce(ssg[:], sq[:], AX.X, ALU.add), [sq], [ssg], dur=700)
            p.act(ssg[:], ssg[:], AF.Ln, [ssg, c.epsb], [ssg], bias=c.epsb[0:64, :], scale=1.0 / 128)
            p.act(ssg[:], ssg[:], AF.Exp, [ssg], [ssg], scale=-0.5)
            p.tt("dve", y_[:], po, ssg[:].unsqueeze(2).broadcast_to([64, H, 128]), ALU.mult, [b3, ssg], [y_])
            b5 = GB()
            pyT = b5[:, 0:256].rearrange("p (h t) -> p h t", h=H)
            for h in range(H):
                p.tr(pyT[:, h, :], y_[:, h, :], c.idf[0:64, 0:64], [y_, c.idf], [b5])
            p.cp("act", yT[:, :, cc], pyT, [b5], [yT])
        o_ = ysg[s_]
        for h in range(H):
            p.stt("dve", o_[:, h, :], yT[:, h, :], gn[:, h:h + 1], bg[s_][:, h, :], ALU.mult, ALU.mult, [yT, gn, bg[s_]], [o_])
        p.dma(sc["YS"][1024:1536, cols].rearrange("(c p) t -> p c t", p=128), o_[:], [o_],
              [] if ysblk is None else [ysblk[tb]])


def phase_rwkv(p, c, S, l, W, sc, TB=128):
    NB = S // TB
    NCH = TB // 64
    H = 8
    C0 = math.exp(-0.5)
    vec = lambda name, ap: _hv(p, name, ap)
    mu_r = vec("r_mur", W["rwkv_mu"][l, 0:512])
    mu_k = vec("r_muk", W["rwkv_mu"][l, 512:1024])
    mu_v = vec("r_muv", W["rwkv_mu"][l, 1024:1536])
    mu_w = p.sbuf("r_muw", [64, 2], F32)
    p.dma(mu_w[:], W["rwkv_mu"][l, 1536:1664].rearrange("(c p) -> p c", p=64), [], [mu_w], allow_slow_non_contiguous=True)
    w0 = vec("r_w0", W["rwkv_w0"][l])
    a0 = vec("r_a0", W["rwkv_a0"][l])
    k_k = vec("r_kk", W["rwkv_k_k"][l])
    k_a = vec("r_ka", W["rwkv_k_a"][l])
    r_k = vec("r_rk", W["rwkv_r_k"][l])
    lnw = vec("r_lnw", W["rwkv_ln_w"][l])
    lnb = vec("r_lnb", W["rwkv_ln_b"][l])
    wup = p.sbuf("r_wup", [64, 512], F32)
    p.dma(wup[:], W["rwkv_w_up"][l], [], [wup])
    aup = p.sbuf("r_aup", [64, 512], F32)
    p.dma(aup[:], W["rwkv_a_up"][l], [], [aup])
    ones = p.sbuf("r_ones", [64, 64], F32)
    p.memset("pool", ones[:], 1.0, [ones])
    gne = p.sbuf("r_gne", [64, 1], F32)
    p.memset("pool", gne[:], 64e-5, [gne])
    maskr = p.sbuf("r_maskr", [64, TB], F32)
    p.memset("pool", maskr[:], 1.0, [maskr])
    p.memset("pool", maskr[:].rearrange("p (c t) -> p c t", t=64)[:, :, 0:1], 0.0, [maskr])

    def mk_mask(name, pat, cm, op, dt=F32):
        m = p.sbuf(name, [64, H, 64], F32)
        p.memset("pool", m[:], 1.0, [m])
        for h in range(H):
            p.op("pool", lambda e, h=h: e.affine_select(m[:, h, :], m[:, h, :], pat, op, 0.0, base=0, channel_multiplier=cm), [m], [m])
        if dt == F32:
            return m
        mb = p.sbuf(name + "b", [64, H, 64], dt)
        p.cp("pool", mb[:], m[:], [m], [mb])
        return mb
    mL = mk_mask("r_mL", [[-1, 64]], 1, ALU.is_gt)
    mSU = mk_mask("r_mSU", [[1, 64]], -1, ALU.is_gt)
    mU = mk_mask("r_mU", [[1, 64]], -1, ALU.is_ge)
    idh = mk_mask("r_idh", [[1, 64]], -1, ALU.is_equal, BF16)
    id64 = c.idb[0:64, 0:64]

    def A3(name, dt=F32, n=TB):
        return p.sbuf(name, [64, H, n], dt)
    r_, k_, vf = A3("r_r"), A3("r_k"), A3("r_v")
    lwa = p.sbuf("r_lwa", [64, 2, TB], F32)
    tmp, tmp2 = A3("r_tmp"), A3("r_tmp2")
    sg, a_, cs = A3("r_sg"), A3("r_a"), A3("r_cs")
    gi, gp = A3("r_gi"), A3("r_gp")
    kkn, kp = A3("r_kkn"), A3("r_kp")
    D2 = lambda name, dt: [A3(name + "0", dt), A3(name + "1", dt)]
    vb2, Ab2, Bb2, Kb2, Rb2 = D2("r_vb", BF16), D2("r_Ab", BF16), D2("r_Bb", BF16), D2("r_Kb", BF16), D2("r_Rb", BF16)
    g2, bon2, yfm2 = D2("r_g", F32), D2("r_bon", F32), D2("r_yfm", F32)
    bgt2, yso2 = D2("r_bgt", BF16), D2("r_yso", BF16)
    T0 = p.sbuf("r_T0", [64, H, 64], F32)
    p.memset("pool", T0[:], 0.0, [T0])
    T0b = p.sbuf("r_T0b", [64, H, 64], BF16)
    p.memset("pool", T0b[:], 0.0, [T0b])
    T1 = p.sbuf("r_T1", [64, H, 64], F32)

    def S3(name, dt=BF16):
        return p.sbuf(name, [64, H, 64], dt)
    S2 = lambda name: [S3(name + "0"), S3(name + "1")]
    P_ = S2("r_P")
    PT_ = S2("r_PT")
    NTa = [S2("r_NTa"), S2("r_NTb")]
    LakT2, ArbT2, ArkT2 = S2("r_LakT"), S2("r_ArbT"), S2("r_ArkT")
    Vtm2, Btm2, Ktm2 = S2("r_Vtm"), S2("r_Btm"), S2("r_Ktm")
    X_, U_ = S3("r_X"), S3("r_U")
    yc, ysq, ynm = S3("r_yc", F32), S3("r_ysq", F32), S3("r_ynm", F32)
    mean = p.sbuf("r_mean", [64, H], F32)
    var = p.sbuf("r_var", [64, H], F32)
    pz = p.psum("r_pz", [64, 2, TB], F32)
    pg = [p.psum("r_pg%d" % i, [64, H, 64], F32) for i in range(5)]
    pb = [p.psum("r_pb%d" % i, [64, H, 64], BF16) for i in range(2)]
    pgi = [0, 0]

    def PG():
        t = pg[pgi[0] % 5]
        pgi[0] += 1
        return t

    def PB():
        t = pb[pgi[1] % 2]
        pgi[1] += 1
        return t

    def bc(t2):
        return t2[:].unsqueeze(2).broadcast_to([64, H, TB])

    def prep(tb):
        t0 = tb * TB
        cols = slice(t0, t0 + TB)
        bp = tb % 2
        vb, Ab, Bb, Kb, Rb, g_, bon, bgt = vb2[bp], Ab2[bp], Bb2[bp], Kb2[bp], Rb2[bp], g2[bp], bon2[bp], bgt2[bp]
        for (dst, r0) in ((r_, 0), (k_, 512), (vf, 1024)):
            p.dma(dst[:], sc["U"][r0:r0 + 512, cols].rearrange("(h d) t -> d h t", d=64), [sc["U"]], [dst])
        p.dma(lwa[:], sc["U"][1536:1664, cols].rearrange("(c p) t -> p c t", p=64), [sc["U"]], [lwa])
        p.dma(bgt[:], sc["BG"][512:1024, cols].rearrange("(h d) t -> d h t", d=64), [sc["BG"]], [bgt])
        p.cp("act", vb[:], vf[:], [vf], [vb])
        p.act(lwa[:, 0, :], lwa[:, 0, :], AF.Tanh, [lwa], [lwa])
        for h in range(H):
            p.mm(pz[:, 0, :], wup[:, h * 64:(h + 1) * 64], lwa[:, 0, :], True, True, [wup, lwa], [pz])
            p.mm(pz[:, 1, :], aup[:, h * 64:(h + 1) * 64], lwa[:, 1, :], True, True, [aup, lwa], [pz])
            p.act(sg[:, h, :], pz[:, 0, :], AF.Sigmoid, [pz, w0], [sg], bias=w0[:, h:h + 1], scale=1.0)
            p.act(a_[:, h, :], pz[:, 1, :], AF.Sigmoid, [pz, a0], [a_], bias=a0[:, h:h + 1], scale=1.0)
        for h in range(H):
            p.op("dve", lambda e, h=h: e.tensor_tensor_scan(cs[:, h, :], maskr[:], sg[:, h, :], 0.0, ALU.mult, ALU.add),
                 [maskr, sg], [cs], dur=100 + 2.1 * TB)
        p.act(g_[:], cs[:], AF.Exp, [cs], [g_], scale=-C0)
        p.act(gi[:], cs[:], AF.Exp, [cs], [gi], scale=C0)
        p.tt("pool", tmp[:], cs[:], sg[:], ALU.subtract, [cs, sg], [tmp])
        p.act(gp[:], tmp[:], AF.Exp, [tmp], [gp], scale=-C0)
        p.tt("dve", kkn[:], k_[:], bc(k_k), ALU.mult, [k_, k_k], [kkn])
        p.act(tmp2[:], kkn[:], AF.Square, [kkn], [tmp2])
        for hp in range(4):
            for j in range(2):
                p.mm(pz[:, j, :], ones[:], tmp2[:, hp * 2 + j, :], True, True, [ones, tmp2], [pz])
            p.ts("dve", sg[:, hp * 2:hp * 2 + 2, :], pz[:], 1e-24, None, ALU.max, None, [pz], [sg])
        p.act(sg[:], sg[:], AF.Ln, [sg], [sg])
        p.act(sg[:], sg[:], AF.Exp, [sg], [sg], scale=-0.5)
        p.tt("pool", kkn[:], kkn[:], sg[:], ALU.mult, [kkn, sg], [kkn])
        p.stt("dve", tmp[:], a_[:], -1.0, bc(k_a), ALU.add, ALU.mult, [a_, k_a], [tmp])
        p.stt("dve", kp[:], tmp[:], 1.0, k_[:], ALU.add, ALU.mult, [tmp, k_], [kp])
        p.stt("dve", Ab[:], kkn[:], -1.0, gp[:], ALU.mult, ALU.mult, [kkn, gp], [Ab])
        p.tt("pool", tmp2[:], kkn[:], a_[:], ALU.mult, [kkn, a_], [tmp2])
        p.tt("dve", Bb[:], tmp2[:], gi[:], ALU.mult, [tmp2, gi], [Bb])
        p.tt("pool", Kb[:], kp[:], gi[:], ALU.mult, [kp, gi], [Kb])
        p.tt("dve", Rb[:], r_[:], g_[:], ALU.mult, [r_, g_], [Rb])
        p.tt("pool", tmp[:], r_[:], kp[:], ALU.mult, [r_, kp], [tmp])
        p.tt("pool", tmp[:], tmp[:], bc(r_k), ALU.mult, [tmp, r_k], [tmp])
        for hp in range(4):
            for j in range(2):
                p.mm(pz[:, j, :], ones[:], tmp[:, hp * 2 + j, :], True, True, [ones, tmp], [pz])
            p.tt("dve", bon[:, hp * 2:hp * 2 + 2, :], pz[:], vf[:, hp * 2:hp * 2 + 2, :], ALU.mult, [pz, vf], [bon])

    def chunk(tb, cch):
        bp = tb % 2
        g = tb * NCH + cch
        cp_ = g % 2
        cc = slice(cch * 64, (cch + 1) * 64)
        vb, Ab, Bb, Kb, Rb, g_, yfm = vb2[bp], Ab2[bp], Bb2[bp], Kb2[bp], Rb2[bp], g2[bp], yfm2[bp]
        LakT, ArbT, ArkT, Vtm, Btm, Ktm = LakT2[cp_], ArbT2[cp_], ArkT2[cp_], Vtm2[cp_], Btm2[cp_], Ktm2[cp_]
        NTp = NTa[cp_]

        def scores(lh, rh, mask, dst, eng):
            ps = PG()
            for h in range(H):
                p.mm(ps[:, h, :], lh[:, h, cc], rh[:, h, cc], True, True, [lh, rh], [ps])
            p.tt(eng, dst[:], ps[:], mask[:], ALU.mult, [ps, mask], [dst])
        scores(Ab, Bb, mL, P_[0], "dve")
        scores(Bb, Ab, mSU, PT_[0], "dve")
        p.tt("pool", NTp[0][:], PT_[0][:], idh[:], ALU.add, [PT_[0], idh], [NTp[0]])
        scores(Kb, Ab, mSU, LakT, "dve")
        scores(Bb, Rb, mU, ArbT, "dve")
        scores(Kb, Rb, mU, ArkT, "dve")

        def transp(src, dst):
            ps = PB()
            for h in range(H):
                p.tr(ps[:, h, :], src[:, h, cc], id64, [src, c.idb], [ps])
            p.cp("act", dst[:], ps[:], [ps], [dst])
        transp(vb, Vtm)
        transp(Bb, Btm)
        transp(Kb, Ktm)
        cur = 0
        nti = 0
        for step in range(5):
            Pn, PTn = P_[1 - cur], PT_[1 - cur]
            ps = PG()
            for h in range(H):
                p.mm(ps[:, h, :], PT_[cur][:, h, :], P_[cur][:, h, :], True, True, [PT_[cur], P_[cur]], [ps])
            p.cp("act", Pn[:], ps[:], [ps], [Pn])
            if step < 4:
                ps2 = PG()
                for h in range(H):
                    p.mm(ps2[:, h, :], P_[cur][:, h, :], PT_[cur][:, h, :], True, True, [PT_[cur], P_[cur]], [ps2])
                p.cp("dve", PTn[:], ps2[:], [ps2], [PTn])
            ps3 = PG()
            NTo, NTn = NTp[nti], NTp[1 - nti]
            for h in range(H):
                p.mm(ps3[:, h, :], id64, NTo[:, h, :], True, False, [c.idb, NTo], [ps3])
                p.mm(ps3[:, h, :], Pn[:, h, :], NTo[:, h, :], False, True, [Pn, NTo], [ps3])
            p.cp("act", NTn[:], ps3[:], [ps3], [NTn])
            nti = 1 - nti
            cur = 1 - cur
        NT = NTp[nti]
        ps = PG()
        for h in range(H):
            p.mm(ps[:, h, :], Ab[:, h, cc], T0b[:, h, :], True, False, [Ab, T0b], [ps])
            p.mm(ps[:, h, :], LakT[:, h, :], Vtm[:, h, :], False, True, [LakT, Vtm], [ps])
        p.cp("act", X_[:], ps[:], [ps], [X_])
        ps = PG()
        for h in range(H):
            p.mm(ps[:, h, :], NT[:, h, :], X_[:, h, :], True, True, [NT, X_], [ps])
        p.cp("act", U_[:], ps[:], [ps], [U_])
        py = PG()
        for h in range(H):
            p.mm(py[:, h, :], Rb[:, h, cc], T0b[:, h, :], True, False, [Rb, T0b], [py])
            p.mm(py[:, h, :], ArbT[:, h, :], U_[:, h, :], False, False, [ArbT, U_], [py])
            p.mm(py[:, h, :], ArkT[:, h, :], Vtm[:, h, :], False, True, [ArkT, Vtm], [py])
        ps = PG()
        for h in range(H):
            p.mm(ps[:, h, :], Btm[:, h, :], U_[:, h, :], True, False, [Btm, U_], [ps])
            p.mm(ps[:, h, :], Ktm[:, h, :], Vtm[:, h, :], False, True, [Ktm, Vtm], [ps])
        p.tt("dve", T1[:], T0[:], ps[:], ALU.add, [T0, ps], [T1])
        gC = g_[:, :, cch * 64 + 63:cch * 64 + 64].broadcast_to([64, H, 64])
        p.tt("dve", T0[:], T1[:], gC, ALU.mult, [T1, g_], [T0])
        p.cp("pool", T0b[:], T0[:], [T0], [T0b])
        p.op("dve", lambda e, py=py: e.tensor_reduce(mean[:], py[:], AX.X, ALU.add), [py], [mean], dur=700)
        p.ts("dve", mean[:], mean[:], 1.0 / 64, None, ALU.mult, None, [mean], [mean])
        p.tt("dve", yc[:], py[:], mean[:].unsqueeze(2).broadcast_to([64, H, 64]), ALU.subtract, [py, mean], [yc])
        p.act(ysq[:], yc[:], AF.Square, [yc], [ysq])
        p.op("dve", lambda e: e.tensor_reduce(var[:], ysq[:], AX.X, ALU.add), [ysq], [var], dur=700)
        p.act(var[:], var[:], AF.Ln, [var, gne], [var], bias=gne[:], scale=1.0 / 64)
        p.act(var[:], var[:], AF.Exp, [var], [var], scale=-0.5)
        p.tt("pool", ynm[:], yc[:], var[:].unsqueeze(2).broadcast_to([64, H, 64]), ALU.mult, [yc, var], [ynm])
        ps = PG()
        for h in range(H):
            p.tr(ps[:, h, :], ynm[:, h, :], c.idf[0:64, 0:64], [ynm, c.idf], [ps])
        p.cp("act", yfm[:, :, cc], ps[:], [ps], [yfm])

    def epilogue(tb):
        bp = tb % 2
        cols = slice(tb * TB, (tb + 1) * TB)
        yfm, bon, bgt, yso = yfm2[bp], bon2[bp], bgt2[bp], yso2[bp]
        p.tt("dve", yfm[:], yfm[:], bc(lnw), ALU.mult, [yfm, lnw], [yfm])
        p.tt("pool", yfm[:], yfm[:], bc(lnb), ALU.add, [yfm, lnb], [yfm])
        p.tt("pool", yfm[:], yfm[:], bon[:], ALU.add, [yfm, bon], [yfm])
        p.tt("dve", yso[:], yfm[:], bgt[:], ALU.mult, [yfm, bgt], [yso])
        p.dma(sc["YS"][512:1024, cols].rearrange("(h d) t -> d h t", d=64), yso[:], [yso], [])

    for tb in range(NB):
        prep(tb)
        for cch in range(NCH):
            chunk(tb, cch)
        epilogue(tb)


def _hv(p, name, ap):
    t = p.sbuf(name, [64, 8], F32)
    p.dma(t[:], ap.rearrange("(h d) -> d h", d=64), [], [t], allow_slow_non_contiguous=True)
    return t


def phase_out_prep(p, c, S, l, W):
    wbo = p.sbuf("wbo", [128, 12, D], BF16)
    wout = p.sbuf("wout", [128, 8, D], BF16)
    gpb = p.sbuf("gpb", [128, D], F32)
    p.dma(gpb[:], W["norm_post"][l:l + 1, :].partition_broadcast(128), [], [gpb])
    mk = p.mark()
    stg = [p.sbuf("wstg%d" % i, [128, 4, D], F32) for i in range(2)]
    for n in range(3):
        p.dma(stg[n % 2][:], W["w_branch_out"][l, n].rearrange("(k p) d -> p k d", p=128), [], [stg[n % 2]])
        p.cp("dve" if n % 2 == 0 else "act", wbo[:, n * 4:(n + 1) * 4, :], stg[n % 2][:], [stg[n % 2]], [wbo])
    for hf in range(2):
        p.dma(stg[(hf + 1) % 2][:], W["w_out"][l, hf * 512:(hf + 1) * 512, :].rearrange("(k p) d -> p k d", p=128), [], [stg[(hf + 1) % 2]])
        p.cp("act" if hf % 2 == 0 else "dve", wout[:, hf * 4:(hf + 1) * 4, :], stg[(hf + 1) % 2][:], [stg[(hf + 1) % 2]], [wout])
    p.release(mk)
    return wbo, wout, gpb


def phase_out(p, c, S, l, xin, xout, W, sc, pre=None, ysblk=None):
    NB = S // 512
    if pre is None:
        pre = phase_out_prep(p, c, S, l, W)
    wbo, wout, gpb = pre
    ysb = [p.sbuf("ysb%d" % i, [128, 12, 512], BF16) for i in range(2)]
    mgb = [p.sbuf("mgb%d" % i, [128, 3, 512], BF16) for i in range(3)]
    mT = p.sbuf("mT", [128, 8, 512], BF16)
    m32 = p.sbuf("m32", [128, 512], F32)
    t32 = [p.sbuf("t32_%d" % i, [128, 512], F32) for i in range(2)]
    xt = [p.sbuf("xo%d" % i, [128, D], F32) for i in range(2)]
    o32a = p.sbuf("o32_0", [128, D], F32)
    o32 = [o32a, o32a]
    junk = p.sbuf("junko", [128, 512], BF16)
    ssq = p.sbuf("ssq", [128, 2], F32)
    rstd = p.sbuf("rstdo", [128, 1], F32)
    pb = [p.psum("pb%d" % i, [128, 512], F32) for i in range(3)]
    po = [p.psum("po%d" % i, [128, 512], F32) for i in range(2)]
    it = 0
    im = 0
    for tb in range(NB):
        cols = slice(tb * 512, (tb + 1) * 512)
        ysx = ysb[tb % 2]
        p.dma(ysx[:], sc["YS"][:, cols].rearrange("(c p) t -> p c t", p=128),
              [sc["YS"] if ysblk is None else ysblk[tb]], [ysx])
        mgv = sc["MG"][:, cols].rearrange("(n c p) t -> p n c t", n=3, p=128)
        for cch in range(8):
            mgx = mgb[im % 3]
            im += 1
            p.dma(mgx[:], mgv[:, :, cch, :], [sc["MG"]], [mgx])
            for n in range(3):
                for k in range(4):
                    p.mm(pb[n][:], wbo[:, n * 4 + k, cch * 128:(cch + 1) * 128], ysx[:, n * 4 + k, :], k == 0, k == 3,
                         [wbo, ysx], [pb[n]])
            p.tt("dve", m32[:], pb[0][:], mgx[:, 0, :], ALU.mult, [pb[0], mgx], [m32])
            p.tt("dve", t32[0][:], pb[1][:], mgx[:, 1, :], ALU.mult, [pb[1], mgx], [t32[0]])
            p.tt("dve", t32[1][:], pb[2][:], mgx[:, 2, :], ALU.mult, [pb[2], mgx], [t32[1]])
            p.tt("pool", m32[:], m32[:], t32[0][:], ALU.add, [m32, t32[0]], [m32])
            p.tt("pool", mT[:, cch, :], m32[:], t32[1][:], ALU.add, [m32, t32[1]], [mT])
        for tsub in range(4):
            s_ = it % 2
            it += 1
            r0 = tb * 512 + tsub * 128
            p.dma(xt[s_][:], xin[r0:r0 + 128, :], [xin], [xt[s_]])
            for hf in range(2):
                for k in range(8):
                    p.mm(po[hf][:], mT[:, k, tsub * 128:(tsub + 1) * 128], wout[:, k, hf * 512:(hf + 1) * 512], k == 0, k == 7,
                         [mT, wout], [po[hf]])
                p.act(junk[:], po[hf][:], AF.Square, [po[hf]], [junk, ssq], accum_out=ssq[:, hf:hf + 1])
            p.tt("dve", rstd[:], ssq[:, 0:1], ssq[:, 1:2], ALU.add, [ssq], [rstd])
            p.act(rstd[:], rstd[:], AF.Ln, [rstd, c.epsb], [rstd], bias=c.epsb[:], scale=1.0 / D)
            p.act(rstd[:], rstd[:], AF.Exp, [rstd], [rstd], scale=-0.5)
            for hf in range(2):
                p.stt("dve", o32[s_][:, hf * 512:(hf + 1) * 512], po[hf][:], rstd[:, 0:1], gpb[:, hf * 512:(hf + 1) * 512],
                      ALU.mult, ALU.mult, [po[hf], rstd, gpb], [o32[s_]])
            p.tt("pool", o32[s_][:], o32[s_][:], xt[s_][:], ALU.add, [o32[s_], xt[s_]], [o32[s_]])
            p.dma(xout[r0:r0 + 128, :], o32[s_][:], [o32[s_]], [])


def make_scratch(p, S, dbg):
    kind = "ExternalOutput" if dbg else "Internal"
    sc = {}
    sc["QT"] = p.dram("QT", [8, 96, S], BF16, kind)
    sc["KT"] = p.dram("KT", [8, 96, S], BF16, kind)
    sc["VA"] = p.dram("VA", [S, 520], BF16, kind)
    sc["U"] = p.dram("U", [1664, S], F32, kind)
    sc["GQ"] = p.dram("GQ", [256, S], F32, kind)
    sc["GK"] = p.dram("GK", [256, S], F32, kind)
    sc["GV"] = p.dram("GV", [S, 512], BF16, kind)
    sc["GL"] = p.dram("GL", [16, S], F32, kind)
    sc["BG"] = p.dram("BG", [1536, S], BF16, kind)
    sc["MG"] = p.dram("MG", [3072, S], BF16, kind)
    sc["YS"] = p.dram("YS", [1536, S], BF16, kind)
    sc["X1"] = p.dram("X1", [S, D], F32, kind)
    return sc


WSPEC = {
    "norm_pre": ([L, D], F32), "w_in": ([L, D, NIN], F32), "wkr2": ([L, D, 192], F32),
    "mla_q_norm": ([L, 256], F32), "mla_kv_norm": ([L, 128], F32),
    "mla_w_uq": ([L, 256, 768], F32), "wuq_sw": ([L, 256, 768], F32),
    "wuk": ([L, 128, 512], F32), "wuv": ([L, 128, 512], F32),
    "rwkv_mu": ([L, 1664], F32), "rwkv_w0": ([L, 512], F32), "rwkv_w_up": ([L, 64, 512], F32),
    "rwkv_a0": ([L, 512], F32), "rwkv_a_up": ([L, 64, 512], F32), "rwkv_k_k": ([L, 512], F32),
    "rwkv_k_a": ([L, 512], F32), "rwkv_r_k": ([L, 512], F32), "rwkv_ln_w": ([L, 512], F32),
    "rwkv_ln_b": ([L, 512], F32), "gla_a_up": ([L, 16, 256], F32), "gla_a_b": ([L, 256], F32),
    "gla_norm": ([L, 512], F32), "w_branch_out": ([L, 3, 512, D], F32), "w_out": ([L, D, D], F32),
    "norm_post": ([L, D], F32),
}


def host_weights(inp):
    w = {}
    for k in WSPEC:
        if k in inp:
            w[k] = np.ascontiguousarray(np.asarray(inp[k], dtype=np.float32))
    win = w["w_in"]
    w["wkr2"] = np.ascontiguousarray(np.concatenate(
        [win[:, :, 320:416], win[:, :, 320:384], win[:, :, 400:416], win[:, :, 384:400]], axis=2))
    uq = w["mla_w_uq"].reshape(L, 256, 8, 96)
    w["wuq_sw"] = np.ascontiguousarray(np.concatenate(
        [uq[..., 0:64], uq[..., 80:96], uq[..., 64:80]], axis=-1).reshape(L, 256, 768))
    ukv = w["mla_w_ukv"] if "mla_w_ukv" in w else np.asarray(inp["mla_w_ukv"], dtype=np.float32)
    ukv = ukv.reshape(L, 128, 8, 128)
    w["wuk"] = np.ascontiguousarray(ukv[..., 0:64].reshape(L, 128, 512))
    w["wuv"] = np.ascontiguousarray(ukv[..., 64:128].reshape(L, 128, 512))
    w["rwkv_r_k"] = w["rwkv_r_k"].reshape(L, 512) if "rwkv_r_k" in w else np.asarray(inp["rwkv_r_k"], np.float32).reshape(L, 512)
    return w


def build(S, phases=("p1",), dbg=True, nlayers=1):
    nc = bass.Bass("TRN2", target_bir_lowering=False)
    p = Prog(nc)
    x_d = p.dram("x", [S, D], F32, "ExternalInput")
    pos_d = p.dram("pos", [1, S], I32, "ExternalInput")
    W = {k: p.dram(k, sh, dt, "ExternalInput").t for k, (sh, dt) in WSPEC.items()}
    out_d = p.dram("out", [S, D], F32, "ExternalOutput")
    sc = make_scratch(p, S, dbg)
    c = setup_consts(p, S, pos_d.t)
    final = [out_d]
    for l in range(nlayers):
        xin = x_d if l == 0 else sc["X1"]
        if "p1" in phases:
            mk = p.mark()
            phase1(p, c, S, l, xin, W, sc)
            p.release(mk)
        if "mla" in phases:
            mk = p.mark()
            phase_mla(p, c, S, l, sc)
            p.release(mk)
        if "rwkv" in phases:
            mk = p.mark()
            phase_rwkv(p, c, S, l, W, sc)
            p.release(mk)
        xo = out_d if l == nlayers - 1 else sc["X1"]
        if "gla" in phases and "out" in phases:
            mk = p.mark()
            pre = phase_out_prep(p, c, S, l, W)
            ysblk = [T(None, "ysblk%d" % i) for i in range(S // 512)]
            phase_gla(p, c, S, l, W, sc, ysblk)
            phase_out(p, c, S, l, xin, xo, W, sc, pre, ysblk)
            p.release(mk)
        else:
            if "gla" in phases:
                mk = p.mark()
                phase_gla(p, c, S, l, W, sc)
                p.release(mk)
            if "out" in phases:
                mk = p.mark()
                phase_out(p, c, S, l, xin, xo, W, sc)
                p.release(mk)
    if dbg:
        final = list(sc.values()) + [out_d]
    p.finish(final)
    return nc, p


ALL_PHASES = ("p1", "mla", "rwkv", "gla", "out")
_CACHE = {}


def kernel(**inputs):
    x = np.asarray(inputs["x"], dtype=np.float32)
    pos = np.asarray(inputs["positions"], dtype=np.int32)
    B, S, _ = x.shape
    Wh = host_weights(inputs)
    nc, _p = build(S, phases=ALL_PHASES, dbg=False, nlayers=L)
    in_maps = []
    for b in range(B):
        m = {"x": np.ascontiguousarray(x[b]), "pos": np.ascontiguousarray(pos[b:b + 1])}
        for k in WSPEC:
            m[k] = Wh[k]
        in_maps.append(m)
    res = run_bass_kernel_spmd(nc, in_maps, core_ids=list(range(B)))
    return np.stack([np.asarray(r["out"]) for r in res.results], axis=0).astype(np.float32)
```

```python
import math
import numpy as np
import concourse.bass as bass
import concourse.mybir as mybir
from concourse.bass_utils import run_bass_kernel_spmd

F32 = mybir.dt.float32
BF16 = mybir.dt.bfloat16
I32 = mybir.dt.int32
ALU = mybir.AluOpType
AF = mybir.ActivationFunctionType
AX = mybir.AxisListType

SEM_CAP = 30000
D = 1024
NIN = 7728
L = 2
EPS = 1e-6


class Buf:
    __slots__ = ("name", "w", "r")

    def __init__(self, name):
        self.name = name
        self.w = None
        self.r = {}


class T:
    def __init__(self, t, name):
        self.t = t
        self.b = Buf(name)

    def __getitem__(self, k):
        return self.t[k]


def _fs(ap):
    n = 1
    for d in ap.shape[1:]:
        n *= int(d)
    return n


def _nbytes(ap):
    n = 1
    for d in ap.shape:
        n *= int(d)
    return n * mybir.dt.size(ap.dtype)


class Prog:
    CE = ("pe", "act", "dve", "pool")
    QE = ("sp",)
    WINDOW = 48
    SYNC = 300.0

    def __init__(self, nc, n_dma_sems=32):
        self.nc = nc
        self.sems = {}
        self._ctx = []
        self._semctx = []
        self.cur = {}
        self.seen = {e: {} for e in self.CE + self.QE}
        self.nsem = 0
        self.old_sems = []
        for e in self.CE:
            self._new_eng_sem(e)
        self.dma_sems = []
        for i in range(n_dma_sems):
            k = self._alloc_sem("dma%d" % i)
            self.dma_sems.append([k, 0])
        self.dma_rr = 0
        self.ninst = 0
        self.ops = []
        self.uid = 0
        self.model_time = 0.0

    def _alloc_sem(self, name):
        cm = self.nc.semaphore(name)
        h = cm.__enter__()
        self._semctx.append(cm)
        self.sems[name] = h
        self.nsem += 1
        return name

    def _new_eng_sem(self, e):
        if e in self.cur:
            self.old_sems.append(tuple(self.cur[e]))
        k = self._alloc_sem("%s_s%d" % (e, self.nsem))
        self.cur[e] = [k, 0]

    def sbuf(self, name, shape, dt):
        self.uid += 1
        name = "%s_u%d" % (name, self.uid)
        cm = self.nc.sbuf_tensor(name, list(shape), dt)
        t = cm.__enter__()
        self._ctx.append(cm)
        return T(t, name)

    def psum(self, name, shape, dt):
        self.uid += 1
        name = "%s_u%d" % (name, self.uid)
        cm = self.nc.psum_tensor(name, list(shape), dt)
        t = cm.__enter__()
        self._ctx.append(cm)
        return T(t, name)

    def dram(self, name, shape, dt, kind="Internal"):
        t = self.nc.dram_tensor(name, list(shape), dt, kind=kind).ap()
        return T(t, name)

    def op(self, eng, fn, R=(), W=(), signal=True, dur=500.0):
        self.ops.append((eng, fn, [x.b for x in R], [x.b for x in W], float(dur), float(dur), False))
        self.ninst += 1

    def dma(self, out_ap, in_ap, R=(), W=(), q="sp", **kw):
        lat = 2000.0 + _nbytes(out_ap) / 150.0
        self.ops.append((q, lambda e: e.dma_start(out=out_ap, in_=in_ap, **kw),
                         [x.b for x in R], [x.b for x in W], 80.0, lat, True))
        self.ninst += 1

    def mm(self, out, lhsT, rhs, start, stop, R, W, **kw):
        n = _fs(rhs)
        d = (50.0 + 1.0 * max(n, 64)) if lhsT.dtype == F32 else (40.0 + 0.42 * max(n, 64))
        self.op("pe", lambda e: e.matmul(out, lhsT, rhs, start=start, stop=stop, **kw), R, W, dur=d)

    def tr(self, out, in_, ident, R, W, signal=True):
        self.op("pe", lambda e: e.transpose(out, in_, ident), R, W, dur=110.0)

    def act(self, out, in_, func, R, W, **kw):
        self.op("act", lambda e: e.activation(out, in_, func, **kw), R, W, dur=260.0 + 0.83 * _fs(out))

    def _vd(self, eng, out, two_src):
        n = _fs(out)
        if eng == "pool":
            return 150.0 + 2.2 * n
        b16 = mybir.dt.size(out.dtype) == 2
        if two_src:
            return 160.0 + (1.04 * n)
        return 160.0 + (0.55 * n)

    def tt(self, eng, out, a, b, op, R, W):
        self.op(eng, lambda e: e.tensor_tensor(out, a, b, op), R, W, dur=self._vd(eng, out, True))

    def ts(self, eng, out, a, s1, s2, op0, op1, R, W):
        d = self._vd(eng, out, False)
        if s2 is None:
            self.op(eng, lambda e: e.tensor_scalar(out, a, s1, None, op0), R, W, dur=d)
        else:
            self.op(eng, lambda e: e.tensor_scalar(out, a, s1, s2, op0, op1), R, W, dur=d)

    def stt(self, eng, out, a, s, b, op0, op1, R, W):
        self.op(eng, lambda e: e.scalar_tensor_tensor(out, a, s, b, op0, op1), R, W, dur=self._vd(eng, out, True))

    def cp(self, eng, out, in_, R, W):
        if eng == "act":
            self.op(eng, lambda e: e.copy(out, in_), R, W, dur=260.0 + 0.83 * _fs(out))
        else:
            self.op(eng, lambda e: e.tensor_copy(out, in_), R, W, dur=self._vd(eng, out, False))

    def memset(self, eng, ap, val, W):
        self.op(eng, lambda e: e.memset(ap, val), (), W, dur=self._vd(eng, ap, False))

    def _eng(self, eng):
        nc = self.nc
        return {"pe": nc.tensor, "act": nc.scalar, "dve": nc.vector, "pool": nc.gpsimd, "sp": nc.sync}[eng]

    def flush(self):
        import bisect
        ops = self.ops
        self.ops = []
        n = len(ops)
        if n == 0:
            return
        engs = self.CE + self.QE
        lastw = {}
        readers = {}
        deps = [None] * n
        for i in range(n):
            eng, fn, R, W, occ, lat, isd = ops[i]
            d = set()
            for b in R:
                w = lastw.get(id(b))
                if w is not None:
                    d.add(w)
            for b in W:
                w = lastw.get(id(b))
                if w is not None:
                    d.add(w)
                rl = readers.get(id(b))
                if rl:
                    d.update(rl)
            d.discard(i)
            deps[i] = d
            for b in R:
                readers.setdefault(id(b), []).append(i)
            for b in W:
                lastw[id(b)] = i
                readers[id(b)] = []
        succ = [[] for _ in range(n)]
        nd = [0] * n
        for i in range(n):
            nd[i] = len(deps[i])
            for j in deps[i]:
                succ[j].append(i)
        bl = [0.0] * n
        for i in range(n - 1, -1, -1):
            m = 0.0
            for s_ in succ[i]:
                if bl[s_] > m:
                    m = bl[s_]
            bl[i] = m + ops[i][5] + 100.0
        ready = [0.0] * n
        free = {e: 0.0 for e in engs}
        cand = {e: [] for e in engs}
        for i in range(n):
            if nd[i] == 0:
                cand[ops[i][0]].append(i)
        order = {e: [] for e in engs}
        done = 0
        W_ = self.WINDOW
        SY = self.SYNC
        tmax = 0.0
        while done < n:
            best = None
            for e in engs:
                c = cand[e]
                if not c:
                    continue
                fe = free[e]
                bst = None
                for i in c[:W_]:
                    st = ready[i] if ready[i] > fe else fe
                    key = (st, -bl[i])
                    if bst is None or key < bst[2]:
                        bst = (st, i, key)
                if best is None or bst[2] < best[3]:
                    best = (bst[0], bst[1], e, bst[2])
            st, i, e = best[0], best[1], best[2]
            cand[e].remove(i)
            order[e].append(i)
            occ, lat = ops[i][4], ops[i][5]
            free[e] = st + occ
            fin = st + lat
            if fin > tmax:
                tmax = fin
            for s_ in succ[i]:
                rt = fin + (SY if ops[s_][0] != e else (0.0 if e == "pe" else 200.0))
                if rt > ready[s_]:
                    ready[s_] = rt
                nd[s_] -= 1
                if nd[s_] == 0:
                    bisect.insort(cand[ops[s_][0]], s_)
            done += 1
        self.model_time += tmax
        busy = {e: 0.0 for e in engs}
        for i in range(n):
            busy[ops[i][0]] += ops[i][4]
        if not hasattr(self, "regions"):
            self.regions = []
        self.regions.append((tmax, busy, n))
        need = [False] * n
        for i in range(n):
            e = ops[i][0]
            if ops[i][6] or not succ[i]:
                need[i] = True
                continue
            for s_ in succ[i]:
                if ops[s_][0] != e or e != "pe":
                    need[i] = True
                    break
        tick = [None] * n
        reuse_wait = {}
        for e in engs:
            for i in order[e]:
                if ops[i][6]:
                    slot = self.dma_sems[self.dma_rr]
                    self.dma_rr = (self.dma_rr + 1) % len(self.dma_sems)
                    if slot[1] > 0:
                        reuse_wait[i] = (slot[0], slot[1])
                    slot[1] += 16
                    tick[i] = (slot[0], slot[1])
                elif need[i]:
                    cur = self.cur[e]
                    if cur[1] >= SEM_CAP:
                        self._new_eng_sem(e)
                        cur = self.cur[e]
                    cur[1] += 1
                    tick[i] = (cur[0], cur[1])
        for e in engs:
            eo = self._eng(e)
            seen = self.seen[e]
            sems = self.sems
            for i in order[e]:
                w = {}
                for j in deps[i]:
                    if e == "pe" and ops[j][0] == "pe":
                        continue
                    k, v = tick[j]
                    if w.get(k, 0) < v:
                        w[k] = v
                if i in reuse_wait:
                    k, v = reuse_wait[i]
                    if w.get(k, 0) < v:
                        w[k] = v
                for k, v in w.items():
                    if seen.get(k, 0) < v:
                        seen[k] = v
                        eo.wait_ge(sems[k], v)
                ins = ops[i][1](eo)
                if tick[i] is not None:
                    ins.then_inc(sems[tick[i][0]], 16 if ops[i][6] else 1)

    def barrier(self):
        self.flush()
        allv = [(k, v) for (k, v) in [tuple(x) for x in self.cur.values()] if v > 0]
        allv += [(k, v) for (k, v) in self.old_sems]
        allv += [(k, v) for (k, v) in [tuple(x) for x in self.dma_sems] if v > 0]
        for eng in self.CE + self.QE:
            eo = self._eng(eng)
            for k, v in allv:
                if self.seen[eng].get(k, 0) < v:
                    self.seen[eng][k] = v
                    eo.wait_ge(self.sems[k], v)

    def mark(self):
        return len(self._ctx)

    def release(self, mark):
        self.barrier()
        while len(self._ctx) > mark:
            self._ctx.pop().__exit__(None, None, None)

    def finish(self, final):
        self.barrier()
        while self._ctx:
            self._ctx.pop().__exit__(None, None, None)
        while self._semctx:
            self._semctx.pop().__exit__(None, None, None)


C_CQ, C_CKV, C_KR, C_U = 0, 256, 384, 416
C_GQ, C_GK, C_GV, C_GL, C_BG, C_MG = 2080, 2336, 2592, 3104, 3120, 4656
SCALE = (64 + 32) ** -0.5


class Ctx:
    pass


def setup_consts(p, S, pos_d):
    c = Ctx()
    idf = p.sbuf("idf", [128, 128], F32)
    p.memset("pool", idf[:], 1.0, [idf])
    p.op("pool", lambda e: e.affine_select(idf[:], idf[:], [[1, 128]], ALU.is_equal, 0.0,
                                           base=0, channel_multiplier=-1), [idf], [idf])
    idb = p.sbuf("idb", [128, 128], BF16)
    p.cp("dve", idb[:], idf[:], [idf], [idb])
    c.idf, c.idb = idf, idb
    onesb = p.sbuf("onesb", [128, 128], BF16)
    p.memset("pool", onesb[:], 1.0, [onesb])
    c.onesb = onesb
    epsb = p.sbuf("epsb", [128, 1], F32)
    p.memset("pool", epsb[:], EPS, [epsb])
    c.epsb = epsb
    lnsc = p.sbuf("lnsc", [128, 1], F32)
    p.memset("pool", lnsc[:], math.log(SCALE), [lnsc])
    c.lnsc = lnsc
    inv = (1.0 / (10000.0 ** (np.arange(0, 32, 2, dtype=np.float32) / np.float32(32)))).astype(np.float32)
    row = p.sbuf("roperow", [1, 64], F32)
    for j in range(16):
        p.memset("pool", row[0:1, j:j + 1], float(inv[j]), [row])
        p.memset("pool", row[0:1, 16 + j:17 + j], float(inv[j]), [row])
    p.memset("pool", row[0:1, 32:48], -1.0, [row])
    p.memset("pool", row[0:1, 48:64], 1.0, [row])
    rd = p.dram("rope_d", [64], F32)
    p.dma(rd[:].rearrange("(o n) -> o n", o=1), row[:], [row], [rd])
    ctab = p.sbuf("ctab", [96, 2], F32)
    p.dma(ctab[64:96, :], rd[:].rearrange("(c p) -> p c", p=32), [rd], [ctab], allow_slow_non_contiguous=True)
    mk = p.mark()
    cos2 = p.sbuf("cos2", [96, S], F32)
    sin2 = p.sbuf("sin2", [96, S], F32)
    posi = p.sbuf("posi", [96, S], I32)
    p.dma(posi[64:96, :], pos_d.partition_broadcast(32), [], [posi])
    ang = p.sbuf("ang", [96, S], F32)
    p.cp("dve", ang[64:96, :], posi[64:96, :], [posi], [ang])
    p.ts("dve", ang[64:96, :], ang[64:96, :], ctab[64:96, 0:1], None, ALU.mult, None, [ang, ctab], [ang])
    kf = p.sbuf("kf", [96, S], F32)
    sl = slice(64, 96)

    def sin_of(dst, shift):
        a = dst
        p.ts("dve", a[sl, :], ang[sl, :], shift, None, ALU.add, None, [ang], [dst])
        p.ts("dve", kf[sl, :], a[sl, :], 1.0 / (2 * math.pi), None, ALU.mult, None, [dst], [kf])
        p.cp("dve", posi[sl, :], kf[sl, :], [kf], [posi])
        p.cp("dve", kf[sl, :], posi[sl, :], [posi], [kf])
        p.stt("dve", a[sl, :], kf[sl, :], -2 * math.pi, a[sl, :], ALU.mult, ALU.add, [kf, dst], [dst])
        p.ts("dve", kf[sl, :], a[sl, :], math.pi, -2 * math.pi, ALU.is_gt, ALU.mult, [dst], [kf])
        p.tt("dve", a[sl, :], a[sl, :], kf[sl, :], ALU.add, [dst, kf], [dst])
        p.ts("dve", kf[sl, :], a[sl, :], -math.pi, 2 * math.pi, ALU.is_lt, ALU.mult, [dst], [kf])
        p.tt("dve", a[sl, :], a[sl, :], kf[sl, :], ALU.add, [dst, kf], [dst])
        p.act(a[sl, :], a[sl, :], AF.Sin, [dst], [dst])
    sin_of(cos2, math.pi / 2)
    sin_of(sin2, 0.0)
    p.ts("dve", sin2[sl, :], sin2[sl, :], ctab[sl, 1:2], None, ALU.mult, None, [sin2, ctab], [sin2])
    c.rope = p.dram("rope_tab", [2, 32, S], F32)
    p.dma(c.rope[0], cos2[sl, :], [cos2], [c.rope])
    p.dma(c.rope[1], sin2[sl, :], [sin2], [c.rope])
    p.release(mk)
    return c


def colvec(p, name, src_ap, n, dt=F32):
    t = p.sbuf(name, [128, n], dt)
    p.dma(t[:], src_ap.rearrange("(c p) -> p c", p=128), [], [t], allow_slow_non_contiguous=True)
    return t


def phase1(p, c, S, l, xin, W, sc):
    NT_ = S // 128
    NB = S // 512
    hT = p.sbuf("hT%d" % l, [128, 8, S], BF16)
    gbc = p.sbuf("gbc%d" % l, [128, D], F32)
    p.dma(gbc[:], W["norm_pre"][l:l + 1, :].partition_broadcast(128), [], [gbc])
    mk1 = p.mark()
    xt = [p.sbuf("xt%d_%d" % (l, i), [128, D], F32) for i in range(3)]
    hb = [p.sbuf("hb%d_%d" % (l, i), [128, D], BF16) for i in range(3)]
    junk = p.sbuf("junk%d" % l, [128, D], BF16)
    ss = [p.sbuf("ss%d_%d" % (l, i), [128, 1], F32) for i in range(3)]
    ptr = [p.psum("ptr%d_%d" % (l, i), [128, 8, 128], BF16) for i in range(3)]
    for i in range(NT_):
        s = i % 3
        p.dma(xt[s][:], xin[i * 128:(i + 1) * 128, :], [xin], [xt[s]])
        p.act(junk[:], xt[s][:], AF.Square, [xt[s]], [junk, ss[s]], accum_out=ss[s][:])
        p.act(ss[s][:], ss[s][:], AF.Ln, [ss[s], c.epsb], [ss[s]], bias=c.epsb[:], scale=1.0 / D)
        p.act(ss[s][:], ss[s][:], AF.Exp, [ss[s]], [ss[s]], scale=-0.5)
        p.stt("dve", hb[s][:], xt[s][:], ss[s][:, 0:1], gbc[:], ALU.mult, ALU.mult,
              [xt[s], ss[s], gbc], [hb[s]])
        for k in range(8):
            p.tr(ptr[s][:, k, :], hb[s][:, k * 128:(k + 1) * 128], c.idb[:], [hb[s], c.idb], [ptr[s]],
                 signal=(k == 7))
        p.cp("dve", hT[:, :, i * 128:(i + 1) * 128], ptr[s][:], [ptr[s]], [hT])

    p.release(mk1)
    mk2 = p.mark()
    wa32 = p.sbuf("wa32_%d" % l, [128, 8, 384], F32)
    p.dma(wa32[:], W["w_in"][l, :, 0:384].rearrange("(c p) n -> p c n", p=128), [], [wa32])
    wa = p.sbuf("wa_%d" % l, [128, 8, 384], BF16)
    p.cp("dve", wa[:], wa32[:], [wa32], [wa])
    wk32 = p.sbuf("wk32_%d" % l, [128, 8, 192], F32)
    p.dma(wk32[:], W["wkr2"][l].rearrange("(c p) n -> p c n", p=128), [], [wk32])
    wkr = p.sbuf("wkr_%d" % l, [128, 8, 192], BF16)
    p.cp("act", wkr[:], wk32[:], [wk32], [wkr])
    wq32 = p.sbuf("wq32_%d" % l, [128, 2, 1536], F32)
    p.dma(wq32[:, :, 0:768], W["mla_w_uq"][l].rearrange("(c p) n -> p c n", p=128), [], [wq32])
    p.dma(wq32[:, :, 768:1536], W["wuq_sw"][l].rearrange("(c p) n -> p c n", p=128), [], [wq32])
    wq = p.sbuf("wq_%d" % l, [128, 2, 1536], BF16)
    p.cp("dve", wq[:], wq32[:], [wq32], [wq])
    wkv32 = p.sbuf("wkv32_%d" % l, [128, 1024], F32)
    p.dma(wkv32[:, 0:512], W["wuk"][l], [], [wkv32])
    p.dma(wkv32[:, 512:1024], W["wuv"][l], [], [wkv32])
    wkv = p.sbuf("wkv_%d" % l, [128, 1024], BF16)
    p.cp("act", wkv[:], wkv32[:], [wkv32], [wkv])
    cos2 = p.sbuf("cos2_%d" % l, [96, S], F32)
    sin2 = p.sbuf("sin2_%d" % l, [96, S], F32)
    p.dma(cos2[64:96, :], c.rope[0], [c.rope], [cos2])
    p.dma(sin2[64:96, :], c.rope[1], [c.rope], [sin2])
    gq = colvec(p, "gq%d" % l, W["mla_q_norm"][l], 2)
    gkv = colvec(p, "gkv%d" % l, W["mla_kv_norm"][l], 1)

    pq = [p.psum("pq%d_%d" % (l, i), [128, 512], F32) for i in range(2)]
    pkv = p.psum("pkv%d" % l, [128, 512], F32)
    pkr = p.psum("pkr%d" % l, [96, 2, 512], F32)
    pn = p.psum("pn%d" % l, [128, 512], F32)
    pw = [p.psum("pw%d_%d" % (l, i), [128, 512], F32) for i in range(2)]
    sq = p.sbuf("sq%d" % l, [128, 2, 512], BF16)
    rq = p.sbuf("rq%d" % l, [128, 512], F32)
    cqn = p.sbuf("cqn%d" % l, [128, 2, 512], BF16)
    ckvn = p.sbuf("ckvn%d" % l, [128, 512], BF16)
    qst = p.sbuf("qst%d" % l, [96, 8, 512], BF16)
    kst = p.sbuf("kst%d" % l, [128, 4, 512], BF16)
    krs = p.sbuf("krs%d" % l, [96, 512], BF16)
    t1s = [p.sbuf("t1_%d_%d" % (l, i), [96, 512], F32) for i in range(2)]
    t2s = [p.sbuf("t2_%d_%d" % (l, i), [96, 512], F32) for i in range(2)]
    t1, t2 = t1s[0], t2s[0]
    vst = p.sbuf("vst%d" % l, [128, 4, 8, 65], BF16)
    p.memset("pool", vst[:], 1.0, [vst])
    rs = slice(64, 96)
    for tb in range(NB):
        ts_ = slice(tb * 512, (tb + 1) * 512)
        for cc in range(2):
            for k in range(8):
                p.mm(pq[cc][:], wa[:, k, cc * 128:(cc + 1) * 128], hT[:, k, ts_], k == 0, k == 7, [wa, hT], [pq[cc]])
        for k in range(8):
            p.mm(pkv[:], wa[:, k, 256:384], hT[:, k, ts_], k == 0, k == 7, [wa, hT], [pkv])
        for v in range(2):
            for k in range(8):
                p.mm(pkr[:, v, :], wkr[:, k, v * 96:(v + 1) * 96], hT[:, k, ts_], k == 0, k == 7, [wkr, hT], [pkr])
        for cc in range(2):
            p.act(sq[:, cc, :], pq[cc][:], AF.Square, [pq[cc]], [sq])
        for cc in range(2):
            p.mm(pn[:], c.onesb[:], sq[:, cc, :], cc == 0, cc == 1, [c.onesb, sq], [pn])
        p.act(rq[:], pn[:], AF.Ln, [pn, c.epsb], [rq], bias=c.epsb[:], scale=1.0 / 256)
        p.act(rq[:], rq[:], AF.Exp, [rq, c.lnsc], [rq], bias=c.lnsc[:], scale=-0.5)
        for cc in range(2):
            p.stt("dve", cqn[:, cc, :], pq[cc][:], gq[:, cc:cc + 1], rq[:], ALU.mult, ALU.mult,
                  [pq[cc], gq, rq], [cqn])
        p.act(sq[:, 0, :], pkv[:], AF.Square, [pkv], [sq])
        p.mm(pn[:], c.onesb[:], sq[:, 0, :], True, True, [c.onesb, sq], [pn])
        p.act(rq[:], pn[:], AF.Ln, [pn, c.epsb], [rq], bias=c.epsb[:], scale=1.0 / 128)
        p.act(rq[:], rq[:], AF.Exp, [rq], [rq], scale=-0.5)
        p.stt("dve", ckvn[:], pkv[:], gkv[:, 0:1], rq[:], ALU.mult, ALU.mult, [pkv, gkv, rq], [ckvn])
        p.tt("dve", t1[rs, :], pkr[rs, 0, :], cos2[rs, ts_], ALU.mult, [pkr, cos2], [t1])
        p.tt("dve", t2[rs, :], pkr[rs, 1, :], sin2[rs, ts_], ALU.mult, [pkr, sin2], [t2])
        p.tt("pool", krs[rs, :], t1[rs, :], t2[rs, :], ALU.add, [t1, t2], [krs])
        for h in range(8):
            p.dma(sc["KT"][h, 64:96, ts_], krs[rs, :], [krs], [])
        for h in range(8):
            a, b = pw[0], pw[1]
            t1, t2 = t1s[h % 2], t2s[h % 2]
            for k in range(2):
                p.mm(a[0:96, :], wq[:, k, h * 96:(h + 1) * 96], cqn[:, k, :], k == 0, k == 1, [wq, cqn], [a])
            for k in range(2):
                p.mm(b[0:96, :], wq[:, k, 768 + h * 96:768 + (h + 1) * 96], cqn[:, k, :], k == 0, k == 1, [wq, cqn], [b])
            p.cp("act", qst[0:64, h, :], a[0:64, :], [a], [qst])
            p.tt("dve", t1[rs, :], a[rs, :], cos2[rs, ts_], ALU.mult, [a, cos2], [t1])
            p.tt("dve", t2[rs, :], b[rs, :], sin2[rs, ts_], ALU.mult, [b, sin2], [t2])
            p.tt("pool", qst[rs, h, :], t1[rs, :], t2[rs, :], ALU.add, [t1, t2], [qst])
        p.dma(sc["QT"][:, :, ts_].rearrange("h p t -> p h t"), qst[:], [qst], [])
        for j in range(4):
            a = pw[j % 2]
            p.mm(a[:], wkv[:, j * 128:(j + 1) * 128], ckvn[:], True, True, [wkv, ckvn], [a])
            p.cp("act", kst[:, j, :], a[:], [a], [kst])
        for two in range(2):
            p.dma(sc["KT"][:, 0:64, ts_].rearrange("(j two) p t -> two p j t", two=2)[two],
                  kst[two * 64:(two + 1) * 64, :, :], [kst], [])
        for tt_ in range(4):
            a = pw[tt_ % 2]
            p.mm(a[:], ckvn[:, tt_ * 128:(tt_ + 1) * 128], wkv[:, 512:1024], True, True, [ckvn, wkv], [a])
            p.cp("act", vst[:, tt_, :, 0:64], a[:].rearrange("p (h v) -> p h v", v=64), [a], [vst])
        p.dma(sc["VA"][ts_, :].rearrange("(tt p) n -> p tt n", p=128),
              vst[:].rearrange("p t h v -> p t (h v)"), [vst], [])

    p.release(mk2)
    mk3 = p.mark()
    pw = [p.psum("pwb%d_%d" % (l, i), [128, 512], F32) for i in range(2)]
    chunks = []
    for i in range(13):
        chunks.append((C_U + i * 128, 128, "mix", "U", i * 128))
    for i in range(2):
        chunks.append((C_GQ + i * 128, 128, "copy32", "GQ", i * 128))
    for i in range(2):
        chunks.append((C_GK + i * 128, 128, "copy32", "GK", i * 128))
    chunks.append((C_GL, 16, "copy32", "GL", 0))
    for i in range(12):
        chunks.append((C_BG + i * 128, 128, "silu", "BG", i * 128))
    for i in range(24):
        chunks.append((C_MG + i * 128, 128, "sigm", "MG", i * 128))
    chunks.sort(key=lambda x: {"mix": 0, "copy32": 0, "silu": 1, "sigm": 2}[x[2]])
    pw = pw + [p.psum("pwd%d_%d" % (l, i), [128, 512], F32) for i in range(2)]
    mu13 = colvec(p, "mu13_%d" % l, W["rwkv_mu"][l], 13)
    wc32 = [p.sbuf("wc32_%d_%d" % (l, i), [128, 8, 128], F32) for i in range(3)]
    wcb = [p.sbuf("wcb_%d_%d" % (l, i), [128, 8, 128], BF16) for i in range(3)]
    st32 = [p.sbuf("st32_%d_%d" % (l, i), [128, 512], F32) for i in range(4)]
    st16 = [p.sbuf("st16_%d_%d" % (l, i), [128, 512], BF16) for i in range(4)]
    stgm = [p.sbuf("stgm_%d_%d" % (l, i), [128, 513], F32) for i in range(3)]
    dmx = [p.sbuf("dmx_%d_%d" % (l, i), [128, 512], F32) for i in range(2)]
    wg32 = p.sbuf("wg32_%d" % l, [128, 8, 512], F32)
    p.dma(wg32[:], W["w_in"][l, :, C_GV:C_GV + 512].rearrange("(c p) n -> p c n", p=128), [], [wg32])
    wg = p.sbuf("wg_%d" % l, [128, 8, 512], BF16)
    p.cp("pool", wg[:], wg32[:], [wg32], [wg])
    n = 0
    nm = 0
    for ci, (col0, m, kind, dest, row0) in enumerate(chunks):
        s = ci % 3
        p.dma(wc32[s][:, :, 0:m], W["w_in"][l, :, col0:col0 + m].rearrange("(c p) n -> p c n", p=128), [], [wc32[s]])
        p.cp("pool", wcb[s][:, :, 0:m], wc32[s][:, :, 0:m], [wc32[s]], [wcb[s]])
        prev = None
        for tb in range(NB):
            ts_ = slice(tb * 512, (tb + 1) * 512)
            a = pw[n % 4]
            for k in range(8):
                p.mm(a[0:m, :], wcb[s][:, k, 0:m], hT[:, k, ts_], k == 0, k == 7, [wcb[s], hT], [a])
            if kind == "mix":
                sg_ = stgm[nm % 3]
                dd = dmx[nm % 2]
                nm += 1
                o = st32[n % 4]
                if prev is None:
                    p.memset("pool", sg_[:, 0:1], 0.0, [sg_])
                else:
                    p.cp("pool", sg_[:, 0:1], prev[:, 512:513], [prev], [sg_])
                p.cp("act", sg_[:, 1:513], a[:], [a], [sg_])
                p.tt("dve", dd[:], sg_[:, 0:512], sg_[:, 1:513], ALU.subtract, [sg_], [dd])
                mi = row0 // 128
                p.stt("dve", o[:], dd[:], mu13[:, mi:mi + 1], sg_[:, 1:513], ALU.mult, ALU.add, [dd, mu13, sg_], [o])
                prev = sg_
            elif kind == "copy32":
                o = st32[n % 4]
                if n % 2 == 0:
                    p.cp("act", o[0:m, :], a[0:m, :], [a], [o])
                else:
                    p.cp("dve", o[0:m, :], a[0:m, :], [a], [o])
            else:
                o = st16[n % 4]
                p.act(o[0:m, :], a[0:m, :], AF.Silu if kind == "silu" else AF.Sigmoid, [a], [o])
            p.dma(sc[dest][row0:row0 + m, ts_], o[0:m, :], [o], [])
            n += 1
    for i in range(NT_):
        a = pw[n % 4]
        o = st16[n % 4]
        n += 1
        for k in range(8):
            p.mm(a[:], hT[:, k, i * 128:(i + 1) * 128], wg[:, k, :], k == 0, k == 7, [hT, wg], [a])
        p.cp("act", o[:], a[:], [a], [o])
        p.dma(sc["GV"][i * 128:(i + 1) * 128, :], o[:], [o], [])
    p.release(mk3)


def phase_mla(p, c, S, l, sc):
    NB = S // 512
    NT_ = S // 128
    kt = p.sbuf("kt", [96, 8, S], BF16)
    for h in range(8):
        p.dma(kt[:, h, :], sc["KT"][h], [sc["KT"]], [kt])
    va = p.sbuf("va", [128, NT_, 520], BF16)
    p.dma(va[:], sc["VA"].t.rearrange("(n p) f -> p n f", p=128), [sc["VA"]], [va])
    qts = [p.sbuf("qt%d" % i, [96, 8, 512], BF16) for i in range(2)]
    bgs = [p.sbuf("bg%d" % i, [128, 4, 512], BF16) for i in range(2)]
    pts = [p.sbuf("pt%d" % i, [128, 512], BF16) for i in range(4)]
    ytm = p.sbuf("ytm", [128, 4, 512], F32)
    ys = [p.sbuf("ysm%d" % i, [128, 4, 512], BF16) for i in range(2)]
    recs = [p.sbuf("rec%d" % i, [128, 1], F32) for i in range(8)]
    pst = [p.psum("pst%d" % i, [128, 512], F32) for i in range(3)]
    acc = [p.psum("acc%d" % i, [128, 512], F32) for i in range(4)]
    pT = p.psum("pT", [128, 4, 128], F32)
    n = 0
    for qb in range(NB):
        qt = qts[qb % 2]
        bg = bgs[qb % 2]
        cols = slice(qb * 512, (qb + 1) * 512)
        p.dma(qt[:], sc["QT"][:, :, cols].rearrange("h p t -> p h t"), [sc["QT"]], [qt])
        p.dma(bg[:], sc["BG"][0:512, cols].rearrange("(c p) t -> p c t", p=128), [sc["BG"]], [bg])
        for h in range(8):
            nkt = 4 * (qb + 1)
            for kti in range(nkt):
                j = kti - 4 * qb
                q0 = 128 * j if j > 0 else 0
                N = 512 - q0
                st = pst[n % 3]
                pt = pts[n % 4]
                p.mm(st[:, 0:N], kt[:, h, kti * 128:(kti + 1) * 128], qt[:, h, q0:512], True, True, [kt, qt], [st])
                p.act(pt[:, 0:N], st[:, 0:N], AF.Exp, [st], [pt])
                if j >= 0:
                    p.op("pool", lambda e, pt=pt: e.affine_select(pt[:, 0:128], pt[:, 0:128], [[1, 128]], ALU.is_ge, 0.0,
                                                                  base=0, channel_multiplier=-1), [pt], [pt])
                for qs in range(q0 // 128, 4):
                    p.mm(acc[qs][:, 0:65], pt[:, qs * 128 - q0:(qs + 1) * 128 - q0], va[:, kti, h * 65:(h + 1) * 65],
                         kti == 0, kti == 4 * qb + qs, [pt, va], [acc[qs]])
                n += 1
            for qs in range(4):
                rc = recs[(h % 2) * 4 + qs]
                p.op("dve", lambda e, qs=qs, rc=rc: e.reciprocal(rc[:], acc[qs][:, 64:65]), [acc[qs]], [rc], dur=200)
                p.ts("dve", ytm[:, qs, h * 64:(h + 1) * 64], acc[qs][:, 0:64], rc[:, 0:1], None, ALU.mult, None,
                     [acc[qs], rc], [ytm])
        yo = ys[qb % 2]
        for fc in range(4):
            for qs in range(4):
                p.tr(pT[:, qs, :], ytm[:, qs, fc * 128:(fc + 1) * 128], c.idf[:], [ytm, c.idf], [pT], signal=(qs == 3))
            p.tt("dve", yo[:, fc, :], pT[:].rearrange("p a b -> p (a b)"), bg[:, fc, :], ALU.mult, [pT, bg], [yo])
        p.dma(sc["YS"][0:512, cols].rearrange("(c p) t -> p c t", p=128), yo[:], [yo], [])


def phase_gla(p, c, S, l, W, sc, ysblk=None):
    NB = S // 512
    H = 4
    aup = p.sbuf("g_aup", [16, 256], F32)
    p.dma(aup[:], W["gla_a_up"][l], [], [aup])
    nab = p.sbuf("g_nab", [64, H], F32)
    p.dma(nab[:], W["gla_a_b"][l].rearrange("(h d) -> d h", d=64), [], [nab], allow_slow_non_contiguous=True)
    p.ts("dve", nab[:], nab[:], -1.0, None, ALU.mult, None, [nab], [nab])
    gn = colvec(p, "g_gn", W["gla_norm"][l], 4)
    one1 = p.sbuf("g_one1", [128, 1], F32)
    p.memset("pool", one1[:], 1.0, [one1])
    maskr = p.sbuf("g_maskr", [64, 512], F32)
    p.memset("pool", maskr[:], 1.0, [maskr])
    p.memset("pool", maskr[:].rearrange("p (c t) -> p c t", t=64)[:, :, 0:1], 0.0, [maskr])
    msk = p.sbuf("g_msk", [64, H, 64], F32)
    p.memset("pool", msk[:], 1.0, [msk])
    for h in range(H):
        p.op("pool", lambda e, h=h: e.affine_select(msk[:, h, :], msk[:, h, :], [[1, 64]], ALU.is_ge, 0.0,
                                                    base=0, channel_multiplier=-1), [msk], [msk])
    st = p.sbuf("g_st", [64, H, 128], F32)
    stb = p.sbuf("g_stb", [64, H, 128], BF16)
    st2 = p.sbuf("g_st2", [64, H, 128], F32)
    p.memset("pool", st[:], 0.0, [st])
    p.memset("pool", stb[:], 0.0, [stb])
    gq1 = p.sbuf("g_q", [64, H, 512], F32)
    gk1 = p.sbuf("g_k", [64, H, 512], F32)
    gq, gk = [gq1, gq1], [gk1, gk1]
    gl1 = p.sbuf("g_l", [16, 512], F32)
    gl = [gl1, gl1]
    gv = [p.sbuf("g_v%d" % i, [64, 8, 512], BF16) for i in range(2)]
    bg1 = p.sbuf("g_bg", [128, H, 512], BF16)
    bg = [bg1, bg1]
    e1 = p.sbuf("g_e1", [64, H, 512], F32)
    cs = p.sbuf("g_cs", [64, H, 512], F32)
    eb = p.sbuf("g_eb", [64, H, 512], F32)
    enb = e1
    qe = p.sbuf("g_qe", [64, H, 512], BF16)
    ke = p.sbuf("g_ke", [64, H, 512], BF16)
    att = [p.sbuf("g_att%d" % i, [64, H, 64], BF16) for i in range(2)]
    ketm = [p.sbuf("g_ketm%d" % i, [64, H, 64], BF16) for i in range(2)]
    sq = p.sbuf("g_sq", [64, H, 128], F32)
    ssg = p.sbuf("g_ss", [64, H], F32)
    yn = [p.sbuf("g_yn%d" % i, [64, H, 128], F32) for i in range(2)]
    yT = p.sbuf("g_yT", [128, H, 512], F32)
    ysg1 = p.sbuf("g_ys", [128, H, 512], BF16)
    ysg = [ysg1, ysg1]
    gbank = [p.psum("g_bank%d" % i, [128, 512], F32) for i in range(3)]
    gbi = [0]

    def GB():
        t = gbank[gbi[0] % 3]
        gbi[0] += 1
        return t
    for tb in range(NB):
        s_ = tb % 2
        cols = slice(tb * 512, (tb + 1) * 512)
        p.dma(gq[s_][:], sc["GQ"][:, cols].rearrange("(h d) t -> d h t", d=64), [sc["GQ"]], [gq[s_]])
        p.dma(gk[s_][:], sc["GK"][:, cols].rearrange("(h d) t -> d h t", d=64), [sc["GK"]], [gk[s_]])
        p.dma(gl[s_][:], sc["GL"][:, cols], [sc["GL"]], [gl[s_]])
        p.dma(gv[s_][:], sc["GV"][cols, :].rearrange("(c p) f -> p c f", p=64), [sc["GV"]], [gv[s_]])
        p.dma(bg[s_][:], sc["BG"][1024:1536, cols].rearrange("(c p) t -> p c t", p=128), [sc["BG"]], [bg[s_]])
        for h in range(H):
            z = GB()
            p.mm(z[0:64, :], aup[:, h * 64:(h + 1) * 64], gl[s_][:], True, True, [aup, gl[s_]], [z])
            p.act(e1[:, h, :], z[0:64, :], AF.Exp, [z, nab], [e1], bias=nab[:, h:h + 1], scale=-1.0)
        p.act(e1[:], e1[:], AF.Ln, [e1, one1], [e1], bias=one1[0:64, :], scale=1.0)
        for h in range(H):
            p.op("dve", lambda e, h=h: e.tensor_tensor_scan(cs[:, h, :], maskr[:], e1[:, h, :], 0.0, ALU.mult, ALU.add),
                 [maskr, e1], [cs])
        p.act(eb[:], cs[:], AF.Exp, [cs], [eb], scale=-1.0 / 16)
        p.act(enb[:], cs[:], AF.Exp, [cs], [enb], scale=1.0 / 16)
        p.stt("dve", qe[:], gq[s_][:], 0.125, eb[:], ALU.mult, ALU.mult, [gq[s_], eb], [qe])
        p.tt("pool", ke[:], gk[s_][:], enb[:], ALU.mult, [gk[s_], enb], [ke])
        for cch in range(8):
            cc = slice(cch * 64, (cch + 1) * 64)
            a_ = att[cch % 2]
            kt_ = ketm[cch % 2]
            y_ = yn[cch % 2]
            b1 = GB()
            pat = b1[0:64, 0:256].rearrange("p (h i) -> p h i", h=H)
            for h in range(H):
                p.mm(pat[:, h, :], ke[:, h, cc], qe[:, h, cc], True, True, [ke, qe], [b1])
            p.tt("dve", a_[:], pat, msk[:], ALU.mult, [b1, msk], [a_])
            b2 = GB()
            pkt = b2[0:64, 0:128].bitcast(BF16).rearrange("p (h i) -> p h i", h=H)
            for h in range(H):
                p.tr(pkt[:, h, :], ke[:, h, cc], c.idb[0:64, 0:64], [ke, c.idb], [b2])
            p.cp("act", kt_[:], pkt, [b2], [kt_])
            b3 = GB()
            po = b3[0:64, :].rearrange("p (h v) -> p h v", h=H)
            for h in range(H):
                p.mm(po[:, h, :], a_[:, h, :], gv[s_][:, cch, h * 128:(h + 1) * 128], True, False, [a_, gv[s_]], [b3])
                p.mm(po[:, h, :], qe[:, h, cc], stb[:, h, :], False, True, [qe, stb], [b3])
            b4 = GB()
            pst = b4[0:64, :].rearrange("p (h v) -> p h v", h=H)
            for h in range(H):
                p.mm(pst[:, h, :], kt_[:, h, :], gv[s_][:, cch, h * 128:(h + 1) * 128], True, True, [kt_, gv[s_]], [b4])
            p.tt("dve", st2[:], st[:], pst, ALU.add, [st, b4], [st2])
            ebl = eb[:, :, cch * 64 + 63:cch * 64 + 64].broadcast_to([64, H, 128])
            p.tt("dve", st[:], st2[:], ebl, ALU.mult, [st2, eb], [st])
            p.cp("pool", stb[:], st[:], [st], [stb])
            p.act(sq[:], po, AF.Square, [b3], [sq])
            p.op("dve", lambda e: e.tensor_reduce(ssg[:], sq[:], AX.X, ALU.add), [sq], [ssg], dur=700)
            p.act(ssg[:], ssg[:], AF.Ln, [ssg, c.epsb], [ssg], bias=c.epsb[0:64, :], scale=1.0 / 128)
            p.act(ssg[:], ssg[:], AF.Exp, [ssg], [ssg], scale=-0.5)
            p.tt("dve", y_[:], po, ssg[:].unsqueeze(2).broadcast_to([64, H, 128]), ALU.mult, [b3, ssg], [y_])
            b5 = GB()
            pyT = b5[:, 0:256].rearrange("p (h t) -> p h t", h=H)
            for h in range(H):
                p.tr(pyT[:, h, :], y_[:, h, :], c.idf[0:64, 0:64], [y_, c.idf], [b5])
            p.cp("act", yT[:, :, cc], pyT, [b5], [yT])
        o_ = ysg[s_]
        for h in range(H):
            p.stt("dve", o_[:, h, :], yT[:, h, :], gn[:, h:h + 1], bg[s_][:, h, :], ALU.mult, ALU.mult, [yT, gn, bg[s_]], [o_])
        p.dma(sc["YS"][1024:1536, cols].rearrange("(c p) t -> p c t", p=128), o_[:], [o_],
              [] if ysblk is None else [ysblk[tb]])


def phase_rwkv(p, c, S, l, W, sc, TB=128):
    NB = S // TB
    NCH = TB // 64
    H = 8
    C0 = math.exp(-0.5)
    vec = lambda name, ap: _hv(p, name, ap)
    mu_r = vec("r_mur", W["rwkv_mu"][l, 0:512])
    mu_k = vec("r_muk", W["rwkv_mu"][l, 512:1024])
    mu_v = vec("r_muv", W["rwkv_mu"][l, 1024:1536])
    mu_w = p.sbuf("r_muw", [64, 2], F32)
    p.dma(mu_w[:], W["rwkv_mu"][l, 1536:1664].rearrange("(c p) -> p c", p=64), [], [mu_w], allow_slow_non_contiguous=True)
    w0 = vec("r_w0", W["rwkv_w0"][l])
    a0 = vec("r_a0", W["rwkv_a0"][l])
    k_k = vec("r_kk", W["rwkv_k_k"][l])
    k_a = vec("r_ka", W["rwkv_k_a"][l])
    r_k = vec("r_rk", W["rwkv_r_k"][l])
    lnw = vec("r_lnw", W["rwkv_ln_w"][l])
    lnb = vec("r_lnb", W["rwkv_ln_b"][l])
    wup = p.sbuf("r_wup", [64, 512], F32)
    p.dma(wup[:], W["rwkv_w_up"][l], [], [wup])
    aup = p.sbuf("r_aup", [64, 512], F32)
    p.dma(aup[:], W["rwkv_a_up"][l], [], [aup])
    ones = p.sbuf("r_ones", [64, 64], F32)
    p.memset("pool", ones[:], 1.0, [ones])
    gne = p.sbuf("r_gne", [64, 1], F32)
    p.memset("pool", gne[:], 64e-5, [gne])
    maskr = p.sbuf("r_maskr", [64, TB], F32)
    p.memset("pool", maskr[:], 1.0, [maskr])
    p.memset("pool", maskr[:].rearrange("p (c t) -> p c t", t=64)[:, :, 0:1], 0.0, [maskr])

    def mk_mask(name, pat, cm, op, dt=F32):
        m = p.sbuf(name, [64, H, 64], F32)
        p.memset("pool", m[:], 1.0, [m])
        for h in range(H):
            p.op("pool", lambda e, h=h: e.affine_select(m[:, h, :], m[:, h, :], pat, op, 0.0, base=0, channel_multiplier=cm), [m], [m])
        if dt == F32:
            return m
        mb = p.sbuf(name + "b", [64, H, 64], dt)
        p.cp("pool", mb[:], m[:], [m], [mb])
        return mb
    mL = mk_mask("r_mL", [[-1, 64]], 1, ALU.is_gt)
    mSU = mk_mask("r_mSU", [[1, 64]], -1, ALU.is_gt)
    mU = mk_mask("r_mU", [[1, 64]], -1, ALU.is_ge)
    idh = mk_mask("r_idh", [[1, 64]], -1, ALU.is_equal, BF16)
    id64 = c.idb[0:64, 0:64]

    def A3(name, dt=F32, n=TB):
        return p.sbuf(name, [64, H, n], dt)
    r_, k_, vf = A3("r_r"), A3("r_k"), A3("r_v")
    lwa = p.sbuf("r_lwa", [64, 2, TB], F32)
    tmp, tmp2 = A3("r_tmp"), A3("r_tmp2")
    sg, a_, cs = A3("r_sg"), A3("r_a"), A3("r_cs")
    gi, gp = A3("r_gi"), A3("r_gp")
    kkn, kp = A3("r_kkn"), A3("r_kp")
    D2 = lambda name, dt: [A3(name + "0", dt), A3(name + "1", dt)]
    vb2, Ab2, Bb2, Kb2, Rb2 = D2("r_vb", BF16), D2("r_Ab", BF16), D2("r_Bb", BF16), D2("r_Kb", BF16), D2("r_Rb", BF16)
    g2, bon2, yfm2 = D2("r_g", F32), D2("r_bon", F32), D2("r_yfm", F32)
    bgt2, yso2 = D2("r_bgt", BF16), D2("r_yso", BF16)
    T0 = p.sbuf("r_T0", [64, H, 64], F32)
    p.memset("pool", T0[:], 0.0, [T0])
    T0b = p.sbuf("r_T0b", [64, H, 64], BF16)
    p.memset("pool", T0b[:], 0.0, [T0b])
    T1 = p.sbuf("r_T1", [64, H, 64], F32)

    def S3(name, dt=BF16):
        return p.sbuf(name, [64, H, 64], dt)
    S2 = lambda name: [S3(name + "0"), S3(name + "1")]
    P_ = S2("r_P")
    PT_ = S2("r_PT")
    NTa = [S2("r_NTa"), S2("r_NTb")]
    LakT2, ArbT2, ArkT2 = S2("r_LakT"), S2("r_ArbT"), S2("r_ArkT")
    Vtm2, Btm2, Ktm2 = S2("r_Vtm"), S2("r_Btm"), S2("r_Ktm")
    X_, U_ = S3("r_X"), S3("r_U")
    yc, ysq, ynm = S3("r_yc", F32), S3("r_ysq", F32), S3("r_ynm", F32)
    mean = p.sbuf("r_mean", [64, H], F32)
    var = p.sbuf("r_var", [64, H], F32)
    pz = p.psum("r_pz", [64, 2, TB], F32)
    pg = [p.psum("r_pg%d" % i, [64, H, 64], F32) for i in range(5)]
    pb = [p.psum("r_pb%d" % i, [64, H, 64], BF16) for i in range(2)]
    pgi = [0, 0]

    def PG():
        t = pg[pgi[0] % 5]
        pgi[0] += 1
        return t

    def PB():
        t = pb[pgi[1] % 2]
        pgi[1] += 1
        return t

    def bc(t2):
        return t2[:].unsqueeze(2).broadcast_to([64, H, TB])

    def prep(tb):
        t0 = tb * TB
        cols = slice(t0, t0 + TB)
        bp = tb % 2
        vb, Ab, Bb, Kb, Rb, g_, bon, bgt = vb2[bp], Ab2[bp], Bb2[bp], Kb2[bp], Rb2[bp], g2[bp], bon2[bp], bgt2[bp]
        for (dst, r0) in ((r_, 0), (k_, 512), (vf, 1024)):
            p.dma(dst[:], sc["U"][r0:r0 + 512, cols].rearrange("(h d) t -> d h t", d=64), [sc["U"]], [dst])
        p.dma(lwa[:], sc["U"][1536:1664, cols].rearrange("(c p) t -> p c t", p=64), [sc["U"]], [lwa])
        p.dma(bgt[:], sc["BG"][512:1024, cols].rearrange("(h d) t -> d h t", d=64), [sc["BG"]], [bgt])
        p.cp("act", vb[:], vf[:], [vf], [vb])
        p.act(lwa[:, 0, :], lwa[:, 0, :], AF.Tanh, [lwa], [lwa])
        for h in range(H):
            p.mm(pz[:, 0, :], wup[:, h * 64:(h + 1) * 64], lwa[:, 0, :], True, True, [wup, lwa], [pz])
            p.mm(pz[:, 1, :], aup[:, h * 64:(h + 1) * 64], lwa[:, 1, :], True, True, [aup, lwa], [pz])
            p.act(sg[:, h, :], pz[:, 0, :], AF.Sigmoid, [pz, w0], [sg], bias=w0[:, h:h + 1], scale=1.0)
            p.act(a_[:, h, :], pz[:, 1, :], AF.Sigmoid, [pz, a0], [a_], bias=a0[:, h:h + 1], scale=1.0)
        for h in range(H):
            p.op("dve", lambda e, h=h: e.tensor_tensor_scan(cs[:, h, :], maskr[:], sg[:, h, :], 0.0, ALU.mult, ALU.add),
                 [maskr, sg], [cs], dur=100 + 2.1 * TB)
        p.act(g_[:], cs[:], AF.Exp, [cs], [g_], scale=-C0)
        p.act(gi[:], cs[:], AF.Exp, [cs], [gi], scale=C0)
        p.tt("pool", tmp[:], cs[:], sg[:], ALU.subtract, [cs, sg], [tmp])
        p.act(gp[:], tmp[:], AF.Exp, [tmp], [gp], scale=-C0)
        p.tt("dve", kkn[:], k_[:], bc(k_k), ALU.mult, [k_, k_k], [kkn])
        p.act(tmp2[:], kkn[:], AF.Square, [kkn], [tmp2])
        for hp in range(4):
            for j in range(2):
                p.mm(pz[:, j, :], ones[:], tmp2[:, hp * 2 + j, :], True, True, [ones, tmp2], [pz])
            p.ts("dve", sg[:, hp * 2:hp * 2 + 2, :], pz[:], 1e-24, None, ALU.max, None, [pz], [sg])
        p.act(sg[:], sg[:], AF.Ln, [sg], [sg])
        p.act(sg[:], sg[:], AF.Exp, [sg], [sg], scale=-0.5)
        p.tt("pool", kkn[:], kkn[:], sg[:], ALU.mult, [kkn, sg], [kkn])
        p.stt("dve", tmp[:], a_[:], -1.0, bc(k_a), ALU.add, ALU.mult, [a_, k_a], [tmp])
        p.stt("dve", kp[:], tmp[:], 1.0, k_[:], ALU.add, ALU.mult, [tmp, k_], [kp])
        p.stt("dve", Ab[:], kkn[:], -1.0, gp[:], ALU.mult, ALU.mult, [kkn, gp], [Ab])
        p.tt("pool", tmp2[:], kkn[:], a_[:], ALU.mult, [kkn, a_], [tmp2])
        p.tt("dve", Bb[:], tmp2[:], gi[:], ALU.mult, [tmp2, gi], [Bb])
        p.tt("pool", Kb[:], kp[:], gi[:], ALU.mult, [kp, gi], [Kb])
        p.tt("dve", Rb[:], r_[:], g_[:], ALU.mult, [r_, g_], [Rb])
        p.tt("pool", tmp[:], r_[:], kp[:], ALU.mult, [r_, kp], [tmp])
        p.tt("pool", tmp[:], tmp[:], bc(r_k), ALU.mult, [tmp, r_k], [tmp])
        for hp in range(4):
            for j in range(2):
                p.mm(pz[:, j, :], ones[:], tmp[:, hp * 2 + j, :], True, True, [ones, tmp], [pz])
            p.tt("dve", bon[:, hp * 2:hp * 2 + 2, :], pz[:], vf[:, hp * 2:hp * 2 + 2, :], ALU.mult, [pz, vf], [bon])

    def chunk(tb, cch):
        bp = tb % 2
        g = tb * NCH + cch
        cp_ = g % 2
        cc = slice(cch * 64, (cch + 1) * 64)
        vb, Ab, Bb, Kb, Rb, g_, yfm = vb2[bp], Ab2[bp], Bb2[bp], Kb2[bp], Rb2[bp], g2[bp], yfm2[bp]
        LakT, ArbT, ArkT, Vtm, Btm, Ktm = LakT2[cp_], ArbT2[cp_], ArkT2[cp_], Vtm2[cp_], Btm2[cp_], Ktm2[cp_]
        NTp = NTa[cp_]

        def scores(lh, rh, mask, dst, eng):
            ps = PG()
            for h in range(H):
                p.mm(ps[:, h, :], lh[:, h, cc], rh[:, h, cc], True, True, [lh, rh], [ps])
            p.tt(eng, dst[:], ps[:], mask[:], ALU.mult, [ps, mask], [dst])
        scores(Ab, Bb, mL, P_[0], "dve")
        scores(Bb, Ab, mSU, PT_[0], "dve")
        p.tt("pool", NTp[0][:], PT_[0][:], idh[:], ALU.add, [PT_[0], idh], [NTp[0]])
        scores(Kb, Ab, mSU, LakT, "dve")
        scores(Bb, Rb, mU, ArbT, "dve")
        scores(Kb, Rb, mU, ArkT, "dve")

        def transp(src, dst):
            ps = PB()
            for h in range(H):
                p.tr(ps[:, h, :], src[:, h, cc], id64, [src, c.idb], [ps])
            p.cp("act", dst[:], ps[:], [ps], [dst])
        transp(vb, Vtm)
        transp(Bb, Btm)
        transp(Kb, Ktm)
        cur = 0
        nti = 0
        for step in range(5):
            Pn, PTn = P_[1 - cur], PT_[1 - cur]
            ps = PG()
            for h in range(H):
                p.mm(ps[:, h, :], PT_[cur][:, h, :], P_[cur][:, h, :], True, True, [PT_[cur], P_[cur]], [ps])
            p.cp("act", Pn[:], ps[:], [ps], [Pn])
            if step < 4:
                ps2 = PG()
                for h in range(H):
                    p.mm(ps2[:, h, :], P_[cur][:, h, :], PT_[cur][:, h, :], True, True, [PT_[cur], P_[cur]], [ps2])
                p.cp("dve", PTn[:], ps2[:], [ps2], [PTn])
            ps3 = PG()
            NTo, NTn = NTp[nti], NTp[1 - nti]
            for h in range(H):
                p.mm(ps3[:, h, :], id64, NTo[:, h, :], True, False, [c.idb, NTo], [ps3])
                p.mm(ps3[:, h, :], Pn[:, h, :], NTo[:, h, :], False, True, [Pn, NTo], [ps3])
            p.cp("act", NTn[:], ps3[:], [ps3], [NTn])
            nti = 1 - nti
            cur = 1 - cur
        NT = NTp[nti]
        ps = PG()
        for h in range(H):
            p.mm(ps[:, h, :], Ab[:, h, cc], T0b[:, h, :], True, False, [Ab, T0b], [ps])
            p.mm(ps[:, h, :], LakT[:, h, :], Vtm[:, h, :], False, True, [LakT, Vtm], [ps])
        p.cp("act", X_[:], ps[:], [ps], [X_])
        ps = PG()
        for h in range(H):
            p.mm(ps[:, h, :], NT[:, h, :], X_[:, h, :], True, True, [NT, X_], [ps])
        p.cp("act", U_[:], ps[:], [ps], [U_])
        py = PG()
        for h in range(H):
            p.mm(py[:, h, :], Rb[:, h, cc], T0b[:, h, :], True, False, [Rb, T0b], [py])
            p.mm(py[:, h, :], ArbT[:, h, :], U_[:, h, :], False, False, [ArbT, U_], [py])
            p.mm(py[:, h, :], ArkT[:, h, :], Vtm[:, h, :], False, True, [ArkT, Vtm], [py])
        ps = PG()
        for h in range(H):
            p.mm(ps[:, h, :], Btm[:, h, :], U_[:, h, :], True, False, [Btm, U_], [ps])
            p.mm(ps[:, h, :], Ktm[:, h, :], Vtm[:, h, :], False, True, [Ktm, Vtm], [ps])
        p.tt("dve", T1[:], T0[:], ps[:], ALU.add, [T0, ps], [T1])
        gC = g_[:, :, cch * 64 + 63:cch * 64 + 64].broadcast_to([64, H, 64])
        p.tt("dve", T0[:], T1[:], gC, ALU.mult, [T1, g_], [T0])
        p.cp("pool", T0b[:], T0[:], [T0], [T0b])
        p.op("dve", lambda e, py=py: e.tensor_reduce(mean[:], py[:], AX.X, ALU.add), [py], [mean], dur=700)
        p.ts("dve", mean[:], mean[:], 1.0 / 64, None, ALU.mult, None, [mean], [mean])
        p.tt("dve", yc[:], py[:], mean[:].unsqueeze(2).broadcast_to([64, H, 64]), ALU.subtract, [py, mean], [yc])
        p.act(ysq[:], yc[:], AF.Square, [yc], [ysq])
        p.op("dve", lambda e: e.tensor_reduce(var[:], ysq[:], AX.X, ALU.add), [ysq], [var], dur=700)
        p.act(var[:], var[:], AF.Ln, [var, gne], [var], bias=gne[:], scale=1.0 / 64)
        p.act(var[:], var[:], AF.Exp, [var], [var], scale=-0.5)
        p.tt("pool", ynm[:], yc[:], var[:].unsqueeze(2).broadcast_to([64, H, 64]), ALU.mult, [yc, var], [ynm])
        ps = PG()
        for h in range(H):
            p.tr(ps[:, h, :], ynm[:, h, :], c.idf[0:64, 0:64], [ynm, c.idf], [ps])
        p.cp("act", yfm[:, :, cc], ps[:], [ps], [yfm])

    def epilogue(tb):
        bp = tb % 2
        cols = slice(tb * TB, (tb + 1) * TB)
        yfm, bon, bgt, yso = yfm2[bp], bon2[bp], bgt2[bp], yso2[bp]
        p.tt("dve", yfm[:], yfm[:], bc(lnw), ALU.mult, [yfm, lnw], [yfm])
        p.tt("pool", yfm[:], yfm[:], bc(lnb), ALU.add, [yfm, lnb], [yfm])
        p.tt("pool", yfm[:], yfm[:], bon[:], ALU.add, [yfm, bon], [yfm])
        p.tt("dve", yso[:], yfm[:], bgt[:], ALU.mult, [yfm, bgt], [yso])
        p.dma(sc["YS"][512:1024, cols].rearrange("(h d) t -> d h t", d=64), yso[:], [yso], [])

    for tb in range(NB):
        prep(tb)
        for cch in range(NCH):
            chunk(tb, cch)
        epilogue(tb)


def _hv(p, name, ap):
    t = p.sbuf(name, [64, 8], F32)
    p.dma(t[:], ap.rearrange("(h d) -> d h", d=64), [], [t], allow_slow_non_contiguous=True)
    return t


def phase_out_prep(p, c, S, l, W):
    wbo = p.sbuf("wbo", [128, 12, D], BF16)
    wout = p.sbuf("wout", [128, 8, D], BF16)
    gpb = p.sbuf("gpb", [128, D], F32)
    p.dma(gpb[:], W["norm_post"][l:l + 1, :].partition_broadcast(128), [], [gpb])
    mk = p.mark()
    stg = [p.sbuf("wstg%d" % i, [128, 4, D], F32) for i in range(2)]
    for n in range(3):
        p.dma(stg[n % 2][:], W["w_branch_out"][l, n].rearrange("(k p) d -> p k d", p=128), [], [stg[n % 2]])
        p.cp("dve" if n % 2 == 0 else "act", wbo[:, n * 4:(n + 1) * 4, :], stg[n % 2][:], [stg[n % 2]], [wbo])
    for hf in range(2):
        p.dma(stg[(hf + 1) % 2][:], W["w_out"][l, hf * 512:(hf + 1) * 512, :].rearrange("(k p) d -> p k d", p=128), [], [stg[(hf + 1) % 2]])
        p.cp("act" if hf % 2 == 0 else "dve", wout[:, hf * 4:(hf + 1) * 4, :], stg[(hf + 1) % 2][:], [stg[(hf + 1) % 2]], [wout])
    p.release(mk)
    return wbo, wout, gpb


def phase_out(p, c, S, l, xin, xout, W, sc, pre=None, ysblk=None):
    NB = S // 512
    if pre is None:
        pre = phase_out_prep(p, c, S, l, W)
    wbo, wout, gpb = pre
    ysb = [p.sbuf("ysb%d" % i, [128, 12, 512], BF16) for i in range(2)]
    mgb = [p.sbuf("mgb%d" % i, [128, 3, 512], BF16) for i in range(3)]
    mT = p.sbuf("mT", [128, 8, 512], BF16)
    m32 = p.sbuf("m32", [128, 512], F32)
    t32 = [p.sbuf("t32_%d" % i, [128, 512], F32) for i in range(2)]
    xt = [p.sbuf("xo%d" % i, [128, D], F32) for i in range(2)]
    o32a = p.sbuf("o32_0", [128, D], F32)
    o32 = [o32a, o32a]
    junk = p.sbuf("junko", [128, 512], BF16)
    ssq = p.sbuf("ssq", [128, 2], F32)
    rstd = p.sbuf("rstdo", [128, 1], F32)
    pb = [p.psum("pb%d" % i, [128, 512], F32) for i in range(3)]
    po = [p.psum("po%d" % i, [128, 512], F32) for i in range(2)]
    it = 0
    im = 0
    for tb in range(NB):
        cols = slice(tb * 512, (tb + 1) * 512)
        ysx = ysb[tb % 2]
        p.dma(ysx[:], sc["YS"][:, cols].rearrange("(c p) t -> p c t", p=128),
              [sc["YS"] if ysblk is None else ysblk[tb]], [ysx])
        mgv = sc["MG"][:, cols].rearrange("(n c p) t -> p n c t", n=3, p=128)
        for cch in range(8):
            mgx = mgb[im % 3]
            im += 1
            p.dma(mgx[:], mgv[:, :, cch, :], [sc["MG"]], [mgx])
            for n in range(3):
                for k in range(4):
                    p.mm(pb[n][:], wbo[:, n * 4 + k, cch * 128:(cch + 1) * 128], ysx[:, n * 4 + k, :], k == 0, k == 3,
                         [wbo, ysx], [pb[n]])
            p.tt("dve", m32[:], pb[0][:], mgx[:, 0, :], ALU.mult, [pb[0], mgx], [m32])
            p.tt("dve", t32[0][:], pb[1][:], mgx[:, 1, :], ALU.mult, [pb[1], mgx], [t32[0]])
            p.tt("dve", t32[1][:], pb[2][:], mgx[:, 2, :], ALU.mult, [pb[2], mgx], [t32[1]])
            p.tt("pool", m32[:], m32[:], t32[0][:], ALU.add, [m32, t32[0]], [m32])
            p.tt("pool", mT[:, cch, :], m32[:], t32[1][:], ALU.add, [m32, t32[1]], [mT])
        for tsub in range(4):
            s_ = it % 2
            it += 1
            r0 = tb * 512 + tsub * 128
            p.dma(xt[s_][:], xin[r0:r0 + 128, :], [xin], [xt[s_]])
            for hf in range(2):
                for k in range(8):
                    p.mm(po[hf][:], mT[:, k, tsub * 128:(tsub + 1) * 128], wout[:, k, hf * 512:(hf + 1) * 512], k == 0, k == 7,
                         [mT, wout], [po[hf]])
                p.act(junk[:], po[hf][:], AF.Square, [po[hf]], [junk, ssq], accum_out=ssq[:, hf:hf + 1])
            p.tt("dve", rstd[:], ssq[:, 0:1], ssq[:, 1:2], ALU.add, [ssq], [rstd])
            p.act(rstd[:], rstd[:], AF.Ln, [rstd, c.epsb], [rstd], bias=c.epsb[:], scale=1.0 / D)
            p.act(rstd[:], rstd[:], AF.Exp, [rstd], [rstd], scale=-0.5)
            for hf in range(2):
                p.stt("dve", o32[s_][:, hf * 512:(hf + 1) * 512], po[hf][:], rstd[:, 0:1], gpb[:, hf * 512:(hf + 1) * 512],
                      ALU.mult, ALU.mult, [po[hf], rstd, gpb], [o32[s_]])
            p.tt("pool", o32[s_][:], o32[s_][:], xt[s_][:], ALU.add, [o32[s_], xt[s_]], [o32[s_]])
            p.dma(xout[r0:r0 + 128, :], o32[s_][:], [o32[s_]], [])


def make_scratch(p, S, dbg):
    kind = "ExternalOutput" if dbg else "Internal"
    sc = {}
    sc["QT"] = p.dram("QT", [8, 96, S], BF16, kind)
    sc["KT"] = p.dram("KT", [8, 96, S], BF16, kind)
    sc["VA"] = p.dram("VA", [S, 520], BF16, kind)
    sc["U"] = p.dram("U", [1664, S], F32, kind)
    sc["GQ"] = p.dram("GQ", [256, S], F32, kind)
    sc["GK"] = p.dram("GK", [256, S], F32, kind)
    sc["GV"] = p.dram("GV", [S, 512], BF16, kind)
    sc["GL"] = p.dram("GL", [16, S], F32, kind)
    sc["BG"] = p.dram("BG", [1536, S], BF16, kind)
    sc["MG"] = p.dram("MG", [3072, S], BF16, kind)
    sc["YS"] = p.dram("YS", [1536, S], BF16, kind)
    sc["X1"] = p.dram("X1", [S, D], F32, kind)
    return sc


WSPEC = {
    "norm_pre": ([L, D], F32), "w_in": ([L, D, NIN], F32), "wkr2": ([L, D, 192], F32),
    "mla_q_norm": ([L, 256], F32), "mla_kv_norm": ([L, 128], F32),
    "mla_w_uq": ([L, 256, 768], F32), "wuq_sw": ([L, 256, 768], F32),
    "wuk": ([L, 128, 512], F32), "wuv": ([L, 128, 512], F32),
    "rwkv_mu": ([L, 1664], F32), "rwkv_w0": ([L, 512], F32), "rwkv_w_up": ([L, 64, 512], F32),
    "rwkv_a0": ([L, 512], F32), "rwkv_a_up": ([L, 64, 512], F32), "rwkv_k_k": ([L, 512], F32),
    "rwkv_k_a": ([L, 512], F32), "rwkv_r_k": ([L, 512], F32), "rwkv_ln_w": ([L, 512], F32),
    "rwkv_ln_b": ([L, 512], F32), "gla_a_up": ([L, 16, 256], F32), "gla_a_b": ([L, 256], F32),
    "gla_norm": ([L, 512], F32), "w_branch_out": ([L, 3, 512, D], F32), "w_out": ([L, D, D], F32),
    "norm_post": ([L, D], F32),
}


def host_weights(inp):
    w = {}
    for k in WSPEC:
        if k in inp:
            w[k] = np.ascontiguousarray(np.asarray(inp[k], dtype=np.float32))
    win = w["w_in"]
    w["wkr2"] = np.ascontiguousarray(np.concatenate(
        [win[:, :, 320:416], win[:, :, 320:384], win[:, :, 400:416], win[:, :, 384:400]], axis=2))
    uq = w["mla_w_uq"].reshape(L, 256, 8, 96)
    w["wuq_sw"] = np.ascontiguousarray(np.concatenate(
        [uq[..., 0:64], uq[..., 80:96], uq[..., 64:80]], axis=-1).reshape(L, 256, 768))
    ukv = w["mla_w_ukv"] if "mla_w_ukv" in w else np.asarray(inp["mla_w_ukv"], dtype=np.float32)
    ukv = ukv.reshape(L, 128, 8, 128)
    w["wuk"] = np.ascontiguousarray(ukv[..., 0:64].reshape(L, 128, 512))
    w["wuv"] = np.ascontiguousarray(ukv[..., 64:128].reshape(L, 128, 512))
    w["rwkv_r_k"] = w["rwkv_r_k"].reshape(L, 512) if "rwkv_r_k" in w else np.asarray(inp["rwkv_r_k"], np.float32).reshape(L, 512)
    return w


def build(S, phases=("p1",), dbg=True, nlayers=1):
    nc = bass.Bass("TRN2", target_bir_lowering=False)
    p = Prog(nc)
    x_d = p.dram("x", [S, D], F32, "ExternalInput")
    pos_d = p.dram("pos", [1, S], I32, "ExternalInput")
    W = {k: p.dram(k, sh, dt, "ExternalInput").t for k, (sh, dt) in WSPEC.items()}
    out_d = p.dram("out", [S, D], F32, "ExternalOutput")
    sc = make_scratch(p, S, dbg)
    c = setup_consts(p, S, pos_d.t)
    final = [out_d]
    for l in range(nlayers):
        xin = x_d if l == 0 else sc["X1"]
        if "p1" in phases:
            mk = p.mark()
            phase1(p, c, S, l, xin, W, sc)
            p.release(mk)
        if "mla" in phases:
            mk = p.mark()
            phase_mla(p, c, S, l, sc)
            p.release(mk)
        if "rwkv" in phases:
            mk = p.mark()
            phase_rwkv(p, c, S, l, W, sc)
            p.release(mk)
        xo = out_d if l == nlayers - 1 else sc["X1"]
        if "gla" in phases and "out" in phases:
            mk = p.mark()
            pre = phase_out_prep(p, c, S, l, W)
            ysblk = [T(None, "ysblk%d" % i) for i in range(S // 512)]
            phase_gla(p, c, S, l, W, sc, ysblk)
            phase_out(p, c, S, l, xin, xo, W, sc, pre, ysblk)
            p.release(mk)
        else:
            if "gla" in phases:
                mk = p.mark()
                phase_gla(p, c, S, l, W, sc)
                p.release(mk)
            if "out" in phases:
                mk = p.mark()
                phase_out(p, c, S, l, xin, xo, W, sc)
                p.release(mk)
    if dbg:
        final = list(sc.values()) + [out_d]
    p.finish(final)
    return nc, p


ALL_PHASES = ("p1", "mla", "rwkv", "gla", "out")
_CACHE = {}


def kernel(**inputs):
    x = np.asarray(inputs["x"], dtype=np.float32)
    pos = np.asarray(inputs["positions"], dtype=np.int32)
    B, S, _ = x.shape
    Wh = host_weights(inputs)
    nc, _p = build(S, phases=ALL_PHASES, dbg=False, nlayers=L)
    in_maps = []
    for b in range(B):
        m = {"x": np.ascontiguousarray(x[b]), "pos": np.ascontiguousarray(pos[b:b + 1])}
        for k in WSPEC:
            m[k] = Wh[k]
        in_maps.append(m)
    res = run_bass_kernel_spmd(nc, in_maps, core_ids=list(range(B)))
    return np.stack([np.asarray(r["out"]) for r in res.results], axis=0).astype(np.float32)
```
